# Optimizing a Trainium2 kernel written in Bass

```python
import jax, jax.numpy as jnp
from jax import lax
import numpy as np

D_MODEL = 1024
BATCH = 4
SEQ = 8192
DEPTH = 1

CHUNK = 64
Q_BLOCK = 2 * CHUNK
HEAD_DIM = 64
N_FOX_HEADS = 8
N_SB_HEADS = 8
FOX_WIDTH = N_FOX_HEADS * HEAD_DIM
SB_WIDTH = N_SB_HEADS * HEAD_DIM
MIX_WIDTH = FOX_WIDTH + SB_WIDTH
IN_COLS = 3 * FOX_WIDTH + N_FOX_HEADS + 3 * SB_WIDTH
PEER_HEADS = 8
PEER_N_KEYS = 128
PEER_N_EXPERTS = PEER_N_KEYS * PEER_N_KEYS
PEER_TOPK = 16
PEER_D_KEY = 256
PEER_HALF = PEER_D_KEY // 2
PEER_TOKEN_BLOCK = 128
N_MOD = 6
EPS = 1e-6

kernel_name = "hybrid_fox_stickbreak_peer_adaln_block"


def rms_norm(x, g):
    xf = x.astype(jnp.float32)
    inv = lax.rsqrt(jnp.mean(xf * xf, axis=-1, keepdims=True) + EPS)
    return (xf * inv).astype(x.dtype) * g


def split_heads(t, n_heads):
    b, s, _ = t.shape
    return t.reshape(b, s, n_heads, HEAD_DIM).transpose(0, 2, 1, 3)


def merge_blocks(o):
    nb, b, h, qb, dh = o.shape
    return o.transpose(1, 0, 3, 2, 4).reshape(b, nb * qb, h, dh)


def forgetting_attention(q, k, v, log_fcum):
    seq = q.shape[2]
    n_blocks = seq // Q_BLOCK
    scale = HEAD_DIM ** -0.5
    k_pos = jnp.arange(seq)

    def block(i):
        start = i * Q_BLOCK
        qb = lax.dynamic_slice_in_dim(q, start, Q_BLOCK, axis=2)
        fq = lax.dynamic_slice_in_dim(log_fcum, start, Q_BLOCK, axis=2)
        q_pos = start + jnp.arange(Q_BLOCK)
        logits = jnp.einsum('bhqd,bhkd->bhqk', qb, k, preferred_element_type=jnp.float32) * scale
        logits = logits + fq[..., :, None] - log_fcum[..., None, :]
        mask = k_pos[None, :] <= q_pos[:, None]
        logits = jnp.where(mask, logits, -jnp.inf)
        p = jax.nn.softmax(logits, axis=-1)
        return jnp.einsum('bhqk,bhkd->bhqd', p.astype(v.dtype), v)

    return merge_blocks(lax.map(block, jnp.arange(n_blocks)))


def stick_breaking_attention(q, k, v):
    seq = q.shape[2]
    n_blocks = seq // Q_BLOCK
    scale = HEAD_DIM ** -0.5
    k_pos = jnp.arange(seq)

    def block(i):
        start = i * Q_BLOCK
        qb = lax.dynamic_slice_in_dim(q, start, Q_BLOCK, axis=2)
        q_pos = start + jnp.arange(Q_BLOCK)
        z = jnp.einsum('bhqd,bhkd->bhqk', qb, k, preferred_element_type=jnp.float32) * scale
        mask = k_pos[None, :] < q_pos[:, None]
        log_beta = jax.nn.log_sigmoid(z)
        log_one_minus = jnp.where(mask, jax.nn.log_sigmoid(-z), 0.0)
        rest = lax.cumsum(log_one_minus, axis=3, reverse=True) - log_one_minus
        a = jnp.where(mask, jnp.exp(log_beta + rest), 0.0)
        return jnp.einsum('bhqk,bhkd->bhqd', a.astype(v.dtype), v)

    return merge_blocks(lax.map(block, jnp.arange(n_blocks)))


def peer_layer(h, w_query, sub_keys, expert_down, expert_up):
    b, s, d = h.shape
    t = b * s
    ht = h.reshape(t, d)
    query = (ht @ w_query).reshape(t, PEER_HEADS, 2, PEER_HALF)
    sub_scores = jnp.einsum('thpd,hpkd->thpk', query, sub_keys,
                            preferred_element_type=jnp.float32)
    half_vals, half_idx = lax.top_k(sub_scores, PEER_TOPK)
    cand = (half_vals[:, :, 0, :, None] + half_vals[:, :, 1, None, :]
            ).reshape(t, PEER_HEADS, PEER_TOPK * PEER_TOPK)
    top_vals, top_pos = lax.top_k(cand, PEER_TOPK)
    i1 = jnp.take_along_axis(half_idx[:, :, 0, :], top_pos // PEER_TOPK, axis=-1)
    i2 = jnp.take_along_axis(half_idx[:, :, 1, :], top_pos % PEER_TOPK, axis=-1)
    experts = i1 * PEER_N_KEYS + i2
    gates = jax.nn.softmax(top_vals, axis=-1).astype(h.dtype)

    n_blk = t // PEER_TOKEN_BLOCK
    xs = (ht.reshape(n_blk, PEER_TOKEN_BLOCK, d),
          experts.reshape(n_blk, PEER_TOKEN_BLOCK, PEER_HEADS, PEER_TOPK),
          gates.reshape(n_blk, PEER_TOKEN_BLOCK, PEER_HEADS, PEER_TOPK))

    def block(args):
        xb, eb, gb = args
        u = expert_down[eb]
        act = jax.nn.gelu(jnp.einsum('td,thkd->thk', xb, u), approximate=False) * gb
        vv = expert_up[eb]
        return jnp.einsum('thk,thkd->td', act, vv)

    return lax.map(block, xs).reshape(b, s, d)


def setup_inputs(seed: int = 0) -> dict:
    key = jax.random.key(seed)
    ks = jax.random.split(key, 17)
    nrm = jax.random.normal
    L, D = DEPTH, D_MODEL
    return {
        "x": nrm(ks[0], (BATCH, SEQ, D), jnp.float32),
        "c": nrm(ks[1], (BATCH, D), jnp.float32),
        "w_ada": nrm(ks[2], (L, D, N_MOD * D), jnp.float32) * D ** -0.5,
        "b_ada": 0.02 * nrm(ks[3], (L, N_MOD * D), jnp.float32),
        "g_norm_mix": 1.0 + 0.05 * nrm(ks[4], (L, D), jnp.float32),
        "w_in": nrm(ks[5], (L, D, IN_COLS), jnp.float32) * D ** -0.5,
        "b_forget": 1.5 + 0.5 * nrm(ks[6], (L, N_FOX_HEADS), jnp.float32),
        "g_out_fox": 1.0 + 0.05 * nrm(ks[7], (L, HEAD_DIM), jnp.float32),
        "g_out_sb": 1.0 + 0.05 * nrm(ks[8], (L, HEAD_DIM), jnp.float32),
        "w_out": nrm(ks[9], (L, MIX_WIDTH, D), jnp.float32) * MIX_WIDTH ** -0.5,
        "g_norm_ffn": 1.0 + 0.05 * nrm(ks[10], (L, D), jnp.float32),
        "w_query": nrm(ks[11], (L, D, PEER_HEADS * PEER_D_KEY), jnp.float32) * D ** -0.5,
        "sub_keys": nrm(ks[12], (L, PEER_HEADS, 2, PEER_N_KEYS, PEER_HALF), jnp.float32) * PEER_HALF ** -0.5,
        "expert_down": nrm(ks[13], (L, PEER_N_EXPERTS, D), jnp.float32) * D ** -0.5,
        "expert_up": nrm(ks[14], (L, PEER_N_EXPERTS, D), jnp.float32) * PEER_HEADS ** -0.5,
        "g_final": 1.0 + 0.05 * nrm(ks[15], (D,), jnp.float32),
    }


def reference(x, c, w_ada, b_ada, g_norm_mix, w_in, b_forget, g_out_fox, g_out_sb,
              w_out, g_norm_ffn, w_query, sub_keys, expert_down, expert_up, g_final):
    b, s, d = x.shape
    c_act = jax.nn.silu(c)
    for l in range(DEPTH):
        mod = c_act @ w_ada[l] + b_ada[l]
        sh1, sc1, gt1, sh2, sc2, gt2 = [m[:, None, :] for m in jnp.split(mod, N_MOD, axis=-1)]

        h = rms_norm(x, g_norm_mix[l]) * (1.0 + sc1) + sh1
        proj = h @ w_in[l]
        o1 = 3 * FOX_WIDTH
        o2 = o1 + N_FOX_HEADS
        fq, fk, fv = jnp.split(proj[..., :o1], 3, axis=-1)
        f_logit = proj[..., o1:o2]
        sq, sk, sv = jnp.split(proj[..., o2:], 3, axis=-1)

        log_f = jax.nn.log_sigmoid((f_logit + b_forget[l]).astype(jnp.float32))
        log_fcum = lax.cumsum(log_f, axis=1).transpose(0, 2, 1)
        fox_out = forgetting_attention(split_heads(fq, N_FOX_HEADS), split_heads(fk, N_FOX_HEADS),
                                       split_heads(fv, N_FOX_HEADS), log_fcum)
        sb_out = stick_breaking_attention(split_heads(sq, N_SB_HEADS), split_heads(sk, N_SB_HEADS),
                                          split_heads(sv, N_SB_HEADS))
        mixed = jnp.concatenate([rms_norm(fox_out, g_out_fox[l]).reshape(b, s, FOX_WIDTH),
                                 rms_norm(sb_out, g_out_sb[l]).reshape(b, s, SB_WIDTH)], axis=-1)
        x = x + gt1 * (mixed @ w_out[l])

        h2 = rms_norm(x, g_norm_ffn[l]) * (1.0 + sc2) + sh2
        x = x + gt2 * peer_layer(h2, w_query[l], sub_keys[l], expert_down[l], expert_up[l])
    return rms_norm(x, g_final)
```

```python
import numpy as np
import ml_dtypes
from contextlib import ExitStack
import concourse.bass as bass
import concourse.mybir as mybir
from concourse.bass_utils import run_bass_kernel_spmd

F32 = mybir.dt.float32
BF16 = mybir.dt.bfloat16
I32 = mybir.dt.int32
U32 = mybir.dt.uint32
AF = mybir.ActivationFunctionType
ALU = mybir.AluOpType
AX = mybir.AxisListType

D = 1024
NH = 16
HD = 64
KA = 70
INC = 3080
NEXP = 16384
EPS = 1e-6
NEG = -30000.0


class Tk:
    __slots__ = ("name", "w", "r")

    def __init__(self, name=""):
        self.name = name
        self.w = None
        self.r = {}


class Chan:
    def __init__(self, S, name):
        self.sem = S.new_sem(name)
        self.key = name
        self.count = 0


class Sched:
    ENGS = ("pe", "act", "dve", "pool", "sp")
    ROT = 20000

    def __init__(self, nc, es):
        self.nc = nc
        self.es = es
        self.eobj = {"pe": nc.tensor, "act": nc.scalar, "dve": nc.vector, "pool": nc.gpsimd, "sp": nc.sync}
        self.sems = {}
        self.chans = []
        self.gen = {k: 0 for k in self.ENGS}
        self.ekey = {}
        self.count = {}
        self.known = {k: {} for k in self.ENGS}
        self.done_keys = []
        for k in self.ENGS:
            self._new_esem(k)
        self.nwaits = 0
        self.ninst = 0

    def _new_esem(self, k):
        key = "e_%s_%d" % (k, self.gen[k])
        self.gen[k] += 1
        self.new_sem(key)
        self.ekey[k] = key
        self.count[k] = 0

    def new_sem(self, name):
        s = self.es.enter_context(self.nc.semaphore(name))
        self.sems[name] = s
        return s

    def chan(self, name):
        c = Chan(self, "c_" + name)
        self.chans.append(c)
        return c

    def _wait(self, eng, ev):
        key, val, clock = ev
        kn = self.known[eng]
        if kn.get(key, 0) >= val:
            return
        self.eobj[eng].wait_ge(self.sems[key], val)
        self.nwaits += 1
        kn[key] = val
        if clock:
            for k2, v2 in clock.items():
                if kn.get(k2, 0) < v2:
                    kn[k2] = v2

    def op(self, eng, fn, reads=(), writes=(), chan=None):
        deps = []
        epref = "e_%s_" % eng
        for t in reads:
            if t.w is not None:
                deps.append(t.w)
        for t in writes:
            for ev in t.r.values():
                if ev[0].startswith(epref):
                    continue
                deps.append(ev)
            if t.w is not None:
                if t.w[0].startswith(epref):
                    continue
                deps.append(t.w)
        for ev in deps:
            self._wait(eng, ev)
        self.ninst += 1
        if chan is None:
            if self.count[eng] >= self.ROT:
                self.done_keys.append((self.ekey[eng], self.count[eng]))
                self._new_esem(eng)
            key = self.ekey[eng]
            self.count[eng] += 1
            val = self.count[eng]
            fn(self.eobj[eng]).then_inc(self.sems[key], 1)
            ev = (key, val, dict(self.known[eng]))
        else:
            chan.count += 16
            fn(self.eobj[eng]).then_inc(chan.sem, 16)
            ev = (chan.key, chan.count, dict(self.known[eng]))
        for t in writes:
            t.w = ev
            t.r = {}
        for t in reads:
            if t in writes:
                continue
            t.r[("e_" + eng) if chan is None else chan.key] = ev
        return ev

    def barrier(self):
        evs = []
        for k in self.ENGS:
            if self.count[k] > 0:
                evs.append((self.ekey[k], self.count[k], None))
        for key, cnt in self.done_keys:
            evs.append((key, cnt, None))
        for c in self.chans:
            if c.count > 0:
                evs.append((c.key, c.count, None))
        for eng in self.ENGS:
            for ev in evs:
                self._wait(eng, ev)


def tile_ids(parity, NT):
    out = []
    for m in range(NT // 4):
        out += [4 * m, 4 * m + 3] if parity == 0 else [4 * m + 1, 4 * m + 2]
    return out


def build(S, dbg=0, phases=(0, 1, 2, 3), nexp=NEXP):
    NT = S // 512
    NB = S // 128
    NTQ = NT // 2
    SQ = NTQ * 512
    NBQ = SQ // 128
    nc = bass.Bass("TRN2", target_bir_lowering=False)

    def din(name, shape, dt=F32):
        return nc.dram_tensor(name, list(shape), dt, kind="ExternalInput").ap()

    x_all = din("x_all", [S, D])
    x_q = din("x_q", [SQ, D])
    cT_d = din("cT", [128, 8])
    w_ada = din("w_ada", [D, 6 * D])
    badaT_d = din("badaT", [128, 48])
    bada_row = din("bada_row", [1, 6 * D])
    gmixT_d = din("gmixT", [128, 8])
    gffnT_d = din("gffnT", [128, 8])
    gffn_row = din("gffn_row", [1, D])
    gfin_row = din("gfin_row", [1, D])
    w_in = din("w_in", [D, INC])
    bf_row = din("bf_row", [1, 8])
    gfox_row = din("gfox_row", [1, HD])
    gsb_row = din("gsb_row", [1, HD])
    w_out = din("w_out", [D, D])
    w_query = din("w_query", [D, 2048])
    skT_d = din("skT", [128, 16, 128])
    e_down = din("e_down", [nexp, D])
    e_up = din("e_up", [nexp, D])
    masks_d = din("masks", [128, 16, 512], BF16)
    masks_sb_d = din("masks_sb", [128, 16, 512], BF16)
    flags_d = din("flags", [8, 2])
    out_d = nc.dram_tensor("out", [SQ, D], F32, kind="ExternalOutput").ap()

    okind = "ExternalOutput" if dbg else None

    def dscr(name, shape, dt):
        if dbg:
            return nc.dram_tensor(name, list(shape), dt, kind="ExternalOutput").ap()
        return nc.dram_tensor(name, list(shape), dt).ap()

    KT_d = dscr("KT_d", [NH, KA, S], BF16)
    QT_d = dscr("QT_d", [NH, KA, SQ], BF16)
    V_d = dscr("V_d", [NH, 128, NB, 65], BF16)
    mixed_d = dscr("mixed_d", [SQ, D], BF16)
    edu_bf = nc.dram_tensor("edu_bf", [nexp, 2, D], BF16).ap()
    if dbg:
        mod_dbg = dscr("mod_dbg", [128, 48], F32)

    with ExitStack() as es:
        S_ = Sched(nc, es)
        op = S_.op

        def sb(stack, name, shape, dt):
            return stack.enter_context(nc.sbuf_tensor("s_" + name, list(shape), dt))

        PS = [es.enter_context(nc.psum_tensor("ps%d" % i, [128, 512], F32)) for i in range(8)]
        TPS = [Tk("ps%d" % i) for i in range(8)]

        ident = sb(es, "ident", [128, 128], BF16); Tident = Tk()
        tri = sb(es, "tri", [128, 128], BF16); Ttri = Tk()
        ones = sb(es, "ones", [128, 128], BF16); Tones = Tk()
        identf = sb(es, "identf", [128, 128], F32); Tidentf = Tk()
        modT = sb(es, "modT", [128, 48], F32); TmodT = Tk()
        G1 = sb(es, "G1", [128, 8], F32); TG1 = Tk()
        G2 = sb(es, "G2", [128, 8], F32); TG2 = Tk()
        bcs = sb(es, "bcs", [128, 5, D], F32)
        Tbcs = [Tk() for _ in range(5)]
        gout = sb(es, "gout", [128, 2, HD], F32); Tgout = Tk()
        flags = sb(es, "flags_sb", [8, 2], F32); Tflags = Tk()

        ch_c = S_.chan("const")
        ch_ld = [S_.chan("ld%d" % i) for i in range(4)]
        ch_st = [S_.chan("st%d" % i) for i in range(4)]
        ch_kts = [S_.chan("kts%d" % i) for i in range(4)]
        ch_aug = [S_.chan("aug%d" % i) for i in range(3)]
        ch_hd = [[S_.chan("hd%d_%d" % (i, j)) for j in range(3)] for i in range(2)]
        ch_stg = [S_.chan("stg%d" % i) for i in range(2)]

        op("pool", lambda e: e.memset(ident[:], 1.0), writes=[Tident])
        op("pool", lambda e: e.affine_select(out=ident[:], in_=ident[:], pattern=[[-1, 128]],
                                             compare_op=ALU.is_equal, fill=0.0, base=0, channel_multiplier=1),
           reads=[Tident], writes=[Tident])
        op("pool", lambda e: e.memset(tri[:], 1.0), writes=[Ttri])
        op("pool", lambda e: e.affine_select(out=tri[:], in_=tri[:], pattern=[[-1, 128]],
                                             compare_op=ALU.is_ge, fill=0.0, base=0, channel_multiplier=1),
           reads=[Ttri], writes=[Ttri])
        op("pool", lambda e: e.memset(ones[:], 1.0), writes=[Tones])
        op("pool", lambda e: e.memset(identf[:], 1.0), writes=[Tidentf])
        op("pool", lambda e: e.affine_select(out=identf[:], in_=identf[:], pattern=[[-1, 128]],
                                             compare_op=ALU.is_equal, fill=0.0, base=0, channel_multiplier=1),
           reads=[Tidentf], writes=[Tidentf])
        op("sp", lambda e: e.dma_start(out=gout[:, 0, :], in_=gfox_row.partition_broadcast(128)), writes=[Tgout], chan=S_.chan("k1"))
        op("sp", lambda e: e.dma_start(out=gout[:, 1, :], in_=gsb_row.partition_broadcast(128)), writes=[Tgout], chan=S_.chan("k2"))
        op("sp", lambda e: e.dma_start(out=flags[:], in_=flags_d), writes=[Tflags], chan=S_.chan("k3"))

        with ExitStack() as p0:
            cT = sb(p0, "cT", [128, 8], F32); TcT = Tk()
            scT = sb(p0, "scT", [128, 8], F32); TscT = Tk()
            screp = sb(p0, "screp", [128, 8, 128], F32); Tscrep = Tk()
            wa = [sb(p0, "wa%d" % i, [128, 8, 512], F32) for i in range(2)]; Twa = [Tk(), Tk()]
            badaT = sb(p0, "badaT", [128, 48], F32); TbadaT = Tk()
            badabc = sb(p0, "badabc", [128, 4, D], F32); Tbadabc = Tk()
            gmixT = sb(p0, "gmixT", [128, 8], F32); TgmixT = Tk()
            gffnT = sb(p0, "gffnT", [128, 8], F32); TgffnT = Tk()
            gffnbc = sb(p0, "gffnbc", [128, D], F32); Tgffnbc = Tk()

            op("sp", lambda e: e.dma_start(out=cT[:], in_=cT_d), writes=[TcT], chan=S_.chan("k4"))
            op("sp", lambda e: e.dma_start(out=badaT[:], in_=badaT_d), writes=[TbadaT], chan=S_.chan("k5"))
            op("sp", lambda e: e.dma_start(out=gmixT[:], in_=gmixT_d), writes=[TgmixT], chan=S_.chan("k6"))
            op("sp", lambda e: e.dma_start(out=gffnT[:], in_=gffnT_d), writes=[TgffnT], chan=S_.chan("k7"))
            op("sp", lambda e: e.dma_start(out=gffnbc[:], in_=gffn_row.partition_broadcast(128)), writes=[Tgffnbc], chan=S_.chan("k8"))
            op("sp", lambda e: e.dma_start(out=bcs[:, 4, :], in_=gfin_row.partition_broadcast(128)), writes=[Tbcs[4]], chan=S_.chan("k9"))
            op("sp", lambda e: e.dma_start(out=badabc[:].rearrange("p a d -> p (a d)"),
                                           in_=bada_row[:, 2 * D:6 * D].partition_broadcast(128)), writes=[Tbadabc], chan=S_.chan("k10"))
            op("act", lambda e: e.activation(out=scT[:], in_=cT[:], func=AF.Silu), reads=[TcT], writes=[TscT])
            op("dve", lambda e: e.tensor_copy(out=screp[:], in_=scT[:].unsqueeze(2).to_broadcast([128, 8, 128])),
               reads=[TscT], writes=[Tscrep])
            w_ada_v = w_ada.rearrange("(k p) c -> p k c", p=128)
            modps = PS[0]
            for cc in range(12):
                b = cc % 2
                op("sp", lambda e, cc=cc, b=b: e.dma_start(out=wa[b][:], in_=w_ada_v[:, :, cc * 512:(cc + 1) * 512]),
                   writes=[Twa[b]], chan=ch_ld[b])
                for f4 in range(4):
                    fc = cc * 4 + f4
                    for k in range(8):
                        op("pe", lambda e, b=b, k=k, f4=f4, fc=fc: e.matmul(
                            modps[:, fc:fc + 1], lhsT=wa[b][:, k, f4 * 128:(f4 + 1) * 128], rhs=scT[:, k:k + 1],
                            start=(k == 0), stop=(k == 7)), reads=[Twa[b], TscT], writes=[TPS[0]])
                if cc >= 4:
                    a = (cc - 4) // 2
                    hh = (cc - 4) % 2
                    pb = PS[1 + (cc % 2)]
                    Tpb = TPS[1 + (cc % 2)]
                    for k in range(8):
                        op("pe", lambda e, b=b, k=k, pb=pb: e.matmul(pb[:, :], lhsT=screp[:, k, :], rhs=wa[b][:, k, :],
                                                                   start=(k == 0), stop=(k == 7)),
                           reads=[Twa[b], Tscrep], writes=[Tpb])
                    op("dve", lambda e, a=a, hh=hh, pb=pb: e.tensor_tensor(
                        out=bcs[:, a, hh * 512:(hh + 1) * 512], in0=pb[:, :], in1=badabc[:, a, hh * 512:(hh + 1) * 512],
                        op=ALU.add), reads=[Tpb, Tbadabc], writes=[Tbcs[a]])
            op("dve", lambda e: e.tensor_tensor(out=modT[:], in0=modps[:, 0:48], in1=badaT[:], op=ALU.add),
               reads=[TPS[0], TbadaT], writes=[TmodT])
            op("dve", lambda e: e.scalar_tensor_tensor(out=G1[:], in0=modT[:, 8:16], scalar=1.0, in1=gmixT[:],
                                                       op0=ALU.add, op1=ALU.mult), reads=[TmodT, TgmixT], writes=[TG1])
            op("dve", lambda e: e.scalar_tensor_tensor(out=G2[:], in0=modT[:, 32:40], scalar=1.0, in1=gffnT[:],
                                                       op0=ALU.add, op1=ALU.mult), reads=[TmodT, TgffnT], writes=[TG2])
            op("dve", lambda e: e.scalar_tensor_tensor(out=bcs[:, 2, :], in0=bcs[:, 2, :], scalar=1.0, in1=gffnbc[:],
                                                       op0=ALU.add, op1=ALU.mult), reads=[Tbcs[2], Tgffnbc], writes=[Tbcs[2]])
            if dbg:
                op("sp", lambda e: e.dma_start(out=mod_dbg, in_=modT[:]), reads=[TmodT], chan=ch_st[0])
            S_.barrier()

        def norm_chunk(xsrc_ap, xt, Txt, ss, Tss, rstd, Trstd, xn, Txn, junk, Tjunk, hT, ThT, Gc, TGc, Bc_ap, TBc, ldchan,
                       psA, psB):
            op("sp", lambda e: e.dma_start(out=xt[:], in_=xsrc_ap.rearrange("(j p) d -> p j d", p=128)),
               writes=[Txt], chan=ldchan)
            for j in range(4):
                op("act", lambda e, j=j: e.activation(out=junk[:], in_=xt[:, j, :], func=AF.Square,
                                                      accum_out=ss[:, j:j + 1]), reads=[Txt], writes=[Tjunk, Tss])
            op("act", lambda e: e.activation(out=rstd[:], in_=ss[:], func=AF.Sqrt, scale=1.0 / D, bias=EPS),
               reads=[Tss], writes=[Trstd])
            op("dve", lambda e: e.reciprocal(out=rstd[:], in_=rstd[:]), reads=[Trstd], writes=[Trstd])
            for j in range(4):
                op("dve", lambda e, j=j: e.tensor_scalar(out=xn[:, j, :], in0=xt[:, j, :], scalar1=rstd[:, j:j + 1],
                                                         scalar2=None, op0=ALU.mult), reads=[Txt, Trstd], writes=[Txn])
            for k in range(8):
                pi = psA if k % 2 == 0 else psB
                pt = PS[pi][:].bitcast(BF16)
                for j in range(4):
                    op("pe", lambda e, j=j, k=k, pt=pt: e.transpose(pt[:, j * 128:(j + 1) * 128], xn[:, j, k * 128:(k + 1) * 128],
                                                                   ident[:]), reads=[Txn, Tident], writes=[TPS[pi]])
                op("act", lambda e, k=k, pt=pt: e.activation(out=hT[:, k, :], in_=pt[:, 0:512], func=AF.Identity,
                                                             scale=Gc[:, k:k + 1], bias=Bc_ap[:, k:k + 1]),
                   reads=[TPS[pi], TGc, TBc], writes=[ThT])

        with ExitStack() as p1:
            Wb = sb(p1, "Wb", [128, 8, INC], BF16); TWb = Tk()
            xt = [sb(p1, "xt%d" % i, [128, 4, D], F32) for i in range(2)]; Txt = [Tk(), Tk()]
            xn = sb(p1, "xn", [128, 4, D], BF16); Txn = Tk()
            junk = sb(p1, "junk", [128, D], BF16); Tjunk = Tk()
            ss = [sb(p1, "ss%d" % i, [128, 4], F32) for i in range(2)]; Tss = [Tk(), Tk()]
            rstd = [sb(p1, "rstd%d" % i, [128, 4], F32) for i in range(2)]; Trstd = [Tk(), Tk()]
            hT = sb(p1, "hT", [128, 8, 512], BF16); ThT = Tk()
            KTs = [sb(p1, "KTs%d" % i, [128, 512], BF16) for i in range(4)]; TKTs = [Tk() for _ in range(4)]
            Vs = [sb(p1, "Vs%d" % i, [128, 4, NH, 65], BF16) for i in range(2)]; TVs = [Tk(), Tk()]
            tribig = sb(p1, "tribig", [128, 4, 512], F32); Ttribig = Tk()
            bfbc = sb(p1, "bfbc", [128, 8], F32); Tbfbc = Tk()
            ub = sb(p1, "ub", [128, 4, 8], F32); Tub = Tk()
            lf = sb(p1, "lf", [128, 4, 8], F32); Tlf = Tk()
            GT = [sb(p1, "GT%d" % i, [8, 512], F32) for i in range(2)]; TGT = [Tk(), Tk()]
            gz = sb(p1, "gz", [8, 1], F32); Tgz = Tk()
            sp_hi = sb(p1, "sp_hi", [8, 3, 512], BF16); Tsp_hi = Tk()
            r1 = sb(p1, "r1", [8, 512], F32); Tr1 = Tk()
            r2 = sb(p1, "r2", [8, 512], F32); Tr2 = Tk()
            gq = sb(p1, "gq", [8, 512], F32); Tgq = Tk()
            spq = sb(p1, "spq", [8, 3, 512], BF16); Tspq = Tk()
            cst = sb(p1, "cst", [8, 3, 512], BF16); Tcst = Tk()

            w_in_v = w_in.rearrange("(k p) c -> p k c", p=128)
            wst = [xt[i][:].rearrange("p j d -> p (j d)")[:, 0:3520].rearrange("p (k c) -> p k c", k=8) for i in range(2)]
            Twst = Txt
            for i in range(7):
                b = i % 2
                op("sp", lambda e, i=i, b=b: e.dma_start(out=wst[b], in_=w_in_v[:, :, i * 440:(i + 1) * 440]),
                   writes=[Twst[b]], chan=ch_ld[b])
                eng = "dve" if i % 2 == 0 else "pool"
                op(eng, lambda e, i=i, b=b: e.tensor_copy(out=Wb[:, :, i * 440:(i + 1) * 440], in_=wst[b]),
                   reads=[Twst[b]], writes=[TWb])
            op("pool", lambda e: e.memset(tribig[:], 1.0), writes=[Ttribig])
            for j in range(4):
                op("pool", lambda e, j=j: e.affine_select(out=tribig[:, j, :], in_=tribig[:, j, :], pattern=[[1, 512]],
                                                          compare_op=ALU.is_ge, fill=0.0, base=-j * 128, channel_multiplier=-1),
                   reads=[Ttribig], writes=[Ttribig])
            op("sp", lambda e: e.dma_start(out=bfbc[:], in_=bf_row.partition_broadcast(128)), writes=[Tbfbc], chan=S_.chan("k11"))
            op("pool", lambda e: e.memset(gz[:], 0.0), writes=[Tgz])
            for i in range(2):
                op("pool", lambda e, i=i: e.memset(Vs[i][:], 1.0), writes=[TVs[i]])
            op("pool", lambda e: e.memset(cst[:], -1.0), writes=[Tcst])
            for c in range(NT):
                op("pool", lambda e, c=c: e.dma_start(
                    out=KT_d[0:8, 67:70, c * 512:(c + 1) * 512], in_=cst[:]),
                   reads=[Tcst], chan=ch_aug[0])
            op("pool", lambda e: e.memset(cst[:], 1.0), reads=[], writes=[Tcst])
            for c in range(NTQ):
                op("pool", lambda e, c=c: e.dma_start(
                    out=QT_d[0:8, 64:67, c * 512:(c + 1) * 512], in_=cst[:]),
                   reads=[Tcst], chan=ch_aug[0])

            o1 = 1536
            o2 = 1544
            kcols = [512 + 128 * i for i in range(4)] + [o2 + 512 + 128 * i for i in range(4)]
            qcols = [0 + 128 * i for i in range(4)] + [o2 + 128 * i for i in range(4)]
            vcols = [1024, o2 + 1024]

            def proj_T(cols_list, dst_d, c, scale, rr):
                for hp in range(8):
                    pi = 2 + (rr[0] % 2); rr[0] += 1
                    for k in range(8):
                        op("pe", lambda e, hp=hp, k=k, pi=pi: e.matmul(
                            PS[pi][:, :], lhsT=Wb[:, k, cols_list[hp]:cols_list[hp] + 128], rhs=hT[:, k, :],
                            start=(k == 0), stop=(k == 7)), reads=[TWb, ThT], writes=[TPS[pi]])
                    kb = rr[1] % 4; rr[1] += 1
                    op("act", lambda e, pi=pi, kb=kb: e.activation(out=KTs[kb][:], in_=PS[pi][:, :], func=AF.Copy, scale=scale),
                       reads=[TPS[pi]], writes=[TKTs[kb]])
                    for i2 in range(2):
                        op("pool", lambda e, hp=hp, kb=kb, i2=i2: e.dma_start(
                            out=dst_d[2 * hp + i2, 0:64, c * 512:(c + 1) * 512],
                            in_=KTs[kb][i2 * 64:(i2 + 1) * 64, :]), reads=[TKTs[kb]], chan=ch_kts[kb])

            rr = [0, 0]
            for c in range(NT):
                b = c % 2
                norm_chunk(x_all[c * 512:(c + 1) * 512, :], xt[b], Txt[b], ss[b], Tss[b], rstd[b], Trstd[b], xn, Txn,
                           junk, Tjunk, hT, ThT, G1, TG1, modT[:, 0:8], TmodT, ch_ld[b], 0, 1)
                proj_T(kcols, KT_d, c, 1.0, rr)
                vb = c % 2
                for j in range(4):
                    for g in range(2):
                        pi = 4 + ((j * 2 + g) % 2)
                        for k in range(8):
                            op("pe", lambda e, j=j, g=g, k=k, pi=pi: e.matmul(
                                PS[pi][:, :], lhsT=hT[:, k, j * 128:(j + 1) * 128], rhs=Wb[:, k, vcols[g]:vcols[g] + 512],
                                start=(k == 0), stop=(k == 7)), reads=[TWb, ThT], writes=[TPS[pi]])
                        op("dve", lambda e, j=j, g=g, pi=pi, vb=vb: e.tensor_copy(
                            out=Vs[vb][:, j, g * 8:(g + 1) * 8, 0:64], in_=PS[pi][:, :].rearrange("p (h d) -> p h d", d=64)),
                           reads=[TPS[pi]], writes=[TVs[vb]])
                for j in range(4):
                    op("pool", lambda e, j=j, vb=vb, c=c: e.dma_start(
                        out=V_d[:, :, 4 * c + j, :].rearrange("h p e -> p h e"), in_=Vs[vb][:, j, :, :]),
                       reads=[TVs[vb]], chan=ch_st[2 + vb])
                for j in range(4):
                    for k in range(8):
                        op("pe", lambda e, j=j, k=k: e.matmul(
                            PS[6][:, j * 8:(j + 1) * 8], lhsT=hT[:, k, j * 128:(j + 1) * 128], rhs=Wb[:, k, o1:o1 + 8],
                            start=(k == 0), stop=(k == 7)), reads=[TWb, ThT], writes=[TPS[6]])
                op("dve", lambda e: e.tensor_tensor(out=ub[:], in0=PS[6][:, 0:32].rearrange("p (j h) -> p j h", h=8),
                                                    in1=bfbc[:].unsqueeze(1).to_broadcast([128, 4, 8]), op=ALU.add),
                   reads=[TPS[6], Tbfbc], writes=[Tub])
                op("act", lambda e: e.activation(out=ub[:], in_=ub[:], func=AF.Exp, scale=-1.0), reads=[Tub], writes=[Tub])
                op("act", lambda e: e.activation(out=lf[:], in_=ub[:], func=AF.Ln, bias=1.0), reads=[Tub], writes=[Tlf])
                for j in range(4):
                    op("pe", lambda e, j=j: e.matmul(PS[7][0:8, :], lhsT=lf[:, j, :], rhs=tribig[:, j, :],
                                                     start=(j == 0), stop=(j == 3)), reads=[Tlf, Ttribig], writes=[TPS[7]])
                gcur, Tgcur = GT[c % 2], TGT[c % 2]
                if c == 0:
                    carry_ap, Tcarry = gz[:, 0:1], Tgz
                else:
                    carry_ap, Tcarry = GT[(c - 1) % 2][:, 511:512], TGT[(c - 1) % 2]
                op("dve", lambda e, gcur=gcur, carry_ap=carry_ap: e.tensor_scalar(
                    out=gcur[:], in0=PS[7][0:8, :], scalar1=carry_ap, scalar2=None, op0=ALU.add),
                   reads=[TPS[7], Tcarry], writes=[Tgcur])

                def split3(src, Tsrc, dst, Tdst):
                    op("dve", lambda e: e.tensor_copy(out=dst[:, 0, :], in_=src[:]), reads=[Tsrc], writes=[Tdst])
                    op("dve", lambda e: e.tensor_tensor(out=r1[:], in0=src[:], in1=dst[:, 0, :], op=ALU.subtract),
                       reads=[Tsrc, Tdst], writes=[Tr1])
                    op("dve", lambda e: e.tensor_copy(out=dst[:, 1, :], in_=r1[:]), reads=[Tr1], writes=[Tdst])
                    op("dve", lambda e: e.tensor_tensor(out=r2[:], in0=r1[:], in1=dst[:, 1, :], op=ALU.subtract),
                       reads=[Tr1, Tdst], writes=[Tr2])
                    op("dve", lambda e: e.tensor_copy(out=dst[:, 2, :], in_=r2[:]), reads=[Tr2], writes=[Tdst])

                split3(gcur, Tgcur, sp_hi, Tsp_hi)
                op("pool", lambda e, c=c: e.dma_start(out=KT_d[0:8, 64:67, c * 512:(c + 1) * 512], in_=sp_hi[:]),
                   reads=[Tsp_hi], chan=ch_aug[1])
                m, ph = c // 4, c % 4
                if ph in (0, 2):
                    fl = flags[:, 0:1] if ph == 0 else flags[:, 1:2]
                    op("dve", lambda e, fl=fl, gcur=gcur: e.tensor_scalar(out=gq[:], in0=gcur[:], scalar1=fl, scalar2=None,
                                                                          op0=ALU.mult), reads=[Tgcur, Tflags], writes=[Tgq])
                else:
                    fl = flags[:, 1:2] if ph == 1 else flags[:, 0:1]
                    op("dve", lambda e, fl=fl, gcur=gcur: e.scalar_tensor_tensor(out=gq[:], in0=gcur[:], scalar=fl, in1=gq[:],
                                                                                 op0=ALU.mult, op1=ALU.add),
                       reads=[Tgcur, Tflags, Tgq], writes=[Tgq])
                    split3(gq, Tgq, spq, Tspq)
                    lt = 2 * m + (0 if ph == 1 else 1)
                    op("pool", lambda e, lt=lt: e.dma_start(out=QT_d[0:8, 67:70, lt * 512:(lt + 1) * 512], in_=spq[:]),
                       reads=[Tspq], chan=ch_aug[2])

            for c in range(NTQ):
                b = c % 2
                norm_chunk(x_q[c * 512:(c + 1) * 512, :], xt[b], Txt[b], ss[b], Tss[b], rstd[b], Trstd[b], xn, Txn,
                           junk, Tjunk, hT, ThT, G1, TG1, modT[:, 0:8], TmodT, ch_ld[b], 0, 1)
                proj_T(qcols, QT_d, c, 0.125, rr)
            S_.barrier()

        if 2 in phases:
          with ExitStack() as p2:
            maskF = sb(p2, "maskF", [128, 16, 512], BF16); TmaskF = Tk()
            maskS = sb(p2, "maskS", [128, 16, 512], BF16); TmaskS = Tk()
            KTh = [sb(p2, "KTh%d" % i, [KA, S], BF16) for i in range(2)]; TKTh = [Tk(), Tk()]
            Vh = [sb(p2, "Vh%d" % i, [128, NB, 65], BF16) for i in range(2)]; TVh = [Tk(), Tk()]
            QTh = [sb(p2, "QTh%d" % i, [KA, SQ], BF16) for i in range(2)]; TQTh = [Tk(), Tk()]
            negQ = [sb(p2, "negQ%d" % i, [64, 512], BF16) for i in range(2)]; TnegQ = [Tk(), Tk()]
            zt = [sb(p2, "zt%d" % i, [128, 512], F32) for i in range(2)]; Tzt = [Tk(), Tk()]
            ee = [sb(p2, "ee%d" % i, [128, 512], F32) for i in range(2)]; Tee = [Tk(), Tk()]
            spb = [sb(p2, "spb%d" % i, [128, 512], BF16) for i in range(2)]; Tspb = [Tk(), Tk()]
            LL = [sb(p2, "LL%d" % i, [128, 512], F32) for i in range(2)]; TLL = [Tk(), Tk()]
            PT = [sb(p2, "PT%d" % i, [128, 512], BF16) for i in range(2)]; TPT = [Tk(), Tk()]
            Rsb = sb(p2, "Rsb", [128, 512], F32); TRsb = Tk()
            osb = sb(p2, "osb", [128, 4, 64], F32); Tosb = Tk()
            sq = sb(p2, "sq", [128, 4, 64], F32); Tsq = Tk()
            o2 = sb(p2, "o2", [128, 4, 64], F32); To2 = Tk()
            omix = [sb(p2, "omix%d" % i, [128, 4, 64], BF16) for i in range(2)]; Tomix = [Tk(), Tk()]
            ssq = sb(p2, "ssq", [128, 4], F32); Tssq = Tk()
            rinv = sb(p2, "rinv", [128, 4], F32); Trinv = Tk()
            OTs = sb(p2, "OTs", [65, 512], F32); TOTs = Tk()

            op("sp", lambda e: e.dma_start(out=maskF[:], in_=masks_d), writes=[TmaskF], chan=S_.chan("k12"))
            op("sp", lambda e: e.dma_start(out=maskS[:], in_=masks_sb_d), writes=[TmaskS], chan=S_.chan("k13"))

            def block_list(lt):
                kind, m = lt % 2, lt // 2
                if kind == 0:
                    masked = [(4 * m + 1, 0), (4 * m, 1)]
                    lower = list(range(4 * m - 1, -1, -1))
                else:
                    masked = [(4 * m + 3, 2), (4 * m + 2, 3)]
                    lower = list(range(4 * m + 1, -1, -1))
                bl = []
                for (T_, ms) in masked:
                    for i in (3, 2, 1, 0):
                        bl.append((4 * T_ + i, ms * 4 + i))
                for T_ in lower:
                    for i in (3, 2, 1, 0):
                        bl.append((4 * T_ + i, None))
                return bl

            bctr = [0]
            octr = [0]
            for h in range(NH):
                hb = h % 2
                is_fox = h < 8
                op("sp", lambda e, h=h, hb=hb: e.dma_start(out=KTh[hb][:], in_=KT_d[h]), writes=[TKTh[hb]], chan=ch_hd[hb][0])
                op("sp", lambda e, h=h, hb=hb: e.dma_start(out=Vh[hb][:], in_=V_d[h]), writes=[TVh[hb]], chan=ch_hd[hb][1])
                op("sp", lambda e, h=h, hb=hb: e.dma_start(out=QTh[hb][:], in_=QT_d[h]), writes=[TQTh[hb]], chan=ch_hd[hb][2])
                kt, vt, qt = KTh[hb], Vh[hb], QTh[hb]
                Tkt, Tvt, Tqt = TKTh[hb], TVh[hb], TQTh[hb]
                for lt in range(NTQ):
                    bl = block_list(lt)
                    n = len(bl)
                    ob = 6 + (octr[0] % 2); octr[0] += 1
                    Ops, TOps = PS[ob], TPS[ob]
                    Ov = Ops[:, 0:260].rearrange("p (j e) -> p j e", e=65)
                    qs = slice(lt * 512, (lt + 1) * 512)
                    base = bctr[0]; bctr[0] += n
                    if is_fox:
                        def A1(k):
                            kb, mi = bl[k]
                            b = (base + k) % 2
                            op("pe", lambda e: e.matmul(PS[b][:, :], lhsT=kt[0:KA, kb * 128:(kb + 1) * 128], rhs=qt[0:KA, qs],
                                                        start=True, stop=True), reads=[Tkt, Tqt], writes=[TPS[b]])
                            if mi is not None:
                                op("dve", lambda e: e.tensor_tensor(out=zt[b][:], in0=PS[b][:, :], in1=maskF[:, mi, :], op=ALU.add),
                                   reads=[TPS[b], TmaskF], writes=[Tzt[b]])
                                op("act", lambda e: e.activation(out=PT[b][:], in_=zt[b][:], func=AF.Exp), reads=[Tzt[b]], writes=[TPT[b]])
                            else:
                                op("act", lambda e: e.activation(out=PT[b][:], in_=PS[b][:, :], func=AF.Exp), reads=[TPS[b]], writes=[TPT[b]])

                        def B(k):
                            kb, mi = bl[k]
                            b = (base + k) % 2
                            op("pe", lambda e: e.matmul(Ops[0:65, :], lhsT=vt[:, kb, :], rhs=PT[b][:, :],
                                                        start=(k == 0), stop=(k == n - 1)), reads=[TPT[b], Tvt], writes=[TOps])
                        for k in range(n + 1):
                            if k < n:
                                A1(k)
                            if k >= 1:
                                B(k - 1)
                    else:
                        nq = negQ[octr[0] % 2]; Tnq = TnegQ[octr[0] % 2]
                        op("pool", lambda e: e.tensor_scalar(out=nq[:], in0=qt[0:64, qs], scalar1=-1.0, scalar2=None, op0=ALU.mult),
                           reads=[Tqt], writes=[Tnq])
                        op("pool", lambda e: e.memset(Rsb[:], 0.0), writes=[TRsb])

                        def A1(k):
                            kb, mi = bl[k]
                            b = (base + k) % 2
                            op("pe", lambda e: e.matmul(PS[b][:, :], lhsT=kt[0:64, kb * 128:(kb + 1) * 128], rhs=qt[0:64, qs],
                                                        start=True, stop=True), reads=[Tkt, Tqt], writes=[TPS[b]])
                            if mi is not None:
                                op("dve", lambda e: e.tensor_tensor(out=zt[b][:], in0=PS[b][:, :], in1=maskS[:, mi, :], op=ALU.add),
                                   reads=[TPS[b], TmaskS], writes=[Tzt[b]])
                                op("act", lambda e: e.activation(out=ee[b][:], in_=zt[b][:], func=AF.Exp), reads=[Tzt[b]], writes=[Tee[b]])
                            else:
                                op("act", lambda e: e.activation(out=ee[b][:], in_=PS[b][:, :], func=AF.Exp), reads=[TPS[b]], writes=[Tee[b]])
                            op("act", lambda e: e.activation(out=spb[b][:], in_=ee[b][:], func=AF.Ln, bias=1.0), reads=[Tee[b]], writes=[Tspb[b]])

                        def A2(k):
                            kb, mi = bl[k]
                            b = (base + k) % 2
                            op("pe", lambda e: e.matmul(PS[2 + b][:, :], lhsT=tri[:], rhs=spb[b][:], start=True, stop=False),
                               reads=[Ttri, Tspb[b]], writes=[TPS[2 + b]])
                            op("pe", lambda e: e.matmul(PS[2 + b][:, :], lhsT=kt[0:64, kb * 128:(kb + 1) * 128], rhs=nq[:],
                                                        start=False, stop=True), reads=[Tkt, Tnq], writes=[TPS[2 + b]])
                            if k < n - 1:
                                op("pe", lambda e: e.matmul(PS[4 + b][:, :], lhsT=ones[:], rhs=spb[b][:], start=True, stop=True),
                                   reads=[Tones, Tspb[b]], writes=[TPS[4 + b]])
                            op("dve", lambda e: e.tensor_tensor(out=LL[b][:], in0=PS[2 + b][:, :], in1=Rsb[:], op=ALU.add),
                               reads=[TPS[2 + b], TRsb], writes=[TLL[b]])
                            if mi is not None:
                                op("dve", lambda e: e.tensor_tensor(out=LL[b][:], in0=LL[b][:], in1=maskS[:, mi, :], op=ALU.subtract),
                                   reads=[TLL[b], TmaskS], writes=[TLL[b]])
                            if k < n - 1:
                                op("dve", lambda e: e.tensor_tensor(out=Rsb[:], in0=PS[4 + b][:, :], in1=Rsb[:], op=ALU.add),
                                   reads=[TPS[4 + b], TRsb], writes=[TRsb])

                        def B(k):
                            kb, mi = bl[k]
                            b = (base + k) % 2
                            op("act", lambda e: e.activation(out=PT[b][:], in_=LL[b][:], func=AF.Exp, scale=-1.0),
                               reads=[TLL[b]], writes=[TPT[b]])
                            op("pe", lambda e: e.matmul(Ops[0:64, :], lhsT=vt[:, kb, 0:64], rhs=PT[b][:, :],
                                                        start=(k == 0), stop=(k == n - 1)), reads=[TPT[b], Tvt], writes=[TOps])
                        for k in range(n + 2):
                            if k < n:
                                A1(k)
                            if 1 <= k <= n:
                                A2(k - 1)
                            if k >= 2:
                                B(k - 2)
                    nr = 65 if is_fox else 64
                    op("act", lambda e: e.activation(out=OTs[0:nr, :], in_=Ops[0:nr, :], func=AF.Copy), reads=[TOps], writes=[TOTs])
                    Ov = PS[5][:, 0:260].rearrange("p (j e) -> p j e", e=65)
                    for j in range(4):
                        op("pe", lambda e, j=j: e.transpose(Ov[:, j, 0:nr], OTs[0:nr, j * 128:(j + 1) * 128], identf[0:nr, 0:nr]),
                           reads=[TOTs, Tidentf], writes=[TPS[5]])
                    TOps = TPS[5]
                    if is_fox:
                        op("dve", lambda e: e.reciprocal(out=rinv[:].unsqueeze(2), in_=Ov[:, :, 64:65]), reads=[TOps], writes=[Trinv])
                        op("dve", lambda e: e.tensor_tensor(out=osb[:], in0=Ov[:, :, 0:64],
                                                            in1=rinv[:].unsqueeze(2).to_broadcast([128, 4, 64]), op=ALU.mult),
                           reads=[TOps, Trinv], writes=[Tosb])
                    else:
                        op("dve", lambda e: e.tensor_copy(out=osb[:], in_=Ov[:, :, 0:64]), reads=[TOps], writes=[Tosb])
                    op("pool", lambda e: e.tensor_tensor(out=sq[:], in0=osb[:], in1=osb[:], op=ALU.mult), reads=[Tosb], writes=[Tsq])
                    op("dve", lambda e: e.tensor_reduce(out=ssq[:], in_=sq[:], axis=AX.X, op=ALU.add), reads=[Tsq], writes=[Tssq])
                    op("act", lambda e: e.activation(out=ssq[:], in_=ssq[:], func=AF.Sqrt, scale=1.0 / HD, bias=EPS),
                       reads=[Tssq], writes=[Tssq])
                    op("dve", lambda e: e.reciprocal(out=ssq[:], in_=ssq[:]), reads=[Tssq], writes=[Tssq])
                    op("pool", lambda e: e.tensor_tensor(out=o2[:], in0=osb[:], in1=ssq[:].unsqueeze(2).to_broadcast([128, 4, 64]),
                                                         op=ALU.mult), reads=[Tosb, Tssq], writes=[To2])
                    om = octr[0] % 2
                    gi = 0 if is_fox else 1
                    op("pool", lambda e, om=om, gi=gi: e.tensor_tensor(out=omix[om][:], in0=o2[:],
                                                                       in1=gout[:, gi, :].unsqueeze(1).to_broadcast([128, 4, 64]),
                                                                       op=ALU.mult), reads=[To2, Tgout], writes=[Tomix[om]])
                    op("pool", lambda e, om=om, h=h, lt=lt: e.dma_start(
                        out=mixed_d[lt * 512:(lt + 1) * 512, h * 64:(h + 1) * 64].rearrange("(j p) d -> p j d", p=128),
                        in_=omix[om][:]), reads=[Tomix[om]], chan=ch_st[om])
            S_.barrier()

        if 3 in phases:
          with ExitStack() as p3:
            NU, GS = 3, 4
            WoB = sb(p3, "WoB", [128, 8, D], BF16); TWoB = Tk()
            WqB = sb(p3, "WqB", [128, 8, 2048], BF16); TWqB = Tk()
            SKb = sb(p3, "SKb", [128, 16, 128], BF16); TSKb = Tk()
            with ExitStack() as pc:
                stgf = [sb(pc, "stgf%d" % i, [128, 4096], F32) for i in range(3)]; Tstgf = [Tk() for _ in range(3)]
                stgb = [sb(pc, "stgb%d" % i, [128, 4096], BF16) for i in range(3)]; Tstgb = [Tk() for _ in range(3)]
                ch_si = [S_.chan("si%d" % i) for i in range(3)]
                ch_so = [S_.chan("so%d" % i) for i in range(3)]
                cengs = ["dve", "act", "pool"]
                sctr = [0]

                def stage_cast(src_ap, shape_str, kw, dst_ap, Tdst):
                    i = sctr[0] % 3; sctr[0] += 1
                    n = 1
                    for v in src_ap.shape[1:]:
                        n *= v
                    stg = stgf[i][:, 0:n].rearrange(shape_str, **kw)
                    op("sp", lambda e: e.dma_start(out=stg, in_=src_ap), writes=[Tstgf[i]], chan=ch_si[i])
                    if cengs[i] == "act":
                        op("act", lambda e: e.activation(out=dst_ap, in_=stg, func=AF.Copy), reads=[Tstgf[i]], writes=[Tdst])
                    else:
                        op(cengs[i], lambda e: e.tensor_copy(out=dst_ap, in_=stg), reads=[Tstgf[i]], writes=[Tdst])
                wo_v = w_out.rearrange("(k p) c -> p k c", p=128)
                wq_v = w_query.rearrange("(k p) c -> p k c", p=128)
                for i in range(2):
                    stage_cast(wo_v[:, :, i * 512:(i + 1) * 512], "p (k c) -> p k c", dict(k=8), WoB[:, :, i * 512:(i + 1) * 512], TWoB)
                for i in range(4):
                    stage_cast(wq_v[:, :, i * 512:(i + 1) * 512], "p (k c) -> p k c", dict(k=8), WqB[:, :, i * 512:(i + 1) * 512], TWqB)
                stage_cast(skT_d, "p (k c) -> p k c", dict(k=16), SKb[:], TSKb)
                RPP = nexp // 128
                for (tsrc, tdst) in ((e_down, edu_bf[:, 0, :]), (e_up, edu_bf[:, 1, :])):
                    sv = tsrc.rearrange("(r p) d -> p r d", p=128)
                    dv = tdst.rearrange("(r p) d -> p r d", p=128)
                    for c4 in range(max(1, RPP // 4)):
                        nr = min(4, RPP)
                        i = sctr[0] % 3; sctr[0] += 1
                        sf = stgf[i][:, 0:nr * D].rearrange("p (r d) -> p r d", d=D)
                        sbv = stgb[i][:, 0:nr * D].rearrange("p (r d) -> p r d", d=D)
                        op("sp", lambda e: e.dma_start(out=sf, in_=sv[:, c4 * 4:c4 * 4 + nr, :]), writes=[Tstgf[i]], chan=ch_si[i])
                        if cengs[i] == "act":
                            op("act", lambda e: e.activation(out=sbv, in_=sf, func=AF.Copy), reads=[Tstgf[i]], writes=[Tstgb[i]])
                        else:
                            op(cengs[i], lambda e: e.tensor_copy(out=sbv, in_=sf), reads=[Tstgf[i]], writes=[Tstgb[i]])
                        op("sp", lambda e: e.dma_start(out=dv[:, c4 * 4:c4 * 4 + nr, :], in_=sbv), reads=[Tstgb[i]], chan=ch_so[i])
                S_.barrier()
            U = [sb(p3, "U%d" % i, [128, GS, 2 * D], BF16) for i in range(NU)]
            TU = [[Tk() for _ in range(GS)] for _ in range(NU)]
            ch_g = [[S_.chan("g%d_%d" % (i, s)) for s in range(GS)] for i in range(NU)]
            dg = [sb(p3, "dg%d" % i, [128, 128], BF16) for i in range(4)]; Tdg = [Tk() for _ in range(4)]
            mxt = [sb(p3, "mxt%d" % i, [128, D], BF16) for i in range(2)]; Tmxt = [Tk(), Tk()]
            xqt = [sb(p3, "xqt%d" % i, [128, D], F32) for i in range(2)]; Txqt = [Tk(), Tk()]
            mT = sb(p3, "mT", [128, 8, 128], BF16); TmT = Tk()
            x1b = [sb(p3, "x1_%d" % i, [128, D], F32) for i in range(2)]; Tx1b = [Tk(), Tk()]
            xn2 = sb(p3, "xn2", [128, D], BF16); Txn2 = Tk()
            h2b = [sb(p3, "h2_%d" % i, [128, D], BF16) for i in range(2)]; Th2b = [Tk(), Tk()]
            h2T = sb(p3, "h2T", [128, 8, 128], BF16); Th2T = Tk()
            qT = sb(p3, "qT", [128, 16, 128], BF16); TqT = Tk()
            sc = sb(p3, "sc", [128, 16, 128], F32); Tsc = Tk()
            scr = sb(p3, "scr", [128, 256], F32); Tscr = Tk()
            v16 = sb(p3, "v16", [128, 16, 16], F32); Tv16 = Tk()
            i16 = sb(p3, "i16", [128, 16, 16], U32); Ti16 = Tk()
            i16f = sb(p3, "i16f", [128, 16, 16], F32); Ti16f = Tk()
            cand = sb(p3, "cand", [128, 8, 256], F32); Tcand = Tk()
            cidx = sb(p3, "cidx", [128, 8, 256], F32); Tcidx = Tk()
            m16 = sb(p3, "m16", [128, 8, 16], F32); Tm16 = Tk()
            eidf = sb(p3, "eidf", [128, 128], F32); Teidf = Tk()
            eid2 = [sb(p3, "eid%d" % i, [128, 128], I32) for i in range(2)]; Teid2 = [Tk(), Tk()]
            gtsb = [sb(p3, "gts%d" % i, [128, 8, 16], F32) for i in range(2)]; Tgtsb = [Tk(), Tk()]
            gsum = sb(p3, "gsum", [128, 8], F32); Tgsum = Tk()
            score = sb(p3, "score", [128, 128], F32); Tscore_g = [Tk() for _ in range(32)]
            actv = sb(p3, "actv", [128, 128], F32); Tactv_g = [Tk() for _ in range(32)]
            junk3 = sb(p3, "junk3", [128, D], BF16); Tjunk3 = Tk()
            junk4 = sb(p3, "junk4", [128, D], BF16); Tjunk4 = Tk()
            x2 = sb(p3, "x2", [128, D], F32); Tx2 = Tk()
            ot0 = sb(p3, "ot0", [128, D], F32); ot = [ot0, ot0]; Tot0 = Tk(); Tot = [Tot0, Tot0]
            st3 = sb(p3, "st3", [128, 4], F32); Tst3 = Tk()
            st4 = sb(p3, "st4", [128, 4], F32); Tst4 = Tk()

            gctr = [0]
            def front_part(tb):
                b = tb % 2
                rows = slice(tb * 128, (tb + 1) * 128)
                x1, Tx1 = x1b[b], Tx1b[b]
                eid, Teid = eid2[b], Teid2[b]
                h2, Th2 = h2b[b], Th2b[b]
                gts, Tgts = gtsb[b], Tgtsb[b]
                op("sp", lambda e: e.dma_start(out=mxt[b][:], in_=mixed_d[rows, :]), writes=[Tmxt[b]], chan=ch_ld[b])
                op("sp", lambda e: e.dma_start(out=xqt[b][:], in_=x_q[rows, :]), writes=[Txqt[b]], chan=ch_ld[2 + b])
                ptb = PS[0][:].bitcast(BF16)
                for k in range(8):
                    op("pe", lambda e, k=k: e.transpose(ptb[:, k * 128:(k + 1) * 128], mxt[b][:, k * 128:(k + 1) * 128], ident[:]),
                       reads=[Tmxt[b], Tident], writes=[TPS[0]])
                op("act", lambda e: e.activation(out=mT[:].rearrange("p k t -> p (k t)"), in_=ptb[:, 0:1024], func=AF.Copy),
                   reads=[TPS[0]], writes=[TmT])
                yield
                for hf in range(2):
                    for k in range(8):
                        op("pe", lambda e, k=k, hf=hf: e.matmul(PS[2 + hf][:, :], lhsT=mT[:, k, :], rhs=WoB[:, k, hf * 512:(hf + 1) * 512],
                                                                start=(k == 0), stop=(k == 7)), reads=[TmT, TWoB], writes=[TPS[2 + hf]])
                    hs = slice(hf * 512, (hf + 1) * 512)
                    op("dve", lambda e, hf=hf, hs=hs: e.tensor_tensor(out=x1[:, hs], in0=PS[2 + hf][:, :], in1=bcs[:, 0, hs], op=ALU.mult),
                       reads=[TPS[2 + hf], Tbcs[0]], writes=[Tx1])
                op("dve", lambda e: e.tensor_tensor(out=x1[:], in0=x1[:], in1=xqt[b][:], op=ALU.add), reads=[Tx1, Txqt[b]], writes=[Tx1])
                yield
                op("act", lambda e: e.activation(out=junk4[:], in_=x1[:], func=AF.Square, accum_out=st3[:, 0:1]),
                   reads=[Tx1], writes=[Tjunk4, Tst3])
                op("act", lambda e: e.activation(out=st3[:, 0:1], in_=st3[:, 0:1], func=AF.Sqrt, scale=1.0 / D, bias=EPS),
                   reads=[Tst3], writes=[Tst3])
                op("dve", lambda e: e.reciprocal(out=st3[:, 0:1], in_=st3[:, 0:1]), reads=[Tst3], writes=[Tst3])
                op("dve", lambda e: e.tensor_scalar(out=xn2[:], in0=x1[:], scalar1=st3[:, 0:1], scalar2=None, op0=ALU.mult),
                   reads=[Tx1, Tst3], writes=[Txn2])
                op("dve", lambda e: e.scalar_tensor_tensor(out=h2[:], in0=x1[:], scalar=st3[:, 0:1], in1=bcs[:, 2, :],
                                                           op0=ALU.mult, op1=ALU.mult), reads=[Tx1, Tst3, Tbcs[2]], writes=[Th2])
                op("dve", lambda e: e.tensor_tensor(out=h2[:], in0=h2[:], in1=bcs[:, 1, :], op=ALU.add), reads=[Th2, Tbcs[1]], writes=[Th2])
                yield
                ptb1 = PS[1][:].bitcast(BF16)
                for k in range(8):
                    op("pe", lambda e, k=k: e.transpose(ptb1[:, k * 128:(k + 1) * 128], xn2[:, k * 128:(k + 1) * 128], ident[:]),
                       reads=[Txn2, Tident], writes=[TPS[1]])
                for k in range(8):
                    op("act", lambda e, k=k: e.activation(out=h2T[:, k, :], in_=ptb1[:, k * 128:(k + 1) * 128], func=AF.Identity,
                                                          scale=G2[:, k:k + 1], bias=modT[:, 24 + k:25 + k]),
                       reads=[TPS[1], TG2, TmodT], writes=[Th2T])
                yield
                for g4 in range(4):
                    pi = g4 % 2
                    for q4 in range(4):
                        hp = g4 * 4 + q4
                        for k in range(8):
                            op("pe", lambda e, k=k, hp=hp, q4=q4, pi=pi: e.matmul(
                                PS[pi][:, q4 * 128:(q4 + 1) * 128], lhsT=WqB[:, k, hp * 128:(hp + 1) * 128], rhs=h2T[:, k, :],
                                start=(k == 0), stop=(k == 7)), reads=[TWqB, Th2T], writes=[TPS[pi]])
                    op("act", lambda e, g4=g4, pi=pi: e.activation(out=qT[:, g4 * 4:(g4 + 1) * 4, :].rearrange("p a t -> p (a t)"),
                                                                   in_=PS[pi][:, :], func=AF.Copy), reads=[TPS[pi]], writes=[TqT])
                yield
                for g4 in range(4):
                    for q4 in range(4):
                        hp = g4 * 4 + q4
                        op("pe", lambda e, hp=hp, q4=q4, g4=g4: e.matmul(PS[2 + g4][:, q4 * 128:(q4 + 1) * 128], lhsT=qT[:, hp, :],
                                                                         rhs=SKb[:, hp, :], start=True, stop=True),
                           reads=[TqT, TSKb], writes=[TPS[2 + g4]])
                    op("act", lambda e, g4=g4: e.activation(out=sc[:, g4 * 4:(g4 + 1) * 4, :].rearrange("p a t -> p (a t)"),
                                                            in_=PS[2 + g4][:, :], func=AF.Copy), reads=[TPS[2 + g4]], writes=[Tsc])
                yield
                for hp in range(16):
                    op("dve", lambda e, hp=hp: e.max(out=v16[:, hp, 0:8], in_=sc[:, hp, :]), reads=[Tsc], writes=[Tv16])
                    op("dve", lambda e, hp=hp: e.max_index(out=i16[:, hp, 0:8], in_max=v16[:, hp, 0:8], in_values=sc[:, hp, :]),
                       reads=[Tsc, Tv16], writes=[Ti16])
                    op("dve", lambda e, hp=hp: e.match_replace(out=scr[:, 0:128], in_to_replace=v16[:, hp, 0:8], in_values=sc[:, hp, :],
                                                               imm_value=-1e30), reads=[Tsc, Tv16], writes=[Tscr])
                    op("dve", lambda e, hp=hp: e.max(out=v16[:, hp, 8:16], in_=scr[:, 0:128]), reads=[Tscr], writes=[Tv16])
                    op("dve", lambda e, hp=hp: e.max_index(out=i16[:, hp, 8:16], in_max=v16[:, hp, 8:16], in_values=scr[:, 0:128]),
                       reads=[Tscr, Tv16], writes=[Ti16])
                    yield
                op("dve", lambda e: e.tensor_copy(out=i16f[:], in_=i16[:]), reads=[Ti16], writes=[Ti16f])
                v4 = v16[:].rearrange("p (h two) a -> p h two a", two=2)
                f4 = i16f[:].rearrange("p (h two) a -> p h two a", two=2)
                c4 = cand[:].rearrange("p h (a b) -> p h a b", b=16)
                x4 = cidx[:].rearrange("p h (a b) -> p h a b", b=16)
                op("dve", lambda e: e.tensor_tensor(out=c4, in0=v4[:, :, 0, :].unsqueeze(3).to_broadcast([128, 8, 16, 16]),
                                                    in1=v4[:, :, 1, :].unsqueeze(2).to_broadcast([128, 8, 16, 16]), op=ALU.add),
                   reads=[Tv16], writes=[Tcand])
                op("dve", lambda e: e.tensor_scalar(out=f4[:, :, 0, :], in0=f4[:, :, 0, :], scalar1=128.0, scalar2=None, op0=ALU.mult),
                   reads=[Ti16f], writes=[Ti16f])
                op("dve", lambda e: e.tensor_tensor(out=x4, in0=f4[:, :, 0, :].unsqueeze(3).to_broadcast([128, 8, 16, 16]),
                                                    in1=f4[:, :, 1, :].unsqueeze(2).to_broadcast([128, 8, 16, 16]), op=ALU.add),
                   reads=[Ti16f], writes=[Tcidx])
                for h in range(8):
                    op("dve", lambda e, h=h: e.max(out=m16[:, h, 0:8], in_=cand[:, h, :]), reads=[Tcand], writes=[Tm16])
                    op("dve", lambda e, h=h: e.match_replace(out=scr[:], in_to_replace=m16[:, h, 0:8], in_values=cand[:, h, :],
                                                             imm_value=-1e30), reads=[Tcand, Tm16], writes=[Tscr])
                    op("dve", lambda e, h=h: e.max(out=m16[:, h, 8:16], in_=scr[:]), reads=[Tscr], writes=[Tm16])
                    yield
                for h in range(8):
                    for k in range(16):
                        op("dve", lambda e, h=h, k=k: e.scalar_tensor_tensor(
                            out=junk3[:, 0:256], in0=cand[:, h, :], scalar=m16[:, h, k:k + 1], in1=cidx[:, h, :],
                            op0=ALU.is_equal, op1=ALU.mult, accum_out=eidf[:, h * 16 + k:h * 16 + k + 1]),
                           reads=[Tcand, Tcidx, Tm16], writes=[Tjunk3, Teidf])
                        if k % 4 == 3:
                            yield
                op("dve", lambda e: e.tensor_scalar(out=eidf[:], in0=eidf[:], scalar1=float(nexp - 1), scalar2=None, op0=ALU.min),
                   reads=[Teidf], writes=[Teidf])
                op("dve", lambda e: e.tensor_copy(out=eid[:], in_=eidf[:]), reads=[Teidf], writes=[Teid])
                yield
                op("dve", lambda e: e.tensor_tensor(out=gts[:], in0=m16[:], in1=m16[:, :, 0:1].to_broadcast([128, 8, 16]),
                                                    op=ALU.subtract), reads=[Tm16], writes=[Tgts])
                op("act", lambda e: e.activation(out=gts[:], in_=gts[:], func=AF.Exp), reads=[Tgts], writes=[Tgts])
                op("dve", lambda e: e.tensor_reduce(out=gsum[:], in_=gts[:], axis=AX.X, op=ALU.add), reads=[Tgts], writes=[Tgsum])
                op("dve", lambda e: e.reciprocal(out=gsum[:], in_=gsum[:]), reads=[Tgsum], writes=[Tgsum])
                op("dve", lambda e: e.tensor_tensor(out=gts[:], in0=gts[:], in1=gsum[:].unsqueeze(2).to_broadcast([128, 8, 16]),
                                                    op=ALU.mult), reads=[Tgts, Tgsum], writes=[Tgts])

            def block_part(tb, gen):
                b = tb % 2
                rows = slice(tb * 128, (tb + 1) * 128)
                x1, Tx1 = x1b[b], Tx1b[b]
                eid, Teid = eid2[b], Teid2[b]
                h2, Th2 = h2b[b], Th2b[b]
                gts, Tgts = gtsb[b], Tgtsb[b]
                gflat = gts[:].rearrange("p h k -> p (h k)")
                for grp in range(128 // GS):
                    ub = gctr[0] % NU; gctr[0] += 1
                    gs = slice(grp * GS, (grp + 1) * GS)
                    for s in range(GS):
                        slot = grp * GS + s
                        op("pool", lambda e, ub=ub, s=s, slot=slot: e.indirect_dma_start(
                            out=U[ub][:, s, :], out_offset=None, in_=edu_bf.rearrange("n t d -> n (t d)"),
                            in_offset=bass.IndirectOffsetOnAxis(ap=eid[:, slot:slot + 1], axis=0)),
                           reads=[Teid], writes=[TU[ub][s]], chan=ch_g[ub][s])
                    for s in range(GS):
                        slot = grp * GS + s
                        op("dve", lambda e, s=s, slot=slot: e.scalar_tensor_tensor(
                            out=junk3[:], in0=U[ub][:, s, 0:D], scalar=1.0, in1=h2[:],
                            op0=ALU.mult, op1=ALU.mult, accum_out=score[:, slot:slot + 1]),
                           reads=[TU[ub][s], Th2], writes=[Tjunk3, Tscore_g[grp]])
                    op("act", lambda e: e.activation(out=actv[:, gs], in_=score[:, gs], func=AF.Gelu),
                       reads=[Tscore_g[grp]], writes=[Tactv_g[grp]])
                    op("dve", lambda e: e.tensor_tensor(out=actv[:, gs], in0=actv[:, gs], in1=gflat[:, gs], op=ALU.mult),
                       reads=[Tactv_g[grp], Tgts], writes=[Tactv_g[grp]])
                    for s in range(GS):
                        slot = grp * GS + s
                        di = slot % 4
                        op("act", lambda e, slot=slot, di=di: e.activation(out=dg[di][:], in_=ident[:], func=AF.Copy,
                                                                           scale=actv[:, slot:slot + 1]),
                           reads=[Tident, Tactv_g[grp]], writes=[Tdg[di]])
                        for hf in range(2):
                            op("pe", lambda e, hf=hf, s=s, di=di, slot=slot: e.matmul(
                                PS[6 + hf][:, :], lhsT=dg[di][:], rhs=U[ub][:, s, D + hf * 512:D + (hf + 1) * 512],
                                start=(slot == 0), stop=(slot == 127)), reads=[Tdg[di], TU[ub][s]], writes=[TPS[6 + hf]])
                    if gen is not None:
                        for _ in range(3):
                            next(gen, None)
                if gen is not None:
                    for _ in gen:
                        pass
                for hf in range(2):
                    hs = slice(hf * 512, (hf + 1) * 512)
                    op("dve", lambda e, hf=hf, hs=hs: e.tensor_tensor(out=x2[:, hs], in0=PS[6 + hf][:, :], in1=bcs[:, 3, hs], op=ALU.mult),
                       reads=[TPS[6 + hf], Tbcs[3]], writes=[Tx2])
                op("dve", lambda e: e.tensor_tensor(out=x2[:], in0=x2[:], in1=x1[:], op=ALU.add), reads=[Tx2, Tx1], writes=[Tx2])
                op("act", lambda e: e.activation(out=junk4[:], in_=x2[:], func=AF.Square, accum_out=st4[:, 1:2]),
                   reads=[Tx2], writes=[Tjunk4, Tst4])
                op("act", lambda e: e.activation(out=st4[:, 1:2], in_=st4[:, 1:2], func=AF.Sqrt, scale=1.0 / D, bias=EPS),
                   reads=[Tst4], writes=[Tst4])
                op("dve", lambda e: e.reciprocal(out=st4[:, 1:2], in_=st4[:, 1:2]), reads=[Tst4], writes=[Tst4])
                op("dve", lambda e: e.scalar_tensor_tensor(out=ot[b][:], in0=x2[:], scalar=st4[:, 1:2], in1=bcs[:, 4, :],
                                                           op0=ALU.mult, op1=ALU.mult), reads=[Tx2, Tst4, Tbcs[4]], writes=[Tot[b]])
                op("sp", lambda e: e.dma_start(out=out_d[rows, :], in_=ot[b][:]), reads=[Tot[b]], chan=ch_st[b])

            for _ in front_part(0):
                pass
            for tb in range(NBQ):
                block_part(tb, front_part(tb + 1) if tb + 1 < NBQ else None)

        S_.barrier()
        print("build: ninst", S_.ninst, "nwaits", S_.nwaits, "sems", len(S_.sems))
    return nc


def make_masks(parity):
    s = np.arange(128)[:, None]
    t = np.arange(512)[None, :]
    out = np.zeros((2, 128, 16, 512), np.float32)
    kinds = ["n", "d", "d", "f"] if parity == 0 else ["d", "f", "n", "d"]
    for ms in range(4):
        for i in range(4):
            for v in range(2):
                if kinds[ms] == "n":
                    m = np.full((128, 512), NEG, np.float32)
                elif kinds[ms] == "f":
                    m = np.zeros((128, 512), np.float32)
                else:
                    kp = i * 128 + s
                    vis = (kp <= t) if v == 0 else (kp < t)
                    m = np.where(vis, 0.0, NEG).astype(np.float32)
                out[v, :, ms * 4 + i, :] = m
    return out.astype(ml_dtypes.bfloat16)


def make_in_maps(inp, S):
    NT = S // 512
    f32 = lambda a: np.ascontiguousarray(a, dtype=np.float32)
    colT = lambda v, n: f32(np.asarray(v).reshape(n, 128).T)
    shared = {
        "w_ada": f32(inp["w_ada"][0]),
        "badaT": colT(inp["b_ada"][0], 48),
        "bada_row": f32(inp["b_ada"][0][None, :]),
        "gmixT": colT(inp["g_norm_mix"][0], 8),
        "gffnT": colT(inp["g_norm_ffn"][0], 8),
        "gffn_row": f32(inp["g_norm_ffn"][0][None, :]),
        "gfin_row": f32(inp["g_final"][None, :]),
        "w_in": f32(inp["w_in"][0]),
        "bf_row": f32(inp["b_forget"][0][None, :]),
        "gfox_row": f32(inp["g_out_fox"][0][None, :]),
        "gsb_row": f32(inp["g_out_sb"][0][None, :]),
        "w_out": f32(inp["w_out"][0]),
        "w_query": f32(inp["w_query"][0]),
        "skT": f32(np.asarray(inp["sub_keys"][0]).reshape(16, 128, 128).transpose(2, 0, 1)),
        "e_down": f32(inp["expert_down"][0]),
        "e_up": f32(inp["expert_up"][0]),
    }
    masks = [make_masks(0), make_masks(1)]
    maps = []
    for core in range(8):
        b, r = core // 2, core % 2
        xb = np.asarray(inp["x"][b])
        tiles = tile_ids(r, NT)
        m = dict(shared)
        m["x_all"] = f32(xb)
        m["x_q"] = f32(np.concatenate([xb[t * 512:(t + 1) * 512] for t in tiles], axis=0))
        m["cT"] = colT(inp["c"][b], 8)
        m["masks"] = np.ascontiguousarray(masks[r][0])
        m["masks_sb"] = np.ascontiguousarray(masks[r][1])
        fl = np.zeros((8, 2), np.float32)
        fl[:, r] = 1.0
        m["flags"] = fl
        maps.append(m)
    return maps


def assemble(results, S):
    NT = S // 512
    out = np.zeros((4, S, D), np.float32)
    for core in range(8):
        b, r = core // 2, core % 2
        o = np.asarray(results[core]["out"])
        for lt, t in enumerate(tile_ids(r, NT)):
            out[b, t * 512:(t + 1) * 512] = o[lt * 512:(lt + 1) * 512]
    return out


_NC_CACHE = {}


def kernel(**inputs):
    S = 8192
    if S not in _NC_CACHE:
        _NC_CACHE[S] = build(S)
    nc = _NC_CACHE[S]
    maps = make_in_maps(inputs, S)
    res = run_bass_kernel_spmd(nc, maps, core_ids=list(range(8)))
    return assemble(res.results, S)
```

```python
import numpy as np
import ml_dtypes
from contextlib import ExitStack
import concourse.bass as bass
import concourse.mybir as mybir
from concourse.bass_utils import run_bass_kernel_spmd

F32 = mybir.dt.float32
BF16 = mybir.dt.bfloat16
I32 = mybir.dt.int32
U32 = mybir.dt.uint32
AF = mybir.ActivationFunctionType
ALU = mybir.AluOpType
AX = mybir.AxisListType

D = 1024
NH = 16
HD = 64
KA = 70
INC = 3080
NEXP = 16384
EPS = 1e-6
NEG = -30000.0


class Tk:
    __slots__ = ("name", "w", "r")

    def __init__(self, name=""):
        self.name = name
        self.w = None
        self.r = {}


class Chan:
    def __init__(self, S, name):
        self.sem = S.new_sem(name)
        self.key = name
        self.count = 0


class Sched:
    ENGS = ("pe", "act", "dve", "pool", "sp")
    ROT = 20000

    def __init__(self, nc, es):
        self.nc = nc
        self.es = es
        self.eobj = {"pe": nc.tensor, "act": nc.scalar, "dve": nc.vector, "pool": nc.gpsimd, "sp": nc.sync}
        self.sems = {}
        self.chans = []
        self.gen = {k: 0 for k in self.ENGS}
        self.ekey = {}
        self.count = {}
        self.known = {k: {} for k in self.ENGS}
        self.done_keys = []
        for k in self.ENGS:
            self._new_esem(k)
        self.nwaits = 0
        self.ninst = 0

    def _new_esem(self, k):
        key = "e_%s_%d" % (k, self.gen[k])
        self.gen[k] += 1
        self.new_sem(key)
        self.ekey[k] = key
        self.count[k] = 0

    def new_sem(self, name):
        s = self.es.enter_context(self.nc.semaphore(name))
        self.sems[name] = s
        return s

    def chan(self, name):
        c = Chan(self, "c_" + name)
        self.chans.append(c)
        return c

    def _wait(self, eng, ev):
        key, val, clock = ev
        kn = self.known[eng]
        if kn.get(key, 0) >= val:
            return
        self.eobj[eng].wait_ge(self.sems[key], val)
        self.nwaits += 1
        kn[key] = val
        if clock:
            for k2, v2 in clock.items():
                if kn.get(k2, 0) < v2:
                    kn[k2] = v2

    def op(self, eng, fn, reads=(), writes=(), chan=None):
        deps = []
        epref = "e_%s_" % eng
        for t in reads:
            if t.w is not None:
                deps.append(t.w)
        for t in writes:
            for ev in t.r.values():
                if ev[0].startswith(epref):
                    continue
                deps.append(ev)
            if t.w is not None:
                if t.w[0].startswith(epref):
                    continue
                deps.append(t.w)
        for ev in deps:
            self._wait(eng, ev)
        self.ninst += 1
        if chan is None:
            if self.count[eng] >= self.ROT:
                self.done_keys.append((self.ekey[eng], self.count[eng]))
                self._new_esem(eng)
            key = self.ekey[eng]
            self.count[eng] += 1
            val = self.count[eng]
            fn(self.eobj[eng]).then_inc(self.sems[key], 1)
            ev = (key, val, dict(self.known[eng]))
        else:
            chan.count += 16
            fn(self.eobj[eng]).then_inc(chan.sem, 16)
            ev = (chan.key, chan.count, dict(self.known[eng]))
        for t in writes:
            t.w = ev
            t.r = {}
        for t in reads:
            if t in writes:
                continue
            t.r[("e_" + eng) if chan is None else chan.key] = ev
        return ev

    def barrier(self):
        evs = []
        for k in self.ENGS:
            if self.count[k] > 0:
                evs.append((self.ekey[k], self.count[k], None))
        for key, cnt in self.done_keys:
            evs.append((key, cnt, None))
        for c in self.chans:
            if c.count > 0:
                evs.append((c.key, c.count, None))
        for eng in self.ENGS:
            for ev in evs:
                self._wait(eng, ev)


def tile_ids(parity, NT):
    out = []
    for m in range(NT // 4):
        out += [4 * m, 4 * m + 3] if parity == 0 else [4 * m + 1, 4 * m + 2]
    return out


def build(S, dbg=0, phases=(0, 1, 2, 3), nexp=NEXP):
    NT = S // 512
    NB = S // 128
    NTQ = NT // 2
    SQ = NTQ * 512
    NBQ = SQ // 128
    nc = bass.Bass("TRN2", target_bir_lowering=False)

    def din(name, shape, dt=F32):
        return nc.dram_tensor(name, list(shape), dt, kind="ExternalInput").ap()

    x_all = din("x_all", [S, D])
    x_q = din("x_q", [SQ, D])
    cT_d = din("cT", [128, 8])
    w_ada = din("w_ada", [D, 6 * D])
    badaT_d = din("badaT", [128, 48])
    bada_row = din("bada_row", [1, 6 * D])
    gmixT_d = din("gmixT", [128, 8])
    gffnT_d = din("gffnT", [128, 8])
    gffn_row = din("gffn_row", [1, D])
    gfin_row = din("gfin_row", [1, D])
    w_in = din("w_in", [D, INC])
    bf_row = din("bf_row", [1, 8])
    gfox_row = din("gfox_row", [1, HD])
    gsb_row = din("gsb_row", [1, HD])
    w_out = din("w_out", [D, D])
    w_query = din("w_query", [D, 2048])
    skT_d = din("skT", [128, 16, 128])
    e_down = din("e_down", [nexp, D])
    e_up = din("e_up", [nexp, D])
    masks_d = din("masks", [128, 16, 512], BF16)
    masks_sb_d = din("masks_sb", [128, 16, 512], BF16)
    flags_d = din("flags", [8, 2])
    out_d = nc.dram_tensor("out", [SQ, D], F32, kind="ExternalOutput").ap()

    okind = "ExternalOutput" if dbg else None

    def dscr(name, shape, dt):
        if dbg:
            return nc.dram_tensor(name, list(shape), dt, kind="ExternalOutput").ap()
        return nc.dram_tensor(name, list(shape), dt).ap()

    KT_d = dscr("KT_d", [NH, KA, S], BF16)
    QT_d = dscr("QT_d", [NH, KA, SQ], BF16)
    V_d = dscr("V_d", [NH, 128, NB, 65], BF16)
    mixed_d = dscr("mixed_d", [SQ, D], BF16)
    edu_bf = nc.dram_tensor("edu_bf", [nexp, 2, D], BF16).ap()
    if dbg:
        mod_dbg = dscr("mod_dbg", [128, 48], F32)

    with ExitStack() as es:
        S_ = Sched(nc, es)
        op = S_.op

        def sb(stack, name, shape, dt):
            return stack.enter_context(nc.sbuf_tensor("s_" + name, list(shape), dt))

        PS = [es.enter_context(nc.psum_tensor("ps%d" % i, [128, 512], F32)) for i in range(8)]
        TPS = [Tk("ps%d" % i) for i in range(8)]

        ident = sb(es, "ident", [128, 128], BF16); Tident = Tk()
        tri = sb(es, "tri", [128, 128], BF16); Ttri = Tk()
        ones = sb(es, "ones", [128, 128], BF16); Tones = Tk()
        modT = sb(es, "modT", [128, 48], F32); TmodT = Tk()
        G1 = sb(es, "G1", [128, 8], F32); TG1 = Tk()
        G2 = sb(es, "G2", [128, 8], F32); TG2 = Tk()
        bcs = sb(es, "bcs", [128, 5, D], F32)
        Tbcs = [Tk() for _ in range(5)]
        gout = sb(es, "gout", [128, 2, HD], F32); Tgout = Tk()
        flags = sb(es, "flags_sb", [8, 2], F32); Tflags = Tk()

        ch_c = S_.chan("const")
        ch_ld = [S_.chan("ld%d" % i) for i in range(4)]
        ch_st = [S_.chan("st%d" % i) for i in range(4)]
        ch_kts = [S_.chan("kts%d" % i) for i in range(4)]
        ch_aug = [S_.chan("aug%d" % i) for i in range(3)]
        ch_hd = [[S_.chan("hd%d_%d" % (i, j)) for j in range(3)] for i in range(2)]
        ch_stg = [S_.chan("stg%d" % i) for i in range(2)]

        op("pool", lambda e: e.memset(ident[:], 1.0), writes=[Tident])
        op("pool", lambda e: e.affine_select(out=ident[:], in_=ident[:], pattern=[[-1, 128]],
                                             compare_op=ALU.is_equal, fill=0.0, base=0, channel_multiplier=1),
           reads=[Tident], writes=[Tident])
        op("pool", lambda e: e.memset(tri[:], 1.0), writes=[Ttri])
        op("pool", lambda e: e.affine_select(out=tri[:], in_=tri[:], pattern=[[-1, 128]],
                                             compare_op=ALU.is_ge, fill=0.0, base=0, channel_multiplier=1),
           reads=[Ttri], writes=[Ttri])
        op("pool", lambda e: e.memset(ones[:], 1.0), writes=[Tones])
        op("sp", lambda e: e.dma_start(out=gout[:, 0, :], in_=gfox_row.partition_broadcast(128)), writes=[Tgout], chan=S_.chan("k1"))
        op("sp", lambda e: e.dma_start(out=gout[:, 1, :], in_=gsb_row.partition_broadcast(128)), writes=[Tgout], chan=S_.chan("k2"))
        op("sp", lambda e: e.dma_start(out=flags[:], in_=flags_d), writes=[Tflags], chan=S_.chan("k3"))

        with ExitStack() as p0:
            cT = sb(p0, "cT", [128, 8], F32); TcT = Tk()
            scT = sb(p0, "scT", [128, 8], F32); TscT = Tk()
            screp = sb(p0, "screp", [128, 8, 128], F32); Tscrep = Tk()
            wa = [sb(p0, "wa%d" % i, [128, 8, 512], F32) for i in range(2)]; Twa = [Tk(), Tk()]
            badaT = sb(p0, "badaT", [128, 48], F32); TbadaT = Tk()
            badabc = sb(p0, "badabc", [128, 4, D], F32); Tbadabc = Tk()
            gmixT = sb(p0, "gmixT", [128, 8], F32); TgmixT = Tk()
            gffnT = sb(p0, "gffnT", [128, 8], F32); TgffnT = Tk()
            gffnbc = sb(p0, "gffnbc", [128, D], F32); Tgffnbc = Tk()

            op("sp", lambda e: e.dma_start(out=cT[:], in_=cT_d), writes=[TcT], chan=S_.chan("k4"))
            op("sp", lambda e: e.dma_start(out=badaT[:], in_=badaT_d), writes=[TbadaT], chan=S_.chan("k5"))
            op("sp", lambda e: e.dma_start(out=gmixT[:], in_=gmixT_d), writes=[TgmixT], chan=S_.chan("k6"))
            op("sp", lambda e: e.dma_start(out=gffnT[:], in_=gffnT_d), writes=[TgffnT], chan=S_.chan("k7"))
            op("sp", lambda e: e.dma_start(out=gffnbc[:], in_=gffn_row.partition_broadcast(128)), writes=[Tgffnbc], chan=S_.chan("k8"))
            op("sp", lambda e: e.dma_start(out=bcs[:, 4, :], in_=gfin_row.partition_broadcast(128)), writes=[Tbcs[4]], chan=S_.chan("k9"))
            op("sp", lambda e: e.dma_start(out=badabc[:].rearrange("p a d -> p (a d)"),
                                           in_=bada_row[:, 2 * D:6 * D].partition_broadcast(128)), writes=[Tbadabc], chan=S_.chan("k10"))
            op("act", lambda e: e.activation(out=scT[:], in_=cT[:], func=AF.Silu), reads=[TcT], writes=[TscT])
            op("dve", lambda e: e.tensor_copy(out=screp[:], in_=scT[:].unsqueeze(2).to_broadcast([128, 8, 128])),
               reads=[TscT], writes=[Tscrep])
            w_ada_v = w_ada.rearrange("(k p) c -> p k c", p=128)
            modps = PS[0]
            for cc in range(12):
                b = cc % 2
                op("sp", lambda e, cc=cc, b=b: e.dma_start(out=wa[b][:], in_=w_ada_v[:, :, cc * 512:(cc + 1) * 512]),
                   writes=[Twa[b]], chan=ch_ld[b])
                for f4 in range(4):
                    fc = cc * 4 + f4
                    for k in range(8):
                        op("pe", lambda e, b=b, k=k, f4=f4, fc=fc: e.matmul(
                            modps[:, fc:fc + 1], lhsT=wa[b][:, k, f4 * 128:(f4 + 1) * 128], rhs=scT[:, k:k + 1],
                            start=(k == 0), stop=(k == 7)), reads=[Twa[b], TscT], writes=[TPS[0]])
                if cc >= 4:
                    a = (cc - 4) // 2
                    hh = (cc - 4) % 2
                    pb = PS[1 + (cc % 2)]
                    Tpb = TPS[1 + (cc % 2)]
                    for k in range(8):
                        op("pe", lambda e, b=b, k=k, pb=pb: e.matmul(pb[:, :], lhsT=screp[:, k, :], rhs=wa[b][:, k, :],
                                                                   start=(k == 0), stop=(k == 7)),
                           reads=[Twa[b], Tscrep], writes=[Tpb])
                    op("dve", lambda e, a=a, hh=hh, pb=pb: e.tensor_tensor(
                        out=bcs[:, a, hh * 512:(hh + 1) * 512], in0=pb[:, :], in1=badabc[:, a, hh * 512:(hh + 1) * 512],
                        op=ALU.add), reads=[Tpb, Tbadabc], writes=[Tbcs[a]])
            op("dve", lambda e: e.tensor_tensor(out=modT[:], in0=modps[:, 0:48], in1=badaT[:], op=ALU.add),
               reads=[TPS[0], TbadaT], writes=[TmodT])
            op("dve", lambda e: e.scalar_tensor_tensor(out=G1[:], in0=modT[:, 8:16], scalar=1.0, in1=gmixT[:],
                                                       op0=ALU.add, op1=ALU.mult), reads=[TmodT, TgmixT], writes=[TG1])
            op("dve", lambda e: e.scalar_tensor_tensor(out=G2[:], in0=modT[:, 32:40], scalar=1.0, in1=gffnT[:],
                                                       op0=ALU.add, op1=ALU.mult), reads=[TmodT, TgffnT], writes=[TG2])
            op("dve", lambda e: e.scalar_tensor_tensor(out=bcs[:, 2, :], in0=bcs[:, 2, :], scalar=1.0, in1=gffnbc[:],
                                                       op0=ALU.add, op1=ALU.mult), reads=[Tbcs[2], Tgffnbc], writes=[Tbcs[2]])
            if dbg:
                op("sp", lambda e: e.dma_start(out=mod_dbg, in_=modT[:]), reads=[TmodT], chan=ch_st[0])
            S_.barrier()

        def norm_chunk(xsrc_ap, xt, Txt, ss, Tss, rstd, Trstd, xn, Txn, junk, Tjunk, hT, ThT, Gc, TGc, Bc_ap, TBc, ldchan,
                       psA, psB):
            op("sp", lambda e: e.dma_start(out=xt[:], in_=xsrc_ap.rearrange("(j p) d -> p j d", p=128)),
               writes=[Txt], chan=ldchan)
            for j in range(4):
                op("act", lambda e, j=j: e.activation(out=junk[:], in_=xt[:, j, :], func=AF.Square,
                                                      accum_out=ss[:, j:j + 1]), reads=[Txt], writes=[Tjunk, Tss])
            op("act", lambda e: e.activation(out=rstd[:], in_=ss[:], func=AF.Sqrt, scale=1.0 / D, bias=EPS),
               reads=[Tss], writes=[Trstd])
            op("dve", lambda e: e.reciprocal(out=rstd[:], in_=rstd[:]), reads=[Trstd], writes=[Trstd])
            for j in range(4):
                op("dve", lambda e, j=j: e.tensor_scalar(out=xn[:, j, :], in0=xt[:, j, :], scalar1=rstd[:, j:j + 1],
                                                         scalar2=None, op0=ALU.mult), reads=[Txt, Trstd], writes=[Txn])
            for k in range(8):
                pi = psA if k % 2 == 0 else psB
                pt = PS[pi][:].bitcast(BF16)
                for j in range(4):
                    op("pe", lambda e, j=j, k=k, pt=pt: e.transpose(pt[:, j * 128:(j + 1) * 128], xn[:, j, k * 128:(k + 1) * 128],
                                                                   ident[:]), reads=[Txn, Tident], writes=[TPS[pi]])
                op("act", lambda e, k=k, pt=pt: e.activation(out=hT[:, k, :], in_=pt[:, 0:512], func=AF.Identity,
                                                             scale=Gc[:, k:k + 1], bias=Bc_ap[:, k:k + 1]),
                   reads=[TPS[pi], TGc, TBc], writes=[ThT])

        with ExitStack() as p1:
            Wb = sb(p1, "Wb", [128, 8, INC], BF16); TWb = Tk()
            xt = [sb(p1, "xt%d" % i, [128, 4, D], F32) for i in range(2)]; Txt = [Tk(), Tk()]
            xn = sb(p1, "xn", [128, 4, D], BF16); Txn = Tk()
            junk = sb(p1, "junk", [128, D], BF16); Tjunk = Tk()
            ss = [sb(p1, "ss%d" % i, [128, 4], F32) for i in range(2)]; Tss = [Tk(), Tk()]
            rstd = [sb(p1, "rstd%d" % i, [128, 4], F32) for i in range(2)]; Trstd = [Tk(), Tk()]
            hT = sb(p1, "hT", [128, 8, 512], BF16); ThT = Tk()
            KTs = [sb(p1, "KTs%d" % i, [128, 512], BF16) for i in range(4)]; TKTs = [Tk() for _ in range(4)]
            Vs = [sb(p1, "Vs%d" % i, [128, 4, NH, 65], BF16) for i in range(2)]; TVs = [Tk(), Tk()]
            tribig = sb(p1, "tribig", [128, 4, 512], F32); Ttribig = Tk()
            bfbc = sb(p1, "bfbc", [128, 8], F32); Tbfbc = Tk()
            ub = sb(p1, "ub", [128, 4, 8], F32); Tub = Tk()
            lf = sb(p1, "lf", [128, 4, 8], F32); Tlf = Tk()
            GT = [sb(p1, "GT%d" % i, [8, 512], F32) for i in range(2)]; TGT = [Tk(), Tk()]
            gz = sb(p1, "gz", [8, 1], F32); Tgz = Tk()
            sp_hi = sb(p1, "sp_hi", [8, 3, 512], BF16); Tsp_hi = Tk()
            r1 = sb(p1, "r1", [8, 512], F32); Tr1 = Tk()
            r2 = sb(p1, "r2", [8, 512], F32); Tr2 = Tk()
            gq = sb(p1, "gq", [8, 512], F32); Tgq = Tk()
            spq = sb(p1, "spq", [8, 3, 512], BF16); Tspq = Tk()
            cst = sb(p1, "cst", [8, 3, 512], BF16); Tcst = Tk()

            w_in_v = w_in.rearrange("(k p) c -> p k c", p=128)
            wst = [xt[i][:].rearrange("p j d -> p (j d)")[:, 0:3520].rearrange("p (k c) -> p k c", k=8) for i in range(2)]
            Twst = Txt
            for i in range(7):
                b = i % 2
                op("sp", lambda e, i=i, b=b: e.dma_start(out=wst[b], in_=w_in_v[:, :, i * 440:(i + 1) * 440]),
                   writes=[Twst[b]], chan=ch_ld[b])
                eng = "dve" if i % 2 == 0 else "pool"
                op(eng, lambda e, i=i, b=b: e.tensor_copy(out=Wb[:, :, i * 440:(i + 1) * 440], in_=wst[b]),
                   reads=[Twst[b]], writes=[TWb])
            op("pool", lambda e: e.memset(tribig[:], 1.0), writes=[Ttribig])
            for j in range(4):
                op("pool", lambda e, j=j: e.affine_select(out=tribig[:, j, :], in_=tribig[:, j, :], pattern=[[1, 512]],
                                                          compare_op=ALU.is_ge, fill=0.0, base=-j * 128, channel_multiplier=-1),
                   reads=[Ttribig], writes=[Ttribig])
            op("sp", lambda e: e.dma_start(out=bfbc[:], in_=bf_row.partition_broadcast(128)), writes=[Tbfbc], chan=S_.chan("k11"))
            op("pool", lambda e: e.memset(gz[:], 0.0), writes=[Tgz])
            for i in range(2):
                op("pool", lambda e, i=i: e.memset(Vs[i][:], 1.0), writes=[TVs[i]])
            op("pool", lambda e: e.memset(cst[:], -1.0), writes=[Tcst])
            for c in range(NT):
                op("pool", lambda e, c=c: e.dma_start(
                    out=KT_d[0:8, 67:70, c * 512:(c + 1) * 512], in_=cst[:]),
                   reads=[Tcst], chan=ch_aug[0])
            op("pool", lambda e: e.memset(cst[:], 1.0), reads=[], writes=[Tcst])
            for c in range(NTQ):
                op("pool", lambda e, c=c: e.dma_start(
                    out=QT_d[0:8, 64:67, c * 512:(c + 1) * 512], in_=cst[:]),
                   reads=[Tcst], chan=ch_aug[0])

            o1 = 1536
            o2 = 1544
            kcols = [512 + 128 * i for i in range(4)] + [o2 + 512 + 128 * i for i in range(4)]
            qcols = [0 + 128 * i for i in range(4)] + [o2 + 128 * i for i in range(4)]
            vcols = [1024, o2 + 1024]

            def proj_T(cols_list, dst_d, c, scale, rr):
                for hp in range(8):
                    pi = 2 + (rr[0] % 2); rr[0] += 1
                    for k in range(8):
                        op("pe", lambda e, hp=hp, k=k, pi=pi: e.matmul(
                            PS[pi][:, :], lhsT=Wb[:, k, cols_list[hp]:cols_list[hp] + 128], rhs=hT[:, k, :],
                            start=(k == 0), stop=(k == 7)), reads=[TWb, ThT], writes=[TPS[pi]])
                    kb = rr[1] % 4; rr[1] += 1
                    op("act", lambda e, pi=pi, kb=kb: e.activation(out=KTs[kb][:], in_=PS[pi][:, :], func=AF.Copy, scale=scale),
                       reads=[TPS[pi]], writes=[TKTs[kb]])
                    for i2 in range(2):
                        op("pool", lambda e, hp=hp, kb=kb, i2=i2: e.dma_start(
                            out=dst_d[2 * hp + i2, 0:64, c * 512:(c + 1) * 512],
                            in_=KTs[kb][i2 * 64:(i2 + 1) * 64, :]), reads=[TKTs[kb]], chan=ch_kts[kb])

            rr = [0, 0]
            for c in range(NT):
                b = c % 2
                norm_chunk(x_all[c * 512:(c + 1) * 512, :], xt[b], Txt[b], ss[b], Tss[b], rstd[b], Trstd[b], xn, Txn,
                           junk, Tjunk, hT, ThT, G1, TG1, modT[:, 0:8], TmodT, ch_ld[b], 0, 1)
                proj_T(kcols, KT_d, c, 1.0, rr)
                vb = c % 2
                for j in range(4):
                    for g in range(2):
                        pi = 4 + ((j * 2 + g) % 2)
                        for k in range(8):
                            op("pe", lambda e, j=j, g=g, k=k, pi=pi: e.matmul(
                                PS[pi][:, :], lhsT=hT[:, k, j * 128:(j + 1) * 128], rhs=Wb[:, k, vcols[g]:vcols[g] + 512],
                                start=(k == 0), stop=(k == 7)), reads=[TWb, ThT], writes=[TPS[pi]])
                        op("dve", lambda e, j=j, g=g, pi=pi, vb=vb: e.tensor_copy(
                            out=Vs[vb][:, j, g * 8:(g + 1) * 8, 0:64], in_=PS[pi][:, :].rearrange("p (h d) -> p h d", d=64)),
                           reads=[TPS[pi]], writes=[TVs[vb]])
                for j in range(4):
                    op("pool", lambda e, j=j, vb=vb, c=c: e.dma_start(
                        out=V_d[:, :, 4 * c + j, :].rearrange("h p e -> p h e"), in_=Vs[vb][:, j, :, :]),
                       reads=[TVs[vb]], chan=ch_st[2 + vb])
                for j in range(4):
                    for k in range(8):
                        op("pe", lambda e, j=j, k=k: e.matmul(
                            PS[6][:, j * 8:(j + 1) * 8], lhsT=hT[:, k, j * 128:(j + 1) * 128], rhs=Wb[:, k, o1:o1 + 8],
                            start=(k == 0), stop=(k == 7)), reads=[TWb, ThT], writes=[TPS[6]])
                op("dve", lambda e: e.tensor_tensor(out=ub[:], in0=PS[6][:, 0:32].rearrange("p (j h) -> p j h", h=8),
                                                    in1=bfbc[:].unsqueeze(1).to_broadcast([128, 4, 8]), op=ALU.add),
                   reads=[TPS[6], Tbfbc], writes=[Tub])
                op("act", lambda e: e.activation(out=ub[:], in_=ub[:], func=AF.Exp, scale=-1.0), reads=[Tub], writes=[Tub])
                op("act", lambda e: e.activation(out=lf[:], in_=ub[:], func=AF.Ln, bias=1.0), reads=[Tub], writes=[Tlf])
                for j in range(4):
                    op("pe", lambda e, j=j: e.matmul(PS[7][0:8, :], lhsT=lf[:, j, :], rhs=tribig[:, j, :],
                                                     start=(j == 0), stop=(j == 3)), reads=[Tlf, Ttribig], writes=[TPS[7]])
                gcur, Tgcur = GT[c % 2], TGT[c % 2]
                if c == 0:
                    carry_ap, Tcarry = gz[:, 0:1], Tgz
                else:
                    carry_ap, Tcarry = GT[(c - 1) % 2][:, 511:512], TGT[(c - 1) % 2]
                op("dve", lambda e, gcur=gcur, carry_ap=carry_ap: e.tensor_scalar(
                    out=gcur[:], in0=PS[7][0:8, :], scalar1=carry_ap, scalar2=None, op0=ALU.add),
                   reads=[TPS[7], Tcarry], writes=[Tgcur])

                def split3(src, Tsrc, dst, Tdst):
                    op("dve", lambda e: e.tensor_copy(out=dst[:, 0, :], in_=src[:]), reads=[Tsrc], writes=[Tdst])
                    op("dve", lambda e: e.tensor_tensor(out=r1[:], in0=src[:], in1=dst[:, 0, :], op=ALU.subtract),
                       reads=[Tsrc, Tdst], writes=[Tr1])
                    op("dve", lambda e: e.tensor_copy(out=dst[:, 1, :], in_=r1[:]), reads=[Tr1], writes=[Tdst])
                    op("dve", lambda e: e.tensor_tensor(out=r2[:], in0=r1[:], in1=dst[:, 1, :], op=ALU.subtract),
                       reads=[Tr1, Tdst], writes=[Tr2])
                    op("dve", lambda e: e.tensor_copy(out=dst[:, 2, :], in_=r2[:]), reads=[Tr2], writes=[Tdst])

                split3(gcur, Tgcur, sp_hi, Tsp_hi)
                op("pool", lambda e, c=c: e.dma_start(out=KT_d[0:8, 64:67, c * 512:(c + 1) * 512], in_=sp_hi[:]),
                   reads=[Tsp_hi], chan=ch_aug[1])
                m, ph = c // 4, c % 4
                if ph in (0, 2):
                    fl = flags[:, 0:1] if ph == 0 else flags[:, 1:2]
                    op("dve", lambda e, fl=fl, gcur=gcur: e.tensor_scalar(out=gq[:], in0=gcur[:], scalar1=fl, scalar2=None,
                                                                          op0=ALU.mult), reads=[Tgcur, Tflags], writes=[Tgq])
                else:
                    fl = flags[:, 1:2] if ph == 1 else flags[:, 0:1]
                    op("dve", lambda e, fl=fl, gcur=gcur: e.scalar_tensor_tensor(out=gq[:], in0=gcur[:], scalar=fl, in1=gq[:],
                                                                                 op0=ALU.mult, op1=ALU.add),
                       reads=[Tgcur, Tflags, Tgq], writes=[Tgq])
                    split3(gq, Tgq, spq, Tspq)
                    lt = 2 * m + (0 if ph == 1 else 1)
                    op("pool", lambda e, lt=lt: e.dma_start(out=QT_d[0:8, 67:70, lt * 512:(lt + 1) * 512], in_=spq[:]),
                       reads=[Tspq], chan=ch_aug[2])

            for c in range(NTQ):
                b = c % 2
                norm_chunk(x_q[c * 512:(c + 1) * 512, :], xt[b], Txt[b], ss[b], Tss[b], rstd[b], Trstd[b], xn, Txn,
                           junk, Tjunk, hT, ThT, G1, TG1, modT[:, 0:8], TmodT, ch_ld[b], 0, 1)
                proj_T(qcols, QT_d, c, 0.125, rr)
            S_.barrier()

        if 2 in phases:
          with ExitStack() as p2:
            maskF = sb(p2, "maskF", [128, 16, 512], BF16); TmaskF = Tk()
            maskS = sb(p2, "maskS", [128, 16, 512], BF16); TmaskS = Tk()
            KTh = [sb(p2, "KTh%d" % i, [KA, S], BF16) for i in range(2)]; TKTh = [Tk(), Tk()]
            Vh = [sb(p2, "Vh%d" % i, [128, NB, 65], BF16) for i in range(2)]; TVh = [Tk(), Tk()]
            QTh = [sb(p2, "QTh%d" % i, [KA, SQ], BF16) for i in range(2)]; TQTh = [Tk(), Tk()]
            negQ = [sb(p2, "negQ%d" % i, [64, 512], BF16) for i in range(2)]; TnegQ = [Tk(), Tk()]
            zt = [sb(p2, "zt%d" % i, [128, 512], F32) for i in range(2)]; Tzt = [Tk(), Tk()]
            ee = [sb(p2, "ee%d" % i, [128, 512], F32) for i in range(2)]; Tee = [Tk(), Tk()]
            spb = [sb(p2, "spb%d" % i, [128, 512], BF16) for i in range(2)]; Tspb = [Tk(), Tk()]
            LL = [sb(p2, "LL%d" % i, [128, 512], F32) for i in range(2)]; TLL = [Tk(), Tk()]
            PT = [sb(p2, "PT%d" % i, [128, 512], BF16) for i in range(6)]; TPT = [Tk() for _ in range(6)]
            Rsb = sb(p2, "Rsb", [128, 512], F32); TRsb = Tk()
            osb = sb(p2, "osb", [128, 4, 64], F32); Tosb = Tk()
            sq = sb(p2, "sq", [128, 4, 64], F32); Tsq = Tk()
            o2 = sb(p2, "o2", [128, 4, 64], F32); To2 = Tk()
            omix = [sb(p2, "omix%d" % i, [128, 4, 64], BF16) for i in range(2)]; Tomix = [Tk(), Tk()]
            ssq = sb(p2, "ssq", [128, 4], F32); Tssq = Tk()
            rinv = sb(p2, "rinv", [128, 4], F32); Trinv = Tk()

            op("sp", lambda e: e.dma_start(out=maskF[:], in_=masks_d), writes=[TmaskF], chan=S_.chan("k12"))
            op("sp", lambda e: e.dma_start(out=maskS[:], in_=masks_sb_d), writes=[TmaskS], chan=S_.chan("k13"))

            def block_list(lt):
                kind, m = lt % 2, lt // 2
                if kind == 0:
                    masked = [(4 * m + 1, 0), (4 * m, 1)]
                    lower = list(range(4 * m - 1, -1, -1))
                else:
                    masked = [(4 * m + 3, 2), (4 * m + 2, 3)]
                    lower = list(range(4 * m + 1, -1, -1))
                bl = []
                for (T_, ms) in masked:
                    for i in (3, 2, 1, 0):
                        bl.append((4 * T_ + i, ms * 4 + i))
                for T_ in lower:
                    for i in (3, 2, 1, 0):
                        bl.append((4 * T_ + i, None))
                return bl

            bctr = [0]
            octr = [0]
            for h in range(NH):
                hb = h % 2
                is_fox = h < 8
                op("sp", lambda e, h=h, hb=hb: e.dma_start(out=KTh[hb][:], in_=KT_d[h]), writes=[TKTh[hb]], chan=ch_hd[hb][0])
                op("sp", lambda e, h=h, hb=hb: e.dma_start(out=Vh[hb][:], in_=V_d[h]), writes=[TVh[hb]], chan=ch_hd[hb][1])
                op("sp", lambda e, h=h, hb=hb: e.dma_start(out=QTh[hb][:], in_=QT_d[h]), writes=[TQTh[hb]], chan=ch_hd[hb][2])
                kt, vt, qt = KTh[hb], Vh[hb], QTh[hb]
                Tkt, Tvt, Tqt = TKTh[hb], TVh[hb], TQTh[hb]
                for lt in range(NTQ):
                    bl = block_list(lt)
                    n = len(bl)
                    ob = 6 + (octr[0] % 2); octr[0] += 1
                    Ops, TOps = PS[ob], TPS[ob]
                    Ov = Ops[:, 0:260].rearrange("p (j e) -> p j e", e=65)
                    qs = slice(lt * 512, (lt + 1) * 512)
                    base = bctr[0]; bctr[0] += n
                    if is_fox:
                        def A1(k):
                            kb, mi = bl[k]
                            b = (base + k) % 6
                            zb = (base + k) % 2
                            op("pe", lambda e: e.matmul(PS[b][:, :], lhsT=kt[0:KA, kb * 128:(kb + 1) * 128], rhs=qt[0:KA, qs],
                                                        start=True, stop=True), reads=[Tkt, Tqt], writes=[TPS[b]])
                            if mi is not None:
                                op("dve", lambda e: e.tensor_tensor(out=zt[zb][:], in0=PS[b][:, :], in1=maskF[:, mi, :], op=ALU.add),
                                   reads=[TPS[b], TmaskF], writes=[Tzt[zb]])
                                op("act", lambda e: e.activation(out=PT[b][:], in_=zt[zb][:], func=AF.Exp), reads=[Tzt[zb]], writes=[TPT[b]])
                            else:
                                op("act", lambda e: e.activation(out=PT[b][:], in_=PS[b][:, :], func=AF.Exp), reads=[TPS[b]], writes=[TPT[b]])

                        def B(k):
                            kb, mi = bl[k]
                            b = (base + k) % 6
                            for j in range(4):
                                op("pe", lambda e, j=j: e.matmul(Ov[:, j, :], lhsT=PT[b][:, j * 128:(j + 1) * 128], rhs=vt[:, kb, :],
                                                                 start=(k == 0), stop=(k == n - 1)), reads=[TPT[b], Tvt], writes=[TOps])
                        for k in range(n + 3):
                            if k < n:
                                A1(k)
                            if k >= 3:
                                B(k - 3)
                    else:
                        nq = negQ[octr[0] % 2]; Tnq = TnegQ[octr[0] % 2]
                        op("pool", lambda e: e.tensor_scalar(out=nq[:], in0=qt[0:64, qs], scalar1=-1.0, scalar2=None, op0=ALU.mult),
                           reads=[Tqt], writes=[Tnq])
                        op("pool", lambda e: e.memset(Rsb[:], 0.0), writes=[TRsb])

                        def A1(k):
                            kb, mi = bl[k]
                            b = (base + k) % 2
                            op("pe", lambda e: e.matmul(PS[b][:, :], lhsT=kt[0:64, kb * 128:(kb + 1) * 128], rhs=qt[0:64, qs],
                                                        start=True, stop=True), reads=[Tkt, Tqt], writes=[TPS[b]])
                            if mi is not None:
                                op("dve", lambda e: e.tensor_tensor(out=zt[b][:], in0=PS[b][:, :], in1=maskS[:, mi, :], op=ALU.add),
                                   reads=[TPS[b], TmaskS], writes=[Tzt[b]])
                                op("act", lambda e: e.activation(out=ee[b][:], in_=zt[b][:], func=AF.Exp), reads=[Tzt[b]], writes=[Tee[b]])
                            else:
                                op("act", lambda e: e.activation(out=ee[b][:], in_=PS[b][:, :], func=AF.Exp), reads=[TPS[b]], writes=[Tee[b]])
                            op("act", lambda e: e.activation(out=spb[b][:], in_=ee[b][:], func=AF.Ln, bias=1.0), reads=[Tee[b]], writes=[Tspb[b]])

                        def A2(k):
                            kb, mi = bl[k]
                            b = (base + k) % 2
                            op("pe", lambda e: e.matmul(PS[2 + b][:, :], lhsT=tri[:], rhs=spb[b][:], start=True, stop=False),
                               reads=[Ttri, Tspb[b]], writes=[TPS[2 + b]])
                            op("pe", lambda e: e.matmul(PS[2 + b][:, :], lhsT=kt[0:64, kb * 128:(kb + 1) * 128], rhs=nq[:],
                                                        start=False, stop=True), reads=[Tkt, Tnq], writes=[TPS[2 + b]])
                            if k < n - 1:
                                op("pe", lambda e: e.matmul(PS[4 + b][:, :], lhsT=ones[:], rhs=spb[b][:], start=True, stop=True),
                                   reads=[Tones, Tspb[b]], writes=[TPS[4 + b]])
                            op("dve", lambda e: e.tensor_tensor(out=LL[b][:], in0=PS[2 + b][:, :], in1=Rsb[:], op=ALU.add),
                               reads=[TPS[2 + b], TRsb], writes=[TLL[b]])
                            if mi is not None:
                                op("dve", lambda e: e.tensor_tensor(out=LL[b][:], in0=LL[b][:], in1=maskS[:, mi, :], op=ALU.subtract),
                                   reads=[TLL[b], TmaskS], writes=[TLL[b]])
                            if k < n - 1:
                                op("dve", lambda e: e.tensor_tensor(out=Rsb[:], in0=PS[4 + b][:, :], in1=Rsb[:], op=ALU.add),
                                   reads=[TPS[4 + b], TRsb], writes=[TRsb])

                        def B(k):
                            kb, mi = bl[k]
                            b = (base + k) % 2
                            op("act", lambda e: e.activation(out=PT[b][:], in_=LL[b][:], func=AF.Exp, scale=-1.0),
                               reads=[TLL[b]], writes=[TPT[b]])
                            for j in range(4):
                                op("pe", lambda e, j=j: e.matmul(Ov[:, j, 0:64], lhsT=PT[b][:, j * 128:(j + 1) * 128], rhs=vt[:, kb, 0:64],
                                                                 start=(k == 0), stop=(k == n - 1)), reads=[TPT[b], Tvt], writes=[TOps])
                        for k in range(n + 2):
                            if k < n:
                                A1(k)
                            if 1 <= k <= n:
                                A2(k - 1)
                            if k >= 2:
                                B(k - 2)
                    if is_fox:
                        op("dve", lambda e: e.reciprocal(out=rinv[:].unsqueeze(2), in_=Ov[:, :, 64:65]), reads=[TOps], writes=[Trinv])
                        op("dve", lambda e: e.tensor_tensor(out=osb[:], in0=Ov[:, :, 0:64],
                                                            in1=rinv[:].unsqueeze(2).to_broadcast([128, 4, 64]), op=ALU.mult),
                           reads=[TOps, Trinv], writes=[Tosb])
                    else:
                        op("dve", lambda e: e.tensor_copy(out=osb[:], in_=Ov[:, :, 0:64]), reads=[TOps], writes=[Tosb])
                    op("pool", lambda e: e.tensor_tensor(out=sq[:], in0=osb[:], in1=osb[:], op=ALU.mult), reads=[Tosb], writes=[Tsq])
                    op("dve", lambda e: e.tensor_reduce(out=ssq[:], in_=sq[:], axis=AX.X, op=ALU.add), reads=[Tsq], writes=[Tssq])
                    op("act", lambda e: e.activation(out=ssq[:], in_=ssq[:], func=AF.Sqrt, scale=1.0 / HD, bias=EPS),
                       reads=[Tssq], writes=[Tssq])
                    op("dve", lambda e: e.reciprocal(out=ssq[:], in_=ssq[:]), reads=[Tssq], writes=[Tssq])
                    op("pool", lambda e: e.tensor_tensor(out=o2[:], in0=osb[:], in1=ssq[:].unsqueeze(2).to_broadcast([128, 4, 64]),
                                                         op=ALU.mult), reads=[Tosb, Tssq], writes=[To2])
                    om = octr[0] % 2
                    gi = 0 if is_fox else 1
                    op("pool", lambda e, om=om, gi=gi: e.tensor_tensor(out=omix[om][:], in0=o2[:],
                                                                       in1=gout[:, gi, :].unsqueeze(1).to_broadcast([128, 4, 64]),
                                                                       op=ALU.mult), reads=[To2, Tgout], writes=[Tomix[om]])
                    op("pool", lambda e, om=om, h=h, lt=lt: e.dma_start(
                        out=mixed_d[lt * 512:(lt + 1) * 512, h * 64:(h + 1) * 64].rearrange("(j p) d -> p j d", p=128),
                        in_=omix[om][:]), reads=[Tomix[om]], chan=ch_st[om])
            S_.barrier()

        if 3 in phases:
          with ExitStack() as p3:
            NU, GS = 3, 4
            WoB = sb(p3, "WoB", [128, 8, D], BF16); TWoB = Tk()
            WqB = sb(p3, "WqB", [128, 8, 2048], BF16); TWqB = Tk()
            SKb = sb(p3, "SKb", [128, 16, 128], BF16); TSKb = Tk()
            with ExitStack() as pc:
                stgf = [sb(pc, "stgf%d" % i, [128, 4096], F32) for i in range(3)]; Tstgf = [Tk() for _ in range(3)]
                stgb = [sb(pc, "stgb%d" % i, [128, 4096], BF16) for i in range(3)]; Tstgb = [Tk() for _ in range(3)]
                ch_si = [S_.chan("si%d" % i) for i in range(3)]
                ch_so = [S_.chan("so%d" % i) for i in range(3)]
                cengs = ["dve", "act", "pool"]
                sctr = [0]

                def stage_cast(src_ap, shape_str, kw, dst_ap, Tdst):
                    i = sctr[0] % 3; sctr[0] += 1
                    n = 1
                    for v in src_ap.shape[1:]:
                        n *= v
                    stg = stgf[i][:, 0:n].rearrange(shape_str, **kw)
                    op("sp", lambda e: e.dma_start(out=stg, in_=src_ap), writes=[Tstgf[i]], chan=ch_si[i])
                    if cengs[i] == "act":
                        op("act", lambda e: e.activation(out=dst_ap, in_=stg, func=AF.Copy), reads=[Tstgf[i]], writes=[Tdst])
                    else:
                        op(cengs[i], lambda e: e.tensor_copy(out=dst_ap, in_=stg), reads=[Tstgf[i]], writes=[Tdst])
                wo_v = w_out.rearrange("(k p) c -> p k c", p=128)
                wq_v = w_query.rearrange("(k p) c -> p k c", p=128)
                for i in range(2):
                    stage_cast(wo_v[:, :, i * 512:(i + 1) * 512], "p (k c) -> p k c", dict(k=8), WoB[:, :, i * 512:(i + 1) * 512], TWoB)
                for i in range(4):
                    stage_cast(wq_v[:, :, i * 512:(i + 1) * 512], "p (k c) -> p k c", dict(k=8), WqB[:, :, i * 512:(i + 1) * 512], TWqB)
                stage_cast(skT_d, "p (k c) -> p k c", dict(k=16), SKb[:], TSKb)
                RPP = nexp // 128
                for (tsrc, tdst) in ((e_down, edu_bf[:, 0, :]), (e_up, edu_bf[:, 1, :])):
                    sv = tsrc.rearrange("(r p) d -> p r d", p=128)
                    dv = tdst.rearrange("(r p) d -> p r d", p=128)
                    for c4 in range(max(1, RPP // 4)):
                        nr = min(4, RPP)
                        i = sctr[0] % 3; sctr[0] += 1
                        sf = stgf[i][:, 0:nr * D].rearrange("p (r d) -> p r d", d=D)
                        sbv = stgb[i][:, 0:nr * D].rearrange("p (r d) -> p r d", d=D)
                        op("sp", lambda e: e.dma_start(out=sf, in_=sv[:, c4 * 4:c4 * 4 + nr, :]), writes=[Tstgf[i]], chan=ch_si[i])
                        if cengs[i] == "act":
                            op("act", lambda e: e.activation(out=sbv, in_=sf, func=AF.Copy), reads=[Tstgf[i]], writes=[Tstgb[i]])
                        else:
                            op(cengs[i], lambda e: e.tensor_copy(out=sbv, in_=sf), reads=[Tstgf[i]], writes=[Tstgb[i]])
                        op("sp", lambda e: e.dma_start(out=dv[:, c4 * 4:c4 * 4 + nr, :], in_=sbv), reads=[Tstgb[i]], chan=ch_so[i])
                S_.barrier()
            U = [sb(p3, "U%d" % i, [128, GS, 2 * D], BF16) for i in range(NU)]
            TU = [[Tk() for _ in range(GS)] for _ in range(NU)]
            ch_g = [[S_.chan("g%d_%d" % (i, s)) for s in range(GS)] for i in range(NU)]
            dg = [sb(p3, "dg%d" % i, [128, 128], BF16) for i in range(4)]; Tdg = [Tk() for _ in range(4)]
            mxt = [sb(p3, "mxt%d" % i, [128, D], BF16) for i in range(2)]; Tmxt = [Tk(), Tk()]
            xqt = [sb(p3, "xqt%d" % i, [128, D], F32) for i in range(2)]; Txqt = [Tk(), Tk()]
            mT = sb(p3, "mT", [128, 8, 128], BF16); TmT = Tk()
            x1b = [sb(p3, "x1_%d" % i, [128, D], F32) for i in range(2)]; Tx1b = [Tk(), Tk()]
            xn2 = sb(p3, "xn2", [128, D], BF16); Txn2 = Tk()
            h2b = [sb(p3, "h2_%d" % i, [128, D], F32) for i in range(2)]; Th2b = [Tk(), Tk()]
            h2T = sb(p3, "h2T", [128, 8, 128], BF16); Th2T = Tk()
            qT = sb(p3, "qT", [128, 16, 128], BF16); TqT = Tk()
            sc = sb(p3, "sc", [128, 16, 128], F32); Tsc = Tk()
            scr = sb(p3, "scr", [128, 256], F32); Tscr = Tk()
            v16 = sb(p3, "v16", [128, 16, 16], F32); Tv16 = Tk()
            i16 = sb(p3, "i16", [128, 16, 16], U32); Ti16 = Tk()
            i16f = sb(p3, "i16f", [128, 16, 16], F32); Ti16f = Tk()
            cand = sb(p3, "cand", [128, 8, 256], F32); Tcand = Tk()
            cidx = sb(p3, "cidx", [128, 8, 256], F32); Tcidx = Tk()
            m16 = sb(p3, "m16", [128, 8, 16], F32); Tm16 = Tk()
            eidf = sb(p3, "eidf", [128, 128], F32); Teidf = Tk()
            eid2 = [sb(p3, "eid%d" % i, [128, 128], I32) for i in range(2)]; Teid2 = [Tk(), Tk()]
            gtsb = [sb(p3, "gts%d" % i, [128, 8, 16], F32) for i in range(2)]; Tgtsb = [Tk(), Tk()]
            gsum = sb(p3, "gsum", [128, 8], F32); Tgsum = Tk()
            score = sb(p3, "score", [128, 128], F32); Tscore_g = [Tk() for _ in range(32)]
            actv = sb(p3, "actv", [128, 128], F32); Tactv_g = [Tk() for _ in range(32)]
            junk3 = sb(p3, "junk3", [128, D], BF16); Tjunk3 = Tk()
            junk4 = sb(p3, "junk4", [128, D], BF16); Tjunk4 = Tk()
            x2 = sb(p3, "x2", [128, D], F32); Tx2 = Tk()
            ot0 = sb(p3, "ot0", [128, D], F32); ot = [ot0, ot0]; Tot0 = Tk(); Tot = [Tot0, Tot0]
            st3 = sb(p3, "st3", [128, 4], F32); Tst3 = Tk()
            st4 = sb(p3, "st4", [128, 4], F32); Tst4 = Tk()

            gctr = [0]
            def front_part(tb):
                b = tb % 2
                rows = slice(tb * 128, (tb + 1) * 128)
                x1, Tx1 = x1b[b], Tx1b[b]
                eid, Teid = eid2[b], Teid2[b]
                h2, Th2 = h2b[b], Th2b[b]
                gts, Tgts = gtsb[b], Tgtsb[b]
                op("sp", lambda e: e.dma_start(out=mxt[b][:], in_=mixed_d[rows, :]), writes=[Tmxt[b]], chan=ch_ld[b])
                op("sp", lambda e: e.dma_start(out=xqt[b][:], in_=x_q[rows, :]), writes=[Txqt[b]], chan=ch_ld[2 + b])
                ptb = PS[0][:].bitcast(BF16)
                for k in range(8):
                    op("pe", lambda e, k=k: e.transpose(ptb[:, k * 128:(k + 1) * 128], mxt[b][:, k * 128:(k + 1) * 128], ident[:]),
                       reads=[Tmxt[b], Tident], writes=[TPS[0]])
                op("act", lambda e: e.activation(out=mT[:].rearrange("p k t -> p (k t)"), in_=ptb[:, 0:1024], func=AF.Copy),
                   reads=[TPS[0]], writes=[TmT])
                yield
                for hf in range(2):
                    for k in range(8):
                        op("pe", lambda e, k=k, hf=hf: e.matmul(PS[2 + hf][:, :], lhsT=mT[:, k, :], rhs=WoB[:, k, hf * 512:(hf + 1) * 512],
                                                                start=(k == 0), stop=(k == 7)), reads=[TmT, TWoB], writes=[TPS[2 + hf]])
                    hs = slice(hf * 512, (hf + 1) * 512)
                    op("dve", lambda e, hf=hf, hs=hs: e.tensor_tensor(out=x1[:, hs], in0=PS[2 + hf][:, :], in1=bcs[:, 0, hs], op=ALU.mult),
                       reads=[TPS[2 + hf], Tbcs[0]], writes=[Tx1])
                op("dve", lambda e: e.tensor_tensor(out=x1[:], in0=x1[:], in1=xqt[b][:], op=ALU.add), reads=[Tx1, Txqt[b]], writes=[Tx1])
                yield
                op("act", lambda e: e.activation(out=junk4[:], in_=x1[:], func=AF.Square, accum_out=st3[:, 0:1]),
                   reads=[Tx1], writes=[Tjunk4, Tst3])
                op("act", lambda e: e.activation(out=st3[:, 0:1], in_=st3[:, 0:1], func=AF.Sqrt, scale=1.0 / D, bias=EPS),
                   reads=[Tst3], writes=[Tst3])
                op("dve", lambda e: e.reciprocal(out=st3[:, 0:1], in_=st3[:, 0:1]), reads=[Tst3], writes=[Tst3])
                op("dve", lambda e: e.tensor_scalar(out=xn2[:], in0=x1[:], scalar1=st3[:, 0:1], scalar2=None, op0=ALU.mult),
                   reads=[Tx1, Tst3], writes=[Txn2])
                op("dve", lambda e: e.scalar_tensor_tensor(out=h2[:], in0=x1[:], scalar=st3[:, 0:1], in1=bcs[:, 2, :],
                                                           op0=ALU.mult, op1=ALU.mult), reads=[Tx1, Tst3, Tbcs[2]], writes=[Th2])
                op("dve", lambda e: e.tensor_tensor(out=h2[:], in0=h2[:], in1=bcs[:, 1, :], op=ALU.add), reads=[Th2, Tbcs[1]], writes=[Th2])
                yield
                ptb1 = PS[1][:].bitcast(BF16)
                for k in range(8):
                    op("pe", lambda e, k=k: e.transpose(ptb1[:, k * 128:(k + 1) * 128], xn2[:, k * 128:(k + 1) * 128], ident[:]),
                       reads=[Txn2, Tident], writes=[TPS[1]])
                for k in range(8):
                    op("act", lambda e, k=k: e.activation(out=h2T[:, k, :], in_=ptb1[:, k * 128:(k + 1) * 128], func=AF.Identity,
                                                          scale=G2[:, k:k + 1], bias=modT[:, 24 + k:25 + k]),
                       reads=[TPS[1], TG2, TmodT], writes=[Th2T])
                yield
                for g4 in range(4):
                    pi = g4 % 2
                    for q4 in range(4):
                        hp = g4 * 4 + q4
                        for k in range(8):
                            op("pe", lambda e, k=k, hp=hp, q4=q4, pi=pi: e.matmul(
                                PS[pi][:, q4 * 128:(q4 + 1) * 128], lhsT=WqB[:, k, hp * 128:(hp + 1) * 128], rhs=h2T[:, k, :],
                                start=(k == 0), stop=(k == 7)), reads=[TWqB, Th2T], writes=[TPS[pi]])
                    op("act", lambda e, g4=g4, pi=pi: e.activation(out=qT[:, g4 * 4:(g4 + 1) * 4, :].rearrange("p a t -> p (a t)"),
                                                                   in_=PS[pi][:, :], func=AF.Copy), reads=[TPS[pi]], writes=[TqT])
                yield
                for g4 in range(4):
                    for q4 in range(4):
                        hp = g4 * 4 + q4
                        op("pe", lambda e, hp=hp, q4=q4, g4=g4: e.matmul(PS[2 + g4][:, q4 * 128:(q4 + 1) * 128], lhsT=qT[:, hp, :],
                                                                         rhs=SKb[:, hp, :], start=True, stop=True),
                           reads=[TqT, TSKb], writes=[TPS[2 + g4]])
                    op("act", lambda e, g4=g4: e.activation(out=sc[:, g4 * 4:(g4 + 1) * 4, :].rearrange("p a t -> p (a t)"),
                                                            in_=PS[2 + g4][:, :], func=AF.Copy), reads=[TPS[2 + g4]], writes=[Tsc])
                yield
                for hp in range(16):
                    op("dve", lambda e, hp=hp: e.max(out=v16[:, hp, 0:8], in_=sc[:, hp, :]), reads=[Tsc], writes=[Tv16])
                    op("dve", lambda e, hp=hp: e.max_index(out=i16[:, hp, 0:8], in_max=v16[:, hp, 0:8], in_values=sc[:, hp, :]),
                       reads=[Tsc, Tv16], writes=[Ti16])
                    op("dve", lambda e, hp=hp: e.match_replace(out=scr[:, 0:128], in_to_replace=v16[:, hp, 0:8], in_values=sc[:, hp, :],
                                                               imm_value=-1e30), reads=[Tsc, Tv16], writes=[Tscr])
                    op("dve", lambda e, hp=hp: e.max(out=v16[:, hp, 8:16], in_=scr[:, 0:128]), reads=[Tscr], writes=[Tv16])
                    op("dve", lambda e, hp=hp: e.max_index(out=i16[:, hp, 8:16], in_max=v16[:, hp, 8:16], in_values=scr[:, 0:128]),
                       reads=[Tscr, Tv16], writes=[Ti16])
                    yield
                op("dve", lambda e: e.tensor_copy(out=i16f[:], in_=i16[:]), reads=[Ti16], writes=[Ti16f])
                v4 = v16[:].rearrange("p (h two) a -> p h two a", two=2)
                f4 = i16f[:].rearrange("p (h two) a -> p h two a", two=2)
                c4 = cand[:].rearrange("p h (a b) -> p h a b", b=16)
                x4 = cidx[:].rearrange("p h (a b) -> p h a b", b=16)
                op("dve", lambda e: e.tensor_tensor(out=c4, in0=v4[:, :, 0, :].unsqueeze(3).to_broadcast([128, 8, 16, 16]),
                                                    in1=v4[:, :, 1, :].unsqueeze(2).to_broadcast([128, 8, 16, 16]), op=ALU.add),
                   reads=[Tv16], writes=[Tcand])
                op("dve", lambda e: e.tensor_scalar(out=f4[:, :, 0, :], in0=f4[:, :, 0, :], scalar1=128.0, scalar2=None, op0=ALU.mult),
                   reads=[Ti16f], writes=[Ti16f])
                op("dve", lambda e: e.tensor_tensor(out=x4, in0=f4[:, :, 0, :].unsqueeze(3).to_broadcast([128, 8, 16, 16]),
                                                    in1=f4[:, :, 1, :].unsqueeze(2).to_broadcast([128, 8, 16, 16]), op=ALU.add),
                   reads=[Ti16f], writes=[Tcidx])
                for h in range(8):
                    op("dve", lambda e, h=h: e.max(out=m16[:, h, 0:8], in_=cand[:, h, :]), reads=[Tcand], writes=[Tm16])
                    op("dve", lambda e, h=h: e.match_replace(out=scr[:], in_to_replace=m16[:, h, 0:8], in_values=cand[:, h, :],
                                                             imm_value=-1e30), reads=[Tcand, Tm16], writes=[Tscr])
                    op("dve", lambda e, h=h: e.max(out=m16[:, h, 8:16], in_=scr[:]), reads=[Tscr], writes=[Tm16])
                    yield
                for h in range(8):
                    for k in range(16):
                        op("dve", lambda e, h=h, k=k: e.scalar_tensor_tensor(
                            out=junk3[:, 0:256], in0=cand[:, h, :], scalar=m16[:, h, k:k + 1], in1=cidx[:, h, :],
                            op0=ALU.is_equal, op1=ALU.mult, accum_out=eidf[:, h * 16 + k:h * 16 + k + 1]),
                           reads=[Tcand, Tcidx, Tm16], writes=[Tjunk3, Teidf])
                        if k % 4 == 3:
                            yield
                op("dve", lambda e: e.tensor_scalar(out=eidf[:], in0=eidf[:], scalar1=float(nexp - 1), scalar2=None, op0=ALU.min),
                   reads=[Teidf], writes=[Teidf])
                op("dve", lambda e: e.tensor_copy(out=eid[:], in_=eidf[:]), reads=[Teidf], writes=[Teid])
                yield
                op("dve", lambda e: e.tensor_tensor(out=gts[:], in0=m16[:], in1=m16[:, :, 0:1].to_broadcast([128, 8, 16]),
                                                    op=ALU.subtract), reads=[Tm16], writes=[Tgts])
                op("act", lambda e: e.activation(out=gts[:], in_=gts[:], func=AF.Exp), reads=[Tgts], writes=[Tgts])
                op("dve", lambda e: e.tensor_reduce(out=gsum[:], in_=gts[:], axis=AX.X, op=ALU.add), reads=[Tgts], writes=[Tgsum])
                op("dve", lambda e: e.reciprocal(out=gsum[:], in_=gsum[:]), reads=[Tgsum], writes=[Tgsum])
                op("dve", lambda e: e.tensor_tensor(out=gts[:], in0=gts[:], in1=gsum[:].unsqueeze(2).to_broadcast([128, 8, 16]),
                                                    op=ALU.mult), reads=[Tgts, Tgsum], writes=[Tgts])

            def block_part(tb, gen):
                b = tb % 2
                rows = slice(tb * 128, (tb + 1) * 128)
                x1, Tx1 = x1b[b], Tx1b[b]
                eid, Teid = eid2[b], Teid2[b]
                h2, Th2 = h2b[b], Th2b[b]
                gts, Tgts = gtsb[b], Tgtsb[b]
                gflat = gts[:].rearrange("p h k -> p (h k)")
                for grp in range(128 // GS):
                    ub = gctr[0] % NU; gctr[0] += 1
                    gs = slice(grp * GS, (grp + 1) * GS)
                    for s in range(GS):
                        slot = grp * GS + s
                        op("pool", lambda e, ub=ub, s=s, slot=slot: e.indirect_dma_start(
                            out=U[ub][:, s, :], out_offset=None, in_=edu_bf.rearrange("n t d -> n (t d)"),
                            in_offset=bass.IndirectOffsetOnAxis(ap=eid[:, slot:slot + 1], axis=0)),
                           reads=[Teid], writes=[TU[ub][s]], chan=ch_g[ub][s])
                    for s in range(GS):
                        slot = grp * GS + s
                        op("dve", lambda e, s=s, slot=slot: e.scalar_tensor_tensor(
                            out=junk3[:], in0=U[ub][:, s, 0:D], scalar=1.0, in1=h2[:],
                            op0=ALU.mult, op1=ALU.mult, accum_out=score[:, slot:slot + 1]),
                           reads=[TU[ub][s], Th2], writes=[Tjunk3, Tscore_g[grp]])
                    op("act", lambda e: e.activation(out=actv[:, gs], in_=score[:, gs], func=AF.Gelu),
                       reads=[Tscore_g[grp]], writes=[Tactv_g[grp]])
                    op("dve", lambda e: e.tensor_tensor(out=actv[:, gs], in0=actv[:, gs], in1=gflat[:, gs], op=ALU.mult),
                       reads=[Tactv_g[grp], Tgts], writes=[Tactv_g[grp]])
                    for s in range(GS):
                        slot = grp * GS + s
                        di = slot % 4
                        op("act", lambda e, slot=slot, di=di: e.activation(out=dg[di][:], in_=ident[:], func=AF.Copy,
                                                                           scale=actv[:, slot:slot + 1]),
                           reads=[Tident, Tactv_g[grp]], writes=[Tdg[di]])
                        for hf in range(2):
                            op("pe", lambda e, hf=hf, s=s, di=di, slot=slot: e.matmul(
                                PS[6 + hf][:, :], lhsT=dg[di][:], rhs=U[ub][:, s, D + hf * 512:D + (hf + 1) * 512],
                                start=(slot == 0), stop=(slot == 127)), reads=[Tdg[di], TU[ub][s]], writes=[TPS[6 + hf]])
                    if gen is not None:
                        for _ in range(3):
                            next(gen, None)
                if gen is not None:
                    for _ in gen:
                        pass
                for hf in range(2):
                    hs = slice(hf * 512, (hf + 1) * 512)
                    op("dve", lambda e, hf=hf, hs=hs: e.tensor_tensor(out=x2[:, hs], in0=PS[6 + hf][:, :], in1=bcs[:, 3, hs], op=ALU.mult),
                       reads=[TPS[6 + hf], Tbcs[3]], writes=[Tx2])
                op("dve", lambda e: e.tensor_tensor(out=x2[:], in0=x2[:], in1=x1[:], op=ALU.add), reads=[Tx2, Tx1], writes=[Tx2])
                op("act", lambda e: e.activation(out=junk4[:], in_=x2[:], func=AF.Square, accum_out=st4[:, 1:2]),
                   reads=[Tx2], writes=[Tjunk4, Tst4])
                op("act", lambda e: e.activation(out=st4[:, 1:2], in_=st4[:, 1:2], func=AF.Sqrt, scale=1.0 / D, bias=EPS),
                   reads=[Tst4], writes=[Tst4])
                op("dve", lambda e: e.reciprocal(out=st4[:, 1:2], in_=st4[:, 1:2]), reads=[Tst4], writes=[Tst4])
                op("dve", lambda e: e.scalar_tensor_tensor(out=ot[b][:], in0=x2[:], scalar=st4[:, 1:2], in1=bcs[:, 4, :],
                                                           op0=ALU.mult, op1=ALU.mult), reads=[Tx2, Tst4, Tbcs[4]], writes=[Tot[b]])
                op("sp", lambda e: e.dma_start(out=out_d[rows, :], in_=ot[b][:]), reads=[Tot[b]], chan=ch_st[b])

            for _ in front_part(0):
                pass
            for tb in range(NBQ):
                block_part(tb, front_part(tb + 1) if tb + 1 < NBQ else None)

        S_.barrier()
        print("build: ninst", S_.ninst, "nwaits", S_.nwaits, "sems", len(S_.sems))
    return nc


def make_masks(parity):
    s = np.arange(128)[:, None]
    t = np.arange(512)[None, :]
    out = np.zeros((2, 128, 16, 512), np.float32)
    kinds = ["n", "d", "d", "f"] if parity == 0 else ["d", "f", "n", "d"]
    for ms in range(4):
        for i in range(4):
            for v in range(2):
                if kinds[ms] == "n":
                    m = np.full((128, 512), NEG, np.float32)
                elif kinds[ms] == "f":
                    m = np.zeros((128, 512), np.float32)
                else:
                    kp = i * 128 + s
                    vis = (kp <= t) if v == 0 else (kp < t)
                    m = np.where(vis, 0.0, NEG).astype(np.float32)
                out[v, :, ms * 4 + i, :] = m
    return out.astype(ml_dtypes.bfloat16)


def make_in_maps(inp, S):
    NT = S // 512
    f32 = lambda a: np.ascontiguousarray(a, dtype=np.float32)
    colT = lambda v, n: f32(np.asarray(v).reshape(n, 128).T)
    shared = {
        "w_ada": f32(inp["w_ada"][0]),
        "badaT": colT(inp["b_ada"][0], 48),
        "bada_row": f32(inp["b_ada"][0][None, :]),
        "gmixT": colT(inp["g_norm_mix"][0], 8),
        "gffnT": colT(inp["g_norm_ffn"][0], 8),
        "gffn_row": f32(inp["g_norm_ffn"][0][None, :]),
        "gfin_row": f32(inp["g_final"][None, :]),
        "w_in": f32(inp["w_in"][0]),
        "bf_row": f32(inp["b_forget"][0][None, :]),
        "gfox_row": f32(inp["g_out_fox"][0][None, :]),
        "gsb_row": f32(inp["g_out_sb"][0][None, :]),
        "w_out": f32(inp["w_out"][0]),
        "w_query": f32(inp["w_query"][0]),
        "skT": f32(np.asarray(inp["sub_keys"][0]).reshape(16, 128, 128).transpose(2, 0, 1)),
        "e_down": f32(inp["expert_down"][0]),
        "e_up": f32(inp["expert_up"][0]),
    }
    masks = [make_masks(0), make_masks(1)]
    maps = []
    for core in range(8):
        b, r = core // 2, core % 2
        xb = np.asarray(inp["x"][b])
        tiles = tile_ids(r, NT)
        m = dict(shared)
        m["x_all"] = f32(xb)
        m["x_q"] = f32(np.concatenate([xb[t * 512:(t + 1) * 512] for t in tiles], axis=0))
        m["cT"] = colT(inp["c"][b], 8)
        m["masks"] = np.ascontiguousarray(masks[r][0])
        m["masks_sb"] = np.ascontiguousarray(masks[r][1])
        fl = np.zeros((8, 2), np.float32)
        fl[:, r] = 1.0
        m["flags"] = fl
        maps.append(m)
    return maps


def assemble(results, S):
    NT = S // 512
    out = np.zeros((4, S, D), np.float32)
    for core in range(8):
        b, r = core // 2, core % 2
        o = np.asarray(results[core]["out"])
        for lt, t in enumerate(tile_ids(r, NT)):
            out[b, t * 512:(t + 1) * 512] = o[lt * 512:(lt + 1) * 512]
    return out


_NC_CACHE = {}


def kernel(**inputs):
    S = 8192
    if S not in _NC_CACHE:
        _NC_CACHE[S] = build(S)
    nc = _NC_CACHE[S]
    maps = make_in_maps(inputs, S)
    res = run_bass_kernel_spmd(nc, maps, core_ids=list(range(8)))
    return assemble(res.results, S)
```

```python
import numpy as np
import ml_dtypes
from contextlib import ExitStack
import concourse.bass as bass
import concourse.mybir as mybir
from concourse.bass_utils import run_bass_kernel_spmd

F32 = mybir.dt.float32
BF16 = mybir.dt.bfloat16
I32 = mybir.dt.int32
U32 = mybir.dt.uint32
AF = mybir.ActivationFunctionType
ALU = mybir.AluOpType
AX = mybir.AxisListType

D = 1024
NH = 16
HD = 64
KA = 70
INC = 3080
NEXP = 16384
EPS = 1e-6
NEG = -30000.0


class Tk:
    __slots__ = ("name", "w", "r")

    def __init__(self, name=""):
        self.name = name
        self.w = None
        self.r = {}


class Chan:
    def __init__(self, S, name):
        self.sem = S.new_sem(name)
        self.key = name
        self.count = 0


class Sched:
    ENGS = ("pe", "act", "dve", "pool", "sp")
    ROT = 20000

    def __init__(self, nc, es):
        self.nc = nc
        self.es = es
        self.eobj = {"pe": nc.tensor, "act": nc.scalar, "dve": nc.vector, "pool": nc.gpsimd, "sp": nc.sync}
        self.sems = {}
        self.chans = []
        self.gen = {k: 0 for k in self.ENGS}
        self.ekey = {}
        self.count = {}
        self.known = {k: {} for k in self.ENGS}
        self.done_keys = []
        for k in self.ENGS:
            self._new_esem(k)
        self.nwaits = 0
        self.ninst = 0

    def _new_esem(self, k):
        key = "e_%s_%d" % (k, self.gen[k])
        self.gen[k] += 1
        self.new_sem(key)
        self.ekey[k] = key
        self.count[k] = 0

    def new_sem(self, name):
        s = self.es.enter_context(self.nc.semaphore(name))
        self.sems[name] = s
        return s

    def chan(self, name):
        c = Chan(self, "c_" + name)
        self.chans.append(c)
        return c

    def _wait(self, eng, ev):
        key, val, clock = ev
        kn = self.known[eng]
        if kn.get(key, 0) >= val:
            return
        self.eobj[eng].wait_ge(self.sems[key], val)
        self.nwaits += 1
        kn[key] = val
        if clock:
            for k2, v2 in clock.items():
                if kn.get(k2, 0) < v2:
                    kn[k2] = v2

    def op(self, eng, fn, reads=(), writes=(), chan=None):
        deps = []
        epref = "e_%s_" % eng
        for t in reads:
            if t.w is not None:
                deps.append(t.w)
        for t in writes:
            for ev in t.r.values():
                if ev[0].startswith(epref):
                    continue
                deps.append(ev)
            if t.w is not None:
                if t.w[0].startswith(epref):
                    continue
                deps.append(t.w)
        for ev in deps:
            self._wait(eng, ev)
        self.ninst += 1
        if chan is None:
            if self.count[eng] >= self.ROT:
                self.done_keys.append((self.ekey[eng], self.count[eng]))
                self._new_esem(eng)
            key = self.ekey[eng]
            self.count[eng] += 1
            val = self.count[eng]
            fn(self.eobj[eng]).then_inc(self.sems[key], 1)
            ev = (key, val, dict(self.known[eng]))
        else:
            chan.count += 16
            fn(self.eobj[eng]).then_inc(chan.sem, 16)
            ev = (chan.key, chan.count, dict(self.known[eng]))
        for t in writes:
            t.w = ev
            t.r = {}
        for t in reads:
            if t in writes:
                continue
            t.r[("e_" + eng) if chan is None else chan.key] = ev
        return ev

    def barrier(self):
        evs = []
        for k in self.ENGS:
            if self.count[k] > 0:
                evs.append((self.ekey[k], self.count[k], None))
        for key, cnt in self.done_keys:
            evs.append((key, cnt, None))
        for c in self.chans:
            if c.count > 0:
                evs.append((c.key, c.count, None))
        for eng in self.ENGS:
            for ev in evs:
                self._wait(eng, ev)


def tile_ids(parity, NT):
    out = []
    for m in range(NT // 4):
        out += [4 * m, 4 * m + 3] if parity == 0 else [4 * m + 1, 4 * m + 2]
    return out


def build(S, dbg=0, phases=(0, 1, 2, 3), nexp=NEXP):
    NT = S // 512
    NB = S // 128
    NTQ = NT // 2
    SQ = NTQ * 512
    NBQ = SQ // 128
    nc = bass.Bass("TRN2", target_bir_lowering=False)

    def din(name, shape, dt=F32):
        return nc.dram_tensor(name, list(shape), dt, kind="ExternalInput").ap()

    x_all = din("x_all", [S, D])
    x_q = din("x_q", [SQ, D])
    cT_d = din("cT", [128, 8])
    w_ada = din("w_ada", [D, 6 * D])
    badaT_d = din("badaT", [128, 48])
    bada_row = din("bada_row", [1, 6 * D])
    gmixT_d = din("gmixT", [128, 8])
    gffnT_d = din("gffnT", [128, 8])
    gffn_row = din("gffn_row", [1, D])
    gfin_row = din("gfin_row", [1, D])
    w_in = din("w_in", [D, INC])
    bf_row = din("bf_row", [1, 8])
    gfox_row = din("gfox_row", [1, HD])
    gsb_row = din("gsb_row", [1, HD])
    w_out = din("w_out", [D, D])
    w_query = din("w_query", [D, 2048])
    skT_d = din("skT", [128, 16, 128])
    e_down = din("e_down", [nexp, D])
    e_up = din("e_up", [nexp, D])
    masks_d = din("masks", [128, 16, 512], BF16)
    masks_sb_d = din("masks_sb", [128, 16, 512], BF16)
    flags_d = din("flags", [8, 2])
    out_d = nc.dram_tensor("out", [SQ, D], F32, kind="ExternalOutput").ap()

    okind = "ExternalOutput" if dbg else None

    def dscr(name, shape, dt):
        if dbg:
            return nc.dram_tensor(name, list(shape), dt, kind="ExternalOutput").ap()
        return nc.dram_tensor(name, list(shape), dt).ap()

    KT_d = dscr("KT_d", [NH, KA, S], BF16)
    QT_d = dscr("QT_d", [NH, KA, SQ], BF16)
    V_d = dscr("V_d", [NH, 128, NB, 65], BF16)
    mixed_d = dscr("mixed_d", [SQ, D], BF16)
    edu_bf = nc.dram_tensor("edu_bf", [nexp, 2, D], BF16).ap()
    if dbg:
        mod_dbg = dscr("mod_dbg", [128, 48], F32)

    with ExitStack() as es:
        S_ = Sched(nc, es)
        op = S_.op

        def sb(stack, name, shape, dt):
            return stack.enter_context(nc.sbuf_tensor("s_" + name, list(shape), dt))

        PS = [es.enter_context(nc.psum_tensor("ps%d" % i, [128, 512], F32)) for i in range(8)]
        TPS = [Tk("ps%d" % i) for i in range(8)]

        ident = sb(es, "ident", [128, 128], BF16); Tident = Tk()
        tri = sb(es, "tri", [128, 128], BF16); Ttri = Tk()
        ones = sb(es, "ones", [128, 128], BF16); Tones = Tk()
        modT = sb(es, "modT", [128, 48], F32); TmodT = Tk()
        G1 = sb(es, "G1", [128, 8], F32); TG1 = Tk()
        G2 = sb(es, "G2", [128, 8], F32); TG2 = Tk()
        bcs = sb(es, "bcs", [128, 5, D], F32)
        Tbcs = [Tk() for _ in range(5)]
        gout = sb(es, "gout", [128, 2, HD], F32); Tgout = Tk()
        flags = sb(es, "flags_sb", [8, 2], F32); Tflags = Tk()

        ch_c = S_.chan("const")
        ch_ld = [S_.chan("ld%d" % i) for i in range(4)]
        ch_st = [S_.chan("st%d" % i) for i in range(4)]
        ch_kts = [S_.chan("kts%d" % i) for i in range(4)]
        ch_aug = [S_.chan("aug%d" % i) for i in range(3)]
        ch_hd = [[S_.chan("hd%d_%d" % (i, j)) for j in range(3)] for i in range(2)]
        ch_stg = [S_.chan("stg%d" % i) for i in range(2)]

        op("pool", lambda e: e.memset(ident[:], 1.0), writes=[Tident])
        op("pool", lambda e: e.affine_select(out=ident[:], in_=ident[:], pattern=[[-1, 128]],
                                             compare_op=ALU.is_equal, fill=0.0, base=0, channel_multiplier=1),
           reads=[Tident], writes=[Tident])
        op("pool", lambda e: e.memset(tri[:], 1.0), writes=[Ttri])
        op("pool", lambda e: e.affine_select(out=tri[:], in_=tri[:], pattern=[[-1, 128]],
                                             compare_op=ALU.is_ge, fill=0.0, base=0, channel_multiplier=1),
           reads=[Ttri], writes=[Ttri])
        op("pool", lambda e: e.memset(ones[:], 1.0), writes=[Tones])
        op("sp", lambda e: e.dma_start(out=gout[:, 0, :], in_=gfox_row.partition_broadcast(128)), writes=[Tgout], chan=S_.chan("k1"))
        op("sp", lambda e: e.dma_start(out=gout[:, 1, :], in_=gsb_row.partition_broadcast(128)), writes=[Tgout], chan=S_.chan("k2"))
        op("sp", lambda e: e.dma_start(out=flags[:], in_=flags_d), writes=[Tflags], chan=S_.chan("k3"))

        with ExitStack() as p0:
            cT = sb(p0, "cT", [128, 8], F32); TcT = Tk()
            scT = sb(p0, "scT", [128, 8], F32); TscT = Tk()
            screp = sb(p0, "screp", [128, 8, 128], F32); Tscrep = Tk()
            wa = [sb(p0, "wa%d" % i, [128, 8, 512], F32) for i in range(2)]; Twa = [Tk(), Tk()]
            badaT = sb(p0, "badaT", [128, 48], F32); TbadaT = Tk()
            badabc = sb(p0, "badabc", [128, 4, D], F32); Tbadabc = Tk()
            gmixT = sb(p0, "gmixT", [128, 8], F32); TgmixT = Tk()
            gffnT = sb(p0, "gffnT", [128, 8], F32); TgffnT = Tk()
            gffnbc = sb(p0, "gffnbc", [128, D], F32); Tgffnbc = Tk()

            op("sp", lambda e: e.dma_start(out=cT[:], in_=cT_d), writes=[TcT], chan=S_.chan("k4"))
            op("sp", lambda e: e.dma_start(out=badaT[:], in_=badaT_d), writes=[TbadaT], chan=S_.chan("k5"))
            op("sp", lambda e: e.dma_start(out=gmixT[:], in_=gmixT_d), writes=[TgmixT], chan=S_.chan("k6"))
            op("sp", lambda e: e.dma_start(out=gffnT[:], in_=gffnT_d), writes=[TgffnT], chan=S_.chan("k7"))
            op("sp", lambda e: e.dma_start(out=gffnbc[:], in_=gffn_row.partition_broadcast(128)), writes=[Tgffnbc], chan=S_.chan("k8"))
            op("sp", lambda e: e.dma_start(out=bcs[:, 4, :], in_=gfin_row.partition_broadcast(128)), writes=[Tbcs[4]], chan=S_.chan("k9"))
            op("sp", lambda e: e.dma_start(out=badabc[:].rearrange("p a d -> p (a d)"),
                                           in_=bada_row[:, 2 * D:6 * D].partition_broadcast(128)), writes=[Tbadabc], chan=S_.chan("k10"))
            op("act", lambda e: e.activation(out=scT[:], in_=cT[:], func=AF.Silu), reads=[TcT], writes=[TscT])
            op("dve", lambda e: e.tensor_copy(out=screp[:], in_=scT[:].unsqueeze(2).to_broadcast([128, 8, 128])),
               reads=[TscT], writes=[Tscrep])
            w_ada_v = w_ada.rearrange("(k p) c -> p k c", p=128)
            modps = PS[0]
            for cc in range(12):
                b = cc % 2
                op("sp", lambda e, cc=cc, b=b: e.dma_start(out=wa[b][:], in_=w_ada_v[:, :, cc * 512:(cc + 1) * 512]),
                   writes=[Twa[b]], chan=ch_ld[b])
                for f4 in range(4):
                    fc = cc * 4 + f4
                    for k in range(8):
                        op("pe", lambda e, b=b, k=k, f4=f4, fc=fc: e.matmul(
                            modps[:, fc:fc + 1], lhsT=wa[b][:, k, f4 * 128:(f4 + 1) * 128], rhs=scT[:, k:k + 1],
                            start=(k == 0), stop=(k == 7)), reads=[Twa[b], TscT], writes=[TPS[0]])
                if cc >= 4:
                    a = (cc - 4) // 2
                    hh = (cc - 4) % 2
                    pb = PS[1 + (cc % 2)]
                    Tpb = TPS[1 + (cc % 2)]
                    for k in range(8):
                        op("pe", lambda e, b=b, k=k, pb=pb: e.matmul(pb[:, :], lhsT=screp[:, k, :], rhs=wa[b][:, k, :],
                                                                   start=(k == 0), stop=(k == 7)),
                           reads=[Twa[b], Tscrep], writes=[Tpb])
                    op("dve", lambda e, a=a, hh=hh, pb=pb: e.tensor_tensor(
                        out=bcs[:, a, hh * 512:(hh + 1) * 512], in0=pb[:, :], in1=badabc[:, a, hh * 512:(hh + 1) * 512],
                        op=ALU.add), reads=[Tpb, Tbadabc], writes=[Tbcs[a]])
            op("dve", lambda e: e.tensor_tensor(out=modT[:], in0=modps[:, 0:48], in1=badaT[:], op=ALU.add),
               reads=[TPS[0], TbadaT], writes=[TmodT])
            op("dve", lambda e: e.scalar_tensor_tensor(out=G1[:], in0=modT[:, 8:16], scalar=1.0, in1=gmixT[:],
                                                       op0=ALU.add, op1=ALU.mult), reads=[TmodT, TgmixT], writes=[TG1])
            op("dve", lambda e: e.scalar_tensor_tensor(out=G2[:], in0=modT[:, 32:40], scalar=1.0, in1=gffnT[:],
                                                       op0=ALU.add, op1=ALU.mult), reads=[TmodT, TgffnT], writes=[TG2])
            op("dve", lambda e: e.scalar_tensor_tensor(out=bcs[:, 2, :], in0=bcs[:, 2, :], scalar=1.0, in1=gffnbc[:],
                                                       op0=ALU.add, op1=ALU.mult), reads=[Tbcs[2], Tgffnbc], writes=[Tbcs[2]])
            if dbg:
                op("sp", lambda e: e.dma_start(out=mod_dbg, in_=modT[:]), reads=[TmodT], chan=ch_st[0])
            S_.barrier()

        def norm_chunk(xsrc_ap, xt, Txt, ss, Tss, rstd, Trstd, xn, Txn, junk, Tjunk, hT, ThT, Gc, TGc, Bc_ap, TBc, ldchan,
                       psA, psB):
            op("sp", lambda e: e.dma_start(out=xt[:], in_=xsrc_ap.rearrange("(j p) d -> p j d", p=128)),
               writes=[Txt], chan=ldchan)
            for j in range(4):
                op("act", lambda e, j=j: e.activation(out=junk[:], in_=xt[:, j, :], func=AF.Square,
                                                      accum_out=ss[:, j:j + 1]), reads=[Txt], writes=[Tjunk, Tss])
            op("act", lambda e: e.activation(out=rstd[:], in_=ss[:], func=AF.Sqrt, scale=1.0 / D, bias=EPS),
               reads=[Tss], writes=[Trstd])
            op("dve", lambda e: e.reciprocal(out=rstd[:], in_=rstd[:]), reads=[Trstd], writes=[Trstd])
            for j in range(4):
                op("dve", lambda e, j=j: e.tensor_scalar(out=xn[:, j, :], in0=xt[:, j, :], scalar1=rstd[:, j:j + 1],
                                                         scalar2=None, op0=ALU.mult), reads=[Txt, Trstd], writes=[Txn])
            for k in range(8):
                pi = psA if k % 2 == 0 else psB
                pt = PS[pi][:].bitcast(BF16)
                for j in range(4):
                    op("pe", lambda e, j=j, k=k, pt=pt: e.transpose(pt[:, j * 128:(j + 1) * 128], xn[:, j, k * 128:(k + 1) * 128],
                                                                   ident[:]), reads=[Txn, Tident], writes=[TPS[pi]])
                op("act", lambda e, k=k, pt=pt: e.activation(out=hT[:, k, :], in_=pt[:, 0:512], func=AF.Identity,
                                                             scale=Gc[:, k:k + 1], bias=Bc_ap[:, k:k + 1]),
                   reads=[TPS[pi], TGc, TBc], writes=[ThT])

        with ExitStack() as p1:
            Wb = sb(p1, "Wb", [128, 8, INC], BF16); TWb = Tk()
            xt = [sb(p1, "xt%d" % i, [128, 4, D], F32) for i in range(2)]; Txt = [Tk(), Tk()]
            xn = sb(p1, "xn", [128, 4, D], BF16); Txn = Tk()
            junk = sb(p1, "junk", [128, D], BF16); Tjunk = Tk()
            ss = [sb(p1, "ss%d" % i, [128, 4], F32) for i in range(2)]; Tss = [Tk(), Tk()]
            rstd = [sb(p1, "rstd%d" % i, [128, 4], F32) for i in range(2)]; Trstd = [Tk(), Tk()]
            hT = sb(p1, "hT", [128, 8, 512], BF16); ThT = Tk()
            KTs = [sb(p1, "KTs%d" % i, [128, 512], BF16) for i in range(4)]; TKTs = [Tk() for _ in range(4)]
            Vs = [sb(p1, "Vs%d" % i, [128, 4, NH, 65], BF16) for i in range(2)]; TVs = [Tk(), Tk()]
            tribig = sb(p1, "tribig", [128, 4, 512], F32); Ttribig = Tk()
            bfbc = sb(p1, "bfbc", [128, 8], F32); Tbfbc = Tk()
            ub = sb(p1, "ub", [128, 4, 8], F32); Tub = Tk()
            lf = sb(p1, "lf", [128, 4, 8], F32); Tlf = Tk()
            GT = [sb(p1, "GT%d" % i, [8, 512], F32) for i in range(2)]; TGT = [Tk(), Tk()]
            gz = sb(p1, "gz", [8, 1], F32); Tgz = Tk()
            sp_hi = sb(p1, "sp_hi", [8, 3, 512], BF16); Tsp_hi = Tk()
            r1 = sb(p1, "r1", [8, 512], F32); Tr1 = Tk()
            r2 = sb(p1, "r2", [8, 512], F32); Tr2 = Tk()
            gq = sb(p1, "gq", [8, 512], F32); Tgq = Tk()
            spq = sb(p1, "spq", [8, 3, 512], BF16); Tspq = Tk()
            cst = sb(p1, "cst", [8, 3, 512], BF16); Tcst = Tk()

            w_in_v = w_in.rearrange("(k p) c -> p k c", p=128)
            wst = [xt[i][:].rearrange("p j d -> p (j d)")[:, 0:3520].rearrange("p (k c) -> p k c", k=8) for i in range(2)]
            Twst = Txt
            for i in range(7):
                b = i % 2
                op("sp", lambda e, i=i, b=b: e.dma_start(out=wst[b], in_=w_in_v[:, :, i * 440:(i + 1) * 440]),
                   writes=[Twst[b]], chan=ch_ld[b])
                eng = "dve" if i % 2 == 0 else "pool"
                op(eng, lambda e, i=i, b=b: e.tensor_copy(out=Wb[:, :, i * 440:(i + 1) * 440], in_=wst[b]),
                   reads=[Twst[b]], writes=[TWb])
            op("pool", lambda e: e.memset(tribig[:], 1.0), writes=[Ttribig])
            for j in range(4):
                op("pool", lambda e, j=j: e.affine_select(out=tribig[:, j, :], in_=tribig[:, j, :], pattern=[[1, 512]],
                                                          compare_op=ALU.is_ge, fill=0.0, base=-j * 128, channel_multiplier=-1),
                   reads=[Ttribig], writes=[Ttribig])
            op("sp", lambda e: e.dma_start(out=bfbc[:], in_=bf_row.partition_broadcast(128)), writes=[Tbfbc], chan=S_.chan("k11"))
            op("pool", lambda e: e.memset(gz[:], 0.0), writes=[Tgz])
            for i in range(2):
                op("pool", lambda e, i=i: e.memset(Vs[i][:], 1.0), writes=[TVs[i]])
            op("pool", lambda e: e.memset(cst[:], -1.0), writes=[Tcst])
            for c in range(NT):
                op("pool", lambda e, c=c: e.dma_start(
                    out=KT_d[0:8, 67:70, c * 512:(c + 1) * 512], in_=cst[:]),
                   reads=[Tcst], chan=ch_aug[0])
            op("pool", lambda e: e.memset(cst[:], 1.0), reads=[], writes=[Tcst])
            for c in range(NTQ):
                op("pool", lambda e, c=c: e.dma_start(
                    out=QT_d[0:8, 64:67, c * 512:(c + 1) * 512], in_=cst[:]),
                   reads=[Tcst], chan=ch_aug[0])

            o1 = 1536
            o2 = 1544
            kcols = [512 + 128 * i for i in range(4)] + [o2 + 512 + 128 * i for i in range(4)]
            qcols = [0 + 128 * i for i in range(4)] + [o2 + 128 * i for i in range(4)]
            vcols = [1024, o2 + 1024]

            def proj_T(cols_list, dst_d, c, scale, rr):
                for hp in range(8):
                    pi = 2 + (rr[0] % 2); rr[0] += 1
                    for k in range(8):
                        op("pe", lambda e, hp=hp, k=k, pi=pi: e.matmul(
                            PS[pi][:, :], lhsT=Wb[:, k, cols_list[hp]:cols_list[hp] + 128], rhs=hT[:, k, :],
                            start=(k == 0), stop=(k == 7)), reads=[TWb, ThT], writes=[TPS[pi]])
                    kb = rr[1] % 4; rr[1] += 1
                    op("act", lambda e, pi=pi, kb=kb: e.activation(out=KTs[kb][:], in_=PS[pi][:, :], func=AF.Copy, scale=scale),
                       reads=[TPS[pi]], writes=[TKTs[kb]])
                    for i2 in range(2):
                        op("pool", lambda e, hp=hp, kb=kb, i2=i2: e.dma_start(
                            out=dst_d[2 * hp + i2, 0:64, c * 512:(c + 1) * 512],
                            in_=KTs[kb][i2 * 64:(i2 + 1) * 64, :]), reads=[TKTs[kb]], chan=ch_kts[kb])

            rr = [0, 0]
            for c in range(NT):
                b = c % 2
                norm_chunk(x_all[c * 512:(c + 1) * 512, :], xt[b], Txt[b], ss[b], Tss[b], rstd[b], Trstd[b], xn, Txn,
                           junk, Tjunk, hT, ThT, G1, TG1, modT[:, 0:8], TmodT, ch_ld[b], 0, 1)
                proj_T(kcols, KT_d, c, 1.0, rr)
                vb = c % 2
                for j in range(4):
                    for g in range(2):
                        pi = 4 + ((j * 2 + g) % 2)
                        for k in range(8):
                            op("pe", lambda e, j=j, g=g, k=k, pi=pi: e.matmul(
                                PS[pi][:, :], lhsT=hT[:, k, j * 128:(j + 1) * 128], rhs=Wb[:, k, vcols[g]:vcols[g] + 512],
                                start=(k == 0), stop=(k == 7)), reads=[TWb, ThT], writes=[TPS[pi]])
                        op("dve", lambda e, j=j, g=g, pi=pi, vb=vb: e.tensor_copy(
                            out=Vs[vb][:, j, g * 8:(g + 1) * 8, 0:64], in_=PS[pi][:, :].rearrange("p (h d) -> p h d", d=64)),
                           reads=[TPS[pi]], writes=[TVs[vb]])
                for j in range(4):
                    op("pool", lambda e, j=j, vb=vb, c=c: e.dma_start(
                        out=V_d[:, :, 4 * c + j, :].rearrange("h p e -> p h e"), in_=Vs[vb][:, j, :, :]),
                       reads=[TVs[vb]], chan=ch_st[2 + vb])
                for j in range(4):
                    for k in range(8):
                        op("pe", lambda e, j=j, k=k: e.matmul(
                            PS[6][:, j * 8:(j + 1) * 8], lhsT=hT[:, k, j * 128:(j + 1) * 128], rhs=Wb[:, k, o1:o1 + 8],
                            start=(k == 0), stop=(k == 7)), reads=[TWb, ThT], writes=[TPS[6]])
                op("dve", lambda e: e.tensor_tensor(out=ub[:], in0=PS[6][:, 0:32].rearrange("p (j h) -> p j h", h=8),
                                                    in1=bfbc[:].unsqueeze(1).to_broadcast([128, 4, 8]), op=ALU.add),
                   reads=[TPS[6], Tbfbc], writes=[Tub])
                op("act", lambda e: e.activation(out=ub[:], in_=ub[:], func=AF.Exp, scale=-1.0), reads=[Tub], writes=[Tub])
                op("act", lambda e: e.activation(out=lf[:], in_=ub[:], func=AF.Ln, bias=1.0), reads=[Tub], writes=[Tlf])
                for j in range(4):
                    op("pe", lambda e, j=j: e.matmul(PS[7][0:8, :], lhsT=lf[:, j, :], rhs=tribig[:, j, :],
                                                     start=(j == 0), stop=(j == 3)), reads=[Tlf, Ttribig], writes=[TPS[7]])
                gcur, Tgcur = GT[c % 2], TGT[c % 2]
                if c == 0:
                    carry_ap, Tcarry = gz[:, 0:1], Tgz
                else:
                    carry_ap, Tcarry = GT[(c - 1) % 2][:, 511:512], TGT[(c - 1) % 2]
                op("dve", lambda e, gcur=gcur, carry_ap=carry_ap: e.tensor_scalar(
                    out=gcur[:], in0=PS[7][0:8, :], scalar1=carry_ap, scalar2=None, op0=ALU.add),
                   reads=[TPS[7], Tcarry], writes=[Tgcur])

                def split3(src, Tsrc, dst, Tdst):
                    op("dve", lambda e: e.tensor_copy(out=dst[:, 0, :], in_=src[:]), reads=[Tsrc], writes=[Tdst])
                    op("dve", lambda e: e.tensor_tensor(out=r1[:], in0=src[:], in1=dst[:, 0, :], op=ALU.subtract),
                       reads=[Tsrc, Tdst], writes=[Tr1])
                    op("dve", lambda e: e.tensor_copy(out=dst[:, 1, :], in_=r1[:]), reads=[Tr1], writes=[Tdst])
                    op("dve", lambda e: e.tensor_tensor(out=r2[:], in0=r1[:], in1=dst[:, 1, :], op=ALU.subtract),
                       reads=[Tr1, Tdst], writes=[Tr2])
                    op("dve", lambda e: e.tensor_copy(out=dst[:, 2, :], in_=r2[:]), reads=[Tr2], writes=[Tdst])

                split3(gcur, Tgcur, sp_hi, Tsp_hi)
                op("pool", lambda e, c=c: e.dma_start(out=KT_d[0:8, 64:67, c * 512:(c + 1) * 512], in_=sp_hi[:]),
                   reads=[Tsp_hi], chan=ch_aug[1])
                m, ph = c // 4, c % 4
                if ph in (0, 2):
                    fl = flags[:, 0:1] if ph == 0 else flags[:, 1:2]
                    op("dve", lambda e, fl=fl, gcur=gcur: e.tensor_scalar(out=gq[:], in0=gcur[:], scalar1=fl, scalar2=None,
                                                                          op0=ALU.mult), reads=[Tgcur, Tflags], writes=[Tgq])
                else:
                    fl = flags[:, 1:2] if ph == 1 else flags[:, 0:1]
                    op("dve", lambda e, fl=fl, gcur=gcur: e.scalar_tensor_tensor(out=gq[:], in0=gcur[:], scalar=fl, in1=gq[:],
                                                                                 op0=ALU.mult, op1=ALU.add),
                       reads=[Tgcur, Tflags, Tgq], writes=[Tgq])
                    split3(gq, Tgq, spq, Tspq)
                    lt = 2 * m + (0 if ph == 1 else 1)
                    op("pool", lambda e, lt=lt: e.dma_start(out=QT_d[0:8, 67:70, lt * 512:(lt + 1) * 512], in_=spq[:]),
                       reads=[Tspq], chan=ch_aug[2])

            for c in range(NTQ):
                b = c % 2
                norm_chunk(x_q[c * 512:(c + 1) * 512, :], xt[b], Txt[b], ss[b], Tss[b], rstd[b], Trstd[b], xn, Txn,
                           junk, Tjunk, hT, ThT, G1, TG1, modT[:, 0:8], TmodT, ch_ld[b], 0, 1)
                proj_T(qcols, QT_d, c, 0.125, rr)
            S_.barrier()

        if 2 in phases:
          with ExitStack() as p2:
            maskF = sb(p2, "maskF", [128, 16, 512], BF16); TmaskF = Tk()
            maskS = sb(p2, "maskS", [128, 16, 512], BF16); TmaskS = Tk()
            KTh = [sb(p2, "KTh%d" % i, [KA, S], BF16) for i in range(2)]; TKTh = [Tk(), Tk()]
            Vh = [sb(p2, "Vh%d" % i, [128, NB, 65], BF16) for i in range(2)]; TVh = [Tk(), Tk()]
            QTh = [sb(p2, "QTh%d" % i, [KA, SQ], BF16) for i in range(2)]; TQTh = [Tk(), Tk()]
            negQ = [sb(p2, "negQ%d" % i, [64, 512], BF16) for i in range(2)]; TnegQ = [Tk(), Tk()]
            zt = [sb(p2, "zt%d" % i, [128, 512], F32) for i in range(2)]; Tzt = [Tk(), Tk()]
            ee = [sb(p2, "ee%d" % i, [128, 512], F32) for i in range(2)]; Tee = [Tk(), Tk()]
            spb = [sb(p2, "spb%d" % i, [128, 512], BF16) for i in range(2)]; Tspb = [Tk(), Tk()]
            LL = [sb(p2, "LL%d" % i, [128, 512], F32) for i in range(2)]; TLL = [Tk(), Tk()]
            PT = [sb(p2, "PT%d" % i, [128, 512], BF16) for i in range(6)]; TPT = [Tk() for _ in range(6)]
            Rsb = sb(p2, "Rsb", [128, 512], F32); TRsb = Tk()
            osb = sb(p2, "osb", [128, 4, 64], F32); Tosb = Tk()
            sq = sb(p2, "sq", [128, 4, 64], F32); Tsq = Tk()
            o2 = sb(p2, "o2", [128, 4, 64], F32); To2 = Tk()
            omix = [sb(p2, "omix%d" % i, [128, 4, 64], BF16) for i in range(2)]; Tomix = [Tk(), Tk()]
            ssq = sb(p2, "ssq", [128, 4], F32); Tssq = Tk()
            rinv = sb(p2, "rinv", [128, 4], F32); Trinv = Tk()

            op("sp", lambda e: e.dma_start(out=maskF[:], in_=masks_d), writes=[TmaskF], chan=S_.chan("k12"))
            op("sp", lambda e: e.dma_start(out=maskS[:], in_=masks_sb_d), writes=[TmaskS], chan=S_.chan("k13"))


            stgf = [sb(p2, "pstgf%d" % i, [128, 4096], F32) for i in range(2)]; Tstgf = [Tk(), Tk()]
            stgb = [sb(p2, "pstgb%d" % i, [128, 4096], BF16) for i in range(2)]; Tstgb = [Tk(), Tk()]
            ch_si = [S_.chan("psi%d" % i) for i in range(2)]
            ch_so = [S_.chan("pso%d" % i) for i in range(2)]

            def precast_gen():
                RPP = nexp // 128
                cnt = 0
                for (tsrc, tdst) in ((e_down, edu_bf[:, 0, :]), (e_up, edu_bf[:, 1, :])):
                    sv = tsrc.rearrange("(r p) d -> p r d", p=128)
                    dv = tdst.rearrange("(r p) d -> p r d", p=128)
                    for c4 in range(max(1, RPP // 4)):
                        nr = min(4, RPP)
                        i = cnt % 2; cnt += 1
                        sf = stgf[i][:, 0:nr * D].rearrange("p (r d) -> p r d", d=D)
                        sbv = stgb[i][:, 0:nr * D].rearrange("p (r d) -> p r d", d=D)
                        op("sp", lambda e: e.dma_start(out=sf, in_=sv[:, c4 * 4:c4 * 4 + nr, :]), writes=[Tstgf[i]], chan=ch_si[i])
                        op("pool", lambda e: e.tensor_copy(out=sbv, in_=sf), reads=[Tstgf[i]], writes=[Tstgb[i]])
                        op("sp", lambda e: e.dma_start(out=dv[:, c4 * 4:c4 * 4 + nr, :], in_=sbv), reads=[Tstgb[i]], chan=ch_so[i])
                        yield
            pgen = precast_gen()

            def block_list(lt):
                kind, m = lt % 2, lt // 2
                if kind == 0:
                    masked = [(4 * m + 1, 0), (4 * m, 1)]
                    lower = list(range(4 * m - 1, -1, -1))
                else:
                    masked = [(4 * m + 3, 2), (4 * m + 2, 3)]
                    lower = list(range(4 * m + 1, -1, -1))
                bl = []
                for (T_, ms) in masked:
                    for i in (3, 2, 1, 0):
                        bl.append((4 * T_ + i, ms * 4 + i))
                for T_ in lower:
                    for i in (3, 2, 1, 0):
                        bl.append((4 * T_ + i, None))
                return bl

            bctr = [0]
            octr = [0]
            for h in range(NH):
                hb = h % 2
                is_fox = h < 8
                op("sp", lambda e, h=h, hb=hb: e.dma_start(out=KTh[hb][:], in_=KT_d[h]), writes=[TKTh[hb]], chan=ch_hd[hb][0])
                op("sp", lambda e, h=h, hb=hb: e.dma_start(out=Vh[hb][:], in_=V_d[h]), writes=[TVh[hb]], chan=ch_hd[hb][1])
                op("sp", lambda e, h=h, hb=hb: e.dma_start(out=QTh[hb][:], in_=QT_d[h]), writes=[TQTh[hb]], chan=ch_hd[hb][2])
                kt, vt, qt = KTh[hb], Vh[hb], QTh[hb]
                Tkt, Tvt, Tqt = TKTh[hb], TVh[hb], TQTh[hb]
                for lt in range(NTQ):
                    bl = block_list(lt)
                    n = len(bl)
                    ob = 6 + (octr[0] % 2); octr[0] += 1
                    Ops, TOps = PS[ob], TPS[ob]
                    Ov = Ops[:, 0:260].rearrange("p (j e) -> p j e", e=65)
                    qs = slice(lt * 512, (lt + 1) * 512)
                    base = bctr[0]; bctr[0] += n
                    if is_fox:
                        def A1(k):
                            kb, mi = bl[k]
                            b = (base + k) % 6
                            zb = (base + k) % 2
                            op("pe", lambda e: e.matmul(PS[b][:, :], lhsT=kt[0:KA, kb * 128:(kb + 1) * 128], rhs=qt[0:KA, qs],
                                                        start=True, stop=True), reads=[Tkt, Tqt], writes=[TPS[b]])
                            if mi is not None:
                                op("dve", lambda e: e.tensor_tensor(out=zt[zb][:], in0=PS[b][:, :], in1=maskF[:, mi, :], op=ALU.add),
                                   reads=[TPS[b], TmaskF], writes=[Tzt[zb]])
                                op("act", lambda e: e.activation(out=PT[b][:], in_=zt[zb][:], func=AF.Exp), reads=[Tzt[zb]], writes=[TPT[b]])
                            else:
                                op("act", lambda e: e.activation(out=PT[b][:], in_=PS[b][:, :], func=AF.Exp), reads=[TPS[b]], writes=[TPT[b]])

                        def B(k):
                            kb, mi = bl[k]
                            b = (base + k) % 6
                            for j in range(4):
                                op("pe", lambda e, j=j: e.matmul(Ov[:, j, :], lhsT=PT[b][:, j * 128:(j + 1) * 128], rhs=vt[:, kb, :],
                                                                 start=(k == 0), stop=(k == n - 1)), reads=[TPT[b], Tvt], writes=[TOps])
                        for k in range(n + 3):
                            if k < n:
                                A1(k)
                            if k >= 3:
                                B(k - 3)
                    else:
                        nq = negQ[octr[0] % 2]; Tnq = TnegQ[octr[0] % 2]
                        op("pool", lambda e: e.tensor_scalar(out=nq[:], in0=qt[0:64, qs], scalar1=-1.0, scalar2=None, op0=ALU.mult),
                           reads=[Tqt], writes=[Tnq])
                        op("pool", lambda e: e.memset(Rsb[:], 0.0), writes=[TRsb])

                        def A1(k):
                            kb, mi = bl[k]
                            b = (base + k) % 2
                            op("pe", lambda e: e.matmul(PS[b][:, :], lhsT=kt[0:64, kb * 128:(kb + 1) * 128], rhs=qt[0:64, qs],
                                                        start=True, stop=True), reads=[Tkt, Tqt], writes=[TPS[b]])
                            if mi is not None:
                                op("dve", lambda e: e.tensor_tensor(out=zt[b][:], in0=PS[b][:, :], in1=maskS[:, mi, :], op=ALU.add),
                                   reads=[TPS[b], TmaskS], writes=[Tzt[b]])
                                op("act", lambda e: e.activation(out=ee[b][:], in_=zt[b][:], func=AF.Exp), reads=[Tzt[b]], writes=[Tee[b]])
                            else:
                                op("act", lambda e: e.activation(out=ee[b][:], in_=PS[b][:, :], func=AF.Exp), reads=[TPS[b]], writes=[Tee[b]])
                            op("act", lambda e: e.activation(out=spb[b][:], in_=ee[b][:], func=AF.Ln, bias=1.0), reads=[Tee[b]], writes=[Tspb[b]])

                        def A2(k):
                            kb, mi = bl[k]
                            b = (base + k) % 2
                            op("pe", lambda e: e.matmul(PS[2 + b][:, :], lhsT=tri[:], rhs=spb[b][:], start=True, stop=False),
                               reads=[Ttri, Tspb[b]], writes=[TPS[2 + b]])
                            op("pe", lambda e: e.matmul(PS[2 + b][:, :], lhsT=kt[0:64, kb * 128:(kb + 1) * 128], rhs=nq[:],
                                                        start=False, stop=True), reads=[Tkt, Tnq], writes=[TPS[2 + b]])
                            if k < n - 1:
                                op("pe", lambda e: e.matmul(PS[4 + b][:, :], lhsT=ones[:], rhs=spb[b][:], start=True, stop=True),
                                   reads=[Tones, Tspb[b]], writes=[TPS[4 + b]])
                            op("dve", lambda e: e.tensor_tensor(out=LL[b][:], in0=PS[2 + b][:, :], in1=Rsb[:], op=ALU.add),
                               reads=[TPS[2 + b], TRsb], writes=[TLL[b]])
                            if mi is not None:
                                op("dve", lambda e: e.tensor_tensor(out=LL[b][:], in0=LL[b][:], in1=maskS[:, mi, :], op=ALU.subtract),
                                   reads=[TLL[b], TmaskS], writes=[TLL[b]])
                            if k < n - 1:
                                op("dve", lambda e: e.tensor_tensor(out=Rsb[:], in0=PS[4 + b][:, :], in1=Rsb[:], op=ALU.add),
                                   reads=[TPS[4 + b], TRsb], writes=[TRsb])

                        def B(k):
                            kb, mi = bl[k]
                            b = (base + k) % 2
                            op("act", lambda e: e.activation(out=PT[b][:], in_=LL[b][:], func=AF.Exp, scale=-1.0),
                               reads=[TLL[b]], writes=[TPT[b]])
                            for j in range(4):
                                op("pe", lambda e, j=j: e.matmul(Ov[:, j, 0:64], lhsT=PT[b][:, j * 128:(j + 1) * 128], rhs=vt[:, kb, 0:64],
                                                                 start=(k == 0), stop=(k == n - 1)), reads=[TPT[b], Tvt], writes=[TOps])
                        for k in range(n + 2):
                            if k < n:
                                A1(k)
                            if 1 <= k <= n:
                                A2(k - 1)
                            if k >= 2:
                                B(k - 2)
                    if is_fox:
                        op("dve", lambda e: e.reciprocal(out=rinv[:].unsqueeze(2), in_=Ov[:, :, 64:65]), reads=[TOps], writes=[Trinv])
                        op("dve", lambda e: e.tensor_tensor(out=osb[:], in0=Ov[:, :, 0:64],
                                                            in1=rinv[:].unsqueeze(2).to_broadcast([128, 4, 64]), op=ALU.mult),
                           reads=[TOps, Trinv], writes=[Tosb])
                    else:
                        op("dve", lambda e: e.tensor_copy(out=osb[:], in_=Ov[:, :, 0:64]), reads=[TOps], writes=[Tosb])
                    op("pool", lambda e: e.tensor_tensor(out=sq[:], in0=osb[:], in1=osb[:], op=ALU.mult), reads=[Tosb], writes=[Tsq])
                    op("dve", lambda e: e.tensor_reduce(out=ssq[:], in_=sq[:], axis=AX.X, op=ALU.add), reads=[Tsq], writes=[Tssq])
                    op("act", lambda e: e.activation(out=ssq[:], in_=ssq[:], func=AF.Sqrt, scale=1.0 / HD, bias=EPS),
                       reads=[Tssq], writes=[Tssq])
                    op("dve", lambda e: e.reciprocal(out=ssq[:], in_=ssq[:]), reads=[Tssq], writes=[Tssq])
                    op("pool", lambda e: e.tensor_tensor(out=o2[:], in0=osb[:], in1=ssq[:].unsqueeze(2).to_broadcast([128, 4, 64]),
                                                         op=ALU.mult), reads=[Tosb, Tssq], writes=[To2])
                    om = octr[0] % 2
                    gi = 0 if is_fox else 1
                    op("pool", lambda e, om=om, gi=gi: e.tensor_tensor(out=omix[om][:], in0=o2[:],
                                                                       in1=gout[:, gi, :].unsqueeze(1).to_broadcast([128, 4, 64]),
                                                                       op=ALU.mult), reads=[To2, Tgout], writes=[Tomix[om]])
                    op("pool", lambda e, om=om, h=h, lt=lt: e.dma_start(
                        out=mixed_d[lt * 512:(lt + 1) * 512, h * 64:(h + 1) * 64].rearrange("(j p) d -> p j d", p=128),
                        in_=omix[om][:]), reads=[Tomix[om]], chan=ch_st[om])
                    next(pgen, None)
            for _ in pgen:
                pass
            S_.barrier()

        if 3 in phases:
          with ExitStack() as p3:
            NU, GS = 3, 4
            WoB = sb(p3, "WoB", [128, 8, D], BF16); TWoB = Tk()
            WqB = sb(p3, "WqB", [128, 8, 2048], BF16); TWqB = Tk()
            SKb = sb(p3, "SKb", [128, 16, 128], BF16); TSKb = Tk()
            with ExitStack() as pc:
                stgf = [sb(pc, "stgf%d" % i, [128, 4096], F32) for i in range(3)]; Tstgf = [Tk() for _ in range(3)]
                stgb = [sb(pc, "stgb%d" % i, [128, 4096], BF16) for i in range(3)]; Tstgb = [Tk() for _ in range(3)]
                ch_si = [S_.chan("si%d" % i) for i in range(3)]
                ch_so = [S_.chan("so%d" % i) for i in range(3)]
                cengs = ["dve", "act", "pool"]
                sctr = [0]

                def stage_cast(src_ap, shape_str, kw, dst_ap, Tdst):
                    i = sctr[0] % 3; sctr[0] += 1
                    n = 1
                    for v in src_ap.shape[1:]:
                        n *= v
                    stg = stgf[i][:, 0:n].rearrange(shape_str, **kw)
                    op("sp", lambda e: e.dma_start(out=stg, in_=src_ap), writes=[Tstgf[i]], chan=ch_si[i])
                    if cengs[i] == "act":
                        op("act", lambda e: e.activation(out=dst_ap, in_=stg, func=AF.Copy), reads=[Tstgf[i]], writes=[Tdst])
                    else:
                        op(cengs[i], lambda e: e.tensor_copy(out=dst_ap, in_=stg), reads=[Tstgf[i]], writes=[Tdst])
                wo_v = w_out.rearrange("(k p) c -> p k c", p=128)
                wq_v = w_query.rearrange("(k p) c -> p k c", p=128)
                for i in range(2):
                    stage_cast(wo_v[:, :, i * 512:(i + 1) * 512], "p (k c) -> p k c", dict(k=8), WoB[:, :, i * 512:(i + 1) * 512], TWoB)
                for i in range(4):
                    stage_cast(wq_v[:, :, i * 512:(i + 1) * 512], "p (k c) -> p k c", dict(k=8), WqB[:, :, i * 512:(i + 1) * 512], TWqB)
                stage_cast(skT_d, "p (k c) -> p k c", dict(k=16), SKb[:], TSKb)
                S_.barrier()
            U = [sb(p3, "U%d" % i, [128, GS, 2 * D], BF16) for i in range(NU)]
            TU = [[Tk() for _ in range(GS)] for _ in range(NU)]
            ch_g = [[S_.chan("g%d_%d" % (i, s)) for s in range(GS)] for i in range(NU)]
            dg = [sb(p3, "dg%d" % i, [128, 128], BF16) for i in range(4)]; Tdg = [Tk() for _ in range(4)]
            mxt = [sb(p3, "mxt%d" % i, [128, D], BF16) for i in range(2)]; Tmxt = [Tk(), Tk()]
            xqt = [sb(p3, "xqt%d" % i, [128, D], F32) for i in range(2)]; Txqt = [Tk(), Tk()]
            mT = sb(p3, "mT", [128, 8, 128], BF16); TmT = Tk()
            x1b = [sb(p3, "x1_%d" % i, [128, D], F32) for i in range(2)]; Tx1b = [Tk(), Tk()]
            xn2 = sb(p3, "xn2", [128, D], BF16); Txn2 = Tk()
            h2b = [sb(p3, "h2_%d" % i, [128, D], F32) for i in range(2)]; Th2b = [Tk(), Tk()]
            h2T = sb(p3, "h2T", [128, 8, 128], BF16); Th2T = Tk()
            qT = sb(p3, "qT", [128, 16, 128], BF16); TqT = Tk()
            sc = sb(p3, "sc", [128, 16, 128], F32); Tsc = Tk()
            scr = sb(p3, "scr", [128, 256], F32); Tscr = Tk()
            v16 = sb(p3, "v16", [128, 16, 16], F32); Tv16 = Tk()
            i16 = sb(p3, "i16", [128, 16, 16], U32); Ti16 = Tk()
            i16f = sb(p3, "i16f", [128, 16, 16], F32); Ti16f = Tk()
            cand = sb(p3, "cand", [128, 8, 256], F32); Tcand = Tk()
            cidx = sb(p3, "cidx", [128, 8, 256], F32); Tcidx = Tk()
            m16 = sb(p3, "m16", [128, 8, 16], F32); Tm16 = Tk()
            eidf = sb(p3, "eidf", [128, 128], F32); Teidf = Tk()
            eid2 = [sb(p3, "eid%d" % i, [128, 128], I32) for i in range(2)]; Teid2 = [Tk(), Tk()]
            gtsb = [sb(p3, "gts%d" % i, [128, 8, 16], F32) for i in range(2)]; Tgtsb = [Tk(), Tk()]
            gsum = sb(p3, "gsum", [128, 8], F32); Tgsum = Tk()
            score = sb(p3, "score", [128, 128], F32); Tscore_g = [Tk() for _ in range(32)]
            actv = sb(p3, "actv", [128, 128], F32); Tactv_g = [Tk() for _ in range(32)]
            junk3 = sb(p3, "junk3", [128, D], BF16); Tjunk3 = Tk()
            junk4 = sb(p3, "junk4", [128, D], BF16); Tjunk4 = Tk()
            x2 = sb(p3, "x2", [128, D], F32); Tx2 = Tk()
            ot0 = sb(p3, "ot0", [128, D], F32); ot = [ot0, ot0]; Tot0 = Tk(); Tot = [Tot0, Tot0]
            st3 = sb(p3, "st3", [128, 4], F32); Tst3 = Tk()
            st4 = sb(p3, "st4", [128, 4], F32); Tst4 = Tk()

            gctr = [0]
            def front_part(tb):
                b = tb % 2
                rows = slice(tb * 128, (tb + 1) * 128)
                x1, Tx1 = x1b[b], Tx1b[b]
                eid, Teid = eid2[b], Teid2[b]
                h2, Th2 = h2b[b], Th2b[b]
                gts, Tgts = gtsb[b], Tgtsb[b]
                op("sp", lambda e: e.dma_start(out=mxt[b][:], in_=mixed_d[rows, :]), writes=[Tmxt[b]], chan=ch_ld[b])
                op("sp", lambda e: e.dma_start(out=xqt[b][:], in_=x_q[rows, :]), writes=[Txqt[b]], chan=ch_ld[2 + b])
                ptb = PS[0][:].bitcast(BF16)
                for k in range(8):
                    op("pe", lambda e, k=k: e.transpose(ptb[:, k * 128:(k + 1) * 128], mxt[b][:, k * 128:(k + 1) * 128], ident[:]),
                       reads=[Tmxt[b], Tident], writes=[TPS[0]])
                op("act", lambda e: e.activation(out=mT[:].rearrange("p k t -> p (k t)"), in_=ptb[:, 0:1024], func=AF.Copy),
                   reads=[TPS[0]], writes=[TmT])
                yield
                for hf in range(2):
                    for k in range(8):
                        op("pe", lambda e, k=k, hf=hf: e.matmul(PS[2 + hf][:, :], lhsT=mT[:, k, :], rhs=WoB[:, k, hf * 512:(hf + 1) * 512],
                                                                start=(k == 0), stop=(k == 7)), reads=[TmT, TWoB], writes=[TPS[2 + hf]])
                    hs = slice(hf * 512, (hf + 1) * 512)
                    op("dve", lambda e, hf=hf, hs=hs: e.tensor_tensor(out=x1[:, hs], in0=PS[2 + hf][:, :], in1=bcs[:, 0, hs], op=ALU.mult),
                       reads=[TPS[2 + hf], Tbcs[0]], writes=[Tx1])
                op("dve", lambda e: e.tensor_tensor(out=x1[:], in0=x1[:], in1=xqt[b][:], op=ALU.add), reads=[Tx1, Txqt[b]], writes=[Tx1])
                yield
                op("act", lambda e: e.activation(out=junk4[:], in_=x1[:], func=AF.Square, accum_out=st3[:, 0:1]),
                   reads=[Tx1], writes=[Tjunk4, Tst3])
                op("act", lambda e: e.activation(out=st3[:, 0:1], in_=st3[:, 0:1], func=AF.Sqrt, scale=1.0 / D, bias=EPS),
                   reads=[Tst3], writes=[Tst3])
                op("dve", lambda e: e.reciprocal(out=st3[:, 0:1], in_=st3[:, 0:1]), reads=[Tst3], writes=[Tst3])
                op("dve", lambda e: e.tensor_scalar(out=xn2[:], in0=x1[:], scalar1=st3[:, 0:1], scalar2=None, op0=ALU.mult),
                   reads=[Tx1, Tst3], writes=[Txn2])
                op("dve", lambda e: e.scalar_tensor_tensor(out=h2[:], in0=x1[:], scalar=st3[:, 0:1], in1=bcs[:, 2, :],
                                                           op0=ALU.mult, op1=ALU.mult), reads=[Tx1, Tst3, Tbcs[2]], writes=[Th2])
                op("dve", lambda e: e.tensor_tensor(out=h2[:], in0=h2[:], in1=bcs[:, 1, :], op=ALU.add), reads=[Th2, Tbcs[1]], writes=[Th2])
                yield
                ptb1 = PS[1][:].bitcast(BF16)
                for k in range(8):
                    op("pe", lambda e, k=k: e.transpose(ptb1[:, k * 128:(k + 1) * 128], xn2[:, k * 128:(k + 1) * 128], ident[:]),
                       reads=[Txn2, Tident], writes=[TPS[1]])
                for k in range(8):
                    op("act", lambda e, k=k: e.activation(out=h2T[:, k, :], in_=ptb1[:, k * 128:(k + 1) * 128], func=AF.Identity,
                                                          scale=G2[:, k:k + 1], bias=modT[:, 24 + k:25 + k]),
                       reads=[TPS[1], TG2, TmodT], writes=[Th2T])
                yield
                for g4 in range(4):
                    pi = g4 % 2
                    for q4 in range(4):
                        hp = g4 * 4 + q4
                        for k in range(8):
                            op("pe", lambda e, k=k, hp=hp, q4=q4, pi=pi: e.matmul(
                                PS[pi][:, q4 * 128:(q4 + 1) * 128], lhsT=WqB[:, k, hp * 128:(hp + 1) * 128], rhs=h2T[:, k, :],
                                start=(k == 0), stop=(k == 7)), reads=[TWqB, Th2T], writes=[TPS[pi]])
                    op("act", lambda e, g4=g4, pi=pi: e.activation(out=qT[:, g4 * 4:(g4 + 1) * 4, :].rearrange("p a t -> p (a t)"),
                                                                   in_=PS[pi][:, :], func=AF.Copy), reads=[TPS[pi]], writes=[TqT])
                yield
                for g4 in range(4):
                    for q4 in range(4):
                        hp = g4 * 4 + q4
                        op("pe", lambda e, hp=hp, q4=q4, g4=g4: e.matmul(PS[2 + g4][:, q4 * 128:(q4 + 1) * 128], lhsT=qT[:, hp, :],
                                                                         rhs=SKb[:, hp, :], start=True, stop=True),
                           reads=[TqT, TSKb], writes=[TPS[2 + g4]])
                    op("act", lambda e, g4=g4: e.activation(out=sc[:, g4 * 4:(g4 + 1) * 4, :].rearrange("p a t -> p (a t)"),
                                                            in_=PS[2 + g4][:, :], func=AF.Copy), reads=[TPS[2 + g4]], writes=[Tsc])
                yield
                for hp in range(16):
                    op("dve", lambda e, hp=hp: e.max(out=v16[:, hp, 0:8], in_=sc[:, hp, :]), reads=[Tsc], writes=[Tv16])
                    op("dve", lambda e, hp=hp: e.max_index(out=i16[:, hp, 0:8], in_max=v16[:, hp, 0:8], in_values=sc[:, hp, :]),
                       reads=[Tsc, Tv16], writes=[Ti16])
                    op("dve", lambda e, hp=hp: e.match_replace(out=scr[:, 0:128], in_to_replace=v16[:, hp, 0:8], in_values=sc[:, hp, :],
                                                               imm_value=-1e30), reads=[Tsc, Tv16], writes=[Tscr])
                    op("dve", lambda e, hp=hp: e.max(out=v16[:, hp, 8:16], in_=scr[:, 0:128]), reads=[Tscr], writes=[Tv16])
                    op("dve", lambda e, hp=hp: e.max_index(out=i16[:, hp, 8:16], in_max=v16[:, hp, 8:16], in_values=scr[:, 0:128]),
                       reads=[Tscr, Tv16], writes=[Ti16])
                    yield
                op("dve", lambda e: e.tensor_copy(out=i16f[:], in_=i16[:]), reads=[Ti16], writes=[Ti16f])
                v4 = v16[:].rearrange("p (h two) a -> p h two a", two=2)
                f4 = i16f[:].rearrange("p (h two) a -> p h two a", two=2)
                c4 = cand[:].rearrange("p h (a b) -> p h a b", b=16)
                x4 = cidx[:].rearrange("p h (a b) -> p h a b", b=16)
                op("dve", lambda e: e.tensor_tensor(out=c4, in0=v4[:, :, 0, :].unsqueeze(3).to_broadcast([128, 8, 16, 16]),
                                                    in1=v4[:, :, 1, :].unsqueeze(2).to_broadcast([128, 8, 16, 16]), op=ALU.add),
                   reads=[Tv16], writes=[Tcand])
                op("dve", lambda e: e.tensor_scalar(out=f4[:, :, 0, :], in0=f4[:, :, 0, :], scalar1=128.0, scalar2=None, op0=ALU.mult),
                   reads=[Ti16f], writes=[Ti16f])
                op("dve", lambda e: e.tensor_tensor(out=x4, in0=f4[:, :, 0, :].unsqueeze(3).to_broadcast([128, 8, 16, 16]),
                                                    in1=f4[:, :, 1, :].unsqueeze(2).to_broadcast([128, 8, 16, 16]), op=ALU.add),
                   reads=[Ti16f], writes=[Tcidx])
                for h in range(8):
                    op("dve", lambda e, h=h: e.max(out=m16[:, h, 0:8], in_=cand[:, h, :]), reads=[Tcand], writes=[Tm16])
                    op("dve", lambda e, h=h: e.match_replace(out=scr[:], in_to_replace=m16[:, h, 0:8], in_values=cand[:, h, :],
                                                             imm_value=-1e30), reads=[Tcand, Tm16], writes=[Tscr])
                    op("dve", lambda e, h=h: e.max(out=m16[:, h, 8:16], in_=scr[:]), reads=[Tscr], writes=[Tm16])
                    yield
                for h in range(8):
                    for k in range(16):
                        op("dve", lambda e, h=h, k=k: e.scalar_tensor_tensor(
                            out=junk3[:, 0:256], in0=cand[:, h, :], scalar=m16[:, h, k:k + 1], in1=cidx[:, h, :],
                            op0=ALU.is_equal, op1=ALU.mult, accum_out=eidf[:, h * 16 + k:h * 16 + k + 1]),
                           reads=[Tcand, Tcidx, Tm16], writes=[Tjunk3, Teidf])
                        if k % 4 == 3:
                            yield
                op("dve", lambda e: e.tensor_scalar(out=eidf[:], in0=eidf[:], scalar1=float(nexp - 1), scalar2=None, op0=ALU.min),
                   reads=[Teidf], writes=[Teidf])
                op("dve", lambda e: e.tensor_copy(out=eid[:], in_=eidf[:]), reads=[Teidf], writes=[Teid])
                yield
                op("dve", lambda e: e.tensor_tensor(out=gts[:], in0=m16[:], in1=m16[:, :, 0:1].to_broadcast([128, 8, 16]),
                                                    op=ALU.subtract), reads=[Tm16], writes=[Tgts])
                op("act", lambda e: e.activation(out=gts[:], in_=gts[:], func=AF.Exp), reads=[Tgts], writes=[Tgts])
                op("dve", lambda e: e.tensor_reduce(out=gsum[:], in_=gts[:], axis=AX.X, op=ALU.add), reads=[Tgts], writes=[Tgsum])
                op("dve", lambda e: e.reciprocal(out=gsum[:], in_=gsum[:]), reads=[Tgsum], writes=[Tgsum])
                op("dve", lambda e: e.tensor_tensor(out=gts[:], in0=gts[:], in1=gsum[:].unsqueeze(2).to_broadcast([128, 8, 16]),
                                                    op=ALU.mult), reads=[Tgts, Tgsum], writes=[Tgts])

            def block_part(tb, gen):
                b = tb % 2
                rows = slice(tb * 128, (tb + 1) * 128)
                x1, Tx1 = x1b[b], Tx1b[b]
                eid, Teid = eid2[b], Teid2[b]
                h2, Th2 = h2b[b], Th2b[b]
                gts, Tgts = gtsb[b], Tgtsb[b]
                gflat = gts[:].rearrange("p h k -> p (h k)")
                for grp in range(128 // GS):
                    ub = gctr[0] % NU; gctr[0] += 1
                    gs = slice(grp * GS, (grp + 1) * GS)
                    for s in range(GS):
                        slot = grp * GS + s
                        op("pool", lambda e, ub=ub, s=s, slot=slot: e.indirect_dma_start(
                            out=U[ub][:, s, :], out_offset=None, in_=edu_bf.rearrange("n t d -> n (t d)"),
                            in_offset=bass.IndirectOffsetOnAxis(ap=eid[:, slot:slot + 1], axis=0)),
                           reads=[Teid], writes=[TU[ub][s]], chan=ch_g[ub][s])
                    for s in range(GS):
                        slot = grp * GS + s
                        op("dve", lambda e, s=s, slot=slot: e.scalar_tensor_tensor(
                            out=junk3[:], in0=U[ub][:, s, 0:D], scalar=1.0, in1=h2[:],
                            op0=ALU.mult, op1=ALU.mult, accum_out=score[:, slot:slot + 1]),
                           reads=[TU[ub][s], Th2], writes=[Tjunk3, Tscore_g[grp]])
                    op("act", lambda e: e.activation(out=actv[:, gs], in_=score[:, gs], func=AF.Gelu),
                       reads=[Tscore_g[grp]], writes=[Tactv_g[grp]])
                    op("dve", lambda e: e.tensor_tensor(out=actv[:, gs], in0=actv[:, gs], in1=gflat[:, gs], op=ALU.mult),
                       reads=[Tactv_g[grp], Tgts], writes=[Tactv_g[grp]])
                    for s in range(GS):
                        slot = grp * GS + s
                        di = slot % 4
                        op("act", lambda e, slot=slot, di=di: e.activation(out=dg[di][:], in_=ident[:], func=AF.Copy,
                                                                           scale=actv[:, slot:slot + 1]),
                           reads=[Tident, Tactv_g[grp]], writes=[Tdg[di]])
                        for hf in range(2):
                            op("pe", lambda e, hf=hf, s=s, di=di, slot=slot: e.matmul(
                                PS[6 + hf][:, :], lhsT=dg[di][:], rhs=U[ub][:, s, D + hf * 512:D + (hf + 1) * 512],
                                start=(slot == 0), stop=(slot == 127)), reads=[Tdg[di], TU[ub][s]], writes=[TPS[6 + hf]])
                    if gen is not None:
                        for _ in range(3):
                            next(gen, None)
                if gen is not None:
                    for _ in gen:
                        pass
                for hf in range(2):
                    hs = slice(hf * 512, (hf + 1) * 512)
                    op("dve", lambda e, hf=hf, hs=hs: e.tensor_tensor(out=x2[:, hs], in0=PS[6 + hf][:, :], in1=bcs[:, 3, hs], op=ALU.mult),
                       reads=[TPS[6 + hf], Tbcs[3]], writes=[Tx2])
                op("dve", lambda e: e.tensor_tensor(out=x2[:], in0=x2[:], in1=x1[:], op=ALU.add), reads=[Tx2, Tx1], writes=[Tx2])
                op("act", lambda e: e.activation(out=junk4[:], in_=x2[:], func=AF.Square, accum_out=st4[:, 1:2]),
                   reads=[Tx2], writes=[Tjunk4, Tst4])
                op("act", lambda e: e.activation(out=st4[:, 1:2], in_=st4[:, 1:2], func=AF.Sqrt, scale=1.0 / D, bias=EPS),
                   reads=[Tst4], writes=[Tst4])
                op("dve", lambda e: e.reciprocal(out=st4[:, 1:2], in_=st4[:, 1:2]), reads=[Tst4], writes=[Tst4])
                op("dve", lambda e: e.scalar_tensor_tensor(out=ot[b][:], in0=x2[:], scalar=st4[:, 1:2], in1=bcs[:, 4, :],
                                                           op0=ALU.mult, op1=ALU.mult), reads=[Tx2, Tst4, Tbcs[4]], writes=[Tot[b]])
                op("sp", lambda e: e.dma_start(out=out_d[rows, :], in_=ot[b][:]), reads=[Tot[b]], chan=ch_st[b])

            for _ in front_part(0):
                pass
            for tb in range(NBQ):
                block_part(tb, front_part(tb + 1) if tb + 1 < NBQ else None)

        S_.barrier()
        print("build: ninst", S_.ninst, "nwaits", S_.nwaits, "sems", len(S_.sems))
    return nc


def make_masks(parity):
    s = np.arange(128)[:, None]
    t = np.arange(512)[None, :]
    out = np.zeros((2, 128, 16, 512), np.float32)
    kinds = ["n", "d", "d", "f"] if parity == 0 else ["d", "f", "n", "d"]
    for ms in range(4):
        for i in range(4):
            for v in range(2):
                if kinds[ms] == "n":
                    m = np.full((128, 512), NEG, np.float32)
                elif kinds[ms] == "f":
                    m = np.zeros((128, 512), np.float32)
                else:
                    kp = i * 128 + s
                    vis = (kp <= t) if v == 0 else (kp < t)
                    m = np.where(vis, 0.0, NEG).astype(np.float32)
                out[v, :, ms * 4 + i, :] = m
    return out.astype(ml_dtypes.bfloat16)


def make_in_maps(inp, S):
    NT = S // 512
    f32 = lambda a: np.ascontiguousarray(a, dtype=np.float32)
    colT = lambda v, n: f32(np.asarray(v).reshape(n, 128).T)
    shared = {
        "w_ada": f32(inp["w_ada"][0]),
        "badaT": colT(inp["b_ada"][0], 48),
        "bada_row": f32(inp["b_ada"][0][None, :]),
        "gmixT": colT(inp["g_norm_mix"][0], 8),
        "gffnT": colT(inp["g_norm_ffn"][0], 8),
        "gffn_row": f32(inp["g_norm_ffn"][0][None, :]),
        "gfin_row": f32(inp["g_final"][None, :]),
        "w_in": f32(inp["w_in"][0]),
        "bf_row": f32(inp["b_forget"][0][None, :]),
        "gfox_row": f32(inp["g_out_fox"][0][None, :]),
        "gsb_row": f32(inp["g_out_sb"][0][None, :]),
        "w_out": f32(inp["w_out"][0]),
        "w_query": f32(inp["w_query"][0]),
        "skT": f32(np.asarray(inp["sub_keys"][0]).reshape(16, 128, 128).transpose(2, 0, 1)),
        "e_down": f32(inp["expert_down"][0]),
        "e_up": f32(inp["expert_up"][0]),
    }
    masks = [make_masks(0), make_masks(1)]
    maps = []
    for core in range(8):
        b, r = core // 2, core % 2
        xb = np.asarray(inp["x"][b])
        tiles = tile_ids(r, NT)
        m = dict(shared)
        m["x_all"] = f32(xb)
        m["x_q"] = f32(np.concatenate([xb[t * 512:(t + 1) * 512] for t in tiles], axis=0))
        m["cT"] = colT(inp["c"][b], 8)
        m["masks"] = np.ascontiguousarray(masks[r][0])
        m["masks_sb"] = np.ascontiguousarray(masks[r][1])
        fl = np.zeros((8, 2), np.float32)
        fl[:, r] = 1.0
        m["flags"] = fl
        maps.append(m)
    return maps


def assemble(results, S):
    NT = S // 512
    out = np.zeros((4, S, D), np.float32)
    for core in range(8):
        b, r = core // 2, core % 2
        o = np.asarray(results[core]["out"])
        for lt, t in enumerate(tile_ids(r, NT)):
            out[b, t * 512:(t + 1) * 512] = o[lt * 512:(lt + 1) * 512]
    return out


_NC_CACHE = {}


def kernel(**inputs):
    S = 8192
    if S not in _NC_CACHE:
        _NC_CACHE[S] = build(S)
    nc = _NC_CACHE[S]
    maps = make_in_maps(inputs, S)
    res = run_bass_kernel_spmd(nc, maps, core_ids=list(range(8)))
    return assemble(res.results, S)
```

```python
import numpy as np
import ml_dtypes
from contextlib import ExitStack
import concourse.bass as bass
import concourse.mybir as mybir
from concourse.bass_utils import run_bass_kernel_spmd

F32 = mybir.dt.float32
BF16 = mybir.dt.bfloat16
I32 = mybir.dt.int32
U32 = mybir.dt.uint32
AF = mybir.ActivationFunctionType
ALU = mybir.AluOpType
AX = mybir.AxisListType

D = 1024
NH = 16
HD = 64
KA = 70
INC = 3080
NEXP = 16384
EPS = 1e-6
NEG = -30000.0


class Tk:
    __slots__ = ("name", "w", "r")

    def __init__(self, name=""):
        self.name = name
        self.w = None
        self.r = {}


class Chan:
    def __init__(self, S, name):
        self.sem = S.new_sem(name)
        self.key = name
        self.count = 0


class Sched:
    ENGS = ("pe", "act", "dve", "pool", "sp")
    ROT = 20000

    def __init__(self, nc, es):
        self.nc = nc
        self.es = es
        self.eobj = {"pe": nc.tensor, "act": nc.scalar, "dve": nc.vector, "pool": nc.gpsimd, "sp": nc.sync}
        self.sems = {}
        self.chans = []
        self.gen = {k: 0 for k in self.ENGS}
        self.ekey = {}
        self.count = {}
        self.known = {k: {} for k in self.ENGS}
        self.done_keys = []
        for k in self.ENGS:
            self._new_esem(k)
        self.nwaits = 0
        self.ninst = 0

    def _new_esem(self, k):
        key = "e_%s_%d" % (k, self.gen[k])
        self.gen[k] += 1
        self.new_sem(key)
        self.ekey[k] = key
        self.count[k] = 0

    def new_sem(self, name):
        s = self.es.enter_context(self.nc.semaphore(name))
        self.sems[name] = s
        return s

    def chan(self, name):
        c = Chan(self, "c_" + name)
        self.chans.append(c)
        return c

    def _wait(self, eng, ev):
        key, val, clock = ev
        kn = self.known[eng]
        if kn.get(key, 0) >= val:
            return
        self.eobj[eng].wait_ge(self.sems[key], val)
        self.nwaits += 1
        kn[key] = val
        if clock:
            for k2, v2 in clock.items():
                if kn.get(k2, 0) < v2:
                    kn[k2] = v2

    def op(self, eng, fn, reads=(), writes=(), chan=None):
        deps = []
        epref = "e_%s_" % eng
        for t in reads:
            if t.w is not None:
                deps.append(t.w)
        for t in writes:
            for ev in t.r.values():
                if ev[0].startswith(epref):
                    continue
                deps.append(ev)
            if t.w is not None:
                if t.w[0].startswith(epref):
                    continue
                deps.append(t.w)
        for ev in deps:
            self._wait(eng, ev)
        self.ninst += 1
        if chan is None:
            if self.count[eng] >= self.ROT:
                self.done_keys.append((self.ekey[eng], self.count[eng]))
                self._new_esem(eng)
            key = self.ekey[eng]
            self.count[eng] += 1
            val = self.count[eng]
            fn(self.eobj[eng]).then_inc(self.sems[key], 1)
            ev = (key, val, dict(self.known[eng]))
        else:
            chan.count += 16
            fn(self.eobj[eng]).then_inc(chan.sem, 16)
            ev = (chan.key, chan.count, dict(self.known[eng]))
        for t in writes:
            t.w = ev
            t.r = {}
        for t in reads:
            if t in writes:
                continue
            t.r[("e_" + eng) if chan is None else chan.key] = ev
        return ev

    def barrier(self):
        evs = []
        for k in self.ENGS:
            if self.count[k] > 0:
                evs.append((self.ekey[k], self.count[k], None))
        for key, cnt in self.done_keys:
            evs.append((key, cnt, None))
        for c in self.chans:
            if c.count > 0:
                evs.append((c.key, c.count, None))
        for eng in self.ENGS:
            for ev in evs:
                self._wait(eng, ev)


def tile_ids(parity, NT):
    out = []
    for m in range(NT // 4):
        out += [4 * m, 4 * m + 3] if parity == 0 else [4 * m + 1, 4 * m + 2]
    return out


def build(S, dbg=0, phases=(0, 1, 2, 3), nexp=NEXP):
    NT = S // 512
    NB = S // 128
    NTQ = NT // 2
    SQ = NTQ * 512
    NBQ = SQ // 128
    nc = bass.Bass("TRN2", target_bir_lowering=False)

    def din(name, shape, dt=F32):
        return nc.dram_tensor(name, list(shape), dt, kind="ExternalInput").ap()

    x_all = din("x_all", [S, D])
    x_q = din("x_q", [SQ, D])
    cT_d = din("cT", [128, 8])
    w_ada = din("w_ada", [D, 6 * D])
    badaT_d = din("badaT", [128, 48])
    bada_row = din("bada_row", [1, 6 * D])
    gmixT_d = din("gmixT", [128, 8])
    gffnT_d = din("gffnT", [128, 8])
    gffn_row = din("gffn_row", [1, D])
    gfin_row = din("gfin_row", [1, D])
    w_in = din("w_in", [D, INC])
    bf_row = din("bf_row", [1, 8])
    gfox_row = din("gfox_row", [1, HD])
    gsb_row = din("gsb_row", [1, HD])
    w_out = din("w_out", [D, D])
    w_query = din("w_query", [D, 2048])
    skT_d = din("skT", [128, 16, 128])
    e_down = din("e_down", [nexp, D])
    e_up = din("e_up", [nexp, D])
    masks_d = din("masks", [128, 16, 512], BF16)
    masks_sb_d = din("masks_sb", [128, 16, 512], BF16)
    flags_d = din("flags", [8, 2])
    out_d = nc.dram_tensor("out", [SQ, D], F32, kind="ExternalOutput").ap()

    okind = "ExternalOutput" if dbg else None

    def dscr(name, shape, dt):
        if dbg:
            return nc.dram_tensor(name, list(shape), dt, kind="ExternalOutput").ap()
        return nc.dram_tensor(name, list(shape), dt).ap()

    KT_d = dscr("KT_d", [NH, KA, S], BF16)
    QT_d = dscr("QT_d", [NH, KA, SQ], BF16)
    V_d = dscr("V_d", [NH, 128, NB, 65], BF16)
    mixed_d = dscr("mixed_d", [SQ, D], BF16)
    edu_bf = nc.dram_tensor("edu_bf", [nexp, 2, D], BF16).ap()
    if dbg:
        mod_dbg = dscr("mod_dbg", [128, 48], F32)

    with ExitStack() as es:
        S_ = Sched(nc, es)
        op = S_.op

        def sb(stack, name, shape, dt):
            return stack.enter_context(nc.sbuf_tensor("s_" + name, list(shape), dt))

        PS = [es.enter_context(nc.psum_tensor("ps%d" % i, [128, 512], F32)) for i in range(8)]
        TPS = [Tk("ps%d" % i) for i in range(8)]

        ident = sb(es, "ident", [128, 128], BF16); Tident = Tk()
        tri = sb(es, "tri", [128, 128], BF16); Ttri = Tk()
        ones = sb(es, "ones", [128, 128], BF16); Tones = Tk()
        modT = sb(es, "modT", [128, 48], F32); TmodT = Tk()
        G1 = sb(es, "G1", [128, 8], F32); TG1 = Tk()
        G2 = sb(es, "G2", [128, 8], F32); TG2 = Tk()
        bcs = sb(es, "bcs", [128, 5, D], F32)
        Tbcs = [Tk() for _ in range(5)]
        gout = sb(es, "gout", [128, 2, HD], F32); Tgout = Tk()
        flags = sb(es, "flags_sb", [8, 2], F32); Tflags = Tk()

        ch_c = S_.chan("const")
        ch_ld = [S_.chan("ld%d" % i) for i in range(4)]
        ch_st = [S_.chan("st%d" % i) for i in range(4)]
        ch_kts = [S_.chan("kts%d" % i) for i in range(4)]
        ch_aug = [S_.chan("aug%d" % i) for i in range(3)]
        ch_hd = [[S_.chan("hd%d_%d" % (i, j)) for j in range(3)] for i in range(2)]
        ch_stg = [S_.chan("stg%d" % i) for i in range(2)]

        op("pool", lambda e: e.memset(ident[:], 1.0), writes=[Tident])
        op("pool", lambda e: e.affine_select(out=ident[:], in_=ident[:], pattern=[[-1, 128]],
                                             compare_op=ALU.is_equal, fill=0.0, base=0, channel_multiplier=1),
           reads=[Tident], writes=[Tident])
        op("pool", lambda e: e.memset(tri[:], 1.0), writes=[Ttri])
        op("pool", lambda e: e.affine_select(out=tri[:], in_=tri[:], pattern=[[-1, 128]],
                                             compare_op=ALU.is_ge, fill=0.0, base=0, channel_multiplier=1),
           reads=[Ttri], writes=[Ttri])
        op("pool", lambda e: e.memset(ones[:], 1.0), writes=[Tones])
        op("sp", lambda e: e.dma_start(out=gout[:, 0, :], in_=gfox_row.partition_broadcast(128)), writes=[Tgout], chan=S_.chan("k1"))
        op("sp", lambda e: e.dma_start(out=gout[:, 1, :], in_=gsb_row.partition_broadcast(128)), writes=[Tgout], chan=S_.chan("k2"))
        op("sp", lambda e: e.dma_start(out=flags[:], in_=flags_d), writes=[Tflags], chan=S_.chan("k3"))

        with ExitStack() as p0:
            cT = sb(p0, "cT", [128, 8], F32); TcT = Tk()
            scT = sb(p0, "scT", [128, 8], F32); TscT = Tk()
            screp = sb(p0, "screp", [128, 8, 128], F32); Tscrep = Tk()
            wa = [sb(p0, "wa%d" % i, [128, 8, 512], F32) for i in range(2)]; Twa = [Tk(), Tk()]
            badaT = sb(p0, "badaT", [128, 48], F32); TbadaT = Tk()
            badabc = sb(p0, "badabc", [128, 4, D], F32); Tbadabc = Tk()
            gmixT = sb(p0, "gmixT", [128, 8], F32); TgmixT = Tk()
            gffnT = sb(p0, "gffnT", [128, 8], F32); TgffnT = Tk()
            gffnbc = sb(p0, "gffnbc", [128, D], F32); Tgffnbc = Tk()

            op("sp", lambda e: e.dma_start(out=cT[:], in_=cT_d), writes=[TcT], chan=S_.chan("k4"))
            op("sp", lambda e: e.dma_start(out=badaT[:], in_=badaT_d), writes=[TbadaT], chan=S_.chan("k5"))
            op("sp", lambda e: e.dma_start(out=gmixT[:], in_=gmixT_d), writes=[TgmixT], chan=S_.chan("k6"))
            op("sp", lambda e: e.dma_start(out=gffnT[:], in_=gffnT_d), writes=[TgffnT], chan=S_.chan("k7"))
            op("sp", lambda e: e.dma_start(out=gffnbc[:], in_=gffn_row.partition_broadcast(128)), writes=[Tgffnbc], chan=S_.chan("k8"))
            op("sp", lambda e: e.dma_start(out=bcs[:, 4, :], in_=gfin_row.partition_broadcast(128)), writes=[Tbcs[4]], chan=S_.chan("k9"))
            op("sp", lambda e: e.dma_start(out=badabc[:].rearrange("p a d -> p (a d)"),
                                           in_=bada_row[:, 2 * D:6 * D].partition_broadcast(128)), writes=[Tbadabc], chan=S_.chan("k10"))
            op("act", lambda e: e.activation(out=scT[:], in_=cT[:], func=AF.Silu), reads=[TcT], writes=[TscT])
            op("dve", lambda e: e.tensor_copy(out=screp[:], in_=scT[:].unsqueeze(2).to_broadcast([128, 8, 128])),
               reads=[TscT], writes=[Tscrep])
            w_ada_v = w_ada.rearrange("(k p) c -> p k c", p=128)
            modps = PS[0]
            for cc in range(12):
                b = cc % 2
                op("sp", lambda e, cc=cc, b=b: e.dma_start(out=wa[b][:], in_=w_ada_v[:, :, cc * 512:(cc + 1) * 512]),
                   writes=[Twa[b]], chan=ch_ld[b])
                for f4 in range(4):
                    fc = cc * 4 + f4
                    for k in range(8):
                        op("pe", lambda e, b=b, k=k, f4=f4, fc=fc: e.matmul(
                            modps[:, fc:fc + 1], lhsT=wa[b][:, k, f4 * 128:(f4 + 1) * 128], rhs=scT[:, k:k + 1],
                            start=(k == 0), stop=(k == 7)), reads=[Twa[b], TscT], writes=[TPS[0]])
                if cc >= 4:
                    a = (cc - 4) // 2
                    hh = (cc - 4) % 2
                    pb = PS[1 + (cc % 2)]
                    Tpb = TPS[1 + (cc % 2)]
                    for k in range(8):
                        op("pe", lambda e, b=b, k=k, pb=pb: e.matmul(pb[:, :], lhsT=screp[:, k, :], rhs=wa[b][:, k, :],
                                                                   start=(k == 0), stop=(k == 7)),
                           reads=[Twa[b], Tscrep], writes=[Tpb])
                    op("dve", lambda e, a=a, hh=hh, pb=pb: e.tensor_tensor(
                        out=bcs[:, a, hh * 512:(hh + 1) * 512], in0=pb[:, :], in1=badabc[:, a, hh * 512:(hh + 1) * 512],
                        op=ALU.add), reads=[Tpb, Tbadabc], writes=[Tbcs[a]])
            op("dve", lambda e: e.tensor_tensor(out=modT[:], in0=modps[:, 0:48], in1=badaT[:], op=ALU.add),
               reads=[TPS[0], TbadaT], writes=[TmodT])
            op("dve", lambda e: e.scalar_tensor_tensor(out=G1[:], in0=modT[:, 8:16], scalar=1.0, in1=gmixT[:],
                                                       op0=ALU.add, op1=ALU.mult), reads=[TmodT, TgmixT], writes=[TG1])
            op("dve", lambda e: e.scalar_tensor_tensor(out=G2[:], in0=modT[:, 32:40], scalar=1.0, in1=gffnT[:],
                                                       op0=ALU.add, op1=ALU.mult), reads=[TmodT, TgffnT], writes=[TG2])
            op("dve", lambda e: e.scalar_tensor_tensor(out=bcs[:, 2, :], in0=bcs[:, 2, :], scalar=1.0, in1=gffnbc[:],
                                                       op0=ALU.add, op1=ALU.mult), reads=[Tbcs[2], Tgffnbc], writes=[Tbcs[2]])
            if dbg:
                op("sp", lambda e: e.dma_start(out=mod_dbg, in_=modT[:]), reads=[TmodT], chan=ch_st[0])
            S_.barrier()

        def norm_chunk(xsrc_ap, xt, Txt, ss, Tss, rstd, Trstd, xn, Txn, junk, Tjunk, hT, ThT, Gc, TGc, Bc_ap, TBc, ldchan,
                       psA, psB):
            op("sp", lambda e: e.dma_start(out=xt[:], in_=xsrc_ap.rearrange("(j p) d -> p j d", p=128)),
               writes=[Txt], chan=ldchan)
            for j in range(4):
                op("act", lambda e, j=j: e.activation(out=junk[:], in_=xt[:, j, :], func=AF.Square,
                                                      accum_out=ss[:, j:j + 1]), reads=[Txt], writes=[Tjunk, Tss])
            op("act", lambda e: e.activation(out=rstd[:], in_=ss[:], func=AF.Sqrt, scale=1.0 / D, bias=EPS),
               reads=[Tss], writes=[Trstd])
            op("dve", lambda e: e.reciprocal(out=rstd[:], in_=rstd[:]), reads=[Trstd], writes=[Trstd])
            for j in range(4):
                op("dve", lambda e, j=j: e.tensor_scalar(out=xn[:, j, :], in0=xt[:, j, :], scalar1=rstd[:, j:j + 1],
                                                         scalar2=None, op0=ALU.mult), reads=[Txt, Trstd], writes=[Txn])
            for k in range(8):
                pi = psA if k % 2 == 0 else psB
                pt = PS[pi][:].bitcast(BF16)
                for j in range(4):
                    op("pe", lambda e, j=j, k=k, pt=pt: e.transpose(pt[:, j * 128:(j + 1) * 128], xn[:, j, k * 128:(k + 1) * 128],
                                                                   ident[:]), reads=[Txn, Tident], writes=[TPS[pi]])
                op("act", lambda e, k=k, pt=pt: e.activation(out=hT[:, k, :], in_=pt[:, 0:512], func=AF.Identity,
                                                             scale=Gc[:, k:k + 1], bias=Bc_ap[:, k:k + 1]),
                   reads=[TPS[pi], TGc, TBc], writes=[ThT])

        with ExitStack() as p1:
            Wb = sb(p1, "Wb", [128, 8, INC], BF16); TWb = Tk()
            xt = [sb(p1, "xt%d" % i, [128, 4, D], F32) for i in range(2)]; Txt = [Tk(), Tk()]
            xn = sb(p1, "xn", [128, 4, D], BF16); Txn = Tk()
            junk = sb(p1, "junk", [128, D], BF16); Tjunk = Tk()
            ss = [sb(p1, "ss%d" % i, [128, 4], F32) for i in range(2)]; Tss = [Tk(), Tk()]
            rstd = [sb(p1, "rstd%d" % i, [128, 4], F32) for i in range(2)]; Trstd = [Tk(), Tk()]
            hT = sb(p1, "hT", [128, 8, 512], BF16); ThT = Tk()
            KTs = [sb(p1, "KTs%d" % i, [128, 512], BF16) for i in range(4)]; TKTs = [Tk() for _ in range(4)]
            Vs = [sb(p1, "Vs%d" % i, [128, 4, NH, 65], BF16) for i in range(2)]; TVs = [Tk(), Tk()]
            tribig = sb(p1, "tribig", [128, 4, 512], F32); Ttribig = Tk()
            bfbc = sb(p1, "bfbc", [128, 8], F32); Tbfbc = Tk()
            ub = sb(p1, "ub", [128, 4, 8], F32); Tub = Tk()
            lf = sb(p1, "lf", [128, 4, 8], F32); Tlf = Tk()
            GT = [sb(p1, "GT%d" % i, [8, 512], F32) for i in range(2)]; TGT = [Tk(), Tk()]
            gz = sb(p1, "gz", [8, 1], F32); Tgz = Tk()
            sp_hi = sb(p1, "sp_hi", [8, 3, 512], BF16); Tsp_hi = Tk()
            r1 = sb(p1, "r1", [8, 512], F32); Tr1 = Tk()
            r2 = sb(p1, "r2", [8, 512], F32); Tr2 = Tk()
            gq = sb(p1, "gq", [8, 512], F32); Tgq = Tk()
            spq = sb(p1, "spq", [8, 3, 512], BF16); Tspq = Tk()
            cst = sb(p1, "cst", [8, 3, 512], BF16); Tcst = Tk()

            w_in_v = w_in.rearrange("(k p) c -> p k c", p=128)
            wst = [xt[i][:].rearrange("p j d -> p (j d)")[:, 0:3520].rearrange("p (k c) -> p k c", k=8) for i in range(2)]
            Twst = Txt
            for i in range(7):
                b = i % 2
                op("sp", lambda e, i=i, b=b: e.dma_start(out=wst[b], in_=w_in_v[:, :, i * 440:(i + 1) * 440]),
                   writes=[Twst[b]], chan=ch_ld[b])
                eng = "dve" if i % 2 == 0 else "pool"
                op(eng, lambda e, i=i, b=b: e.tensor_copy(out=Wb[:, :, i * 440:(i + 1) * 440], in_=wst[b]),
                   reads=[Twst[b]], writes=[TWb])
            op("pool", lambda e: e.memset(tribig[:], 1.0), writes=[Ttribig])
            for j in range(4):
                op("pool", lambda e, j=j: e.affine_select(out=tribig[:, j, :], in_=tribig[:, j, :], pattern=[[1, 512]],
                                                          compare_op=ALU.is_ge, fill=0.0, base=-j * 128, channel_multiplier=-1),
                   reads=[Ttribig], writes=[Ttribig])
            op("sp", lambda e: e.dma_start(out=bfbc[:], in_=bf_row.partition_broadcast(128)), writes=[Tbfbc], chan=S_.chan("k11"))
            op("pool", lambda e: e.memset(gz[:], 0.0), writes=[Tgz])
            for i in range(2):
                op("pool", lambda e, i=i: e.memset(Vs[i][:], 1.0), writes=[TVs[i]])
            op("pool", lambda e: e.memset(cst[:], -1.0), writes=[Tcst])
            for c in range(NT):
                op("pool", lambda e, c=c: e.dma_start(
                    out=KT_d[0:8, 67:70, c * 512:(c + 1) * 512], in_=cst[:]),
                   reads=[Tcst], chan=ch_aug[0])
            op("pool", lambda e: e.memset(cst[:], 1.0), reads=[], writes=[Tcst])
            for c in range(NTQ):
                op("pool", lambda e, c=c: e.dma_start(
                    out=QT_d[0:8, 64:67, c * 512:(c + 1) * 512], in_=cst[:]),
                   reads=[Tcst], chan=ch_aug[0])

            o1 = 1536
            o2 = 1544
            kcols = [512 + 128 * i for i in range(4)] + [o2 + 512 + 128 * i for i in range(4)]
            qcols = [0 + 128 * i for i in range(4)] + [o2 + 128 * i for i in range(4)]
            vcols = [1024, o2 + 1024]

            def proj_T(cols_list, dst_d, c, scale, rr):
                for hp in range(8):
                    pi = 2 + (rr[0] % 2); rr[0] += 1
                    for k in range(8):
                        op("pe", lambda e, hp=hp, k=k, pi=pi: e.matmul(
                            PS[pi][:, :], lhsT=Wb[:, k, cols_list[hp]:cols_list[hp] + 128], rhs=hT[:, k, :],
                            start=(k == 0), stop=(k == 7)), reads=[TWb, ThT], writes=[TPS[pi]])
                    kb = rr[1] % 4; rr[1] += 1
                    op("act", lambda e, pi=pi, kb=kb: e.activation(out=KTs[kb][:], in_=PS[pi][:, :], func=AF.Copy, scale=scale),
                       reads=[TPS[pi]], writes=[TKTs[kb]])
                    for i2 in range(2):
                        op("pool", lambda e, hp=hp, kb=kb, i2=i2: e.dma_start(
                            out=dst_d[2 * hp + i2, 0:64, c * 512:(c + 1) * 512],
                            in_=KTs[kb][i2 * 64:(i2 + 1) * 64, :]), reads=[TKTs[kb]], chan=ch_kts[kb])

            rr = [0, 0]
            for c in range(NT):
                b = c % 2
                norm_chunk(x_all[c * 512:(c + 1) * 512, :], xt[b], Txt[b], ss[b], Tss[b], rstd[b], Trstd[b], xn, Txn,
                           junk, Tjunk, hT, ThT, G1, TG1, modT[:, 0:8], TmodT, ch_ld[b], 0, 1)
                proj_T(kcols, KT_d, c, 1.0, rr)
                vb = c % 2
                for j in range(4):
                    for g in range(2):
                        pi = 4 + ((j * 2 + g) % 2)
                        for k in range(8):
                            op("pe", lambda e, j=j, g=g, k=k, pi=pi: e.matmul(
                                PS[pi][:, :], lhsT=hT[:, k, j * 128:(j + 1) * 128], rhs=Wb[:, k, vcols[g]:vcols[g] + 512],
                                start=(k == 0), stop=(k == 7)), reads=[TWb, ThT], writes=[TPS[pi]])
                        op("dve", lambda e, j=j, g=g, pi=pi, vb=vb: e.tensor_copy(
                            out=Vs[vb][:, j, g * 8:(g + 1) * 8, 0:64], in_=PS[pi][:, :].rearrange("p (h d) -> p h d", d=64)),
                           reads=[TPS[pi]], writes=[TVs[vb]])
                for j in range(4):
                    op("pool", lambda e, j=j, vb=vb, c=c: e.dma_start(
                        out=V_d[:, :, 4 * c + j, :].rearrange("h p e -> p h e"), in_=Vs[vb][:, j, :, :]),
                       reads=[TVs[vb]], chan=ch_st[2 + vb])
                for j in range(4):
                    for k in range(8):
                        op("pe", lambda e, j=j, k=k: e.matmul(
                            PS[6][:, j * 8:(j + 1) * 8], lhsT=hT[:, k, j * 128:(j + 1) * 128], rhs=Wb[:, k, o1:o1 + 8],
                            start=(k == 0), stop=(k == 7)), reads=[TWb, ThT], writes=[TPS[6]])
                op("dve", lambda e: e.tensor_tensor(out=ub[:], in0=PS[6][:, 0:32].rearrange("p (j h) -> p j h", h=8),
                                                    in1=bfbc[:].unsqueeze(1).to_broadcast([128, 4, 8]), op=ALU.add),
                   reads=[TPS[6], Tbfbc], writes=[Tub])
                op("act", lambda e: e.activation(out=ub[:], in_=ub[:], func=AF.Exp, scale=-1.0), reads=[Tub], writes=[Tub])
                op("act", lambda e: e.activation(out=lf[:], in_=ub[:], func=AF.Ln, bias=1.0), reads=[Tub], writes=[Tlf])
                for j in range(4):
                    op("pe", lambda e, j=j: e.matmul(PS[7][0:8, :], lhsT=lf[:, j, :], rhs=tribig[:, j, :],
                                                     start=(j == 0), stop=(j == 3)), reads=[Tlf, Ttribig], writes=[TPS[7]])
                gcur, Tgcur = GT[c % 2], TGT[c % 2]
                if c == 0:
                    carry_ap, Tcarry = gz[:, 0:1], Tgz
                else:
                    carry_ap, Tcarry = GT[(c - 1) % 2][:, 511:512], TGT[(c - 1) % 2]
                op("dve", lambda e, gcur=gcur, carry_ap=carry_ap: e.tensor_scalar(
                    out=gcur[:], in0=PS[7][0:8, :], scalar1=carry_ap, scalar2=None, op0=ALU.add),
                   reads=[TPS[7], Tcarry], writes=[Tgcur])

                def split3(src, Tsrc, dst, Tdst):
                    op("dve", lambda e: e.tensor_copy(out=dst[:, 0, :], in_=src[:]), reads=[Tsrc], writes=[Tdst])
                    op("dve", lambda e: e.tensor_tensor(out=r1[:], in0=src[:], in1=dst[:, 0, :], op=ALU.subtract),
                       reads=[Tsrc, Tdst], writes=[Tr1])
                    op("dve", lambda e: e.tensor_copy(out=dst[:, 1, :], in_=r1[:]), reads=[Tr1], writes=[Tdst])
                    op("dve", lambda e: e.tensor_tensor(out=r2[:], in0=r1[:], in1=dst[:, 1, :], op=ALU.subtract),
                       reads=[Tr1, Tdst], writes=[Tr2])
                    op("dve", lambda e: e.tensor_copy(out=dst[:, 2, :], in_=r2[:]), reads=[Tr2], writes=[Tdst])

                split3(gcur, Tgcur, sp_hi, Tsp_hi)
                op("pool", lambda e, c=c: e.dma_start(out=KT_d[0:8, 64:67, c * 512:(c + 1) * 512], in_=sp_hi[:]),
                   reads=[Tsp_hi], chan=ch_aug[1])
                m, ph = c // 4, c % 4
                if ph in (0, 2):
                    fl = flags[:, 0:1] if ph == 0 else flags[:, 1:2]
                    op("dve", lambda e, fl=fl, gcur=gcur: e.tensor_scalar(out=gq[:], in0=gcur[:], scalar1=fl, scalar2=None,
                                                                          op0=ALU.mult), reads=[Tgcur, Tflags], writes=[Tgq])
                else:
                    fl = flags[:, 1:2] if ph == 1 else flags[:, 0:1]
                    op("dve", lambda e, fl=fl, gcur=gcur: e.scalar_tensor_tensor(out=gq[:], in0=gcur[:], scalar=fl, in1=gq[:],
                                                                                 op0=ALU.mult, op1=ALU.add),
                       reads=[Tgcur, Tflags, Tgq], writes=[Tgq])
                    split3(gq, Tgq, spq, Tspq)
                    lt = 2 * m + (0 if ph == 1 else 1)
                    op("pool", lambda e, lt=lt: e.dma_start(out=QT_d[0:8, 67:70, lt * 512:(lt + 1) * 512], in_=spq[:]),
                       reads=[Tspq], chan=ch_aug[2])

            for c in range(NTQ):
                b = c % 2
                norm_chunk(x_q[c * 512:(c + 1) * 512, :], xt[b], Txt[b], ss[b], Tss[b], rstd[b], Trstd[b], xn, Txn,
                           junk, Tjunk, hT, ThT, G1, TG1, modT[:, 0:8], TmodT, ch_ld[b], 0, 1)
                proj_T(qcols, QT_d, c, 0.125, rr)
            S_.barrier()

        if 2 in phases:
          with ExitStack() as p2:
            maskF = sb(p2, "maskF", [128, 16, 512], BF16); TmaskF = Tk()
            maskS = sb(p2, "maskS", [128, 16, 512], BF16); TmaskS = Tk()
            KTh = [sb(p2, "KTh%d" % i, [KA, S], BF16) for i in range(2)]; TKTh = [Tk(), Tk()]
            Vh = [sb(p2, "Vh%d" % i, [128, NB, 65], BF16) for i in range(2)]; TVh = [Tk(), Tk()]
            QTh = [sb(p2, "QTh%d" % i, [KA, SQ], BF16) for i in range(2)]; TQTh = [Tk(), Tk()]
            negQ = [sb(p2, "negQ%d" % i, [64, 512], BF16) for i in range(2)]; TnegQ = [Tk(), Tk()]
            zt = [sb(p2, "zt%d" % i, [128, 512], F32) for i in range(2)]; Tzt = [Tk(), Tk()]
            ee = [sb(p2, "ee%d" % i, [128, 512], F32) for i in range(2)]; Tee = [Tk(), Tk()]
            spb = [sb(p2, "spb%d" % i, [128, 512], BF16) for i in range(2)]; Tspb = [Tk(), Tk()]
            LL = [sb(p2, "LL%d" % i, [128, 512], F32) for i in range(2)]; TLL = [Tk(), Tk()]
            PT = [sb(p2, "PT%d" % i, [128, 512], BF16) for i in range(6)]; TPT = [Tk() for _ in range(6)]
            Rsb = sb(p2, "Rsb", [128, 512], F32); TRsb = Tk()
            osb = sb(p2, "osb", [128, 4, 64], F32); Tosb = Tk()
            sq = sb(p2, "sq", [128, 4, 64], F32); Tsq = Tk()
            o2 = sb(p2, "o2", [128, 4, 64], F32); To2 = Tk()
            omix = [sb(p2, "omix%d" % i, [128, 4, 64], BF16) for i in range(2)]; Tomix = [Tk(), Tk()]
            ssq = sb(p2, "ssq", [128, 4], F32); Tssq = Tk()
            rinv = sb(p2, "rinv", [128, 4], F32); Trinv = Tk()

            op("sp", lambda e: e.dma_start(out=maskF[:], in_=masks_d), writes=[TmaskF], chan=S_.chan("k12"))
            op("sp", lambda e: e.dma_start(out=maskS[:], in_=masks_sb_d), writes=[TmaskS], chan=S_.chan("k13"))


            stgf = [sb(p2, "pstgf%d" % i, [128, 4096], F32) for i in range(2)]; Tstgf = [Tk(), Tk()]
            stgb = [sb(p2, "pstgb%d" % i, [128, 4096], BF16) for i in range(2)]; Tstgb = [Tk(), Tk()]
            ch_si = [S_.chan("psi%d" % i) for i in range(2)]
            ch_so = [S_.chan("pso%d" % i) for i in range(2)]

            def precast_gen():
                RPP = nexp // 128
                cnt = 0
                for (tsrc, tdst) in ((e_down, edu_bf[:, 0, :]), (e_up, edu_bf[:, 1, :])):
                    sv = tsrc.rearrange("(r p) d -> p r d", p=128)
                    dv = tdst.rearrange("(r p) d -> p r d", p=128)
                    for c4 in range(max(1, RPP // 4)):
                        nr = min(4, RPP)
                        i = cnt % 2; cnt += 1
                        sf = stgf[i][:, 0:nr * D].rearrange("p (r d) -> p r d", d=D)
                        sbv = stgb[i][:, 0:nr * D].rearrange("p (r d) -> p r d", d=D)
                        op("sp", lambda e: e.dma_start(out=sf, in_=sv[:, c4 * 4:c4 * 4 + nr, :]), writes=[Tstgf[i]], chan=ch_si[i])
                        op("pool", lambda e: e.tensor_copy(out=sbv, in_=sf), reads=[Tstgf[i]], writes=[Tstgb[i]])
                        op("sp", lambda e: e.dma_start(out=dv[:, c4 * 4:c4 * 4 + nr, :], in_=sbv), reads=[Tstgb[i]], chan=ch_so[i])
                        yield
            pgen = precast_gen()

            def block_list(lt):
                kind, m = lt % 2, lt // 2
                if kind == 0:
                    masked = [(4 * m + 1, 0), (4 * m, 1)]
                    lower = list(range(4 * m - 1, -1, -1))
                else:
                    masked = [(4 * m + 3, 2), (4 * m + 2, 3)]
                    lower = list(range(4 * m + 1, -1, -1))
                bl = []
                for (T_, ms) in masked:
                    for i in (3, 2, 1, 0):
                        bl.append((4 * T_ + i, ms * 4 + i))
                for T_ in lower:
                    for i in (3, 2, 1, 0):
                        bl.append((4 * T_ + i, None))
                return bl

            bctr = [0]
            octr = [0]
            for h in range(NH):
                hb = h % 2
                is_fox = h < 8
                op("sp", lambda e, h=h, hb=hb: e.dma_start(out=KTh[hb][:], in_=KT_d[h]), writes=[TKTh[hb]], chan=ch_hd[hb][0])
                op("sp", lambda e, h=h, hb=hb: e.dma_start(out=Vh[hb][:], in_=V_d[h]), writes=[TVh[hb]], chan=ch_hd[hb][1])
                op("sp", lambda e, h=h, hb=hb: e.dma_start(out=QTh[hb][:], in_=QT_d[h]), writes=[TQTh[hb]], chan=ch_hd[hb][2])
                kt, vt, qt = KTh[hb], Vh[hb], QTh[hb]
                Tkt, Tvt, Tqt = TKTh[hb], TVh[hb], TQTh[hb]
                for lt in range(NTQ):
                    bl = block_list(lt)
                    n = len(bl)
                    ob = 6 + (octr[0] % 2); octr[0] += 1
                    Ops, TOps = PS[ob], TPS[ob]
                    Ov = Ops[:, 0:260].rearrange("p (j e) -> p j e", e=65)
                    qs = slice(lt * 512, (lt + 1) * 512)
                    base = bctr[0]; bctr[0] += n
                    if is_fox:
                        def A1(k):
                            kb, mi = bl[k]
                            b = (base + k) % 6
                            zb = (base + k) % 2
                            op("pe", lambda e: e.matmul(PS[b][:, :], lhsT=kt[0:KA, kb * 128:(kb + 1) * 128], rhs=qt[0:KA, qs],
                                                        start=True, stop=True), reads=[Tkt, Tqt], writes=[TPS[b]])
                            if mi is not None:
                                op("dve", lambda e: e.tensor_tensor(out=zt[zb][:], in0=PS[b][:, :], in1=maskF[:, mi, :], op=ALU.add),
                                   reads=[TPS[b], TmaskF], writes=[Tzt[zb]])
                                op("act", lambda e: e.activation(out=PT[b][:], in_=zt[zb][:], func=AF.Exp), reads=[Tzt[zb]], writes=[TPT[b]])
                            else:
                                op("act", lambda e: e.activation(out=PT[b][:], in_=PS[b][:, :], func=AF.Exp), reads=[TPS[b]], writes=[TPT[b]])

                        def B(k):
                            kb, mi = bl[k]
                            b = (base + k) % 6
                            for j in range(4):
                                op("pe", lambda e, j=j: e.matmul(Ov[:, j, :], lhsT=PT[b][:, j * 128:(j + 1) * 128], rhs=vt[:, kb, :],
                                                                 start=(k == 0), stop=(k == n - 1)), reads=[TPT[b], Tvt], writes=[TOps])
                        for k in range(n + 3):
                            if k < n:
                                A1(k)
                            if k >= 3:
                                B(k - 3)
                    else:
                        nq = negQ[octr[0] % 2]; Tnq = TnegQ[octr[0] % 2]
                        op("pool", lambda e: e.tensor_scalar(out=nq[:], in0=qt[0:64, qs], scalar1=-1.0, scalar2=None, op0=ALU.mult),
                           reads=[Tqt], writes=[Tnq])
                        op("pool", lambda e: e.memset(Rsb[:], 0.0), writes=[TRsb])

                        def A1(k):
                            kb, mi = bl[k]
                            b = (base + k) % 2
                            op("pe", lambda e: e.matmul(PS[b][:, :], lhsT=kt[0:64, kb * 128:(kb + 1) * 128], rhs=qt[0:64, qs],
                                                        start=True, stop=True), reads=[Tkt, Tqt], writes=[TPS[b]])
                            if mi is not None:
                                op("dve", lambda e: e.tensor_tensor(out=zt[b][:], in0=PS[b][:, :], in1=maskS[:, mi, :], op=ALU.add),
                                   reads=[TPS[b], TmaskS], writes=[Tzt[b]])
                                op("act", lambda e: e.activation(out=ee[b][:], in_=zt[b][:], func=AF.Exp), reads=[Tzt[b]], writes=[Tee[b]])
                            else:
                                op("act", lambda e: e.activation(out=ee[b][:], in_=PS[b][:, :], func=AF.Exp), reads=[TPS[b]], writes=[Tee[b]])
                            op("act", lambda e: e.activation(out=spb[b][:], in_=ee[b][:], func=AF.Ln, bias=1.0), reads=[Tee[b]], writes=[Tspb[b]])

                        def A2(k):
                            kb, mi = bl[k]
                            b = (base + k) % 2
                            op("pe", lambda e: e.matmul(PS[2 + b][:, :], lhsT=tri[:], rhs=spb[b][:], start=True, stop=False),
                               reads=[Ttri, Tspb[b]], writes=[TPS[2 + b]])
                            op("pe", lambda e: e.matmul(PS[2 + b][:, :], lhsT=kt[0:64, kb * 128:(kb + 1) * 128], rhs=nq[:],
                                                        start=False, stop=True), reads=[Tkt, Tnq], writes=[TPS[2 + b]])
                            if k < n - 1:
                                op("pe", lambda e: e.matmul(PS[4 + b][:, :], lhsT=ones[:], rhs=spb[b][:], start=True, stop=True),
                                   reads=[Tones, Tspb[b]], writes=[TPS[4 + b]])
                            op("dve", lambda e: e.tensor_tensor(out=LL[b][:], in0=PS[2 + b][:, :], in1=Rsb[:], op=ALU.add),
                               reads=[TPS[2 + b], TRsb], writes=[TLL[b]])
                            if mi is not None:
                                op("dve", lambda e: e.tensor_tensor(out=LL[b][:], in0=LL[b][:], in1=maskS[:, mi, :], op=ALU.subtract),
                                   reads=[TLL[b], TmaskS], writes=[TLL[b]])
                            if k < n - 1:
                                op("dve", lambda e: e.tensor_tensor(out=Rsb[:], in0=PS[4 + b][:, :], in1=Rsb[:], op=ALU.add),
                                   reads=[TPS[4 + b], TRsb], writes=[TRsb])

                        def B(k):
                            kb, mi = bl[k]
                            b = (base + k) % 2
                            op("act", lambda e: e.activation(out=PT[b][:], in_=LL[b][:], func=AF.Exp, scale=-1.0),
                               reads=[TLL[b]], writes=[TPT[b]])
                            for j in range(4):
                                op("pe", lambda e, j=j: e.matmul(Ov[:, j, 0:64], lhsT=PT[b][:, j * 128:(j + 1) * 128], rhs=vt[:, kb, 0:64],
                                                                 start=(k == 0), stop=(k == n - 1)), reads=[TPT[b], Tvt], writes=[TOps])
                        for k in range(n + 2):
                            if k < n:
                                A1(k)
                            if 1 <= k <= n:
                                A2(k - 1)
                            if k >= 2:
                                B(k - 2)
                    if is_fox:
                        op("dve", lambda e: e.reciprocal(out=rinv[:].unsqueeze(2), in_=Ov[:, :, 64:65]), reads=[TOps], writes=[Trinv])
                        op("dve", lambda e: e.tensor_tensor(out=osb[:], in0=Ov[:, :, 0:64],
                                                            in1=rinv[:].unsqueeze(2).to_broadcast([128, 4, 64]), op=ALU.mult),
                           reads=[TOps, Trinv], writes=[Tosb])
                    else:
                        op("dve", lambda e: e.tensor_copy(out=osb[:], in_=Ov[:, :, 0:64]), reads=[TOps], writes=[Tosb])
                    op("pool", lambda e: e.tensor_tensor(out=sq[:], in0=osb[:], in1=osb[:], op=ALU.mult), reads=[Tosb], writes=[Tsq])
                    op("dve", lambda e: e.tensor_reduce(out=ssq[:], in_=sq[:], axis=AX.X, op=ALU.add), reads=[Tsq], writes=[Tssq])
                    op("act", lambda e: e.activation(out=ssq[:], in_=ssq[:], func=AF.Sqrt, scale=1.0 / HD, bias=EPS),
                       reads=[Tssq], writes=[Tssq])
                    op("dve", lambda e: e.reciprocal(out=ssq[:], in_=ssq[:]), reads=[Tssq], writes=[Tssq])
                    op("pool", lambda e: e.tensor_tensor(out=o2[:], in0=osb[:], in1=ssq[:].unsqueeze(2).to_broadcast([128, 4, 64]),
                                                         op=ALU.mult), reads=[Tosb, Tssq], writes=[To2])
                    om = octr[0] % 2
                    gi = 0 if is_fox else 1
                    op("pool", lambda e, om=om, gi=gi: e.tensor_tensor(out=omix[om][:], in0=o2[:],
                                                                       in1=gout[:, gi, :].unsqueeze(1).to_broadcast([128, 4, 64]),
                                                                       op=ALU.mult), reads=[To2, Tgout], writes=[Tomix[om]])
                    op("pool", lambda e, om=om, h=h, lt=lt: e.dma_start(
                        out=mixed_d[lt * 512:(lt + 1) * 512, h * 64:(h + 1) * 64].rearrange("(j p) d -> p j d", p=128),
                        in_=omix[om][:]), reads=[Tomix[om]], chan=ch_st[om])
                    next(pgen, None)
            for _ in pgen:
                pass
            S_.barrier()

        if 3 in phases:
          with ExitStack() as p3:
            NU, GS = 3, 4
            WoB = sb(p3, "WoB", [128, 8, D], BF16); TWoB = Tk()
            WqB = sb(p3, "WqB", [128, 8, 2048], BF16); TWqB = Tk()
            SKb = sb(p3, "SKb", [128, 16, 128], BF16); TSKb = Tk()
            with ExitStack() as pc:
                stgf = [sb(pc, "stgf%d" % i, [128, 4096], F32) for i in range(3)]; Tstgf = [Tk() for _ in range(3)]
                stgb = [sb(pc, "stgb%d" % i, [128, 4096], BF16) for i in range(3)]; Tstgb = [Tk() for _ in range(3)]
                ch_si = [S_.chan("si%d" % i) for i in range(3)]
                ch_so = [S_.chan("so%d" % i) for i in range(3)]
                cengs = ["dve", "act", "pool"]
                sctr = [0]

                def stage_cast(src_ap, shape_str, kw, dst_ap, Tdst):
                    i = sctr[0] % 3; sctr[0] += 1
                    n = 1
                    for v in src_ap.shape[1:]:
                        n *= v
                    stg = stgf[i][:, 0:n].rearrange(shape_str, **kw)
                    op("sp", lambda e: e.dma_start(out=stg, in_=src_ap), writes=[Tstgf[i]], chan=ch_si[i])
                    if cengs[i] == "act":
                        op("act", lambda e: e.activation(out=dst_ap, in_=stg, func=AF.Copy), reads=[Tstgf[i]], writes=[Tdst])
                    else:
                        op(cengs[i], lambda e: e.tensor_copy(out=dst_ap, in_=stg), reads=[Tstgf[i]], writes=[Tdst])
                wo_v = w_out.rearrange("(k p) c -> p k c", p=128)
                wq_v = w_query.rearrange("(k p) c -> p k c", p=128)
                for i in range(2):
                    stage_cast(wo_v[:, :, i * 512:(i + 1) * 512], "p (k c) -> p k c", dict(k=8), WoB[:, :, i * 512:(i + 1) * 512], TWoB)
                for i in range(4):
                    stage_cast(wq_v[:, :, i * 512:(i + 1) * 512], "p (k c) -> p k c", dict(k=8), WqB[:, :, i * 512:(i + 1) * 512], TWqB)
                stage_cast(skT_d, "p (k c) -> p k c", dict(k=16), SKb[:], TSKb)
                S_.barrier()
            U = [sb(p3, "U%d" % i, [128, GS, 2 * D], BF16) for i in range(NU)]
            TU = [[Tk() for _ in range(GS)] for _ in range(NU)]
            ch_g = [[S_.chan("g%d_%d" % (i, s)) for s in range(GS)] for i in range(NU)]
            dg = [sb(p3, "dg%d" % i, [128, 128], BF16) for i in range(4)]; Tdg = [Tk() for _ in range(4)]
            mxt = [sb(p3, "mxt%d" % i, [128, D], BF16) for i in range(2)]; Tmxt = [Tk(), Tk()]
            xqt = [sb(p3, "xqt%d" % i, [128, D], F32) for i in range(2)]; Txqt = [Tk(), Tk()]
            mT = sb(p3, "mT", [128, 8, 128], BF16); TmT = Tk()
            x1b = [sb(p3, "x1_%d" % i, [128, D], F32) for i in range(2)]; Tx1b = [Tk(), Tk()]
            xn2 = sb(p3, "xn2", [128, D], BF16); Txn2 = Tk()
            h2b = [sb(p3, "h2_%d" % i, [128, D], F32) for i in range(2)]; Th2b = [Tk(), Tk()]
            h2T = sb(p3, "h2T", [128, 8, 128], BF16); Th2T = Tk()
            qT = sb(p3, "qT", [128, 16, 128], BF16); TqT = Tk()
            sc = sb(p3, "sc", [128, 16, 128], F32); Tsc = Tk()
            scr = sb(p3, "scr", [128, 256], F32); Tscr = Tk()
            v16 = sb(p3, "v16", [128, 16, 16], F32); Tv16 = Tk()
            i16 = sb(p3, "i16", [128, 16, 16], U32); Ti16 = Tk()
            i16f = sb(p3, "i16f", [128, 16, 16], F32); Ti16f = Tk()
            cand = sb(p3, "cand", [128, 8, 256], F32); Tcand = Tk()
            cidx = sb(p3, "cidx", [128, 8, 256], F32); Tcidx = Tk()
            m16 = sb(p3, "m16", [128, 8, 16], F32); Tm16 = Tk()
            pos = sb(p3, "pos", [128, 8, 16], U32); Tpos = Tk()
            pab = sb(p3, "pab", [128, 2, 8, 16], U32); Tpab = Tk()
            pabf = sb(p3, "pabf", [128, 2, 8, 16], F32); Tpabf = Tk()
            e12 = sb(p3, "e12", [128, 2, 8, 16], F32); Te12 = Tk()
            iota16 = sb(p3, "iota16", [128, 16], F32); Tiota16 = Tk()
            for a_ in range(16):
                op("pool", lambda e, a_=a_: e.memset(iota16[:, a_:a_ + 1], float(a_)), writes=[Tiota16])
            eidf = sb(p3, "eidf", [128, 128], F32); Teidf = Tk()
            eid2 = [sb(p3, "eid%d" % i, [128, 128], I32) for i in range(2)]; Teid2 = [Tk(), Tk()]
            gtsb = [sb(p3, "gts%d" % i, [128, 8, 16], F32) for i in range(2)]; Tgtsb = [Tk(), Tk()]
            gsum = sb(p3, "gsum", [128, 8], F32); Tgsum = Tk()
            score = sb(p3, "score", [128, 128], F32); Tscore_g = [Tk() for _ in range(32)]
            actv = sb(p3, "actv", [128, 128], F32); Tactv_g = [Tk() for _ in range(32)]
            junk3 = sb(p3, "junk3", [128, D], BF16); Tjunk3 = Tk()
            junk4 = sb(p3, "junk4", [128, D], BF16); Tjunk4 = Tk()
            x2 = sb(p3, "x2", [128, D], F32); Tx2 = Tk()
            ot = [x2, x2]; Tot = [Tx2, Tx2]
            st3 = sb(p3, "st3", [128, 4], F32); Tst3 = Tk()
            st4 = sb(p3, "st4", [128, 4], F32); Tst4 = Tk()

            gctr = [0]
            def front_part(tb):
                b = tb % 2
                rows = slice(tb * 128, (tb + 1) * 128)
                x1, Tx1 = x1b[b], Tx1b[b]
                eid, Teid = eid2[b], Teid2[b]
                h2, Th2 = h2b[b], Th2b[b]
                gts, Tgts = gtsb[b], Tgtsb[b]
                op("sp", lambda e: e.dma_start(out=mxt[b][:], in_=mixed_d[rows, :]), writes=[Tmxt[b]], chan=ch_ld[b])
                op("sp", lambda e: e.dma_start(out=xqt[b][:], in_=x_q[rows, :]), writes=[Txqt[b]], chan=ch_ld[2 + b])
                ptb = PS[0][:].bitcast(BF16)
                for k in range(8):
                    op("pe", lambda e, k=k: e.transpose(ptb[:, k * 128:(k + 1) * 128], mxt[b][:, k * 128:(k + 1) * 128], ident[:]),
                       reads=[Tmxt[b], Tident], writes=[TPS[0]])
                op("act", lambda e: e.activation(out=mT[:].rearrange("p k t -> p (k t)"), in_=ptb[:, 0:1024], func=AF.Copy),
                   reads=[TPS[0]], writes=[TmT])
                yield
                for hf in range(2):
                    for k in range(8):
                        op("pe", lambda e, k=k, hf=hf: e.matmul(PS[2 + hf][:, :], lhsT=mT[:, k, :], rhs=WoB[:, k, hf * 512:(hf + 1) * 512],
                                                                start=(k == 0), stop=(k == 7)), reads=[TmT, TWoB], writes=[TPS[2 + hf]])
                    hs = slice(hf * 512, (hf + 1) * 512)
                    op("dve", lambda e, hf=hf, hs=hs: e.tensor_tensor(out=x1[:, hs], in0=PS[2 + hf][:, :], in1=bcs[:, 0, hs], op=ALU.mult),
                       reads=[TPS[2 + hf], Tbcs[0]], writes=[Tx1])
                op("dve", lambda e: e.tensor_tensor(out=x1[:], in0=x1[:], in1=xqt[b][:], op=ALU.add), reads=[Tx1, Txqt[b]], writes=[Tx1])
                yield
                op("act", lambda e: e.activation(out=junk4[:], in_=x1[:], func=AF.Square, accum_out=st3[:, 0:1]),
                   reads=[Tx1], writes=[Tjunk4, Tst3])
                op("act", lambda e: e.activation(out=st3[:, 0:1], in_=st3[:, 0:1], func=AF.Sqrt, scale=1.0 / D, bias=EPS),
                   reads=[Tst3], writes=[Tst3])
                op("dve", lambda e: e.reciprocal(out=st3[:, 0:1], in_=st3[:, 0:1]), reads=[Tst3], writes=[Tst3])
                op("dve", lambda e: e.tensor_scalar(out=xn2[:], in0=x1[:], scalar1=st3[:, 0:1], scalar2=None, op0=ALU.mult),
                   reads=[Tx1, Tst3], writes=[Txn2])
                op("dve", lambda e: e.scalar_tensor_tensor(out=h2[:], in0=x1[:], scalar=st3[:, 0:1], in1=bcs[:, 2, :],
                                                           op0=ALU.mult, op1=ALU.mult), reads=[Tx1, Tst3, Tbcs[2]], writes=[Th2])
                op("dve", lambda e: e.tensor_tensor(out=h2[:], in0=h2[:], in1=bcs[:, 1, :], op=ALU.add), reads=[Th2, Tbcs[1]], writes=[Th2])
                yield
                ptb1 = PS[1][:].bitcast(BF16)
                for k in range(8):
                    op("pe", lambda e, k=k: e.transpose(ptb1[:, k * 128:(k + 1) * 128], xn2[:, k * 128:(k + 1) * 128], ident[:]),
                       reads=[Txn2, Tident], writes=[TPS[1]])
                for k in range(8):
                    op("act", lambda e, k=k: e.activation(out=h2T[:, k, :], in_=ptb1[:, k * 128:(k + 1) * 128], func=AF.Identity,
                                                          scale=G2[:, k:k + 1], bias=modT[:, 24 + k:25 + k]),
                       reads=[TPS[1], TG2, TmodT], writes=[Th2T])
                yield
                for g4 in range(4):
                    pi = g4 % 2
                    for q4 in range(4):
                        hp = g4 * 4 + q4
                        for k in range(8):
                            op("pe", lambda e, k=k, hp=hp, q4=q4, pi=pi: e.matmul(
                                PS[pi][:, q4 * 128:(q4 + 1) * 128], lhsT=WqB[:, k, hp * 128:(hp + 1) * 128], rhs=h2T[:, k, :],
                                start=(k == 0), stop=(k == 7)), reads=[TWqB, Th2T], writes=[TPS[pi]])
                    op("act", lambda e, g4=g4, pi=pi: e.activation(out=qT[:, g4 * 4:(g4 + 1) * 4, :].rearrange("p a t -> p (a t)"),
                                                                   in_=PS[pi][:, :], func=AF.Copy), reads=[TPS[pi]], writes=[TqT])
                yield
                for g4 in range(4):
                    for q4 in range(4):
                        hp = g4 * 4 + q4
                        op("pe", lambda e, hp=hp, q4=q4, g4=g4: e.matmul(PS[2 + g4][:, q4 * 128:(q4 + 1) * 128], lhsT=qT[:, hp, :],
                                                                         rhs=SKb[:, hp, :], start=True, stop=True),
                           reads=[TqT, TSKb], writes=[TPS[2 + g4]])
                    op("act", lambda e, g4=g4: e.activation(out=sc[:, g4 * 4:(g4 + 1) * 4, :].rearrange("p a t -> p (a t)"),
                                                            in_=PS[2 + g4][:, :], func=AF.Copy), reads=[TPS[2 + g4]], writes=[Tsc])
                yield
                for hp in range(16):
                    op("dve", lambda e, hp=hp: e.max(out=v16[:, hp, 0:8], in_=sc[:, hp, :]), reads=[Tsc], writes=[Tv16])
                    op("dve", lambda e, hp=hp: e.max_index(out=i16[:, hp, 0:8], in_max=v16[:, hp, 0:8], in_values=sc[:, hp, :]),
                       reads=[Tsc, Tv16], writes=[Ti16])
                    op("dve", lambda e, hp=hp: e.match_replace(out=scr[:, 0:128], in_to_replace=v16[:, hp, 0:8], in_values=sc[:, hp, :],
                                                               imm_value=-1e30), reads=[Tsc, Tv16], writes=[Tscr])
                    op("dve", lambda e, hp=hp: e.max(out=v16[:, hp, 8:16], in_=scr[:, 0:128]), reads=[Tscr], writes=[Tv16])
                    op("dve", lambda e, hp=hp: e.max_index(out=i16[:, hp, 8:16], in_max=v16[:, hp, 8:16], in_values=scr[:, 0:128]),
                       reads=[Tscr, Tv16], writes=[Ti16])
                    yield
                op("dve", lambda e: e.tensor_copy(out=i16f[:], in_=i16[:]), reads=[Ti16], writes=[Ti16f])
                v4 = v16[:].rearrange("p (h two) a -> p h two a", two=2)
                f4 = i16f[:].rearrange("p (h two) a -> p h two a", two=2)
                c4 = cand[:].rearrange("p h (a b) -> p h a b", b=16)
                x4 = cidx[:].rearrange("p h (a b) -> p h a b", b=16)
                op("dve", lambda e: e.tensor_tensor(out=c4, in0=v4[:, :, 0, :].unsqueeze(3).to_broadcast([128, 8, 16, 16]),
                                                    in1=v4[:, :, 1, :].unsqueeze(2).to_broadcast([128, 8, 16, 16]), op=ALU.add),
                   reads=[Tv16], writes=[Tcand])
                op("dve", lambda e: e.tensor_scalar(out=f4[:, :, 0, :], in0=f4[:, :, 0, :], scalar1=128.0, scalar2=None, op0=ALU.mult),
                   reads=[Ti16f], writes=[Ti16f])
                for h in range(8):
                    op("dve", lambda e, h=h: e.max(out=m16[:, h, 0:8], in_=cand[:, h, :]), reads=[Tcand], writes=[Tm16])
                    op("dve", lambda e, h=h: e.max_index(out=pos[:, h, 0:8], in_max=m16[:, h, 0:8], in_values=cand[:, h, :]),
                       reads=[Tcand, Tm16], writes=[Tpos])
                    op("dve", lambda e, h=h: e.match_replace(out=scr[:], in_to_replace=m16[:, h, 0:8], in_values=cand[:, h, :],
                                                             imm_value=-1e30), reads=[Tcand, Tm16], writes=[Tscr])
                    op("dve", lambda e, h=h: e.max(out=m16[:, h, 8:16], in_=scr[:]), reads=[Tscr], writes=[Tm16])
                    op("dve", lambda e, h=h: e.max_index(out=pos[:, h, 8:16], in_max=m16[:, h, 8:16], in_values=scr[:]),
                       reads=[Tscr, Tm16], writes=[Tpos])
                    yield
                op("dve", lambda e: e.tensor_single_scalar(out=pab[:, 0], in_=pos[:], scalar=4, op=ALU.logical_shift_right),
                   reads=[Tpos], writes=[Tpab])
                op("dve", lambda e: e.tensor_single_scalar(out=pab[:, 1], in_=pos[:], scalar=15, op=ALU.bitwise_and),
                   reads=[Tpos], writes=[Tpab])
                op("dve", lambda e: e.tensor_copy(out=pabf[:], in_=pab[:]), reads=[Tpab], writes=[Tpabf])
                yield
                oh4 = cand[:].rearrange("p h (k a) -> p h k a", a=16)
                pr4 = cidx[:].rearrange("p h (k a) -> p h k a", a=16)
                io4 = iota16[:].unsqueeze(1).unsqueeze(1).to_broadcast([128, 8, 16, 16])
                for half in range(2):
                    op("dve", lambda e, half=half: e.tensor_tensor(
                        out=oh4, in0=pabf[:, half].unsqueeze(3).to_broadcast([128, 8, 16, 16]), in1=io4, op=ALU.is_equal),
                       reads=[Tpabf, Tiota16, Tm16, Tpos], writes=[Tcand])
                    op("dve", lambda e, half=half: e.tensor_tensor(
                        out=pr4, in0=oh4, in1=f4[:, :, half, :].unsqueeze(2).to_broadcast([128, 8, 16, 16]), op=ALU.mult),
                       reads=[Tcand, Ti16f], writes=[Tcidx])
                    op("dve", lambda e, half=half: e.tensor_reduce(out=e12[:, half], in_=pr4, axis=AX.X, op=ALU.add),
                       reads=[Tcidx], writes=[Te12])
                    yield
                op("dve", lambda e: e.tensor_tensor(out=eidf[:].rearrange("p (h k) -> p h k", k=16), in0=e12[:, 0], in1=e12[:, 1], op=ALU.add),
                   reads=[Te12], writes=[Teidf])
                op("dve", lambda e: e.tensor_scalar(out=eidf[:], in0=eidf[:], scalar1=float(nexp - 1), scalar2=None, op0=ALU.min),
                   reads=[Teidf], writes=[Teidf])
                op("dve", lambda e: e.tensor_copy(out=eid[:], in_=eidf[:]), reads=[Teidf], writes=[Teid])
                yield
                op("dve", lambda e: e.tensor_tensor(out=gts[:], in0=m16[:], in1=m16[:, :, 0:1].to_broadcast([128, 8, 16]),
                                                    op=ALU.subtract), reads=[Tm16], writes=[Tgts])
                op("act", lambda e: e.activation(out=gts[:], in_=gts[:], func=AF.Exp), reads=[Tgts], writes=[Tgts])
                op("dve", lambda e: e.tensor_reduce(out=gsum[:], in_=gts[:], axis=AX.X, op=ALU.add), reads=[Tgts], writes=[Tgsum])
                op("dve", lambda e: e.reciprocal(out=gsum[:], in_=gsum[:]), reads=[Tgsum], writes=[Tgsum])
                op("dve", lambda e: e.tensor_tensor(out=gts[:], in0=gts[:], in1=gsum[:].unsqueeze(2).to_broadcast([128, 8, 16]),
                                                    op=ALU.mult), reads=[Tgts, Tgsum], writes=[Tgts])

            def block_part(tb, gen):
                b = tb % 2
                rows = slice(tb * 128, (tb + 1) * 128)
                x1, Tx1 = x1b[b], Tx1b[b]
                eid, Teid = eid2[b], Teid2[b]
                h2, Th2 = h2b[b], Th2b[b]
                gts, Tgts = gtsb[b], Tgtsb[b]
                gflat = gts[:].rearrange("p h k -> p (h k)")
                for grp in range(128 // GS):
                    ub = gctr[0] % NU; gctr[0] += 1
                    gs = slice(grp * GS, (grp + 1) * GS)
                    for s in range(GS):
                        slot = grp * GS + s
                        op("pool", lambda e, ub=ub, s=s, slot=slot: e.indirect_dma_start(
                            out=U[ub][:, s, :], out_offset=None, in_=edu_bf.rearrange("n t d -> n (t d)"),
                            in_offset=bass.IndirectOffsetOnAxis(ap=eid[:, slot:slot + 1], axis=0)),
                           reads=[Teid], writes=[TU[ub][s]], chan=ch_g[ub][s])
                    for s in range(GS):
                        slot = grp * GS + s
                        op("dve", lambda e, s=s, slot=slot: e.scalar_tensor_tensor(
                            out=junk3[:], in0=U[ub][:, s, 0:D], scalar=1.0, in1=h2[:],
                            op0=ALU.mult, op1=ALU.mult, accum_out=score[:, slot:slot + 1]),
                           reads=[TU[ub][s], Th2], writes=[Tjunk3, Tscore_g[grp]])
                    op("act", lambda e: e.activation(out=actv[:, gs], in_=score[:, gs], func=AF.Gelu),
                       reads=[Tscore_g[grp]], writes=[Tactv_g[grp]])
                    op("dve", lambda e: e.tensor_tensor(out=actv[:, gs], in0=actv[:, gs], in1=gflat[:, gs], op=ALU.mult),
                       reads=[Tactv_g[grp], Tgts], writes=[Tactv_g[grp]])
                    for s in range(GS):
                        slot = grp * GS + s
                        di = slot % 4
                        op("act", lambda e, slot=slot, di=di: e.activation(out=dg[di][:], in_=ident[:], func=AF.Copy,
                                                                           scale=actv[:, slot:slot + 1]),
                           reads=[Tident, Tactv_g[grp]], writes=[Tdg[di]])
                        for hf in range(2):
                            op("pe", lambda e, hf=hf, s=s, di=di, slot=slot: e.matmul(
                                PS[6 + hf][:, :], lhsT=dg[di][:], rhs=U[ub][:, s, D + hf * 512:D + (hf + 1) * 512],
                                start=(slot == 0), stop=(slot == 127)), reads=[Tdg[di], TU[ub][s]], writes=[TPS[6 + hf]])
                    if gen is not None:
                        for _ in range(3):
                            next(gen, None)
                if gen is not None:
                    for _ in gen:
                        pass
                for hf in range(2):
                    hs = slice(hf * 512, (hf + 1) * 512)
                    op("dve", lambda e, hf=hf, hs=hs: e.tensor_tensor(out=x2[:, hs], in0=PS[6 + hf][:, :], in1=bcs[:, 3, hs], op=ALU.mult),
                       reads=[TPS[6 + hf], Tbcs[3]], writes=[Tx2])
                op("dve", lambda e: e.tensor_tensor(out=x2[:], in0=x2[:], in1=x1[:], op=ALU.add), reads=[Tx2, Tx1], writes=[Tx2])
                op("act", lambda e: e.activation(out=junk4[:], in_=x2[:], func=AF.Square, accum_out=st4[:, 1:2]),
                   reads=[Tx2], writes=[Tjunk4, Tst4])
                op("act", lambda e: e.activation(out=st4[:, 1:2], in_=st4[:, 1:2], func=AF.Sqrt, scale=1.0 / D, bias=EPS),
                   reads=[Tst4], writes=[Tst4])
                op("dve", lambda e: e.reciprocal(out=st4[:, 1:2], in_=st4[:, 1:2]), reads=[Tst4], writes=[Tst4])
                op("dve", lambda e: e.scalar_tensor_tensor(out=ot[b][:], in0=x2[:], scalar=st4[:, 1:2], in1=bcs[:, 4, :],
                                                           op0=ALU.mult, op1=ALU.mult), reads=[Tx2, Tst4, Tbcs[4]], writes=[Tx2])
                op("sp", lambda e: e.dma_start(out=out_d[rows, :], in_=ot[b][:]), reads=[Tot[b]], chan=ch_st[b])

            for _ in front_part(0):
                pass
            for tb in range(NBQ):
                block_part(tb, front_part(tb + 1) if tb + 1 < NBQ else None)

        S_.barrier()
        print("build: ninst", S_.ninst, "nwaits", S_.nwaits, "sems", len(S_.sems))
    return nc


def make_masks(parity):
    s = np.arange(128)[:, None]
    t = np.arange(512)[None, :]
    out = np.zeros((2, 128, 16, 512), np.float32)
    kinds = ["n", "d", "d", "f"] if parity == 0 else ["d", "f", "n", "d"]
    for ms in range(4):
        for i in range(4):
            for v in range(2):
                if kinds[ms] == "n":
                    m = np.full((128, 512), NEG, np.float32)
                elif kinds[ms] == "f":
                    m = np.zeros((128, 512), np.float32)
                else:
                    kp = i * 128 + s
                    vis = (kp <= t) if v == 0 else (kp < t)
                    m = np.where(vis, 0.0, NEG).astype(np.float32)
                out[v, :, ms * 4 + i, :] = m
    return out.astype(ml_dtypes.bfloat16)


def make_in_maps(inp, S):
    NT = S // 512
    f32 = lambda a: np.ascontiguousarray(a, dtype=np.float32)
    colT = lambda v, n: f32(np.asarray(v).reshape(n, 128).T)
    shared = {
        "w_ada": f32(inp["w_ada"][0]),
        "badaT": colT(inp["b_ada"][0], 48),
        "bada_row": f32(inp["b_ada"][0][None, :]),
        "gmixT": colT(inp["g_norm_mix"][0], 8),
        "gffnT": colT(inp["g_norm_ffn"][0], 8),
        "gffn_row": f32(inp["g_norm_ffn"][0][None, :]),
        "gfin_row": f32(inp["g_final"][None, :]),
        "w_in": f32(inp["w_in"][0]),
        "bf_row": f32(inp["b_forget"][0][None, :]),
        "gfox_row": f32(inp["g_out_fox"][0][None, :]),
        "gsb_row": f32(inp["g_out_sb"][0][None, :]),
        "w_out": f32(inp["w_out"][0]),
        "w_query": f32(inp["w_query"][0]),
        "skT": f32(np.asarray(inp["sub_keys"][0]).reshape(16, 128, 128).transpose(2, 0, 1)),
        "e_down": f32(inp["expert_down"][0]),
        "e_up": f32(inp["expert_up"][0]),
    }
    masks = [make_masks(0), make_masks(1)]
    maps = []
    for core in range(8):
        b, r = core // 2, core % 2
        xb = np.asarray(inp["x"][b])
        tiles = tile_ids(r, NT)
        m = dict(shared)
        m["x_all"] = f32(xb)
        m["x_q"] = f32(np.concatenate([xb[t * 512:(t + 1) * 512] for t in tiles], axis=0))
        m["cT"] = colT(inp["c"][b], 8)
        m["masks"] = np.ascontiguousarray(masks[r][0])
        m["masks_sb"] = np.ascontiguousarray(masks[r][1])
        fl = np.zeros((8, 2), np.float32)
        fl[:, r] = 1.0
        m["flags"] = fl
        maps.append(m)
    return maps


def assemble(results, S):
    NT = S // 512
    out = np.zeros((4, S, D), np.float32)
    for core in range(8):
        b, r = core // 2, core % 2
        o = np.asarray(results[core]["out"])
        for lt, t in enumerate(tile_ids(r, NT)):
            out[b, t * 512:(t + 1) * 512] = o[lt * 512:(lt + 1) * 512]
    return out


_NC_CACHE = {}


def kernel(**inputs):
    S = 8192
    if S not in _NC_CACHE:
        _NC_CACHE[S] = build(S)
    nc = _NC_CACHE[S]
    maps = make_in_maps(inputs, S)
    res = run_bass_kernel_spmd(nc, maps, core_ids=list(range(8)))
    return assemble(res.results, S)
```

```python
import numpy as np
import ml_dtypes
from contextlib import ExitStack
import concourse.bass as bass
import concourse.mybir as mybir
from concourse.bass_utils import run_bass_kernel_spmd

F32 = mybir.dt.float32
BF16 = mybir.dt.bfloat16
I32 = mybir.dt.int32
U32 = mybir.dt.uint32
AF = mybir.ActivationFunctionType
ALU = mybir.AluOpType
AX = mybir.AxisListType

D = 1024
NH = 16
HD = 64
KA = 70
INC = 3080
NEXP = 16384
EPS = 1e-6
NEG = -30000.0


class Tk:
    __slots__ = ("name", "w", "r")

    def __init__(self, name=""):
        self.name = name
        self.w = None
        self.r = {}


class Chan:
    def __init__(self, S, name):
        self.sem = S.new_sem(name)
        self.key = name
        self.count = 0


class Sched:
    ENGS = ("pe", "act", "dve", "pool", "sp")
    ROT = 20000

    def __init__(self, nc, es):
        self.nc = nc
        self.es = es
        self.eobj = {"pe": nc.tensor, "act": nc.scalar, "dve": nc.vector, "pool": nc.gpsimd, "sp": nc.sync}
        self.sems = {}
        self.chans = []
        self.gen = {k: 0 for k in self.ENGS}
        self.ekey = {}
        self.count = {}
        self.known = {k: {} for k in self.ENGS}
        self.done_keys = []
        for k in self.ENGS:
            self._new_esem(k)
        self.nwaits = 0
        self.ninst = 0

    def _new_esem(self, k):
        key = "e_%s_%d" % (k, self.gen[k])
        self.gen[k] += 1
        self.new_sem(key)
        self.ekey[k] = key
        self.count[k] = 0

    def new_sem(self, name):
        s = self.es.enter_context(self.nc.semaphore(name))
        self.sems[name] = s
        return s

    def chan(self, name):
        c = Chan(self, "c_" + name)
        self.chans.append(c)
        return c

    def _wait(self, eng, ev):
        key, val, clock = ev
        kn = self.known[eng]
        if kn.get(key, 0) >= val:
            return
        self.eobj[eng].wait_ge(self.sems[key], val)
        self.nwaits += 1
        kn[key] = val
        if clock:
            for k2, v2 in clock.items():
                if kn.get(k2, 0) < v2:
                    kn[k2] = v2

    def op(self, eng, fn, reads=(), writes=(), chan=None):
        deps = []
        epref = "e_%s_" % eng
        for t in reads:
            if t.w is not None:
                deps.append(t.w)
        for t in writes:
            for ev in t.r.values():
                if ev[0].startswith(epref):
                    continue
                deps.append(ev)
            if t.w is not None:
                if t.w[0].startswith(epref):
                    continue
                deps.append(t.w)
        for ev in deps:
            self._wait(eng, ev)
        self.ninst += 1
        if chan is None:
            if self.count[eng] >= self.ROT:
                self.done_keys.append((self.ekey[eng], self.count[eng]))
                self._new_esem(eng)
            key = self.ekey[eng]
            self.count[eng] += 1
            val = self.count[eng]
            fn(self.eobj[eng]).then_inc(self.sems[key], 1)
            ev = (key, val, dict(self.known[eng]))
        else:
            chan.count += 16
            fn(self.eobj[eng]).then_inc(chan.sem, 16)
            ev = (chan.key, chan.count, dict(self.known[eng]))
        for t in writes:
            t.w = ev
            t.r = {}
        for t in reads:
            if t in writes:
                continue
            t.r[("e_" + eng) if chan is None else chan.key] = ev
        return ev

    def barrier(self):
        evs = []
        for k in self.ENGS:
            if self.count[k] > 0:
                evs.append((self.ekey[k], self.count[k], None))
        for key, cnt in self.done_keys:
            evs.append((key, cnt, None))
        for c in self.chans:
            if c.count > 0:
                evs.append((c.key, c.count, None))
        for eng in self.ENGS:
            for ev in evs:
                self._wait(eng, ev)


def tile_ids(parity, NT):
    out = []
    for m in range(NT // 4):
        out += [4 * m, 4 * m + 3] if parity == 0 else [4 * m + 1, 4 * m + 2]
    return out


def build(S, dbg=0, phases=(0, 1, 2, 3), nexp=NEXP):
    NT = S // 512
    NB = S // 128
    NTQ = NT // 2
    SQ = NTQ * 512
    NBQ = SQ // 128
    nc = bass.Bass("TRN2", target_bir_lowering=False)

    def din(name, shape, dt=F32):
        return nc.dram_tensor(name, list(shape), dt, kind="ExternalInput").ap()

    x_all = din("x_all", [S, D])
    x_q = din("x_q", [SQ, D])
    cT_d = din("cT", [128, 8])
    w_ada = din("w_ada", [D, 6 * D])
    badaT_d = din("badaT", [128, 48])
    bada_row = din("bada_row", [1, 6 * D])
    gmixT_d = din("gmixT", [128, 8])
    gffnT_d = din("gffnT", [128, 8])
    gffn_row = din("gffn_row", [1, D])
    gfin_row = din("gfin_row", [1, D])
    w_in = din("w_in", [D, INC])
    bf_row = din("bf_row", [1, 8])
    gfox_row = din("gfox_row", [1, HD])
    gsb_row = din("gsb_row", [1, HD])
    w_out = din("w_out", [D, D])
    w_query = din("w_query", [D, 2048])
    skT_d = din("skT", [128, 16, 128])
    e_down = din("e_down", [nexp, D])
    e_up = din("e_up", [nexp, D])
    masks_d = din("masks", [128, 16, 512], BF16)
    masks_sb_d = din("masks_sb", [128, 16, 512], BF16)
    flags_d = din("flags", [8, 2])
    out_d = nc.dram_tensor("out", [SQ, D], F32, kind="ExternalOutput").ap()

    okind = "ExternalOutput" if dbg else None

    def dscr(name, shape, dt):
        if dbg:
            return nc.dram_tensor(name, list(shape), dt, kind="ExternalOutput").ap()
        return nc.dram_tensor(name, list(shape), dt).ap()

    KT_d = dscr("KT_d", [NH, KA, S], BF16)
    QT_d = dscr("QT_d", [NH, KA, SQ], BF16)
    V_d = dscr("V_d", [NH, 128, NB, 65], BF16)
    mixed_d = dscr("mixed_d", [SQ, D], BF16)
    edu_bf = nc.dram_tensor("edu_bf", [nexp, 2, D], BF16).ap()
    if dbg:
        mod_dbg = dscr("mod_dbg", [128, 48], F32)

    with ExitStack() as es:
        S_ = Sched(nc, es)
        op = S_.op

        def sb(stack, name, shape, dt):
            return stack.enter_context(nc.sbuf_tensor("s_" + name, list(shape), dt))

        PS = [es.enter_context(nc.psum_tensor("ps%d" % i, [128, 512], F32)) for i in range(8)]
        TPS = [Tk("ps%d" % i) for i in range(8)]

        ident = sb(es, "ident", [128, 128], BF16); Tident = Tk()
        tri = sb(es, "tri", [128, 128], BF16); Ttri = Tk()
        ones = sb(es, "ones", [128, 128], BF16); Tones = Tk()
        identf = sb(es, "identf", [128, 128], F32); Tidentf = Tk()
        modT = sb(es, "modT", [128, 48], F32); TmodT = Tk()
        G1 = sb(es, "G1", [128, 8], F32); TG1 = Tk()
        G2 = sb(es, "G2", [128, 8], F32); TG2 = Tk()
        bcs = sb(es, "bcs", [128, 5, D], F32)
        Tbcs = [Tk() for _ in range(5)]
        gout = sb(es, "gout", [128, 2, HD], F32); Tgout = Tk()
        flags = sb(es, "flags_sb", [8, 2], F32); Tflags = Tk()

        ch_c = S_.chan("const")
        ch_ld = [S_.chan("ld%d" % i) for i in range(4)]
        ch_st = [S_.chan("st%d" % i) for i in range(4)]
        ch_kts = [S_.chan("kts%d" % i) for i in range(4)]
        ch_aug = [S_.chan("aug%d" % i) for i in range(3)]
        ch_hd = [[S_.chan("hd%d_%d" % (i, j)) for j in range(3)] for i in range(2)]
        ch_stg = [S_.chan("stg%d" % i) for i in range(2)]

        op("pool", lambda e: e.memset(ident[:], 1.0), writes=[Tident])
        op("pool", lambda e: e.affine_select(out=ident[:], in_=ident[:], pattern=[[-1, 128]],
                                             compare_op=ALU.is_equal, fill=0.0, base=0, channel_multiplier=1),
           reads=[Tident], writes=[Tident])
        op("pool", lambda e: e.memset(tri[:], 1.0), writes=[Ttri])
        op("pool", lambda e: e.affine_select(out=tri[:], in_=tri[:], pattern=[[-1, 128]],
                                             compare_op=ALU.is_ge, fill=0.0, base=0, channel_multiplier=1),
           reads=[Ttri], writes=[Ttri])
        op("pool", lambda e: e.memset(ones[:], 1.0), writes=[Tones])
        op("pool", lambda e: e.memset(identf[:], 1.0), writes=[Tidentf])
        op("pool", lambda e: e.affine_select(out=identf[:], in_=identf[:], pattern=[[-1, 128]],
                                             compare_op=ALU.is_equal, fill=0.0, base=0, channel_multiplier=1),
           reads=[Tidentf], writes=[Tidentf])
        op("sp", lambda e: e.dma_start(out=gout[:, 0, :], in_=gfox_row.partition_broadcast(128)), writes=[Tgout], chan=S_.chan("k1"))
        op("sp", lambda e: e.dma_start(out=gout[:, 1, :], in_=gsb_row.partition_broadcast(128)), writes=[Tgout], chan=S_.chan("k2"))
        op("sp", lambda e: e.dma_start(out=flags[:], in_=flags_d), writes=[Tflags], chan=S_.chan("k3"))

        with ExitStack() as p0:
            cT = sb(p0, "cT", [128, 8], F32); TcT = Tk()
            scT = sb(p0, "scT", [128, 8], F32); TscT = Tk()
            screp = sb(p0, "screp", [128, 8, 128], F32); Tscrep = Tk()
            wa = [sb(p0, "wa%d" % i, [128, 8, 512], F32) for i in range(2)]; Twa = [Tk(), Tk()]
            badaT = sb(p0, "badaT", [128, 48], F32); TbadaT = Tk()
            badabc = sb(p0, "badabc", [128, 4, D], F32); Tbadabc = Tk()
            gmixT = sb(p0, "gmixT", [128, 8], F32); TgmixT = Tk()
            gffnT = sb(p0, "gffnT", [128, 8], F32); TgffnT = Tk()
            gffnbc = sb(p0, "gffnbc", [128, D], F32); Tgffnbc = Tk()

            op("sp", lambda e: e.dma_start(out=cT[:], in_=cT_d), writes=[TcT], chan=S_.chan("k4"))
            op("sp", lambda e: e.dma_start(out=badaT[:], in_=badaT_d), writes=[TbadaT], chan=S_.chan("k5"))
            op("sp", lambda e: e.dma_start(out=gmixT[:], in_=gmixT_d), writes=[TgmixT], chan=S_.chan("k6"))
            op("sp", lambda e: e.dma_start(out=gffnT[:], in_=gffnT_d), writes=[TgffnT], chan=S_.chan("k7"))
            op("sp", lambda e: e.dma_start(out=gffnbc[:], in_=gffn_row.partition_broadcast(128)), writes=[Tgffnbc], chan=S_.chan("k8"))
            op("sp", lambda e: e.dma_start(out=bcs[:, 4, :], in_=gfin_row.partition_broadcast(128)), writes=[Tbcs[4]], chan=S_.chan("k9"))
            op("sp", lambda e: e.dma_start(out=badabc[:].rearrange("p a d -> p (a d)"),
                                           in_=bada_row[:, 2 * D:6 * D].partition_broadcast(128)), writes=[Tbadabc], chan=S_.chan("k10"))
            op("act", lambda e: e.activation(out=scT[:], in_=cT[:], func=AF.Silu), reads=[TcT], writes=[TscT])
            op("dve", lambda e: e.tensor_copy(out=screp[:], in_=scT[:].unsqueeze(2).to_broadcast([128, 8, 128])),
               reads=[TscT], writes=[Tscrep])
            w_ada_v = w_ada.rearrange("(k p) c -> p k c", p=128)
            modps = PS[0]
            for cc in range(12):
                b = cc % 2
                op("sp", lambda e, cc=cc, b=b: e.dma_start(out=wa[b][:], in_=w_ada_v[:, :, cc * 512:(cc + 1) * 512]),
                   writes=[Twa[b]], chan=ch_ld[b])
                for f4 in range(4):
                    fc = cc * 4 + f4
                    for k in range(8):
                        op("pe", lambda e, b=b, k=k, f4=f4, fc=fc: e.matmul(
                            modps[:, fc:fc + 1], lhsT=wa[b][:, k, f4 * 128:(f4 + 1) * 128], rhs=scT[:, k:k + 1],
                            start=(k == 0), stop=(k == 7)), reads=[Twa[b], TscT], writes=[TPS[0]])
                if cc >= 4:
                    a = (cc - 4) // 2
                    hh = (cc - 4) % 2
                    pb = PS[1 + (cc % 2)]
                    Tpb = TPS[1 + (cc % 2)]
                    for k in range(8):
                        op("pe", lambda e, b=b, k=k, pb=pb: e.matmul(pb[:, :], lhsT=screp[:, k, :], rhs=wa[b][:, k, :],
                                                                   start=(k == 0), stop=(k == 7)),
                           reads=[Twa[b], Tscrep], writes=[Tpb])
                    op("dve", lambda e, a=a, hh=hh, pb=pb: e.tensor_tensor(
                        out=bcs[:, a, hh * 512:(hh + 1) * 512], in0=pb[:, :], in1=badabc[:, a, hh * 512:(hh + 1) * 512],
                        op=ALU.add), reads=[Tpb, Tbadabc], writes=[Tbcs[a]])
            op("dve", lambda e: e.tensor_tensor(out=modT[:], in0=modps[:, 0:48], in1=badaT[:], op=ALU.add),
               reads=[TPS[0], TbadaT], writes=[TmodT])
            op("dve", lambda e: e.scalar_tensor_tensor(out=G1[:], in0=modT[:, 8:16], scalar=1.0, in1=gmixT[:],
                                                       op0=ALU.add, op1=ALU.mult), reads=[TmodT, TgmixT], writes=[TG1])
            op("dve", lambda e: e.scalar_tensor_tensor(out=G2[:], in0=modT[:, 32:40], scalar=1.0, in1=gffnT[:],
                                                       op0=ALU.add, op1=ALU.mult), reads=[TmodT, TgffnT], writes=[TG2])
            op("dve", lambda e: e.scalar_tensor_tensor(out=bcs[:, 2, :], in0=bcs[:, 2, :], scalar=1.0, in1=gffnbc[:],
                                                       op0=ALU.add, op1=ALU.mult), reads=[Tbcs[2], Tgffnbc], writes=[Tbcs[2]])
            if dbg:
                op("sp", lambda e: e.dma_start(out=mod_dbg, in_=modT[:]), reads=[TmodT], chan=ch_st[0])
            S_.barrier()

        def norm_chunk(xsrc_ap, xt, Txt, ss, Tss, rstd, Trstd, xn, Txn, junk, Tjunk, hT, ThT, Gc, TGc, Bc_ap, TBc, ldchan,
                       psA, psB):
            op("sp", lambda e: e.dma_start(out=xt[:], in_=xsrc_ap.rearrange("(j p) d -> p j d", p=128)),
               writes=[Txt], chan=ldchan)
            for j in range(4):
                op("act", lambda e, j=j: e.activation(out=junk[:], in_=xt[:, j, :], func=AF.Square,
                                                      accum_out=ss[:, j:j + 1]), reads=[Txt], writes=[Tjunk, Tss])
            op("act", lambda e: e.activation(out=rstd[:], in_=ss[:], func=AF.Sqrt, scale=1.0 / D, bias=EPS),
               reads=[Tss], writes=[Trstd])
            op("dve", lambda e: e.reciprocal(out=rstd[:], in_=rstd[:]), reads=[Trstd], writes=[Trstd])
            for j in range(4):
                op("dve", lambda e, j=j: e.tensor_scalar(out=xn[:, j, :], in0=xt[:, j, :], scalar1=rstd[:, j:j + 1],
                                                         scalar2=None, op0=ALU.mult), reads=[Txt, Trstd], writes=[Txn])
            for k in range(8):
                pi = psA if k % 2 == 0 else psB
                pt = PS[pi][:].bitcast(BF16)
                for j in range(4):
                    op("pe", lambda e, j=j, k=k, pt=pt: e.transpose(pt[:, j * 128:(j + 1) * 128], xn[:, j, k * 128:(k + 1) * 128],
                                                                   ident[:]), reads=[Txn, Tident], writes=[TPS[pi]])
                op("act", lambda e, k=k, pt=pt: e.activation(out=hT[:, k, :], in_=pt[:, 0:512], func=AF.Identity,
                                                             scale=Gc[:, k:k + 1], bias=Bc_ap[:, k:k + 1]),
                   reads=[TPS[pi], TGc, TBc], writes=[ThT])

        with ExitStack() as p1:
            Wb = sb(p1, "Wb", [128, 8, INC], BF16); TWb = Tk()
            xt = [sb(p1, "xt%d" % i, [128, 4, D], F32) for i in range(2)]; Txt = [Tk(), Tk()]
            xn = sb(p1, "xn", [128, 4, D], BF16); Txn = Tk()
            junk = sb(p1, "junk", [128, D], BF16); Tjunk = Tk()
            ss = [sb(p1, "ss%d" % i, [128, 4], F32) for i in range(2)]; Tss = [Tk(), Tk()]
            rstd = [sb(p1, "rstd%d" % i, [128, 4], F32) for i in range(2)]; Trstd = [Tk(), Tk()]
            hT = sb(p1, "hT", [128, 8, 512], BF16); ThT = Tk()
            KTs = [sb(p1, "KTs%d" % i, [128, 512], BF16) for i in range(4)]; TKTs = [Tk() for _ in range(4)]
            Vs = [sb(p1, "Vs%d" % i, [128, 4, NH, 65], BF16) for i in range(2)]; TVs = [Tk(), Tk()]
            tribig = sb(p1, "tribig", [128, 4, 512], F32); Ttribig = Tk()
            bfbc = sb(p1, "bfbc", [128, 8], F32); Tbfbc = Tk()
            ub = sb(p1, "ub", [128, 4, 8], F32); Tub = Tk()
            lf = sb(p1, "lf", [128, 4, 8], F32); Tlf = Tk()
            GT = [sb(p1, "GT%d" % i, [8, 512], F32) for i in range(2)]; TGT = [Tk(), Tk()]
            gz = sb(p1, "gz", [8, 1], F32); Tgz = Tk()
            sp_hi = sb(p1, "sp_hi", [8, 3, 512], BF16); Tsp_hi = Tk()
            r1 = sb(p1, "r1", [8, 512], F32); Tr1 = Tk()
            r2 = sb(p1, "r2", [8, 512], F32); Tr2 = Tk()
            gq = sb(p1, "gq", [8, 512], F32); Tgq = Tk()
            spq = sb(p1, "spq", [8, 3, 512], BF16); Tspq = Tk()
            cst = sb(p1, "cst", [8, 3, 512], BF16); Tcst = Tk()

            w_in_v = w_in.rearrange("(k p) c -> p k c", p=128)
            wst = [xt[i][:].rearrange("p j d -> p (j d)")[:, 0:3520].rearrange("p (k c) -> p k c", k=8) for i in range(2)]
            Twst = Txt
            for i in range(7):
                b = i % 2
                op("sp", lambda e, i=i, b=b: e.dma_start(out=wst[b], in_=w_in_v[:, :, i * 440:(i + 1) * 440]),
                   writes=[Twst[b]], chan=ch_ld[b])
                eng = "dve" if i % 2 == 0 else "pool"
                op(eng, lambda e, i=i, b=b: e.tensor_copy(out=Wb[:, :, i * 440:(i + 1) * 440], in_=wst[b]),
                   reads=[Twst[b]], writes=[TWb])
            op("pool", lambda e: e.memset(tribig[:], 1.0), writes=[Ttribig])
            for j in range(4):
                op("pool", lambda e, j=j: e.affine_select(out=tribig[:, j, :], in_=tribig[:, j, :], pattern=[[1, 512]],
                                                          compare_op=ALU.is_ge, fill=0.0, base=-j * 128, channel_multiplier=-1),
                   reads=[Ttribig], writes=[Ttribig])
            op("sp", lambda e: e.dma_start(out=bfbc[:], in_=bf_row.partition_broadcast(128)), writes=[Tbfbc], chan=S_.chan("k11"))
            op("pool", lambda e: e.memset(gz[:], 0.0), writes=[Tgz])
            for i in range(2):
                op("pool", lambda e, i=i: e.memset(Vs[i][:], 1.0), writes=[TVs[i]])
            op("pool", lambda e: e.memset(cst[:], -1.0), writes=[Tcst])
            for c in range(NT):
                op("pool", lambda e, c=c: e.dma_start(
                    out=KT_d[0:8, 67:70, c * 512:(c + 1) * 512], in_=cst[:]),
                   reads=[Tcst], chan=ch_aug[0])
            op("pool", lambda e: e.memset(cst[:], 1.0), reads=[], writes=[Tcst])
            for c in range(NTQ):
                op("pool", lambda e, c=c: e.dma_start(
                    out=QT_d[0:8, 64:67, c * 512:(c + 1) * 512], in_=cst[:]),
                   reads=[Tcst], chan=ch_aug[0])

            o1 = 1536
            o2 = 1544
            kcols = [512 + 128 * i for i in range(4)] + [o2 + 512 + 128 * i for i in range(4)]
            qcols = [0 + 128 * i for i in range(4)] + [o2 + 128 * i for i in range(4)]
            vcols = [1024, o2 + 1024]

            def proj_T(cols_list, dst_d, c, scale, rr):
                for hp in range(8):
                    pi = 2 + (rr[0] % 2); rr[0] += 1
                    for k in range(8):
                        op("pe", lambda e, hp=hp, k=k, pi=pi: e.matmul(
                            PS[pi][:, :], lhsT=Wb[:, k, cols_list[hp]:cols_list[hp] + 128], rhs=hT[:, k, :],
                            start=(k == 0), stop=(k == 7)), reads=[TWb, ThT], writes=[TPS[pi]])
                    kb = rr[1] % 4; rr[1] += 1
                    op("act", lambda e, pi=pi, kb=kb: e.activation(out=KTs[kb][:], in_=PS[pi][:, :], func=AF.Copy, scale=scale),
                       reads=[TPS[pi]], writes=[TKTs[kb]])
                    for i2 in range(2):
                        op("pool", lambda e, hp=hp, kb=kb, i2=i2: e.dma_start(
                            out=dst_d[2 * hp + i2, 0:64, c * 512:(c + 1) * 512],
                            in_=KTs[kb][i2 * 64:(i2 + 1) * 64, :]), reads=[TKTs[kb]], chan=ch_kts[kb])

            rr = [0, 0]
            for c in range(NT):
                b = c % 2
                norm_chunk(x_all[c * 512:(c + 1) * 512, :], xt[b], Txt[b], ss[b], Tss[b], rstd[b], Trstd[b], xn, Txn,
                           junk, Tjunk, hT, ThT, G1, TG1, modT[:, 0:8], TmodT, ch_ld[b], 0, 1)
                proj_T(kcols, KT_d, c, 1.0, rr)
                vb = c % 2
                for j in range(4):
                    for g in range(2):
                        pi = 4 + ((j * 2 + g) % 2)
                        for k in range(8):
                            op("pe", lambda e, j=j, g=g, k=k, pi=pi: e.matmul(
                                PS[pi][:, :], lhsT=hT[:, k, j * 128:(j + 1) * 128], rhs=Wb[:, k, vcols[g]:vcols[g] + 512],
                                start=(k == 0), stop=(k == 7)), reads=[TWb, ThT], writes=[TPS[pi]])
                        op("dve", lambda e, j=j, g=g, pi=pi, vb=vb: e.tensor_copy(
                            out=Vs[vb][:, j, g * 8:(g + 1) * 8, 0:64], in_=PS[pi][:, :].rearrange("p (h d) -> p h d", d=64)),
                           reads=[TPS[pi]], writes=[TVs[vb]])
                for j in range(4):
                    op("pool", lambda e, j=j, vb=vb, c=c: e.dma_start(
                        out=V_d[:, :, 4 * c + j, :].rearrange("h p e -> p h e"), in_=Vs[vb][:, j, :, :]),
                       reads=[TVs[vb]], chan=ch_st[2 + vb])
                for j in range(4):
                    for k in range(8):
                        op("pe", lambda e, j=j, k=k: e.matmul(
                            PS[6][:, j * 8:(j + 1) * 8], lhsT=hT[:, k, j * 128:(j + 1) * 128], rhs=Wb[:, k, o1:o1 + 8],
                            start=(k == 0), stop=(k == 7)), reads=[TWb, ThT], writes=[TPS[6]])
                op("dve", lambda e: e.tensor_tensor(out=ub[:], in0=PS[6][:, 0:32].rearrange("p (j h) -> p j h", h=8),
                                                    in1=bfbc[:].unsqueeze(1).to_broadcast([128, 4, 8]), op=ALU.add),
                   reads=[TPS[6], Tbfbc], writes=[Tub])
                op("act", lambda e: e.activation(out=ub[:], in_=ub[:], func=AF.Exp, scale=-1.0), reads=[Tub], writes=[Tub])
                op("act", lambda e: e.activation(out=lf[:], in_=ub[:], func=AF.Ln, bias=1.0), reads=[Tub], writes=[Tlf])
                for j in range(4):
                    op("pe", lambda e, j=j: e.matmul(PS[7][0:8, :], lhsT=lf[:, j, :], rhs=tribig[:, j, :],
                                                     start=(j == 0), stop=(j == 3)), reads=[Tlf, Ttribig], writes=[TPS[7]])
                gcur, Tgcur = GT[c % 2], TGT[c % 2]
                if c == 0:
                    carry_ap, Tcarry = gz[:, 0:1], Tgz
                else:
                    carry_ap, Tcarry = GT[(c - 1) % 2][:, 511:512], TGT[(c - 1) % 2]
                op("dve", lambda e, gcur=gcur, carry_ap=carry_ap: e.tensor_scalar(
                    out=gcur[:], in0=PS[7][0:8, :], scalar1=carry_ap, scalar2=None, op0=ALU.add),
                   reads=[TPS[7], Tcarry], writes=[Tgcur])

                def split3(src, Tsrc, dst, Tdst):
                    op("dve", lambda e: e.tensor_copy(out=dst[:, 0, :], in_=src[:]), reads=[Tsrc], writes=[Tdst])
                    op("dve", lambda e: e.tensor_tensor(out=r1[:], in0=src[:], in1=dst[:, 0, :], op=ALU.subtract),
                       reads=[Tsrc, Tdst], writes=[Tr1])
                    op("dve", lambda e: e.tensor_copy(out=dst[:, 1, :], in_=r1[:]), reads=[Tr1], writes=[Tdst])
                    op("dve", lambda e: e.tensor_tensor(out=r2[:], in0=r1[:], in1=dst[:, 1, :], op=ALU.subtract),
                       reads=[Tr1, Tdst], writes=[Tr2])
                    op("dve", lambda e: e.tensor_copy(out=dst[:, 2, :], in_=r2[:]), reads=[Tr2], writes=[Tdst])

                split3(gcur, Tgcur, sp_hi, Tsp_hi)
                op("pool", lambda e, c=c: e.dma_start(out=KT_d[0:8, 64:67, c * 512:(c + 1) * 512], in_=sp_hi[:]),
                   reads=[Tsp_hi], chan=ch_aug[1])
                m, ph = c // 4, c % 4
                if ph in (0, 2):
                    fl = flags[:, 0:1] if ph == 0 else flags[:, 1:2]
                    op("dve", lambda e, fl=fl, gcur=gcur: e.tensor_scalar(out=gq[:], in0=gcur[:], scalar1=fl, scalar2=None,
                                                                          op0=ALU.mult), reads=[Tgcur, Tflags], writes=[Tgq])
                else:
                    fl = flags[:, 1:2] if ph == 1 else flags[:, 0:1]
                    op("dve", lambda e, fl=fl, gcur=gcur: e.scalar_tensor_tensor(out=gq[:], in0=gcur[:], scalar=fl, in1=gq[:],
                                                                                 op0=ALU.mult, op1=ALU.add),
                       reads=[Tgcur, Tflags, Tgq], writes=[Tgq])
                    split3(gq, Tgq, spq, Tspq)
                    lt = 2 * m + (0 if ph == 1 else 1)
                    op("pool", lambda e, lt=lt: e.dma_start(out=QT_d[0:8, 67:70, lt * 512:(lt + 1) * 512], in_=spq[:]),
                       reads=[Tspq], chan=ch_aug[2])

            for c in range(NTQ):
                b = c % 2
                norm_chunk(x_q[c * 512:(c + 1) * 512, :], xt[b], Txt[b], ss[b], Tss[b], rstd[b], Trstd[b], xn, Txn,
                           junk, Tjunk, hT, ThT, G1, TG1, modT[:, 0:8], TmodT, ch_ld[b], 0, 1)
                proj_T(qcols, QT_d, c, 0.125, rr)
            S_.barrier()

        if 2 in phases:
          with ExitStack() as p2:
            maskF = sb(p2, "maskF", [128, 16, 512], BF16); TmaskF = Tk()
            maskS = sb(p2, "maskS", [128, 16, 512], BF16); TmaskS = Tk()
            KTh = [sb(p2, "KTh%d" % i, [KA, S], BF16) for i in range(2)]; TKTh = [Tk(), Tk()]
            Vh = [sb(p2, "Vh%d" % i, [128, NB, 65], BF16) for i in range(2)]; TVh = [Tk(), Tk()]
            QTh = [sb(p2, "QTh%d" % i, [KA, SQ], BF16) for i in range(2)]; TQTh = [Tk(), Tk()]
            negQ = [sb(p2, "negQ%d" % i, [64, 512], BF16) for i in range(2)]; TnegQ = [Tk(), Tk()]
            zt = [sb(p2, "zt%d" % i, [128, 512], F32) for i in range(2)]; Tzt = [Tk(), Tk()]
            ee = [sb(p2, "ee%d" % i, [128, 512], F32) for i in range(2)]; Tee = [Tk(), Tk()]
            spb = [sb(p2, "spb%d" % i, [128, 512], BF16) for i in range(2)]; Tspb = [Tk(), Tk()]
            LL = [sb(p2, "LL%d" % i, [128, 512], F32) for i in range(2)]; TLL = [Tk(), Tk()]
            PT = [sb(p2, "PT%d" % i, [128, 512], BF16) for i in range(6)]; TPT = [Tk() for _ in range(6)]
            Rsb = sb(p2, "Rsb", [128, 512], F32); TRsb = Tk()
            osb = sb(p2, "osb", [128, 4, 64], F32); Tosb = Tk()
            sq = sb(p2, "sq", [128, 4, 64], F32); Tsq = Tk()
            o2 = sb(p2, "o2", [128, 4, 64], F32); To2 = Tk()
            omix = [sb(p2, "omix%d" % i, [128, 4, 64], BF16) for i in range(2)]; Tomix = [Tk(), Tk()]
            ssq = sb(p2, "ssq", [128, 4], F32); Tssq = Tk()
            rinv = sb(p2, "rinv", [128, 4], F32); Trinv = Tk()
            OTs = sb(p2, "OTs", [65, 512], F32); TOTs = Tk()

            op("sp", lambda e: e.dma_start(out=maskF[:], in_=masks_d), writes=[TmaskF], chan=S_.chan("k12"))
            op("sp", lambda e: e.dma_start(out=maskS[:], in_=masks_sb_d), writes=[TmaskS], chan=S_.chan("k13"))


            stgf = [sb(p2, "pstgf%d" % i, [128, 4096], F32) for i in range(2)]; Tstgf = [Tk(), Tk()]
            stgb = [sb(p2, "pstgb%d" % i, [128, 4096], BF16) for i in range(2)]; Tstgb = [Tk(), Tk()]
            ch_si = [S_.chan("psi%d" % i) for i in range(2)]
            ch_so = [S_.chan("pso%d" % i) for i in range(2)]

            def precast_gen():
                RPP = nexp // 128
                cnt = 0
                for (tsrc, tdst) in ((e_down, edu_bf[:, 0, :]), (e_up, edu_bf[:, 1, :])):
                    sv = tsrc.rearrange("(r p) d -> p r d", p=128)
                    dv = tdst.rearrange("(r p) d -> p r d", p=128)
                    for c4 in range(max(1, RPP // 4)):
                        nr = min(4, RPP)
                        i = cnt % 2; cnt += 1
                        sf = stgf[i][:, 0:nr * D].rearrange("p (r d) -> p r d", d=D)
                        sbv = stgb[i][:, 0:nr * D].rearrange("p (r d) -> p r d", d=D)
                        op("sp", lambda e: e.dma_start(out=sf, in_=sv[:, c4 * 4:c4 * 4 + nr, :]), writes=[Tstgf[i]], chan=ch_si[i])
                        op("pool", lambda e: e.tensor_copy(out=sbv, in_=sf), reads=[Tstgf[i]], writes=[Tstgb[i]])
                        op("sp", lambda e: e.dma_start(out=dv[:, c4 * 4:c4 * 4 + nr, :], in_=sbv), reads=[Tstgb[i]], chan=ch_so[i])
                        yield
            pgen = precast_gen()

            def block_list(lt):
                kind, m = lt % 2, lt // 2
                if kind == 0:
                    masked = [(4 * m + 1, 0), (4 * m, 1)]
                    lower = list(range(4 * m - 1, -1, -1))
                else:
                    masked = [(4 * m + 3, 2), (4 * m + 2, 3)]
                    lower = list(range(4 * m + 1, -1, -1))
                bl = []
                for (T_, ms) in masked:
                    for i in (3, 2, 1, 0):
                        bl.append((4 * T_ + i, ms * 4 + i))
                for T_ in lower:
                    for i in (3, 2, 1, 0):
                        bl.append((4 * T_ + i, None))
                return bl

            bctr = [0]
            octr = [0]
            for h in range(NH):
                hb = h % 2
                is_fox = h < 8
                op("sp", lambda e, h=h, hb=hb: e.dma_start(out=KTh[hb][:], in_=KT_d[h]), writes=[TKTh[hb]], chan=ch_hd[hb][0])
                op("sp", lambda e, h=h, hb=hb: e.dma_start(out=Vh[hb][:], in_=V_d[h]), writes=[TVh[hb]], chan=ch_hd[hb][1])
                op("sp", lambda e, h=h, hb=hb: e.dma_start(out=QTh[hb][:], in_=QT_d[h]), writes=[TQTh[hb]], chan=ch_hd[hb][2])
                kt, vt, qt = KTh[hb], Vh[hb], QTh[hb]
                Tkt, Tvt, Tqt = TKTh[hb], TVh[hb], TQTh[hb]
                for lt in range(NTQ):
                    bl = block_list(lt)
                    n = len(bl)
                    ob = 6 + (octr[0] % 2); octr[0] += 1
                    Ops, TOps = PS[ob], TPS[ob]
                    Ov = Ops[:, 0:260].rearrange("p (j e) -> p j e", e=65)
                    qs = slice(lt * 512, (lt + 1) * 512)
                    base = bctr[0]; bctr[0] += n
                    if is_fox:
                        def A1(k):
                            kb, mi = bl[k]
                            b = (base + k) % 6
                            zb = (base + k) % 2
                            op("pe", lambda e: e.matmul(PS[b][:, :], lhsT=kt[0:KA, kb * 128:(kb + 1) * 128], rhs=qt[0:KA, qs],
                                                        start=True, stop=True), reads=[Tkt, Tqt], writes=[TPS[b]])
                            if mi is not None:
                                op("dve", lambda e: e.tensor_tensor(out=zt[zb][:], in0=PS[b][:, :], in1=maskF[:, mi, :], op=ALU.add),
                                   reads=[TPS[b], TmaskF], writes=[Tzt[zb]])
                                op("act", lambda e: e.activation(out=PT[b][:], in_=zt[zb][:], func=AF.Exp), reads=[Tzt[zb]], writes=[TPT[b]])
                            else:
                                op("act", lambda e: e.activation(out=PT[b][:], in_=PS[b][:, :], func=AF.Exp), reads=[TPS[b]], writes=[TPT[b]])

                        def B(k):
                            kb, mi = bl[k]
                            b = (base + k) % 6
                            op("pe", lambda e: e.matmul(Ops[0:65, :], lhsT=vt[:, kb, :], rhs=PT[b][:, :],
                                                        start=(k == 0), stop=(k == n - 1)), reads=[TPT[b], Tvt], writes=[TOps])
                        for k in range(n + 3):
                            if k < n:
                                A1(k)
                            if k >= 3:
                                B(k - 3)
                    else:
                        nq = negQ[octr[0] % 2]; Tnq = TnegQ[octr[0] % 2]
                        op("pool", lambda e: e.tensor_scalar(out=nq[:], in0=qt[0:64, qs], scalar1=-1.0, scalar2=None, op0=ALU.mult),
                           reads=[Tqt], writes=[Tnq])
                        op("pool", lambda e: e.memset(Rsb[:], 0.0), writes=[TRsb])

                        def A1(k):
                            kb, mi = bl[k]
                            b = (base + k) % 2
                            op("pe", lambda e: e.matmul(PS[b][:, :], lhsT=kt[0:64, kb * 128:(kb + 1) * 128], rhs=qt[0:64, qs],
                                                        start=True, stop=True), reads=[Tkt, Tqt], writes=[TPS[b]])
                            if mi is not None:
                                op("dve", lambda e: e.tensor_tensor(out=zt[b][:], in0=PS[b][:, :], in1=maskS[:, mi, :], op=ALU.add),
                                   reads=[TPS[b], TmaskS], writes=[Tzt[b]])
                                op("act", lambda e: e.activation(out=ee[b][:], in_=zt[b][:], func=AF.Exp), reads=[Tzt[b]], writes=[Tee[b]])
                            else:
                                op("act", lambda e: e.activation(out=ee[b][:], in_=PS[b][:, :], func=AF.Exp), reads=[TPS[b]], writes=[Tee[b]])
                            op("act", lambda e: e.activation(out=spb[b][:], in_=ee[b][:], func=AF.Ln, bias=1.0), reads=[Tee[b]], writes=[Tspb[b]])

                        def A2(k):
                            kb, mi = bl[k]
                            b = (base + k) % 2
                            op("pe", lambda e: e.matmul(PS[2 + b][:, :], lhsT=tri[:], rhs=spb[b][:], start=True, stop=False),
                               reads=[Ttri, Tspb[b]], writes=[TPS[2 + b]])
                            op("pe", lambda e: e.matmul(PS[2 + b][:, :], lhsT=kt[0:64, kb * 128:(kb + 1) * 128], rhs=nq[:],
                                                        start=False, stop=True), reads=[Tkt, Tnq], writes=[TPS[2 + b]])
                            if k < n - 1:
                                op("pe", lambda e: e.matmul(PS[4 + b][:, :], lhsT=ones[:], rhs=spb[b][:], start=True, stop=True),
                                   reads=[Tones, Tspb[b]], writes=[TPS[4 + b]])
                            op("dve", lambda e: e.tensor_tensor(out=LL[b][:], in0=PS[2 + b][:, :], in1=Rsb[:], op=ALU.add),
                               reads=[TPS[2 + b], TRsb], writes=[TLL[b]])
                            if mi is not None:
                                op("dve", lambda e: e.tensor_tensor(out=LL[b][:], in0=LL[b][:], in1=maskS[:, mi, :], op=ALU.subtract),
                                   reads=[TLL[b], TmaskS], writes=[TLL[b]])
                            if k < n - 1:
                                op("dve", lambda e: e.tensor_tensor(out=Rsb[:], in0=PS[4 + b][:, :], in1=Rsb[:], op=ALU.add),
                                   reads=[TPS[4 + b], TRsb], writes=[TRsb])

                        def B(k):
                            kb, mi = bl[k]
                            b = (base + k) % 2
                            op("act", lambda e: e.activation(out=PT[b][:], in_=LL[b][:], func=AF.Exp, scale=-1.0),
                               reads=[TLL[b]], writes=[TPT[b]])
                            for j in range(4):
                                op("pe", lambda e, j=j: e.matmul(Ov[:, j, 0:64], lhsT=PT[b][:, j * 128:(j + 1) * 128], rhs=vt[:, kb, 0:64],
                                                                 start=(k == 0), stop=(k == n - 1)), reads=[TPT[b], Tvt], writes=[TOps])
                        for k in range(n + 2):
                            if k < n:
                                A1(k)
                            if 1 <= k <= n:
                                A2(k - 1)
                            if k >= 2:
                                B(k - 2)
                    if is_fox:
                        op("act", lambda e: e.activation(out=OTs[:, :], in_=Ops[0:65, :], func=AF.Copy), reads=[TOps], writes=[TOTs])
                        Ov = PS[5][:, 0:260].rearrange("p (j e) -> p j e", e=65)
                        for j in range(4):
                            op("pe", lambda e, j=j: e.transpose(Ov[:, j, :], OTs[:, j * 128:(j + 1) * 128], identf[0:65, 0:65]),
                               reads=[TOTs, Tidentf], writes=[TPS[5]])
                        TOps = TPS[5]
                        op("dve", lambda e: e.reciprocal(out=rinv[:].unsqueeze(2), in_=Ov[:, :, 64:65]), reads=[TOps], writes=[Trinv])
                        op("dve", lambda e: e.tensor_tensor(out=osb[:], in0=Ov[:, :, 0:64],
                                                            in1=rinv[:].unsqueeze(2).to_broadcast([128, 4, 64]), op=ALU.mult),
                           reads=[TOps, Trinv], writes=[Tosb])
                    else:
                        op("dve", lambda e: e.tensor_copy(out=osb[:], in_=Ov[:, :, 0:64]), reads=[TOps], writes=[Tosb])
                    op("pool", lambda e: e.tensor_tensor(out=sq[:], in0=osb[:], in1=osb[:], op=ALU.mult), reads=[Tosb], writes=[Tsq])
                    op("dve", lambda e: e.tensor_reduce(out=ssq[:], in_=sq[:], axis=AX.X, op=ALU.add), reads=[Tsq], writes=[Tssq])
                    op("act", lambda e: e.activation(out=ssq[:], in_=ssq[:], func=AF.Sqrt, scale=1.0 / HD, bias=EPS),
                       reads=[Tssq], writes=[Tssq])
                    op("dve", lambda e: e.reciprocal(out=ssq[:], in_=ssq[:]), reads=[Tssq], writes=[Tssq])
                    op("pool", lambda e: e.tensor_tensor(out=o2[:], in0=osb[:], in1=ssq[:].unsqueeze(2).to_broadcast([128, 4, 64]),
                                                         op=ALU.mult), reads=[Tosb, Tssq], writes=[To2])
                    om = octr[0] % 2
                    gi = 0 if is_fox else 1
                    op("pool", lambda e, om=om, gi=gi: e.tensor_tensor(out=omix[om][:], in0=o2[:],
                                                                       in1=gout[:, gi, :].unsqueeze(1).to_broadcast([128, 4, 64]),
                                                                       op=ALU.mult), reads=[To2, Tgout], writes=[Tomix[om]])
                    op("pool", lambda e, om=om, h=h, lt=lt: e.dma_start(
                        out=mixed_d[lt * 512:(lt + 1) * 512, h * 64:(h + 1) * 64].rearrange("(j p) d -> p j d", p=128),
                        in_=omix[om][:]), reads=[Tomix[om]], chan=ch_st[om])
                    next(pgen, None)
            for _ in pgen:
                pass
            S_.barrier()

        if 3 in phases:
          with ExitStack() as p3:
            NU, GS = 3, 4
            WoB = sb(p3, "WoB", [128, 8, D], BF16); TWoB = Tk()
            WqB = sb(p3, "WqB", [128, 8, 2048], BF16); TWqB = Tk()
            SKb = sb(p3, "SKb", [128, 16, 128], BF16); TSKb = Tk()
            with ExitStack() as pc:
                stgf = [sb(pc, "stgf%d" % i, [128, 4096], F32) for i in range(3)]; Tstgf = [Tk() for _ in range(3)]
                stgb = [sb(pc, "stgb%d" % i, [128, 4096], BF16) for i in range(3)]; Tstgb = [Tk() for _ in range(3)]
                ch_si = [S_.chan("si%d" % i) for i in range(3)]
                ch_so = [S_.chan("so%d" % i) for i in range(3)]
                cengs = ["dve", "act", "pool"]
                sctr = [0]

                def stage_cast(src_ap, shape_str, kw, dst_ap, Tdst):
                    i = sctr[0] % 3; sctr[0] += 1
                    n = 1
                    for v in src_ap.shape[1:]:
                        n *= v
                    stg = stgf[i][:, 0:n].rearrange(shape_str, **kw)
                    op("sp", lambda e: e.dma_start(out=stg, in_=src_ap), writes=[Tstgf[i]], chan=ch_si[i])
                    if cengs[i] == "act":
                        op("act", lambda e: e.activation(out=dst_ap, in_=stg, func=AF.Copy), reads=[Tstgf[i]], writes=[Tdst])
                    else:
                        op(cengs[i], lambda e: e.tensor_copy(out=dst_ap, in_=stg), reads=[Tstgf[i]], writes=[Tdst])
                wo_v = w_out.rearrange("(k p) c -> p k c", p=128)
                wq_v = w_query.rearrange("(k p) c -> p k c", p=128)
                for i in range(2):
                    stage_cast(wo_v[:, :, i * 512:(i + 1) * 512], "p (k c) -> p k c", dict(k=8), WoB[:, :, i * 512:(i + 1) * 512], TWoB)
                for i in range(4):
                    stage_cast(wq_v[:, :, i * 512:(i + 1) * 512], "p (k c) -> p k c", dict(k=8), WqB[:, :, i * 512:(i + 1) * 512], TWqB)
                stage_cast(skT_d, "p (k c) -> p k c", dict(k=16), SKb[:], TSKb)
                S_.barrier()
            U = [sb(p3, "U%d" % i, [128, GS, 2 * D], BF16) for i in range(NU)]
            TU = [[Tk() for _ in range(GS)] for _ in range(NU)]
            ch_g = [[S_.chan("g%d_%d" % (i, s)) for s in range(GS)] for i in range(NU)]
            dg = [sb(p3, "dg%d" % i, [128, 128], BF16) for i in range(4)]; Tdg = [Tk() for _ in range(4)]
            mxt = [sb(p3, "mxt%d" % i, [128, D], BF16) for i in range(2)]; Tmxt = [Tk(), Tk()]
            xqt = [sb(p3, "xqt%d" % i, [128, D], F32) for i in range(2)]; Txqt = [Tk(), Tk()]
            mT = sb(p3, "mT", [128, 8, 128], BF16); TmT = Tk()
            x1b = [sb(p3, "x1_%d" % i, [128, D], F32) for i in range(2)]; Tx1b = [Tk(), Tk()]
            xn2 = sb(p3, "xn2", [128, D], BF16); Txn2 = Tk()
            h2b = [sb(p3, "h2_%d" % i, [128, D], F32) for i in range(2)]; Th2b = [Tk(), Tk()]
            h2T = sb(p3, "h2T", [128, 8, 128], BF16); Th2T = Tk()
            qT = sb(p3, "qT", [128, 16, 128], BF16); TqT = Tk()
            sc = sb(p3, "sc", [128, 16, 128], F32); Tsc = Tk()
            scr = sb(p3, "scr", [128, 256], F32); Tscr = Tk()
            v16 = sb(p3, "v16", [128, 16, 16], F32); Tv16 = Tk()
            i16 = sb(p3, "i16", [128, 16, 16], U32); Ti16 = Tk()
            i16f = sb(p3, "i16f", [128, 16, 16], F32); Ti16f = Tk()
            cand = sb(p3, "cand", [128, 8, 256], F32); Tcand = Tk()
            cidx = sb(p3, "cidx", [128, 8, 256], F32); Tcidx = Tk()
            m16 = sb(p3, "m16", [128, 8, 16], F32); Tm16 = Tk()
            pos = sb(p3, "pos", [128, 8, 16], U32); Tpos = Tk()
            pab = sb(p3, "pab", [128, 2, 8, 16], U32); Tpab = Tk()
            pabf = sb(p3, "pabf", [128, 2, 8, 16], F32); Tpabf = Tk()
            e12 = sb(p3, "e12", [128, 2, 8, 16], F32); Te12 = Tk()
            iota16 = sb(p3, "iota16", [128, 16], F32); Tiota16 = Tk()
            for a_ in range(16):
                op("pool", lambda e, a_=a_: e.memset(iota16[:, a_:a_ + 1], float(a_)), writes=[Tiota16])
            eidf = sb(p3, "eidf", [128, 128], F32); Teidf = Tk()
            eid2 = [sb(p3, "eid%d" % i, [128, 128], I32) for i in range(2)]; Teid2 = [Tk(), Tk()]
            gtsb = [sb(p3, "gts%d" % i, [128, 8, 16], F32) for i in range(2)]; Tgtsb = [Tk(), Tk()]
            gsum = sb(p3, "gsum", [128, 8], F32); Tgsum = Tk()
            score = sb(p3, "score", [128, 128], F32); Tscore_g = [Tk() for _ in range(32)]
            actv = sb(p3, "actv", [128, 128], F32); Tactv_g = [Tk() for _ in range(32)]
            junk3 = sb(p3, "junk3", [128, D], BF16); Tjunk3 = Tk()
            junk4 = sb(p3, "junk4", [128, D], BF16); Tjunk4 = Tk()
            x2 = sb(p3, "x2", [128, D], F32); Tx2 = Tk()
            ot = [x2, x2]; Tot = [Tx2, Tx2]
            st3 = sb(p3, "st3", [128, 4], F32); Tst3 = Tk()
            st4 = sb(p3, "st4", [128, 4], F32); Tst4 = Tk()

            gctr = [0]
            def front_part(tb):
                b = tb % 2
                rows = slice(tb * 128, (tb + 1) * 128)
                x1, Tx1 = x1b[b], Tx1b[b]
                eid, Teid = eid2[b], Teid2[b]
                h2, Th2 = h2b[b], Th2b[b]
                gts, Tgts = gtsb[b], Tgtsb[b]
                op("sp", lambda e: e.dma_start(out=mxt[b][:], in_=mixed_d[rows, :]), writes=[Tmxt[b]], chan=ch_ld[b])
                op("sp", lambda e: e.dma_start(out=xqt[b][:], in_=x_q[rows, :]), writes=[Txqt[b]], chan=ch_ld[2 + b])
                ptb = PS[0][:].bitcast(BF16)
                for k in range(8):
                    op("pe", lambda e, k=k: e.transpose(ptb[:, k * 128:(k + 1) * 128], mxt[b][:, k * 128:(k + 1) * 128], ident[:]),
                       reads=[Tmxt[b], Tident], writes=[TPS[0]])
                op("act", lambda e: e.activation(out=mT[:].rearrange("p k t -> p (k t)"), in_=ptb[:, 0:1024], func=AF.Copy),
                   reads=[TPS[0]], writes=[TmT])
                yield
                for hf in range(2):
                    for k in range(8):
                        op("pe", lambda e, k=k, hf=hf: e.matmul(PS[2 + hf][:, :], lhsT=mT[:, k, :], rhs=WoB[:, k, hf * 512:(hf + 1) * 512],
                                                                start=(k == 0), stop=(k == 7)), reads=[TmT, TWoB], writes=[TPS[2 + hf]])
                    hs = slice(hf * 512, (hf + 1) * 512)
                    op("dve", lambda e, hf=hf, hs=hs: e.tensor_tensor(out=x1[:, hs], in0=PS[2 + hf][:, :], in1=bcs[:, 0, hs], op=ALU.mult),
                       reads=[TPS[2 + hf], Tbcs[0]], writes=[Tx1])
                op("dve", lambda e: e.tensor_tensor(out=x1[:], in0=x1[:], in1=xqt[b][:], op=ALU.add), reads=[Tx1, Txqt[b]], writes=[Tx1])
                yield
                op("act", lambda e: e.activation(out=junk4[:], in_=x1[:], func=AF.Square, accum_out=st3[:, 0:1]),
                   reads=[Tx1], writes=[Tjunk4, Tst3])
                op("act", lambda e: e.activation(out=st3[:, 0:1], in_=st3[:, 0:1], func=AF.Sqrt, scale=1.0 / D, bias=EPS),
                   reads=[Tst3], writes=[Tst3])
                op("dve", lambda e: e.reciprocal(out=st3[:, 0:1], in_=st3[:, 0:1]), reads=[Tst3], writes=[Tst3])
                op("dve", lambda e: e.tensor_scalar(out=xn2[:], in0=x1[:], scalar1=st3[:, 0:1], scalar2=None, op0=ALU.mult),
                   reads=[Tx1, Tst3], writes=[Txn2])
                op("dve", lambda e: e.scalar_tensor_tensor(out=h2[:], in0=x1[:], scalar=st3[:, 0:1], in1=bcs[:, 2, :],
                                                           op0=ALU.mult, op1=ALU.mult), reads=[Tx1, Tst3, Tbcs[2]], writes=[Th2])
                op("dve", lambda e: e.tensor_tensor(out=h2[:], in0=h2[:], in1=bcs[:, 1, :], op=ALU.add), reads=[Th2, Tbcs[1]], writes=[Th2])
                yield
                ptb1 = PS[1][:].bitcast(BF16)
                for k in range(8):
                    op("pe", lambda e, k=k: e.transpose(ptb1[:, k * 128:(k + 1) * 128], xn2[:, k * 128:(k + 1) * 128], ident[:]),
                       reads=[Txn2, Tident], writes=[TPS[1]])
                for k in range(8):
                    op("act", lambda e, k=k: e.activation(out=h2T[:, k, :], in_=ptb1[:, k * 128:(k + 1) * 128], func=AF.Identity,
                                                          scale=G2[:, k:k + 1], bias=modT[:, 24 + k:25 + k]),
                       reads=[TPS[1], TG2, TmodT], writes=[Th2T])
                yield
                for g4 in range(4):
                    pi = g4 % 2
                    for q4 in range(4):
                        hp = g4 * 4 + q4
                        for k in range(8):
                            op("pe", lambda e, k=k, hp=hp, q4=q4, pi=pi: e.matmul(
                                PS[pi][:, q4 * 128:(q4 + 1) * 128], lhsT=WqB[:, k, hp * 128:(hp + 1) * 128], rhs=h2T[:, k, :],
                                start=(k == 0), stop=(k == 7)), reads=[TWqB, Th2T], writes=[TPS[pi]])
                    op("act", lambda e, g4=g4, pi=pi: e.activation(out=qT[:, g4 * 4:(g4 + 1) * 4, :].rearrange("p a t -> p (a t)"),
                                                                   in_=PS[pi][:, :], func=AF.Copy), reads=[TPS[pi]], writes=[TqT])
                yield
                for g4 in range(4):
                    for q4 in range(4):
                        hp = g4 * 4 + q4
                        op("pe", lambda e, hp=hp, q4=q4, g4=g4: e.matmul(PS[2 + g4][:, q4 * 128:(q4 + 1) * 128], lhsT=qT[:, hp, :],
                                                                         rhs=SKb[:, hp, :], start=True, stop=True),
                           reads=[TqT, TSKb], writes=[TPS[2 + g4]])
                    op("act", lambda e, g4=g4: e.activation(out=sc[:, g4 * 4:(g4 + 1) * 4, :].rearrange("p a t -> p (a t)"),
                                                            in_=PS[2 + g4][:, :], func=AF.Copy), reads=[TPS[2 + g4]], writes=[Tsc])
                yield
                for hp in range(16):
                    op("dve", lambda e, hp=hp: e.max(out=v16[:, hp, 0:8], in_=sc[:, hp, :]), reads=[Tsc], writes=[Tv16])
                    op("dve", lambda e, hp=hp: e.max_index(out=i16[:, hp, 0:8], in_max=v16[:, hp, 0:8], in_values=sc[:, hp, :]),
                       reads=[Tsc, Tv16], writes=[Ti16])
                    op("dve", lambda e, hp=hp: e.match_replace(out=scr[:, 0:128], in_to_replace=v16[:, hp, 0:8], in_values=sc[:, hp, :],
                                                               imm_value=-1e30), reads=[Tsc, Tv16], writes=[Tscr])
                    op("dve", lambda e, hp=hp: e.max(out=v16[:, hp, 8:16], in_=scr[:, 0:128]), reads=[Tscr], writes=[Tv16])
                    op("dve", lambda e, hp=hp: e.max_index(out=i16[:, hp, 8:16], in_max=v16[:, hp, 8:16], in_values=scr[:, 0:128]),
                       reads=[Tscr, Tv16], writes=[Ti16])
                    yield
                op("dve", lambda e: e.tensor_copy(out=i16f[:], in_=i16[:]), reads=[Ti16], writes=[Ti16f])
                v4 = v16[:].rearrange("p (h two) a -> p h two a", two=2)
                f4 = i16f[:].rearrange("p (h two) a -> p h two a", two=2)
                c4 = cand[:].rearrange("p h (a b) -> p h a b", b=16)
                x4 = cidx[:].rearrange("p h (a b) -> p h a b", b=16)
                op("dve", lambda e: e.tensor_tensor(out=c4, in0=v4[:, :, 0, :].unsqueeze(3).to_broadcast([128, 8, 16, 16]),
                                                    in1=v4[:, :, 1, :].unsqueeze(2).to_broadcast([128, 8, 16, 16]), op=ALU.add),
                   reads=[Tv16], writes=[Tcand])
                op("dve", lambda e: e.tensor_scalar(out=f4[:, :, 0, :], in0=f4[:, :, 0, :], scalar1=128.0, scalar2=None, op0=ALU.mult),
                   reads=[Ti16f], writes=[Ti16f])
                for h in range(8):
                    op("dve", lambda e, h=h: e.max(out=m16[:, h, 0:8], in_=cand[:, h, :]), reads=[Tcand], writes=[Tm16])
                    op("dve", lambda e, h=h: e.max_index(out=pos[:, h, 0:8], in_max=m16[:, h, 0:8], in_values=cand[:, h, :]),
                       reads=[Tcand, Tm16], writes=[Tpos])
                    op("dve", lambda e, h=h: e.match_replace(out=scr[:], in_to_replace=m16[:, h, 0:8], in_values=cand[:, h, :],
                                                             imm_value=-1e30), reads=[Tcand, Tm16], writes=[Tscr])
                    op("dve", lambda e, h=h: e.max(out=m16[:, h, 8:16], in_=scr[:]), reads=[Tscr], writes=[Tm16])
                    op("dve", lambda e, h=h: e.max_index(out=pos[:, h, 8:16], in_max=m16[:, h, 8:16], in_values=scr[:]),
                       reads=[Tscr, Tm16], writes=[Tpos])
                    yield
                op("dve", lambda e: e.tensor_single_scalar(out=pab[:, 0], in_=pos[:], scalar=4, op=ALU.logical_shift_right),
                   reads=[Tpos], writes=[Tpab])
                op("dve", lambda e: e.tensor_single_scalar(out=pab[:, 1], in_=pos[:], scalar=15, op=ALU.bitwise_and),
                   reads=[Tpos], writes=[Tpab])
                op("dve", lambda e: e.tensor_copy(out=pabf[:], in_=pab[:]), reads=[Tpab], writes=[Tpabf])
                yield
                oh4 = cand[:].rearrange("p h (k a) -> p h k a", a=16)
                pr4 = cidx[:].rearrange("p h (k a) -> p h k a", a=16)
                io4 = iota16[:].unsqueeze(1).unsqueeze(1).to_broadcast([128, 8, 16, 16])
                for half in range(2):
                    op("dve", lambda e, half=half: e.tensor_tensor(
                        out=oh4, in0=pabf[:, half].unsqueeze(3).to_broadcast([128, 8, 16, 16]), in1=io4, op=ALU.is_equal),
                       reads=[Tpabf, Tiota16, Tm16, Tpos], writes=[Tcand])
                    op("dve", lambda e, half=half: e.tensor_tensor(
                        out=pr4, in0=oh4, in1=f4[:, :, half, :].unsqueeze(2).to_broadcast([128, 8, 16, 16]), op=ALU.mult),
                       reads=[Tcand, Ti16f], writes=[Tcidx])
                    op("dve", lambda e, half=half: e.tensor_reduce(out=e12[:, half], in_=pr4, axis=AX.X, op=ALU.add),
                       reads=[Tcidx], writes=[Te12])
                    yield
                op("dve", lambda e: e.tensor_tensor(out=eidf[:].rearrange("p (h k) -> p h k", k=16), in0=e12[:, 0], in1=e12[:, 1], op=ALU.add),
                   reads=[Te12], writes=[Teidf])
                op("dve", lambda e: e.tensor_scalar(out=eidf[:], in0=eidf[:], scalar1=float(nexp - 1), scalar2=None, op0=ALU.min),
                   reads=[Teidf], writes=[Teidf])
                op("dve", lambda e: e.tensor_copy(out=eid[:], in_=eidf[:]), reads=[Teidf], writes=[Teid])
                yield
                op("dve", lambda e: e.tensor_tensor(out=gts[:], in0=m16[:], in1=m16[:, :, 0:1].to_broadcast([128, 8, 16]),
                                                    op=ALU.subtract), reads=[Tm16], writes=[Tgts])
                op("act", lambda e: e.activation(out=gts[:], in_=gts[:], func=AF.Exp), reads=[Tgts], writes=[Tgts])
                op("dve", lambda e: e.tensor_reduce(out=gsum[:], in_=gts[:], axis=AX.X, op=ALU.add), reads=[Tgts], writes=[Tgsum])
                op("dve", lambda e: e.reciprocal(out=gsum[:], in_=gsum[:]), reads=[Tgsum], writes=[Tgsum])
                op("dve", lambda e: e.tensor_tensor(out=gts[:], in0=gts[:], in1=gsum[:].unsqueeze(2).to_broadcast([128, 8, 16]),
                                                    op=ALU.mult), reads=[Tgts, Tgsum], writes=[Tgts])

            def block_part(tb, gen):
                b = tb % 2
                rows = slice(tb * 128, (tb + 1) * 128)
                x1, Tx1 = x1b[b], Tx1b[b]
                eid, Teid = eid2[b], Teid2[b]
                h2, Th2 = h2b[b], Th2b[b]
                gts, Tgts = gtsb[b], Tgtsb[b]
                gflat = gts[:].rearrange("p h k -> p (h k)")
                for grp in range(128 // GS):
                    ub = gctr[0] % NU; gctr[0] += 1
                    gs = slice(grp * GS, (grp + 1) * GS)
                    for s in range(GS):
                        slot = grp * GS + s
                        op("pool", lambda e, ub=ub, s=s, slot=slot: e.indirect_dma_start(
                            out=U[ub][:, s, :], out_offset=None, in_=edu_bf.rearrange("n t d -> n (t d)"),
                            in_offset=bass.IndirectOffsetOnAxis(ap=eid[:, slot:slot + 1], axis=0)),
                           reads=[Teid], writes=[TU[ub][s]], chan=ch_g[ub][s])
                    for s in range(GS):
                        slot = grp * GS + s
                        op("dve", lambda e, s=s, slot=slot: e.scalar_tensor_tensor(
                            out=junk3[:], in0=U[ub][:, s, 0:D], scalar=1.0, in1=h2[:],
                            op0=ALU.mult, op1=ALU.mult, accum_out=score[:, slot:slot + 1]),
                           reads=[TU[ub][s], Th2], writes=[Tjunk3, Tscore_g[grp]])
                    op("act", lambda e: e.activation(out=actv[:, gs], in_=score[:, gs], func=AF.Gelu),
                       reads=[Tscore_g[grp]], writes=[Tactv_g[grp]])
                    op("dve", lambda e: e.tensor_tensor(out=actv[:, gs], in0=actv[:, gs], in1=gflat[:, gs], op=ALU.mult),
                       reads=[Tactv_g[grp], Tgts], writes=[Tactv_g[grp]])
                    for s in range(GS):
                        slot = grp * GS + s
                        di = slot % 4
                        op("act", lambda e, slot=slot, di=di: e.activation(out=dg[di][:], in_=ident[:], func=AF.Copy,
                                                                           scale=actv[:, slot:slot + 1]),
                           reads=[Tident, Tactv_g[grp]], writes=[Tdg[di]])
                        for hf in range(2):
                            op("pe", lambda e, hf=hf, s=s, di=di, slot=slot: e.matmul(
                                PS[6 + hf][:, :], lhsT=dg[di][:], rhs=U[ub][:, s, D + hf * 512:D + (hf + 1) * 512],
                                start=(slot == 0), stop=(slot == 127)), reads=[Tdg[di], TU[ub][s]], writes=[TPS[6 + hf]])
                    if gen is not None:
                        for _ in range(3):
                            next(gen, None)
                if gen is not None:
                    for _ in gen:
                        pass
                for hf in range(2):
                    hs = slice(hf * 512, (hf + 1) * 512)
                    op("dve", lambda e, hf=hf, hs=hs: e.tensor_tensor(out=x2[:, hs], in0=PS[6 + hf][:, :], in1=bcs[:, 3, hs], op=ALU.mult),
                       reads=[TPS[6 + hf], Tbcs[3]], writes=[Tx2])
                op("dve", lambda e: e.tensor_tensor(out=x2[:], in0=x2[:], in1=x1[:], op=ALU.add), reads=[Tx2, Tx1], writes=[Tx2])
                op("act", lambda e: e.activation(out=junk4[:], in_=x2[:], func=AF.Square, accum_out=st4[:, 1:2]),
                   reads=[Tx2], writes=[Tjunk4, Tst4])
                op("act", lambda e: e.activation(out=st4[:, 1:2], in_=st4[:, 1:2], func=AF.Sqrt, scale=1.0 / D, bias=EPS),
                   reads=[Tst4], writes=[Tst4])
                op("dve", lambda e: e.reciprocal(out=st4[:, 1:2], in_=st4[:, 1:2]), reads=[Tst4], writes=[Tst4])
                op("dve", lambda e: e.scalar_tensor_tensor(out=ot[b][:], in0=x2[:], scalar=st4[:, 1:2], in1=bcs[:, 4, :],
                                                           op0=ALU.mult, op1=ALU.mult), reads=[Tx2, Tst4, Tbcs[4]], writes=[Tx2])
                op("sp", lambda e: e.dma_start(out=out_d[rows, :], in_=ot[b][:]), reads=[Tot[b]], chan=ch_st[b])

            for _ in front_part(0):
                pass
            for tb in range(NBQ):
                block_part(tb, front_part(tb + 1) if tb + 1 < NBQ else None)

        S_.barrier()
        print("build: ninst", S_.ninst, "nwaits", S_.nwaits, "sems", len(S_.sems))
    return nc


def make_masks(parity):
    s = np.arange(128)[:, None]
    t = np.arange(512)[None, :]
    out = np.zeros((2, 128, 16, 512), np.float32)
    kinds = ["n", "d", "d", "f"] if parity == 0 else ["d", "f", "n", "d"]
    for ms in range(4):
        for i in range(4):
            for v in range(2):
                if kinds[ms] == "n":
                    m = np.full((128, 512), NEG, np.float32)
                elif kinds[ms] == "f":
                    m = np.zeros((128, 512), np.float32)
                else:
                    kp = i * 128 + s
                    vis = (kp <= t) if v == 0 else (kp < t)
                    m = np.where(vis, 0.0, NEG).astype(np.float32)
                out[v, :, ms * 4 + i, :] = m
    return out.astype(ml_dtypes.bfloat16)


def make_in_maps(inp, S):
    NT = S // 512
    f32 = lambda a: np.ascontiguousarray(a, dtype=np.float32)
    colT = lambda v, n: f32(np.asarray(v).reshape(n, 128).T)
    shared = {
        "w_ada": f32(inp["w_ada"][0]),
        "badaT": colT(inp["b_ada"][0], 48),
        "bada_row": f32(inp["b_ada"][0][None, :]),
        "gmixT": colT(inp["g_norm_mix"][0], 8),
        "gffnT": colT(inp["g_norm_ffn"][0], 8),
        "gffn_row": f32(inp["g_norm_ffn"][0][None, :]),
        "gfin_row": f32(inp["g_final"][None, :]),
        "w_in": f32(inp["w_in"][0]),
        "bf_row": f32(inp["b_forget"][0][None, :]),
        "gfox_row": f32(inp["g_out_fox"][0][None, :]),
        "gsb_row": f32(inp["g_out_sb"][0][None, :]),
        "w_out": f32(inp["w_out"][0]),
        "w_query": f32(inp["w_query"][0]),
        "skT": f32(np.asarray(inp["sub_keys"][0]).reshape(16, 128, 128).transpose(2, 0, 1)),
        "e_down": f32(inp["expert_down"][0]),
        "e_up": f32(inp["expert_up"][0]),
    }
    masks = [make_masks(0), make_masks(1)]
    maps = []
    for core in range(8):
        b, r = core // 2, core % 2
        xb = np.asarray(inp["x"][b])
        tiles = tile_ids(r, NT)
        m = dict(shared)
        m["x_all"] = f32(xb)
        m["x_q"] = f32(np.concatenate([xb[t * 512:(t + 1) * 512] for t in tiles], axis=0))
        m["cT"] = colT(inp["c"][b], 8)
        m["masks"] = np.ascontiguousarray(masks[r][0])
        m["masks_sb"] = np.ascontiguousarray(masks[r][1])
        fl = np.zeros((8, 2), np.float32)
        fl[:, r] = 1.0
        m["flags"] = fl
        maps.append(m)
    return maps


def assemble(results, S):
    NT = S // 512
    out = np.zeros((4, S, D), np.float32)
    for core in range(8):
        b, r = core // 2, core % 2
        o = np.asarray(results[core]["out"])
        for lt, t in enumerate(tile_ids(r, NT)):
            out[b, t * 512:(t + 1) * 512] = o[lt * 512:(lt + 1) * 512]
    return out


_NC_CACHE = {}


def kernel(**inputs):
    S = 8192
    if S not in _NC_CACHE:
        _NC_CACHE[S] = build(S)
    nc = _NC_CACHE[S]
    maps = make_in_maps(inputs, S)
    res = run_bass_kernel_spmd(nc, maps, core_ids=list(range(8)))
    return assemble(res.results, S)
```

```python
import numpy as np
import ml_dtypes
from contextlib import ExitStack
import concourse.bass as bass
import concourse.mybir as mybir
from concourse.bass_utils import run_bass_kernel_spmd

F32 = mybir.dt.float32
BF16 = mybir.dt.bfloat16
I32 = mybir.dt.int32
U32 = mybir.dt.uint32
AF = mybir.ActivationFunctionType
ALU = mybir.AluOpType
AX = mybir.AxisListType

D = 1024
NH = 16
HD = 64
KA = 70
INC = 3080
NEXP = 16384
EPS = 1e-6
NEG = -30000.0


class Tk:
    __slots__ = ("name", "w", "r")

    def __init__(self, name=""):
        self.name = name
        self.w = None
        self.r = {}


class Chan:
    def __init__(self, S, name):
        self.sem = S.new_sem(name)
        self.key = name
        self.count = 0


class Sched:
    ENGS = ("pe", "act", "dve", "pool", "sp")
    ROT = 20000

    def __init__(self, nc, es):
        self.nc = nc
        self.es = es
        self.eobj = {"pe": nc.tensor, "act": nc.scalar, "dve": nc.vector, "pool": nc.gpsimd, "sp": nc.sync}
        self.sems = {}
        self.chans = []
        self.gen = {k: 0 for k in self.ENGS}
        self.ekey = {}
        self.count = {}
        self.known = {k: {} for k in self.ENGS}
        self.done_keys = []
        for k in self.ENGS:
            self._new_esem(k)
        self.nwaits = 0
        self.ninst = 0

    def _new_esem(self, k):
        key = "e_%s_%d" % (k, self.gen[k])
        self.gen[k] += 1
        self.new_sem(key)
        self.ekey[k] = key
        self.count[k] = 0

    def new_sem(self, name):
        s = self.es.enter_context(self.nc.semaphore(name))
        self.sems[name] = s
        return s

    def chan(self, name):
        c = Chan(self, "c_" + name)
        self.chans.append(c)
        return c

    def _wait(self, eng, ev):
        key, val, clock = ev
        kn = self.known[eng]
        if kn.get(key, 0) >= val:
            return
        self.eobj[eng].wait_ge(self.sems[key], val)
        self.nwaits += 1
        kn[key] = val
        if clock:
            for k2, v2 in clock.items():
                if kn.get(k2, 0) < v2:
                    kn[k2] = v2

    def op(self, eng, fn, reads=(), writes=(), chan=None):
        deps = []
        epref = "e_%s_" % eng
        for t in reads:
            if t.w is not None:
                deps.append(t.w)
        for t in writes:
            for ev in t.r.values():
                if ev[0].startswith(epref):
                    continue
                deps.append(ev)
            if t.w is not None:
                if t.w[0].startswith(epref):
                    continue
                deps.append(t.w)
        for ev in deps:
            self._wait(eng, ev)
        self.ninst += 1
        if chan is None:
            if self.count[eng] >= self.ROT:
                self.done_keys.append((self.ekey[eng], self.count[eng]))
                self._new_esem(eng)
            key = self.ekey[eng]
            self.count[eng] += 1
            val = self.count[eng]
            fn(self.eobj[eng]).then_inc(self.sems[key], 1)
            ev = (key, val, dict(self.known[eng]))
        else:
            chan.count += 16
            fn(self.eobj[eng]).then_inc(chan.sem, 16)
            ev = (chan.key, chan.count, dict(self.known[eng]))
        for t in writes:
            t.w = ev
            t.r = {}
        for t in reads:
            if t in writes:
                continue
            t.r[("e_" + eng) if chan is None else chan.key] = ev
        return ev

    def barrier(self):
        evs = []
        for k in self.ENGS:
            if self.count[k] > 0:
                evs.append((self.ekey[k], self.count[k], None))
        for key, cnt in self.done_keys:
            evs.append((key, cnt, None))
        for c in self.chans:
            if c.count > 0:
                evs.append((c.key, c.count, None))
        for eng in self.ENGS:
            for ev in evs:
                self._wait(eng, ev)


def tile_ids(parity, NT):
    out = []
    for m in range(NT // 4):
        out += [4 * m, 4 * m + 3] if parity == 0 else [4 * m + 1, 4 * m + 2]
    return out


def build(S, dbg=0, phases=(0, 1, 2, 3), nexp=NEXP):
    NT = S // 512
    NB = S // 128
    NTQ = NT // 2
    SQ = NTQ * 512
    NBQ = SQ // 128
    nc = bass.Bass("TRN2", target_bir_lowering=False)

    def din(name, shape, dt=F32):
        return nc.dram_tensor(name, list(shape), dt, kind="ExternalInput").ap()

    x_all = din("x_all", [S, D])
    x_q = din("x_q", [SQ, D])
    cT_d = din("cT", [128, 8])
    w_ada = din("w_ada", [D, 6 * D])
    badaT_d = din("badaT", [128, 48])
    bada_row = din("bada_row", [1, 6 * D])
    gmixT_d = din("gmixT", [128, 8])
    gffnT_d = din("gffnT", [128, 8])
    gffn_row = din("gffn_row", [1, D])
    gfin_row = din("gfin_row", [1, D])
    w_in = din("w_in", [D, INC])
    bf_row = din("bf_row", [1, 8])
    gfox_row = din("gfox_row", [1, HD])
    gsb_row = din("gsb_row", [1, HD])
    w_out = din("w_out", [D, D])
    w_query = din("w_query", [D, 2048])
    skT_d = din("skT", [128, 16, 128])
    e_down = din("e_down", [nexp, D])
    e_up = din("e_up", [nexp, D])
    masks_d = din("masks", [128, 16, 512], BF16)
    masks_sb_d = din("masks_sb", [128, 16, 512], BF16)
    flags_d = din("flags", [8, 2])
    out_d = nc.dram_tensor("out", [SQ, D], F32, kind="ExternalOutput").ap()

    okind = "ExternalOutput" if dbg else None

    def dscr(name, shape, dt):
        if dbg:
            return nc.dram_tensor(name, list(shape), dt, kind="ExternalOutput").ap()
        return nc.dram_tensor(name, list(shape), dt).ap()

    KT_d = dscr("KT_d", [NH, KA, S], BF16)
    QT_d = dscr("QT_d", [NH, KA, SQ], BF16)
    V_d = dscr("V_d", [NH, 128, NB, 65], BF16)
    mixed_d = dscr("mixed_d", [SQ, D], BF16)
    edu_bf = nc.dram_tensor("edu_bf", [nexp, 2, D], BF16).ap()
    if dbg:
        mod_dbg = dscr("mod_dbg", [128, 48], F32)

    with ExitStack() as es:
        S_ = Sched(nc, es)
        op = S_.op

        def sb(stack, name, shape, dt):
            return stack.enter_context(nc.sbuf_tensor("s_" + name, list(shape), dt))

        PS = [es.enter_context(nc.psum_tensor("ps%d" % i, [128, 512], F32)) for i in range(8)]
        TPS = [Tk("ps%d" % i) for i in range(8)]

        ident = sb(es, "ident", [128, 128], BF16); Tident = Tk()
        tri = sb(es, "tri", [128, 128], BF16); Ttri = Tk()
        ones = sb(es, "ones", [128, 128], BF16); Tones = Tk()
        identf = sb(es, "identf", [128, 128], F32); Tidentf = Tk()
        modT = sb(es, "modT", [128, 48], F32); TmodT = Tk()
        G1 = sb(es, "G1", [128, 8], F32); TG1 = Tk()
        G2 = sb(es, "G2", [128, 8], F32); TG2 = Tk()
        bcs = sb(es, "bcs", [128, 5, D], F32)
        Tbcs = [Tk() for _ in range(5)]
        gout = sb(es, "gout", [128, 2, HD], F32); Tgout = Tk()
        flags = sb(es, "flags_sb", [8, 2], F32); Tflags = Tk()

        ch_c = S_.chan("const")
        ch_ld = [S_.chan("ld%d" % i) for i in range(4)]
        ch_st = [S_.chan("st%d" % i) for i in range(4)]
        ch_kts = [S_.chan("kts%d" % i) for i in range(4)]
        ch_aug = [S_.chan("aug%d" % i) for i in range(3)]
        ch_hd = [[S_.chan("hd%d_%d" % (i, j)) for j in range(3)] for i in range(2)]
        ch_stg = [S_.chan("stg%d" % i) for i in range(2)]

        op("pool", lambda e: e.memset(ident[:], 1.0), writes=[Tident])
        op("pool", lambda e: e.affine_select(out=ident[:], in_=ident[:], pattern=[[-1, 128]],
                                             compare_op=ALU.is_equal, fill=0.0, base=0, channel_multiplier=1),
           reads=[Tident], writes=[Tident])
        op("pool", lambda e: e.memset(tri[:], 1.0), writes=[Ttri])
        op("pool", lambda e: e.affine_select(out=tri[:], in_=tri[:], pattern=[[-1, 128]],
                                             compare_op=ALU.is_ge, fill=0.0, base=0, channel_multiplier=1),
           reads=[Ttri], writes=[Ttri])
        op("pool", lambda e: e.memset(ones[:], 1.0), writes=[Tones])
        op("pool", lambda e: e.memset(identf[:], 1.0), writes=[Tidentf])
        op("pool", lambda e: e.affine_select(out=identf[:], in_=identf[:], pattern=[[-1, 128]],
                                             compare_op=ALU.is_equal, fill=0.0, base=0, channel_multiplier=1),
           reads=[Tidentf], writes=[Tidentf])
        op("sp", lambda e: e.dma_start(out=gout[:, 0, :], in_=gfox_row.partition_broadcast(128)), writes=[Tgout], chan=S_.chan("k1"))
        op("sp", lambda e: e.dma_start(out=gout[:, 1, :], in_=gsb_row.partition_broadcast(128)), writes=[Tgout], chan=S_.chan("k2"))
        op("sp", lambda e: e.dma_start(out=flags[:], in_=flags_d), writes=[Tflags], chan=S_.chan("k3"))

        with ExitStack() as p0:
            cT = sb(p0, "cT", [128, 8], F32); TcT = Tk()
            scT = sb(p0, "scT", [128, 8], F32); TscT = Tk()
            screp = sb(p0, "screp", [128, 8, 128], F32); Tscrep = Tk()
            wa = [sb(p0, "wa%d" % i, [128, 8, 512], F32) for i in range(2)]; Twa = [Tk(), Tk()]
            badaT = sb(p0, "badaT", [128, 48], F32); TbadaT = Tk()
            badabc = sb(p0, "badabc", [128, 4, D], F32); Tbadabc = Tk()
            gmixT = sb(p0, "gmixT", [128, 8], F32); TgmixT = Tk()
            gffnT = sb(p0, "gffnT", [128, 8], F32); TgffnT = Tk()
            gffnbc = sb(p0, "gffnbc", [128, D], F32); Tgffnbc = Tk()

            op("sp", lambda e: e.dma_start(out=cT[:], in_=cT_d), writes=[TcT], chan=S_.chan("k4"))
            op("sp", lambda e: e.dma_start(out=badaT[:], in_=badaT_d), writes=[TbadaT], chan=S_.chan("k5"))
            op("sp", lambda e: e.dma_start(out=gmixT[:], in_=gmixT_d), writes=[TgmixT], chan=S_.chan("k6"))
            op("sp", lambda e: e.dma_start(out=gffnT[:], in_=gffnT_d), writes=[TgffnT], chan=S_.chan("k7"))
            op("sp", lambda e: e.dma_start(out=gffnbc[:], in_=gffn_row.partition_broadcast(128)), writes=[Tgffnbc], chan=S_.chan("k8"))
            op("sp", lambda e: e.dma_start(out=bcs[:, 4, :], in_=gfin_row.partition_broadcast(128)), writes=[Tbcs[4]], chan=S_.chan("k9"))
            op("sp", lambda e: e.dma_start(out=badabc[:].rearrange("p a d -> p (a d)"),
                                           in_=bada_row[:, 2 * D:6 * D].partition_broadcast(128)), writes=[Tbadabc], chan=S_.chan("k10"))
            op("act", lambda e: e.activation(out=scT[:], in_=cT[:], func=AF.Silu), reads=[TcT], writes=[TscT])
            op("dve", lambda e: e.tensor_copy(out=screp[:], in_=scT[:].unsqueeze(2).to_broadcast([128, 8, 128])),
               reads=[TscT], writes=[Tscrep])
            w_ada_v = w_ada.rearrange("(k p) c -> p k c", p=128)
            modps = PS[0]
            for cc in range(12):
                b = cc % 2
                op("sp", lambda e, cc=cc, b=b: e.dma_start(out=wa[b][:], in_=w_ada_v[:, :, cc * 512:(cc + 1) * 512]),
                   writes=[Twa[b]], chan=ch_ld[b])
                for f4 in range(4):
                    fc = cc * 4 + f4
                    for k in range(8):
                        op("pe", lambda e, b=b, k=k, f4=f4, fc=fc: e.matmul(
                            modps[:, fc:fc + 1], lhsT=wa[b][:, k, f4 * 128:(f4 + 1) * 128], rhs=scT[:, k:k + 1],
                            start=(k == 0), stop=(k == 7)), reads=[Twa[b], TscT], writes=[TPS[0]])
                if cc >= 4:
                    a = (cc - 4) // 2
                    hh = (cc - 4) % 2
                    pb = PS[1 + (cc % 2)]
                    Tpb = TPS[1 + (cc % 2)]
                    for k in range(8):
                        op("pe", lambda e, b=b, k=k, pb=pb: e.matmul(pb[:, :], lhsT=screp[:, k, :], rhs=wa[b][:, k, :],
                                                                   start=(k == 0), stop=(k == 7)),
                           reads=[Twa[b], Tscrep], writes=[Tpb])
                    op("dve", lambda e, a=a, hh=hh, pb=pb: e.tensor_tensor(
                        out=bcs[:, a, hh * 512:(hh + 1) * 512], in0=pb[:, :], in1=badabc[:, a, hh * 512:(hh + 1) * 512],
                        op=ALU.add), reads=[Tpb, Tbadabc], writes=[Tbcs[a]])
            op("dve", lambda e: e.tensor_tensor(out=modT[:], in0=modps[:, 0:48], in1=badaT[:], op=ALU.add),
               reads=[TPS[0], TbadaT], writes=[TmodT])
            op("dve", lambda e: e.scalar_tensor_tensor(out=G1[:], in0=modT[:, 8:16], scalar=1.0, in1=gmixT[:],
                                                       op0=ALU.add, op1=ALU.mult), reads=[TmodT, TgmixT], writes=[TG1])
            op("dve", lambda e: e.scalar_tensor_tensor(out=G2[:], in0=modT[:, 32:40], scalar=1.0, in1=gffnT[:],
                                                       op0=ALU.add, op1=ALU.mult), reads=[TmodT, TgffnT], writes=[TG2])
            op("dve", lambda e: e.scalar_tensor_tensor(out=bcs[:, 2, :], in0=bcs[:, 2, :], scalar=1.0, in1=gffnbc[:],
                                                       op0=ALU.add, op1=ALU.mult), reads=[Tbcs[2], Tgffnbc], writes=[Tbcs[2]])
            if dbg:
                op("sp", lambda e: e.dma_start(out=mod_dbg, in_=modT[:]), reads=[TmodT], chan=ch_st[0])
            S_.barrier()

        def norm_chunk(xsrc_ap, xt, Txt, ss, Tss, rstd, Trstd, xn, Txn, junk, Tjunk, hT, ThT, Gc, TGc, Bc_ap, TBc, ldchan,
                       psA, psB):
            op("sp", lambda e: e.dma_start(out=xt[:], in_=xsrc_ap.rearrange("(j p) d -> p j d", p=128)),
               writes=[Txt], chan=ldchan)
            for j in range(4):
                op("act", lambda e, j=j: e.activation(out=junk[:], in_=xt[:, j, :], func=AF.Square,
                                                      accum_out=ss[:, j:j + 1]), reads=[Txt], writes=[Tjunk, Tss])
            op("act", lambda e: e.activation(out=rstd[:], in_=ss[:], func=AF.Sqrt, scale=1.0 / D, bias=EPS),
               reads=[Tss], writes=[Trstd])
            op("dve", lambda e: e.reciprocal(out=rstd[:], in_=rstd[:]), reads=[Trstd], writes=[Trstd])
            for j in range(4):
                op("dve", lambda e, j=j: e.tensor_scalar(out=xn[:, j, :], in0=xt[:, j, :], scalar1=rstd[:, j:j + 1],
                                                         scalar2=None, op0=ALU.mult), reads=[Txt, Trstd], writes=[Txn])
            for k in range(8):
                pi = psA if k % 2 == 0 else psB
                pt = PS[pi][:].bitcast(BF16)
                for j in range(4):
                    op("pe", lambda e, j=j, k=k, pt=pt: e.transpose(pt[:, j * 128:(j + 1) * 128], xn[:, j, k * 128:(k + 1) * 128],
                                                                   ident[:]), reads=[Txn, Tident], writes=[TPS[pi]])
                op("act", lambda e, k=k, pt=pt: e.activation(out=hT[:, k, :], in_=pt[:, 0:512], func=AF.Identity,
                                                             scale=Gc[:, k:k + 1], bias=Bc_ap[:, k:k + 1]),
                   reads=[TPS[pi], TGc, TBc], writes=[ThT])

        with ExitStack() as p1:
            Wb = sb(p1, "Wb", [128, 8, INC], BF16); TWb = Tk()
            xt = [sb(p1, "xt%d" % i, [128, 4, D], F32) for i in range(2)]; Txt = [Tk(), Tk()]
            xn = sb(p1, "xn", [128, 4, D], BF16); Txn = Tk()
            junk = sb(p1, "junk", [128, D], BF16); Tjunk = Tk()
            ss = [sb(p1, "ss%d" % i, [128, 4], F32) for i in range(2)]; Tss = [Tk(), Tk()]
            rstd = [sb(p1, "rstd%d" % i, [128, 4], F32) for i in range(2)]; Trstd = [Tk(), Tk()]
            hT = sb(p1, "hT", [128, 8, 512], BF16); ThT = Tk()
            KTs = [sb(p1, "KTs%d" % i, [128, 512], BF16) for i in range(4)]; TKTs = [Tk() for _ in range(4)]
            Vs = [sb(p1, "Vs%d" % i, [128, 4, NH, 65], BF16) for i in range(2)]; TVs = [Tk(), Tk()]
            tribig = sb(p1, "tribig", [128, 4, 512], F32); Ttribig = Tk()
            bfbc = sb(p1, "bfbc", [128, 8], F32); Tbfbc = Tk()
            ub = sb(p1, "ub", [128, 4, 8], F32); Tub = Tk()
            lf = sb(p1, "lf", [128, 4, 8], F32); Tlf = Tk()
            GT = [sb(p1, "GT%d" % i, [8, 512], F32) for i in range(2)]; TGT = [Tk(), Tk()]
            gz = sb(p1, "gz", [8, 1], F32); Tgz = Tk()
            sp_hi = sb(p1, "sp_hi", [8, 3, 512], BF16); Tsp_hi = Tk()
            r1 = sb(p1, "r1", [8, 512], F32); Tr1 = Tk()
            r2 = sb(p1, "r2", [8, 512], F32); Tr2 = Tk()
            gq = sb(p1, "gq", [8, 512], F32); Tgq = Tk()
            spq = sb(p1, "spq", [8, 3, 512], BF16); Tspq = Tk()
            cst = sb(p1, "cst", [8, 3, 512], BF16); Tcst = Tk()

            w_in_v = w_in.rearrange("(k p) c -> p k c", p=128)
            wst = [xt[i][:].rearrange("p j d -> p (j d)")[:, 0:3520].rearrange("p (k c) -> p k c", k=8) for i in range(2)]
            Twst = Txt
            for i in range(7):
                b = i % 2
                op("sp", lambda e, i=i, b=b: e.dma_start(out=wst[b], in_=w_in_v[:, :, i * 440:(i + 1) * 440]),
                   writes=[Twst[b]], chan=ch_ld[b])
                eng = "dve" if i % 2 == 0 else "pool"
                op(eng, lambda e, i=i, b=b: e.tensor_copy(out=Wb[:, :, i * 440:(i + 1) * 440], in_=wst[b]),
                   reads=[Twst[b]], writes=[TWb])
            op("pool", lambda e: e.memset(tribig[:], 1.0), writes=[Ttribig])
            for j in range(4):
                op("pool", lambda e, j=j: e.affine_select(out=tribig[:, j, :], in_=tribig[:, j, :], pattern=[[1, 512]],
                                                          compare_op=ALU.is_ge, fill=0.0, base=-j * 128, channel_multiplier=-1),
                   reads=[Ttribig], writes=[Ttribig])
            op("sp", lambda e: e.dma_start(out=bfbc[:], in_=bf_row.partition_broadcast(128)), writes=[Tbfbc], chan=S_.chan("k11"))
            op("pool", lambda e: e.memset(gz[:], 0.0), writes=[Tgz])
            for i in range(2):
                op("pool", lambda e, i=i: e.memset(Vs[i][:], 1.0), writes=[TVs[i]])
            op("pool", lambda e: e.memset(cst[:], -1.0), writes=[Tcst])
            for c in range(NT):
                op("pool", lambda e, c=c: e.dma_start(
                    out=KT_d[0:8, 67:70, c * 512:(c + 1) * 512], in_=cst[:]),
                   reads=[Tcst], chan=ch_aug[0])
            op("pool", lambda e: e.memset(cst[:], 1.0), reads=[], writes=[Tcst])
            for c in range(NTQ):
                op("pool", lambda e, c=c: e.dma_start(
                    out=QT_d[0:8, 64:67, c * 512:(c + 1) * 512], in_=cst[:]),
                   reads=[Tcst], chan=ch_aug[0])

            o1 = 1536
            o2 = 1544
            kcols = [512 + 128 * i for i in range(4)] + [o2 + 512 + 128 * i for i in range(4)]
            qcols = [0 + 128 * i for i in range(4)] + [o2 + 128 * i for i in range(4)]
            vcols = [1024, o2 + 1024]

            def proj_T(cols_list, dst_d, c, scale, rr):
                for hp in range(8):
                    pi = 2 + (rr[0] % 2); rr[0] += 1
                    for k in range(8):
                        op("pe", lambda e, hp=hp, k=k, pi=pi: e.matmul(
                            PS[pi][:, :], lhsT=Wb[:, k, cols_list[hp]:cols_list[hp] + 128], rhs=hT[:, k, :],
                            start=(k == 0), stop=(k == 7)), reads=[TWb, ThT], writes=[TPS[pi]])
                    kb = rr[1] % 4; rr[1] += 1
                    op("act", lambda e, pi=pi, kb=kb: e.activation(out=KTs[kb][:], in_=PS[pi][:, :], func=AF.Copy, scale=scale),
                       reads=[TPS[pi]], writes=[TKTs[kb]])
                    for i2 in range(2):
                        op("pool", lambda e, hp=hp, kb=kb, i2=i2: e.dma_start(
                            out=dst_d[2 * hp + i2, 0:64, c * 512:(c + 1) * 512],
                            in_=KTs[kb][i2 * 64:(i2 + 1) * 64, :]), reads=[TKTs[kb]], chan=ch_kts[kb])

            rr = [0, 0]
            for c in range(NT):
                b = c % 2
                norm_chunk(x_all[c * 512:(c + 1) * 512, :], xt[b], Txt[b], ss[b], Tss[b], rstd[b], Trstd[b], xn, Txn,
                           junk, Tjunk, hT, ThT, G1, TG1, modT[:, 0:8], TmodT, ch_ld[b], 0, 1)
                proj_T(kcols, KT_d, c, 1.0, rr)
                vb = c % 2
                for j in range(4):
                    for g in range(2):
                        pi = 4 + ((j * 2 + g) % 2)
                        for k in range(8):
                            op("pe", lambda e, j=j, g=g, k=k, pi=pi: e.matmul(
                                PS[pi][:, :], lhsT=hT[:, k, j * 128:(j + 1) * 128], rhs=Wb[:, k, vcols[g]:vcols[g] + 512],
                                start=(k == 0), stop=(k == 7)), reads=[TWb, ThT], writes=[TPS[pi]])
                        op("dve", lambda e, j=j, g=g, pi=pi, vb=vb: e.tensor_copy(
                            out=Vs[vb][:, j, g * 8:(g + 1) * 8, 0:64], in_=PS[pi][:, :].rearrange("p (h d) -> p h d", d=64)),
                           reads=[TPS[pi]], writes=[TVs[vb]])
                for j in range(4):
                    op("pool", lambda e, j=j, vb=vb, c=c: e.dma_start(
                        out=V_d[:, :, 4 * c + j, :].rearrange("h p e -> p h e"), in_=Vs[vb][:, j, :, :]),
                       reads=[TVs[vb]], chan=ch_st[2 + vb])
                for j in range(4):
                    for k in range(8):
                        op("pe", lambda e, j=j, k=k: e.matmul(
                            PS[6][:, j * 8:(j + 1) * 8], lhsT=hT[:, k, j * 128:(j + 1) * 128], rhs=Wb[:, k, o1:o1 + 8],
                            start=(k == 0), stop=(k == 7)), reads=[TWb, ThT], writes=[TPS[6]])
                op("dve", lambda e: e.tensor_tensor(out=ub[:], in0=PS[6][:, 0:32].rearrange("p (j h) -> p j h", h=8),
                                                    in1=bfbc[:].unsqueeze(1).to_broadcast([128, 4, 8]), op=ALU.add),
                   reads=[TPS[6], Tbfbc], writes=[Tub])
                op("act", lambda e: e.activation(out=ub[:], in_=ub[:], func=AF.Exp, scale=-1.0), reads=[Tub], writes=[Tub])
                op("act", lambda e: e.activation(out=lf[:], in_=ub[:], func=AF.Ln, bias=1.0), reads=[Tub], writes=[Tlf])
                for j in range(4):
                    op("pe", lambda e, j=j: e.matmul(PS[7][0:8, :], lhsT=lf[:, j, :], rhs=tribig[:, j, :],
                                                     start=(j == 0), stop=(j == 3)), reads=[Tlf, Ttribig], writes=[TPS[7]])
                gcur, Tgcur = GT[c % 2], TGT[c % 2]
                if c == 0:
                    carry_ap, Tcarry = gz[:, 0:1], Tgz
                else:
                    carry_ap, Tcarry = GT[(c - 1) % 2][:, 511:512], TGT[(c - 1) % 2]
                op("dve", lambda e, gcur=gcur, carry_ap=carry_ap: e.tensor_scalar(
                    out=gcur[:], in0=PS[7][0:8, :], scalar1=carry_ap, scalar2=None, op0=ALU.add),
                   reads=[TPS[7], Tcarry], writes=[Tgcur])

                def split3(src, Tsrc, dst, Tdst):
                    op("dve", lambda e: e.tensor_copy(out=dst[:, 0, :], in_=src[:]), reads=[Tsrc], writes=[Tdst])
                    op("dve", lambda e: e.tensor_tensor(out=r1[:], in0=src[:], in1=dst[:, 0, :], op=ALU.subtract),
                       reads=[Tsrc, Tdst], writes=[Tr1])
                    op("dve", lambda e: e.tensor_copy(out=dst[:, 1, :], in_=r1[:]), reads=[Tr1], writes=[Tdst])
                    op("dve", lambda e: e.tensor_tensor(out=r2[:], in0=r1[:], in1=dst[:, 1, :], op=ALU.subtract),
                       reads=[Tr1, Tdst], writes=[Tr2])
                    op("dve", lambda e: e.tensor_copy(out=dst[:, 2, :], in_=r2[:]), reads=[Tr2], writes=[Tdst])

                split3(gcur, Tgcur, sp_hi, Tsp_hi)
                op("pool", lambda e, c=c: e.dma_start(out=KT_d[0:8, 64:67, c * 512:(c + 1) * 512], in_=sp_hi[:]),
                   reads=[Tsp_hi], chan=ch_aug[1])
                m, ph = c // 4, c % 4
                if ph in (0, 2):
                    fl = flags[:, 0:1] if ph == 0 else flags[:, 1:2]
                    op("dve", lambda e, fl=fl, gcur=gcur: e.tensor_scalar(out=gq[:], in0=gcur[:], scalar1=fl, scalar2=None,
                                                                          op0=ALU.mult), reads=[Tgcur, Tflags], writes=[Tgq])
                else:
                    fl = flags[:, 1:2] if ph == 1 else flags[:, 0:1]
                    op("dve", lambda e, fl=fl, gcur=gcur: e.scalar_tensor_tensor(out=gq[:], in0=gcur[:], scalar=fl, in1=gq[:],
                                                                                 op0=ALU.mult, op1=ALU.add),
                       reads=[Tgcur, Tflags, Tgq], writes=[Tgq])
                    split3(gq, Tgq, spq, Tspq)
                    lt = 2 * m + (0 if ph == 1 else 1)
                    op("pool", lambda e, lt=lt: e.dma_start(out=QT_d[0:8, 67:70, lt * 512:(lt + 1) * 512], in_=spq[:]),
                       reads=[Tspq], chan=ch_aug[2])

            for c in range(NTQ):
                b = c % 2
                norm_chunk(x_q[c * 512:(c + 1) * 512, :], xt[b], Txt[b], ss[b], Tss[b], rstd[b], Trstd[b], xn, Txn,
                           junk, Tjunk, hT, ThT, G1, TG1, modT[:, 0:8], TmodT, ch_ld[b], 0, 1)
                proj_T(qcols, QT_d, c, 0.125, rr)
            S_.barrier()

        if 2 in phases:
          with ExitStack() as p2:
            maskF = sb(p2, "maskF", [128, 16, 512], BF16); TmaskF = Tk()
            maskS = sb(p2, "maskS", [128, 16, 512], BF16); TmaskS = Tk()
            KTh = [sb(p2, "KTh%d" % i, [KA, S], BF16) for i in range(2)]; TKTh = [Tk(), Tk()]
            Vh = [sb(p2, "Vh%d" % i, [128, NB, 65], BF16) for i in range(2)]; TVh = [Tk(), Tk()]
            QTh = [sb(p2, "QTh%d" % i, [KA, SQ], BF16) for i in range(2)]; TQTh = [Tk(), Tk()]
            negQ = [sb(p2, "negQ%d" % i, [64, 512], BF16) for i in range(2)]; TnegQ = [Tk(), Tk()]
            zt = [sb(p2, "zt%d" % i, [128, 512], F32) for i in range(2)]; Tzt = [Tk(), Tk()]
            ee = [sb(p2, "ee%d" % i, [128, 512], F32) for i in range(2)]; Tee = [Tk(), Tk()]
            spb = [sb(p2, "spb%d" % i, [128, 512], BF16) for i in range(2)]; Tspb = [Tk(), Tk()]
            LL = [sb(p2, "LL%d" % i, [128, 512], F32) for i in range(2)]; TLL = [Tk(), Tk()]
            PT = [sb(p2, "PT%d" % i, [128, 512], BF16) for i in range(6)]; TPT = [Tk() for _ in range(6)]
            Rsb = sb(p2, "Rsb", [128, 512], F32); TRsb = Tk()
            osb = sb(p2, "osb", [128, 4, 64], F32); Tosb = Tk()
            sq = sb(p2, "sq", [128, 4, 64], F32); Tsq = Tk()
            o2 = sb(p2, "o2", [128, 4, 64], F32); To2 = Tk()
            omix = [sb(p2, "omix%d" % i, [128, 4, 64], BF16) for i in range(2)]; Tomix = [Tk(), Tk()]
            ssq = sb(p2, "ssq", [128, 4], F32); Tssq = Tk()
            rinv = sb(p2, "rinv", [128, 4], F32); Trinv = Tk()
            OTs = sb(p2, "OTs", [65, 512], F32); TOTs = Tk()

            op("sp", lambda e: e.dma_start(out=maskF[:], in_=masks_d), writes=[TmaskF], chan=S_.chan("k12"))
            op("sp", lambda e: e.dma_start(out=maskS[:], in_=masks_sb_d), writes=[TmaskS], chan=S_.chan("k13"))


            stgf = [sb(p2, "pstgf%d" % i, [128, 4096], F32) for i in range(2)]; Tstgf = [Tk(), Tk()]
            stgb = [sb(p2, "pstgb%d" % i, [128, 4096], BF16) for i in range(2)]; Tstgb = [Tk(), Tk()]
            ch_si = [S_.chan("psi%d" % i) for i in range(2)]
            ch_so = [S_.chan("pso%d" % i) for i in range(2)]

            def precast_gen():
                RPP = nexp // 128
                cnt = 0
                for (tsrc, tdst) in ((e_down, edu_bf[:, 0, :]), (e_up, edu_bf[:, 1, :])):
                    sv = tsrc.rearrange("(r p) d -> p r d", p=128)
                    dv = tdst.rearrange("(r p) d -> p r d", p=128)
                    for c4 in range(max(1, RPP // 4)):
                        nr = min(4, RPP)
                        i = cnt % 2; cnt += 1
                        sf = stgf[i][:, 0:nr * D].rearrange("p (r d) -> p r d", d=D)
                        sbv = stgb[i][:, 0:nr * D].rearrange("p (r d) -> p r d", d=D)
                        op("sp", lambda e: e.dma_start(out=sf, in_=sv[:, c4 * 4:c4 * 4 + nr, :]), writes=[Tstgf[i]], chan=ch_si[i])
                        op("pool", lambda e: e.tensor_copy(out=sbv, in_=sf), reads=[Tstgf[i]], writes=[Tstgb[i]])
                        op("sp", lambda e: e.dma_start(out=dv[:, c4 * 4:c4 * 4 + nr, :], in_=sbv), reads=[Tstgb[i]], chan=ch_so[i])
                        yield
            pgen = precast_gen()

            def block_list(lt):
                kind, m = lt % 2, lt // 2
                if kind == 0:
                    masked = [(4 * m + 1, 0), (4 * m, 1)]
                    lower = list(range(4 * m - 1, -1, -1))
                else:
                    masked = [(4 * m + 3, 2), (4 * m + 2, 3)]
                    lower = list(range(4 * m + 1, -1, -1))
                bl = []
                for (T_, ms) in masked:
                    for i in (3, 2, 1, 0):
                        bl.append((4 * T_ + i, ms * 4 + i))
                for T_ in lower:
                    for i in (3, 2, 1, 0):
                        bl.append((4 * T_ + i, None))
                return bl

            bctr = [0]
            octr = [0]
            for h in range(NH):
                hb = h % 2
                is_fox = h < 8
                op("sp", lambda e, h=h, hb=hb: e.dma_start(out=KTh[hb][:], in_=KT_d[h]), writes=[TKTh[hb]], chan=ch_hd[hb][0])
                op("sp", lambda e, h=h, hb=hb: e.dma_start(out=Vh[hb][:], in_=V_d[h]), writes=[TVh[hb]], chan=ch_hd[hb][1])
                op("sp", lambda e, h=h, hb=hb: e.dma_start(out=QTh[hb][:], in_=QT_d[h]), writes=[TQTh[hb]], chan=ch_hd[hb][2])
                kt, vt, qt = KTh[hb], Vh[hb], QTh[hb]
                Tkt, Tvt, Tqt = TKTh[hb], TVh[hb], TQTh[hb]
                for lt in range(NTQ):
                    bl = block_list(lt)
                    n = len(bl)
                    ob = 6 + (octr[0] % 2); octr[0] += 1
                    Ops, TOps = PS[ob], TPS[ob]
                    Ov = Ops[:, 0:260].rearrange("p (j e) -> p j e", e=65)
                    qs = slice(lt * 512, (lt + 1) * 512)
                    base = bctr[0]; bctr[0] += n
                    if is_fox:
                        def A1(k):
                            kb, mi = bl[k]
                            b = (base + k) % 6
                            zb = (base + k) % 2
                            op("pe", lambda e: e.matmul(PS[b][:, :], lhsT=kt[0:KA, kb * 128:(kb + 1) * 128], rhs=qt[0:KA, qs],
                                                        start=True, stop=True), reads=[Tkt, Tqt], writes=[TPS[b]])
                            if mi is not None:
                                op("dve", lambda e: e.tensor_tensor(out=zt[zb][:], in0=PS[b][:, :], in1=maskF[:, mi, :], op=ALU.add),
                                   reads=[TPS[b], TmaskF], writes=[Tzt[zb]])
                                op("act", lambda e: e.activation(out=PT[b][:], in_=zt[zb][:], func=AF.Exp), reads=[Tzt[zb]], writes=[TPT[b]])
                            else:
                                op("act", lambda e: e.activation(out=PT[b][:], in_=PS[b][:, :], func=AF.Exp), reads=[TPS[b]], writes=[TPT[b]])

                        def B(k):
                            kb, mi = bl[k]
                            b = (base + k) % 6
                            op("pe", lambda e: e.matmul(Ops[0:65, :], lhsT=vt[:, kb, :], rhs=PT[b][:, :],
                                                        start=(k == 0), stop=(k == n - 1)), reads=[TPT[b], Tvt], writes=[TOps])
                        for k in range(n + 5):
                            if k < n:
                                A1(k)
                            if k >= 5:
                                B(k - 5)
                    else:
                        nq = negQ[octr[0] % 2]; Tnq = TnegQ[octr[0] % 2]
                        op("pool", lambda e: e.tensor_scalar(out=nq[:], in0=qt[0:64, qs], scalar1=-1.0, scalar2=None, op0=ALU.mult),
                           reads=[Tqt], writes=[Tnq])
                        op("pool", lambda e: e.memset(Rsb[:], 0.0), writes=[TRsb])

                        def A1(k):
                            kb, mi = bl[k]
                            b = (base + k) % 2
                            op("pe", lambda e: e.matmul(PS[b][:, :], lhsT=kt[0:64, kb * 128:(kb + 1) * 128], rhs=qt[0:64, qs],
                                                        start=True, stop=True), reads=[Tkt, Tqt], writes=[TPS[b]])
                            if mi is not None:
                                op("dve", lambda e: e.tensor_tensor(out=zt[b][:], in0=PS[b][:, :], in1=maskS[:, mi, :], op=ALU.add),
                                   reads=[TPS[b], TmaskS], writes=[Tzt[b]])
                                op("act", lambda e: e.activation(out=ee[b][:], in_=zt[b][:], func=AF.Exp), reads=[Tzt[b]], writes=[Tee[b]])
                            else:
                                op("act", lambda e: e.activation(out=ee[b][:], in_=PS[b][:, :], func=AF.Exp), reads=[TPS[b]], writes=[Tee[b]])
                            op("act", lambda e: e.activation(out=spb[b][:], in_=ee[b][:], func=AF.Ln, bias=1.0), reads=[Tee[b]], writes=[Tspb[b]])

                        def A2(k):
                            kb, mi = bl[k]
                            b = (base + k) % 2
                            op("pe", lambda e: e.matmul(PS[2 + b][:, :], lhsT=tri[:], rhs=spb[b][:], start=True, stop=False),
                               reads=[Ttri, Tspb[b]], writes=[TPS[2 + b]])
                            op("pe", lambda e: e.matmul(PS[2 + b][:, :], lhsT=kt[0:64, kb * 128:(kb + 1) * 128], rhs=nq[:],
                                                        start=False, stop=True), reads=[Tkt, Tnq], writes=[TPS[2 + b]])
                            if k < n - 1:
                                op("pe", lambda e: e.matmul(PS[4 + b][:, :], lhsT=ones[:], rhs=spb[b][:], start=True, stop=True),
                                   reads=[Tones, Tspb[b]], writes=[TPS[4 + b]])
                            op("dve", lambda e: e.tensor_tensor(out=LL[b][:], in0=PS[2 + b][:, :], in1=Rsb[:], op=ALU.add),
                               reads=[TPS[2 + b], TRsb], writes=[TLL[b]])
                            if mi is not None:
                                op("dve", lambda e: e.tensor_tensor(out=LL[b][:], in0=LL[b][:], in1=maskS[:, mi, :], op=ALU.subtract),
                                   reads=[TLL[b], TmaskS], writes=[TLL[b]])
                            if k < n - 1:
                                op("dve", lambda e: e.tensor_tensor(out=Rsb[:], in0=PS[4 + b][:, :], in1=Rsb[:], op=ALU.add),
                                   reads=[TPS[4 + b], TRsb], writes=[TRsb])

                        def B(k):
                            kb, mi = bl[k]
                            b = (base + k) % 2
                            op("act", lambda e: e.activation(out=PT[b][:], in_=LL[b][:], func=AF.Exp, scale=-1.0),
                               reads=[TLL[b]], writes=[TPT[b]])
                            for j in range(4):
                                op("pe", lambda e, j=j: e.matmul(Ov[:, j, 0:64], lhsT=PT[b][:, j * 128:(j + 1) * 128], rhs=vt[:, kb, 0:64],
                                                                 start=(k == 0), stop=(k == n - 1)), reads=[TPT[b], Tvt], writes=[TOps])
                        for k in range(n + 2):
                            if k < n:
                                A1(k)
                            if 1 <= k <= n:
                                A2(k - 1)
                            if k >= 2:
                                B(k - 2)
                    if is_fox:
                        op("act", lambda e: e.activation(out=OTs[:, :], in_=Ops[0:65, :], func=AF.Copy), reads=[TOps], writes=[TOTs])
                        Ov = PS[5][:, 0:260].rearrange("p (j e) -> p j e", e=65)
                        for j in range(4):
                            op("pe", lambda e, j=j: e.transpose(Ov[:, j, :], OTs[:, j * 128:(j + 1) * 128], identf[0:65, 0:65]),
                               reads=[TOTs, Tidentf], writes=[TPS[5]])
                        TOps = TPS[5]
                        op("dve", lambda e: e.reciprocal(out=rinv[:].unsqueeze(2), in_=Ov[:, :, 64:65]), reads=[TOps], writes=[Trinv])
                        op("dve", lambda e: e.tensor_tensor(out=osb[:], in0=Ov[:, :, 0:64],
                                                            in1=rinv[:].unsqueeze(2).to_broadcast([128, 4, 64]), op=ALU.mult),
                           reads=[TOps, Trinv], writes=[Tosb])
                    else:
                        op("dve", lambda e: e.tensor_copy(out=osb[:], in_=Ov[:, :, 0:64]), reads=[TOps], writes=[Tosb])
                    op("pool", lambda e: e.tensor_tensor(out=sq[:], in0=osb[:], in1=osb[:], op=ALU.mult), reads=[Tosb], writes=[Tsq])
                    op("dve", lambda e: e.tensor_reduce(out=ssq[:], in_=sq[:], axis=AX.X, op=ALU.add), reads=[Tsq], writes=[Tssq])
                    op("act", lambda e: e.activation(out=ssq[:], in_=ssq[:], func=AF.Sqrt, scale=1.0 / HD, bias=EPS),
                       reads=[Tssq], writes=[Tssq])
                    op("dve", lambda e: e.reciprocal(out=ssq[:], in_=ssq[:]), reads=[Tssq], writes=[Tssq])
                    op("pool", lambda e: e.tensor_tensor(out=o2[:], in0=osb[:], in1=ssq[:].unsqueeze(2).to_broadcast([128, 4, 64]),
                                                         op=ALU.mult), reads=[Tosb, Tssq], writes=[To2])
                    om = octr[0] % 2
                    gi = 0 if is_fox else 1
                    op("pool", lambda e, om=om, gi=gi: e.tensor_tensor(out=omix[om][:], in0=o2[:],
                                                                       in1=gout[:, gi, :].unsqueeze(1).to_broadcast([128, 4, 64]),
                                                                       op=ALU.mult), reads=[To2, Tgout], writes=[Tomix[om]])
                    op("pool", lambda e, om=om, h=h, lt=lt: e.dma_start(
                        out=mixed_d[lt * 512:(lt + 1) * 512, h * 64:(h + 1) * 64].rearrange("(j p) d -> p j d", p=128),
                        in_=omix[om][:]), reads=[Tomix[om]], chan=ch_st[om])
                    next(pgen, None)
            for _ in pgen:
                pass
            S_.barrier()

        if 3 in phases:
          with ExitStack() as p3:
            NU, GS = 3, 4
            WoB = sb(p3, "WoB", [128, 8, D], BF16); TWoB = Tk()
            WqB = sb(p3, "WqB", [128, 8, 2048], BF16); TWqB = Tk()
            SKb = sb(p3, "SKb", [128, 16, 128], BF16); TSKb = Tk()
            with ExitStack() as pc:
                stgf = [sb(pc, "stgf%d" % i, [128, 4096], F32) for i in range(3)]; Tstgf = [Tk() for _ in range(3)]
                stgb = [sb(pc, "stgb%d" % i, [128, 4096], BF16) for i in range(3)]; Tstgb = [Tk() for _ in range(3)]
                ch_si = [S_.chan("si%d" % i) for i in range(3)]
                ch_so = [S_.chan("so%d" % i) for i in range(3)]
                cengs = ["dve", "act", "pool"]
                sctr = [0]

                def stage_cast(src_ap, shape_str, kw, dst_ap, Tdst):
                    i = sctr[0] % 3; sctr[0] += 1
                    n = 1
                    for v in src_ap.shape[1:]:
                        n *= v
                    stg = stgf[i][:, 0:n].rearrange(shape_str, **kw)
                    op("sp", lambda e: e.dma_start(out=stg, in_=src_ap), writes=[Tstgf[i]], chan=ch_si[i])
                    if cengs[i] == "act":
                        op("act", lambda e: e.activation(out=dst_ap, in_=stg, func=AF.Copy), reads=[Tstgf[i]], writes=[Tdst])
                    else:
                        op(cengs[i], lambda e: e.tensor_copy(out=dst_ap, in_=stg), reads=[Tstgf[i]], writes=[Tdst])
                wo_v = w_out.rearrange("(k p) c -> p k c", p=128)
                wq_v = w_query.rearrange("(k p) c -> p k c", p=128)
                for i in range(2):
                    stage_cast(wo_v[:, :, i * 512:(i + 1) * 512], "p (k c) -> p k c", dict(k=8), WoB[:, :, i * 512:(i + 1) * 512], TWoB)
                for i in range(4):
                    stage_cast(wq_v[:, :, i * 512:(i + 1) * 512], "p (k c) -> p k c", dict(k=8), WqB[:, :, i * 512:(i + 1) * 512], TWqB)
                stage_cast(skT_d, "p (k c) -> p k c", dict(k=16), SKb[:], TSKb)
                S_.barrier()
            U = [sb(p3, "U%d" % i, [128, GS, 2 * D], BF16) for i in range(NU)]
            TU = [[Tk() for _ in range(GS)] for _ in range(NU)]
            ch_g = [[S_.chan("g%d_%d" % (i, s)) for s in range(GS)] for i in range(NU)]
            dg = [sb(p3, "dg%d" % i, [128, 128], BF16) for i in range(4)]; Tdg = [Tk() for _ in range(4)]
            mxt = [sb(p3, "mxt%d" % i, [128, D], BF16) for i in range(2)]; Tmxt = [Tk(), Tk()]
            xqt = [sb(p3, "xqt%d" % i, [128, D], F32) for i in range(2)]; Txqt = [Tk(), Tk()]
            mT = sb(p3, "mT", [128, 8, 128], BF16); TmT = Tk()
            x1b = [sb(p3, "x1_%d" % i, [128, D], F32) for i in range(2)]; Tx1b = [Tk(), Tk()]
            xn2 = sb(p3, "xn2", [128, D], BF16); Txn2 = Tk()
            h2b = [sb(p3, "h2_%d" % i, [128, D], F32) for i in range(2)]; Th2b = [Tk(), Tk()]
            h2T = sb(p3, "h2T", [128, 8, 128], BF16); Th2T = Tk()
            qT = sb(p3, "qT", [128, 16, 128], BF16); TqT = Tk()
            sc = sb(p3, "sc", [128, 16, 128], F32); Tsc = Tk()
            scr = sb(p3, "scr", [128, 256], F32); Tscr = Tk()
            v16 = sb(p3, "v16", [128, 16, 16], F32); Tv16 = Tk()
            i16 = sb(p3, "i16", [128, 16, 16], U32); Ti16 = Tk()
            i16f = sb(p3, "i16f", [128, 16, 16], F32); Ti16f = Tk()
            cand = sb(p3, "cand", [128, 8, 256], F32); Tcand = Tk()
            cidx = sb(p3, "cidx", [128, 8, 256], F32); Tcidx = Tk()
            m16 = sb(p3, "m16", [128, 8, 16], F32); Tm16 = Tk()
            pos = sb(p3, "pos", [128, 8, 16], U32); Tpos = Tk()
            pab = sb(p3, "pab", [128, 2, 8, 16], U32); Tpab = Tk()
            pabf = sb(p3, "pabf", [128, 2, 8, 16], F32); Tpabf = Tk()
            e12 = sb(p3, "e12", [128, 2, 8, 16], F32); Te12 = Tk()
            iota16 = sb(p3, "iota16", [128, 16], F32); Tiota16 = Tk()
            for a_ in range(16):
                op("pool", lambda e, a_=a_: e.memset(iota16[:, a_:a_ + 1], float(a_)), writes=[Tiota16])
            eidf = sb(p3, "eidf", [128, 128], F32); Teidf = Tk()
            eid2 = [sb(p3, "eid%d" % i, [128, 128], I32) for i in range(2)]; Teid2 = [Tk(), Tk()]
            gtsb = [sb(p3, "gts%d" % i, [128, 8, 16], F32) for i in range(2)]; Tgtsb = [Tk(), Tk()]
            gsum = sb(p3, "gsum", [128, 8], F32); Tgsum = Tk()
            score = sb(p3, "score", [128, 128], F32); Tscore_g = [Tk() for _ in range(32)]
            actv = sb(p3, "actv", [128, 128], F32); Tactv_g = [Tk() for _ in range(32)]
            junk3 = sb(p3, "junk3", [128, D], BF16); Tjunk3 = Tk()
            junk4 = sb(p3, "junk4", [128, D], BF16); Tjunk4 = Tk()
            x2 = sb(p3, "x2", [128, D], F32); Tx2 = Tk()
            ot = [x2, x2]; Tot = [Tx2, Tx2]
            st3 = sb(p3, "st3", [128, 4], F32); Tst3 = Tk()
            st4 = sb(p3, "st4", [128, 4], F32); Tst4 = Tk()

            gctr = [0]
            def front_part(tb):
                b = tb % 2
                rows = slice(tb * 128, (tb + 1) * 128)
                x1, Tx1 = x1b[b], Tx1b[b]
                eid, Teid = eid2[b], Teid2[b]
                h2, Th2 = h2b[b], Th2b[b]
                gts, Tgts = gtsb[b], Tgtsb[b]
                op("sp", lambda e: e.dma_start(out=mxt[b][:], in_=mixed_d[rows, :]), writes=[Tmxt[b]], chan=ch_ld[b])
                op("sp", lambda e: e.dma_start(out=xqt[b][:], in_=x_q[rows, :]), writes=[Txqt[b]], chan=ch_ld[2 + b])
                ptb = PS[0][:].bitcast(BF16)
                for k in range(8):
                    op("pe", lambda e, k=k: e.transpose(ptb[:, k * 128:(k + 1) * 128], mxt[b][:, k * 128:(k + 1) * 128], ident[:]),
                       reads=[Tmxt[b], Tident], writes=[TPS[0]])
                op("act", lambda e: e.activation(out=mT[:].rearrange("p k t -> p (k t)"), in_=ptb[:, 0:1024], func=AF.Copy),
                   reads=[TPS[0]], writes=[TmT])
                yield
                for hf in range(2):
                    for k in range(8):
                        op("pe", lambda e, k=k, hf=hf: e.matmul(PS[2 + hf][:, :], lhsT=mT[:, k, :], rhs=WoB[:, k, hf * 512:(hf + 1) * 512],
                                                                start=(k == 0), stop=(k == 7)), reads=[TmT, TWoB], writes=[TPS[2 + hf]])
                    hs = slice(hf * 512, (hf + 1) * 512)
                    op("dve", lambda e, hf=hf, hs=hs: e.tensor_tensor(out=x1[:, hs], in0=PS[2 + hf][:, :], in1=bcs[:, 0, hs], op=ALU.mult),
                       reads=[TPS[2 + hf], Tbcs[0]], writes=[Tx1])
                op("dve", lambda e: e.tensor_tensor(out=x1[:], in0=x1[:], in1=xqt[b][:], op=ALU.add), reads=[Tx1, Txqt[b]], writes=[Tx1])
                yield
                op("act", lambda e: e.activation(out=junk4[:], in_=x1[:], func=AF.Square, accum_out=st3[:, 0:1]),
                   reads=[Tx1], writes=[Tjunk4, Tst3])
                op("act", lambda e: e.activation(out=st3[:, 0:1], in_=st3[:, 0:1], func=AF.Sqrt, scale=1.0 / D, bias=EPS),
                   reads=[Tst3], writes=[Tst3])
                op("dve", lambda e: e.reciprocal(out=st3[:, 0:1], in_=st3[:, 0:1]), reads=[Tst3], writes=[Tst3])
                op("dve", lambda e: e.tensor_scalar(out=xn2[:], in0=x1[:], scalar1=st3[:, 0:1], scalar2=None, op0=ALU.mult),
                   reads=[Tx1, Tst3], writes=[Txn2])
                op("dve", lambda e: e.scalar_tensor_tensor(out=h2[:], in0=x1[:], scalar=st3[:, 0:1], in1=bcs[:, 2, :],
                                                           op0=ALU.mult, op1=ALU.mult), reads=[Tx1, Tst3, Tbcs[2]], writes=[Th2])
                op("dve", lambda e: e.tensor_tensor(out=h2[:], in0=h2[:], in1=bcs[:, 1, :], op=ALU.add), reads=[Th2, Tbcs[1]], writes=[Th2])
                yield
                ptb1 = PS[1][:].bitcast(BF16)
                for k in range(8):
                    op("pe", lambda e, k=k: e.transpose(ptb1[:, k * 128:(k + 1) * 128], xn2[:, k * 128:(k + 1) * 128], ident[:]),
                       reads=[Txn2, Tident], writes=[TPS[1]])
                for k in range(8):
                    op("act", lambda e, k=k: e.activation(out=h2T[:, k, :], in_=ptb1[:, k * 128:(k + 1) * 128], func=AF.Identity,
                                                          scale=G2[:, k:k + 1], bias=modT[:, 24 + k:25 + k]),
                       reads=[TPS[1], TG2, TmodT], writes=[Th2T])
                yield
                for g4 in range(4):
                    pi = g4 % 2
                    for q4 in range(4):
                        hp = g4 * 4 + q4
                        for k in range(8):
                            op("pe", lambda e, k=k, hp=hp, q4=q4, pi=pi: e.matmul(
                                PS[pi][:, q4 * 128:(q4 + 1) * 128], lhsT=WqB[:, k, hp * 128:(hp + 1) * 128], rhs=h2T[:, k, :],
                                start=(k == 0), stop=(k == 7)), reads=[TWqB, Th2T], writes=[TPS[pi]])
                    op("act", lambda e, g4=g4, pi=pi: e.activation(out=qT[:, g4 * 4:(g4 + 1) * 4, :].rearrange("p a t -> p (a t)"),
                                                                   in_=PS[pi][:, :], func=AF.Copy), reads=[TPS[pi]], writes=[TqT])
                yield
                for g4 in range(4):
                    for q4 in range(4):
                        hp = g4 * 4 + q4
                        op("pe", lambda e, hp=hp, q4=q4, g4=g4: e.matmul(PS[2 + g4][:, q4 * 128:(q4 + 1) * 128], lhsT=qT[:, hp, :],
                                                                         rhs=SKb[:, hp, :], start=True, stop=True),
                           reads=[TqT, TSKb], writes=[TPS[2 + g4]])
                    op("act", lambda e, g4=g4: e.activation(out=sc[:, g4 * 4:(g4 + 1) * 4, :].rearrange("p a t -> p (a t)"),
                                                            in_=PS[2 + g4][:, :], func=AF.Copy), reads=[TPS[2 + g4]], writes=[Tsc])
                yield
                for hp in range(16):
                    op("dve", lambda e, hp=hp: e.max(out=v16[:, hp, 0:8], in_=sc[:, hp, :]), reads=[Tsc], writes=[Tv16])
                    op("dve", lambda e, hp=hp: e.max_index(out=i16[:, hp, 0:8], in_max=v16[:, hp, 0:8], in_values=sc[:, hp, :]),
                       reads=[Tsc, Tv16], writes=[Ti16])
                    op("dve", lambda e, hp=hp: e.match_replace(out=scr[:, 0:128], in_to_replace=v16[:, hp, 0:8], in_values=sc[:, hp, :],
                                                               imm_value=-1e30), reads=[Tsc, Tv16], writes=[Tscr])
                    op("dve", lambda e, hp=hp: e.max(out=v16[:, hp, 8:16], in_=scr[:, 0:128]), reads=[Tscr], writes=[Tv16])
                    op("dve", lambda e, hp=hp: e.max_index(out=i16[:, hp, 8:16], in_max=v16[:, hp, 8:16], in_values=scr[:, 0:128]),
                       reads=[Tscr, Tv16], writes=[Ti16])
                    yield
                op("dve", lambda e: e.tensor_copy(out=i16f[:], in_=i16[:]), reads=[Ti16], writes=[Ti16f])
                v4 = v16[:].rearrange("p (h two) a -> p h two a", two=2)
                f4 = i16f[:].rearrange("p (h two) a -> p h two a", two=2)
                c4 = cand[:].rearrange("p h (a b) -> p h a b", b=16)
                x4 = cidx[:].rearrange("p h (a b) -> p h a b", b=16)
                op("dve", lambda e: e.tensor_tensor(out=c4, in0=v4[:, :, 0, :].unsqueeze(3).to_broadcast([128, 8, 16, 16]),
                                                    in1=v4[:, :, 1, :].unsqueeze(2).to_broadcast([128, 8, 16, 16]), op=ALU.add),
                   reads=[Tv16], writes=[Tcand])
                op("dve", lambda e: e.tensor_scalar(out=f4[:, :, 0, :], in0=f4[:, :, 0, :], scalar1=128.0, scalar2=None, op0=ALU.mult),
                   reads=[Ti16f], writes=[Ti16f])
                for h in range(8):
                    op("dve", lambda e, h=h: e.max(out=m16[:, h, 0:8], in_=cand[:, h, :]), reads=[Tcand], writes=[Tm16])
                    op("dve", lambda e, h=h: e.max_index(out=pos[:, h, 0:8], in_max=m16[:, h, 0:8], in_values=cand[:, h, :]),
                       reads=[Tcand, Tm16], writes=[Tpos])
                    op("dve", lambda e, h=h: e.match_replace(out=scr[:], in_to_replace=m16[:, h, 0:8], in_values=cand[:, h, :],
                                                             imm_value=-1e30), reads=[Tcand, Tm16], writes=[Tscr])
                    op("dve", lambda e, h=h: e.max(out=m16[:, h, 8:16], in_=scr[:]), reads=[Tscr], writes=[Tm16])
                    op("dve", lambda e, h=h: e.max_index(out=pos[:, h, 8:16], in_max=m16[:, h, 8:16], in_values=scr[:]),
                       reads=[Tscr, Tm16], writes=[Tpos])
                    yield
                op("dve", lambda e: e.tensor_single_scalar(out=pab[:, 0], in_=pos[:], scalar=4, op=ALU.logical_shift_right),
                   reads=[Tpos], writes=[Tpab])
                op("dve", lambda e: e.tensor_single_scalar(out=pab[:, 1], in_=pos[:], scalar=15, op=ALU.bitwise_and),
                   reads=[Tpos], writes=[Tpab])
                op("dve", lambda e: e.tensor_copy(out=pabf[:], in_=pab[:]), reads=[Tpab], writes=[Tpabf])
                yield
                oh4 = cand[:].rearrange("p h (k a) -> p h k a", a=16)
                pr4 = cidx[:].rearrange("p h (k a) -> p h k a", a=16)
                io4 = iota16[:].unsqueeze(1).unsqueeze(1).to_broadcast([128, 8, 16, 16])
                for half in range(2):
                    op("dve", lambda e, half=half: e.tensor_tensor(
                        out=oh4, in0=pabf[:, half].unsqueeze(3).to_broadcast([128, 8, 16, 16]), in1=io4, op=ALU.is_equal),
                       reads=[Tpabf, Tiota16, Tm16, Tpos], writes=[Tcand])
                    op("dve", lambda e, half=half: e.tensor_tensor(
                        out=pr4, in0=oh4, in1=f4[:, :, half, :].unsqueeze(2).to_broadcast([128, 8, 16, 16]), op=ALU.mult),
                       reads=[Tcand, Ti16f], writes=[Tcidx])
                    op("dve", lambda e, half=half: e.tensor_reduce(out=e12[:, half], in_=pr4, axis=AX.X, op=ALU.add),
                       reads=[Tcidx], writes=[Te12])
                    yield
                op("dve", lambda e: e.tensor_tensor(out=eidf[:].rearrange("p (h k) -> p h k", k=16), in0=e12[:, 0], in1=e12[:, 1], op=ALU.add),
                   reads=[Te12], writes=[Teidf])
                op("dve", lambda e: e.tensor_scalar(out=eidf[:], in0=eidf[:], scalar1=float(nexp - 1), scalar2=None, op0=ALU.min),
                   reads=[Teidf], writes=[Teidf])
                op("dve", lambda e: e.tensor_copy(out=eid[:], in_=eidf[:]), reads=[Teidf], writes=[Teid])
                yield
                op("dve", lambda e: e.tensor_tensor(out=gts[:], in0=m16[:], in1=m16[:, :, 0:1].to_broadcast([128, 8, 16]),
                                                    op=ALU.subtract), reads=[Tm16], writes=[Tgts])
                op("act", lambda e: e.activation(out=gts[:], in_=gts[:], func=AF.Exp), reads=[Tgts], writes=[Tgts])
                op("dve", lambda e: e.tensor_reduce(out=gsum[:], in_=gts[:], axis=AX.X, op=ALU.add), reads=[Tgts], writes=[Tgsum])
                op("dve", lambda e: e.reciprocal(out=gsum[:], in_=gsum[:]), reads=[Tgsum], writes=[Tgsum])
                op("dve", lambda e: e.tensor_tensor(out=gts[:], in0=gts[:], in1=gsum[:].unsqueeze(2).to_broadcast([128, 8, 16]),
                                                    op=ALU.mult), reads=[Tgts, Tgsum], writes=[Tgts])

            def block_part(tb, gen):
                b = tb % 2
                rows = slice(tb * 128, (tb + 1) * 128)
                x1, Tx1 = x1b[b], Tx1b[b]
                eid, Teid = eid2[b], Teid2[b]
                h2, Th2 = h2b[b], Th2b[b]
                gts, Tgts = gtsb[b], Tgtsb[b]
                gflat = gts[:].rearrange("p h k -> p (h k)")
                for grp in range(128 // GS):
                    ub = gctr[0] % NU; gctr[0] += 1
                    gs = slice(grp * GS, (grp + 1) * GS)
                    for s in range(GS):
                        slot = grp * GS + s
                        op("pool", lambda e, ub=ub, s=s, slot=slot: e.indirect_dma_start(
                            out=U[ub][:, s, :], out_offset=None, in_=edu_bf.rearrange("n t d -> n (t d)"),
                            in_offset=bass.IndirectOffsetOnAxis(ap=eid[:, slot:slot + 1], axis=0)),
                           reads=[Teid], writes=[TU[ub][s]], chan=ch_g[ub][s])
                    for s in range(GS):
                        slot = grp * GS + s
                        op("dve", lambda e, s=s, slot=slot: e.scalar_tensor_tensor(
                            out=junk3[:], in0=U[ub][:, s, 0:D], scalar=1.0, in1=h2[:],
                            op0=ALU.mult, op1=ALU.mult, accum_out=score[:, slot:slot + 1]),
                           reads=[TU[ub][s], Th2], writes=[Tjunk3, Tscore_g[grp]])
                    op("act", lambda e: e.activation(out=actv[:, gs], in_=score[:, gs], func=AF.Gelu),
                       reads=[Tscore_g[grp]], writes=[Tactv_g[grp]])
                    op("dve", lambda e: e.tensor_tensor(out=actv[:, gs], in0=actv[:, gs], in1=gflat[:, gs], op=ALU.mult),
                       reads=[Tactv_g[grp], Tgts], writes=[Tactv_g[grp]])
                    for s in range(GS):
                        slot = grp * GS + s
                        di = slot % 4
                        op("act", lambda e, slot=slot, di=di: e.activation(out=dg[di][:], in_=ident[:], func=AF.Copy,
                                                                           scale=actv[:, slot:slot + 1]),
                           reads=[Tident, Tactv_g[grp]], writes=[Tdg[di]])
                        for hf in range(2):
                            op("pe", lambda e, hf=hf, s=s, di=di, slot=slot: e.matmul(
                                PS[6 + hf][:, :], lhsT=dg[di][:], rhs=U[ub][:, s, D + hf * 512:D + (hf + 1) * 512],
                                start=(slot == 0), stop=(slot == 127)), reads=[Tdg[di], TU[ub][s]], writes=[TPS[6 + hf]])
                    if gen is not None:
                        for _ in range(3):
                            next(gen, None)
                if gen is not None:
                    for _ in gen:
                        pass
                for hf in range(2):
                    hs = slice(hf * 512, (hf + 1) * 512)
                    op("dve", lambda e, hf=hf, hs=hs: e.tensor_tensor(out=x2[:, hs], in0=PS[6 + hf][:, :], in1=bcs[:, 3, hs], op=ALU.mult),
                       reads=[TPS[6 + hf], Tbcs[3]], writes=[Tx2])
                op("dve", lambda e: e.tensor_tensor(out=x2[:], in0=x2[:], in1=x1[:], op=ALU.add), reads=[Tx2, Tx1], writes=[Tx2])
                op("act", lambda e: e.activation(out=junk4[:], in_=x2[:], func=AF.Square, accum_out=st4[:, 1:2]),
                   reads=[Tx2], writes=[Tjunk4, Tst4])
                op("act", lambda e: e.activation(out=st4[:, 1:2], in_=st4[:, 1:2], func=AF.Sqrt, scale=1.0 / D, bias=EPS),
                   reads=[Tst4], writes=[Tst4])
                op("dve", lambda e: e.reciprocal(out=st4[:, 1:2], in_=st4[:, 1:2]), reads=[Tst4], writes=[Tst4])
                op("dve", lambda e: e.scalar_tensor_tensor(out=ot[b][:], in0=x2[:], scalar=st4[:, 1:2], in1=bcs[:, 4, :],
                                                           op0=ALU.mult, op1=ALU.mult), reads=[Tx2, Tst4, Tbcs[4]], writes=[Tx2])
                op("sp", lambda e: e.dma_start(out=out_d[rows, :], in_=ot[b][:]), reads=[Tot[b]], chan=ch_st[b])

            for _ in front_part(0):
                pass
            for tb in range(NBQ):
                block_part(tb, front_part(tb + 1) if tb + 1 < NBQ else None)

        S_.barrier()
        print("build: ninst", S_.ninst, "nwaits", S_.nwaits, "sems", len(S_.sems))
    return nc


def make_masks(parity):
    s = np.arange(128)[:, None]
    t = np.arange(512)[None, :]
    out = np.zeros((2, 128, 16, 512), np.float32)
    kinds = ["n", "d", "d", "f"] if parity == 0 else ["d", "f", "n", "d"]
    for ms in range(4):
        for i in range(4):
            for v in range(2):
                if kinds[ms] == "n":
                    m = np.full((128, 512), NEG, np.float32)
                elif kinds[ms] == "f":
                    m = np.zeros((128, 512), np.float32)
                else:
                    kp = i * 128 + s
                    vis = (kp <= t) if v == 0 else (kp < t)
                    m = np.where(vis, 0.0, NEG).astype(np.float32)
                out[v, :, ms * 4 + i, :] = m
    return out.astype(ml_dtypes.bfloat16)


def make_in_maps(inp, S):
    NT = S // 512
    f32 = lambda a: np.ascontiguousarray(a, dtype=np.float32)
    colT = lambda v, n: f32(np.asarray(v).reshape(n, 128).T)
    shared = {
        "w_ada": f32(inp["w_ada"][0]),
        "badaT": colT(inp["b_ada"][0], 48),
        "bada_row": f32(inp["b_ada"][0][None, :]),
        "gmixT": colT(inp["g_norm_mix"][0], 8),
        "gffnT": colT(inp["g_norm_ffn"][0], 8),
        "gffn_row": f32(inp["g_norm_ffn"][0][None, :]),
        "gfin_row": f32(inp["g_final"][None, :]),
        "w_in": f32(inp["w_in"][0]),
        "bf_row": f32(inp["b_forget"][0][None, :]),
        "gfox_row": f32(inp["g_out_fox"][0][None, :]),
        "gsb_row": f32(inp["g_out_sb"][0][None, :]),
        "w_out": f32(inp["w_out"][0]),
        "w_query": f32(inp["w_query"][0]),
        "skT": f32(np.asarray(inp["sub_keys"][0]).reshape(16, 128, 128).transpose(2, 0, 1)),
        "e_down": f32(inp["expert_down"][0]),
        "e_up": f32(inp["expert_up"][0]),
    }
    masks = [make_masks(0), make_masks(1)]
    maps = []
    for core in range(8):
        b, r = core // 2, core % 2
        xb = np.asarray(inp["x"][b])
        tiles = tile_ids(r, NT)
        m = dict(shared)
        m["x_all"] = f32(xb)
        m["x_q"] = f32(np.concatenate([xb[t * 512:(t + 1) * 512] for t in tiles], axis=0))
        m["cT"] = colT(inp["c"][b], 8)
        m["masks"] = np.ascontiguousarray(masks[r][0])
        m["masks_sb"] = np.ascontiguousarray(masks[r][1])
        fl = np.zeros((8, 2), np.float32)
        fl[:, r] = 1.0
        m["flags"] = fl
        maps.append(m)
    return maps


def assemble(results, S):
    NT = S // 512
    out = np.zeros((4, S, D), np.float32)
    for core in range(8):
        b, r = core // 2, core % 2
        o = np.asarray(results[core]["out"])
        for lt, t in enumerate(tile_ids(r, NT)):
            out[b, t * 512:(t + 1) * 512] = o[lt * 512:(lt + 1) * 512]
    return out


_NC_CACHE = {}


def kernel(**inputs):
    S = 8192
    if S not in _NC_CACHE:
        _NC_CACHE[S] = build(S)
    nc = _NC_CACHE[S]
    maps = make_in_maps(inputs, S)
    res = run_bass_kernel_spmd(nc, maps, core_ids=list(range(8)))
    return assemble(res.results, S)
```
